# Optimizing a Trainium2 kernel written in Bass

```python
import jax, jax.numpy as jnp
from jax import lax
import numpy as np

D_MODEL = 1024
BATCH = 4
SEQ = 8192
DEPTH = 1

N_META = 16
BLOCK = 128
WINDOW = 128
N_PAD = BLOCK - N_META
ROPE_THETA = 10000.0
SWA_HEADS = 8
SWA_KV_HEADS = 2
SWA_HEAD_DIM = 64
MLA_HEADS = 4
MLA_Q_RANK = 256
MLA_KV_RANK = 256
MLA_NOPE_DIM = 128
MLA_ROPE_DIM = 64
MLA_V_DIM = 128
SWA_Q_COLS = SWA_HEADS * SWA_HEAD_DIM
SWA_KV_COLS = SWA_KV_HEADS * SWA_HEAD_DIM
MLA_OUT_COLS = MLA_HEADS * MLA_V_DIM
D_MIX = SWA_Q_COLS + MLA_OUT_COLS
IN_SPLITS = (SWA_Q_COLS, SWA_Q_COLS + SWA_KV_COLS, SWA_Q_COLS + 2 * SWA_KV_COLS,
             SWA_Q_COLS + 2 * SWA_KV_COLS + MLA_Q_RANK,
             SWA_Q_COLS + 2 * SWA_KV_COLS + MLA_Q_RANK + MLA_KV_RANK)
IN_COLS = SWA_Q_COLS + 2 * SWA_KV_COLS + MLA_Q_RANK + MLA_KV_RANK + MLA_ROPE_DIM
N_GROUPS = 4
EXPERTS_PER_GROUP = 8
N_EXPERTS = N_GROUPS * EXPERTS_PER_GROUP
TOP_K = 2
D_EXPERT = 256
LN_EPS = 1e-5
RMS_EPS = 1e-6
ALPHA = (2.0 * DEPTH) ** 0.25
BETA = (8.0 * DEPTH) ** -0.25
NEG = -1e30

kernel_name = "hymba_swa_sink_mla_hier_moe_deepnorm"


def layer_norm(x, g, b):
    xf = x.astype(jnp.float32)
    mu = jnp.mean(xf, axis=-1, keepdims=True)
    var = jnp.mean(jnp.square(xf - mu), axis=-1, keepdims=True)
    return ((xf - mu) * lax.rsqrt(var + LN_EPS) * g + b).astype(x.dtype)


def rms_norm(x, g):
    xf = x.astype(jnp.float32)
    ms = jnp.mean(jnp.square(xf), axis=-1, keepdims=True)
    return (xf * lax.rsqrt(ms + RMS_EPS) * g).astype(x.dtype)


def rope(x, pos):
    d = x.shape[-1]
    inv_freq = ROPE_THETA ** (-jnp.arange(0, d, 2, dtype=jnp.float32) / d)
    ang = pos[:, None] * inv_freq[None, :]
    cos = jnp.concatenate([jnp.cos(ang), jnp.cos(ang)], axis=-1)[:, None, :]
    sin = jnp.concatenate([jnp.sin(ang), jnp.sin(ang)], axis=-1)[:, None, :]
    xf = x.astype(jnp.float32)
    rot = jnp.concatenate([-xf[..., d // 2:], xf[..., :d // 2]], axis=-1)
    return (xf * cos + rot * sin).astype(x.dtype)


def swa_sink_attention(q, k, v, sinks, pos):
    B, Lp = q.shape[0], q.shape[1]
    nb = Lp // BLOCK
    G = SWA_HEADS // SWA_KV_HEADS
    q = rope(q, pos)
    k = rope(k, pos)
    qb = q.reshape(B, nb, BLOCK, SWA_KV_HEADS, G, SWA_HEAD_DIM)
    kb = k.reshape(B, nb, BLOCK, SWA_KV_HEADS, SWA_HEAD_DIM)
    vb = v.reshape(B, nb, BLOCK, SWA_KV_HEADS, SWA_HEAD_DIM)

    def with_prev(t):
        prev = jnp.pad(t[:, :-1], ((0, 0), (1, 0), (0, 0), (0, 0), (0, 0)))
        return jnp.concatenate([prev, t], axis=2)

    kw, vw = with_prev(kb), with_prev(vb)
    q_idx = jnp.arange(Lp).reshape(nb, BLOCK)
    k_idx = q_idx[:, :1] - BLOCK + jnp.arange(2 * BLOCK)[None, :]
    diff = q_idx[:, :, None] - k_idx[:, None, :]
    mask = (diff >= 0) & (diff < WINDOW) & (k_idx[:, None, :] >= N_PAD)
    s = jnp.einsum('bnqhgd,bnkhd->bnhgqk', qb, kw).astype(jnp.float32) * (SWA_HEAD_DIM ** -0.5)
    s = jnp.where(mask[None, :, None, None], s, NEG)
    sink = sinks.astype(jnp.float32).reshape(SWA_KV_HEADS, G)[None, None, :, :, None, None]
    m = jnp.maximum(jnp.max(s, axis=-1, keepdims=True), sink)
    e = jnp.exp(s - m)
    p = e / (jnp.sum(e, axis=-1, keepdims=True) + jnp.exp(sink - m))
    o = jnp.einsum('bnhgqk,bnkhd->bnqhgd', p.astype(v.dtype), vw)
    return o.reshape(B, Lp, SWA_Q_COLS)


def mla_attention(c_q, c_kv, k_rope, q_norm_g, w_uq, kv_norm_g, w_ukv, pos):
    B, Lp = c_q.shape[0], c_q.shape[1]
    H = MLA_HEADS
    q = (rms_norm(c_q, q_norm_g) @ w_uq).reshape(B, Lp, H, MLA_NOPE_DIM + MLA_ROPE_DIM)
    q = jnp.concatenate([q[..., :MLA_NOPE_DIM], rope(q[..., MLA_NOPE_DIM:], pos)], axis=-1)
    kv = (rms_norm(c_kv, kv_norm_g) @ w_ukv).reshape(B, Lp, H, MLA_NOPE_DIM + MLA_V_DIM)
    k_nope, v = kv[..., :MLA_NOPE_DIM], kv[..., MLA_NOPE_DIM:]
    k_r = jnp.broadcast_to(rope(k_rope[:, :, None, :], pos), (B, Lp, H, MLA_ROPE_DIM))
    k = jnp.concatenate([k_nope, k_r], axis=-1)
    scale = (MLA_NOPE_DIM + MLA_ROPE_DIM) ** -0.5
    nb = Lp // BLOCK
    qb = jnp.moveaxis(q.reshape(B, nb, BLOCK, H, MLA_NOPE_DIM + MLA_ROPE_DIM), 1, 0)
    k_idx = jnp.arange(Lp)

    def block_attn(args):
        q_blk, n = args
        q_idx = n * BLOCK + jnp.arange(BLOCK)
        mask = (k_idx[None, :] <= q_idx[:, None]) & (k_idx[None, :] >= N_PAD)
        s = jnp.einsum('bqhd,bkhd->bhqk', q_blk, k).astype(jnp.float32) * scale
        p = jax.nn.softmax(jnp.where(mask[None, None], s, NEG), axis=-1)
        return jnp.einsum('bhqk,bkhd->bqhd', p.astype(v.dtype), v)

    o = lax.map(block_attn, (qb, jnp.arange(nb)))
    return jnp.moveaxis(o, 0, 1).reshape(B, Lp, MLA_OUT_COLS)


def hierarchical_moe(h, w_group, b_group, w_router, b_router, w_gate, w_up, w_down):
    B, Lp, D = h.shape
    t = h.reshape(B * Lp, D)
    T = t.shape[0]
    group_probs = jax.nn.softmax((t @ w_group).astype(jnp.float32) + b_group, axis=-1)
    g_top, g_idx = lax.top_k(group_probs, 1)
    e_logits = ((t @ w_router).astype(jnp.float32) + b_router).reshape(T, N_GROUPS, EXPERTS_PER_GROUP)
    sel = jnp.broadcast_to(g_idx[:, :, None], (T, 1, EXPERTS_PER_GROUP))
    in_group = jnp.take_along_axis(e_logits, sel, axis=1)[:, 0]
    e_top, e_idx = lax.top_k(in_group, TOP_K)
    weights = g_top * jax.nn.softmax(e_top, axis=-1)
    expert_ids = g_idx * EXPERTS_PER_GROUP + e_idx
    combine = jnp.einsum('tk,tke->te', weights, jax.nn.one_hot(expert_ids, N_EXPERTS, dtype=jnp.float32))
    nblk = T // BLOCK

    def expert_block(args):
        xb, cb = args
        hid = jax.nn.silu(jnp.einsum('td,edf->tef', xb, w_gate)) * jnp.einsum('td,edf->tef', xb, w_up)
        hid = hid * cb.astype(hid.dtype)[:, :, None]
        return jnp.einsum('tef,efd->td', hid, w_down)

    out = lax.map(expert_block, (t.reshape(nblk, BLOCK, D), combine.reshape(nblk, BLOCK, N_EXPERTS)))
    return out.reshape(B, Lp, D)


def setup_inputs(seed: int = 0) -> dict:
    key = jax.random.key(seed)
    ks = jax.random.split(key, 24)
    f32 = jnp.float32

    def nrm(k, shape, scale):
        return jax.random.normal(k, shape, f32) * scale

    L = DEPTH
    return {
        "x": nrm(ks[0], (BATCH, SEQ, D_MODEL), 1.0),
        "meta_tokens": nrm(ks[1], (N_META, D_MODEL), 1.0),
        "ln_in_g": 1.0 + nrm(ks[2], (D_MODEL,), 0.02),
        "ln_in_b": nrm(ks[3], (D_MODEL,), 0.02),
        "w_in": nrm(ks[4], (L, D_MODEL, IN_COLS), D_MODEL ** -0.5),
        "swa_sinks": nrm(ks[5], (L, SWA_HEADS), 0.5),
        "mla_q_norm_g": 1.0 + nrm(ks[6], (L, MLA_Q_RANK), 0.02),
        "mla_w_uq": nrm(ks[7], (L, MLA_Q_RANK, MLA_HEADS * (MLA_NOPE_DIM + MLA_ROPE_DIM)), MLA_Q_RANK ** -0.5),
        "mla_kv_norm_g": 1.0 + nrm(ks[8], (L, MLA_KV_RANK), 0.02),
        "mla_w_ukv": nrm(ks[9], (L, MLA_KV_RANK, MLA_HEADS * (MLA_NOPE_DIM + MLA_V_DIM)), MLA_KV_RANK ** -0.5),
        "swa_out_norm_g": 1.0 + nrm(ks[10], (L, SWA_Q_COLS), 0.02),
        "mla_out_norm_g": 1.0 + nrm(ks[11], (L, MLA_OUT_COLS), 0.02),
        "w_o": nrm(ks[12], (L, D_MIX, D_MODEL), (D_MIX ** -0.5) * BETA),
        "ln1_g": 1.0 + nrm(ks[13], (L, D_MODEL), 0.02),
        "ln1_b": nrm(ks[14], (L, D_MODEL), 0.02),
        "moe_w_group": nrm(ks[15], (L, D_MODEL, N_GROUPS), D_MODEL ** -0.5),
        "moe_b_group": nrm(ks[16], (L, N_GROUPS), 0.01),
        "moe_w_router": nrm(ks[17], (L, D_MODEL, N_EXPERTS), D_MODEL ** -0.5),
        "moe_b_router": nrm(ks[18], (L, N_EXPERTS), 0.01),
        "moe_w_gate": nrm(ks[19], (L, N_EXPERTS, D_MODEL, D_EXPERT), D_MODEL ** -0.5),
        "moe_w_up": nrm(ks[20], (L, N_EXPERTS, D_MODEL, D_EXPERT), D_MODEL ** -0.5),
        "moe_w_down": nrm(ks[21], (L, N_EXPERTS, D_EXPERT, D_MODEL), (D_EXPERT ** -0.5) * BETA),
        "ln2_g": 1.0 + nrm(ks[22], (L, D_MODEL), 0.02),
        "ln2_b": nrm(ks[23], (L, D_MODEL), 0.02),
    }


def reference(x, meta_tokens, ln_in_g, ln_in_b, w_in, swa_sinks, mla_q_norm_g, mla_w_uq,
              mla_kv_norm_g, mla_w_ukv, swa_out_norm_g, mla_out_norm_g, w_o, ln1_g, ln1_b,
              moe_w_group, moe_b_group, moe_w_router, moe_b_router, moe_w_gate, moe_w_up,
              moe_w_down, ln2_g, ln2_b):
    B = x.shape[0]
    pads = jnp.zeros((B, N_PAD, D_MODEL), x.dtype)
    meta = jnp.broadcast_to(meta_tokens.astype(x.dtype)[None], (B, N_META, D_MODEL))
    h = jnp.concatenate([pads, meta, x], axis=1)
    Lp = h.shape[1]
    pos = jnp.maximum(jnp.arange(Lp) - N_PAD, 0).astype(jnp.float32)
    h = layer_norm(h, ln_in_g, ln_in_b)
    for l in range(DEPTH):
        u = h @ w_in[l]
        q_a, k_a, v_a, c_q, c_kv, k_rope = jnp.split(u, IN_SPLITS, axis=-1)
        a_out = swa_sink_attention(
            q_a.reshape(B, Lp, SWA_HEADS, SWA_HEAD_DIM),
            k_a.reshape(B, Lp, SWA_KV_HEADS, SWA_HEAD_DIM),
            v_a.reshape(B, Lp, SWA_KV_HEADS, SWA_HEAD_DIM),
            swa_sinks[l], pos)
        b_out = mla_attention(c_q, c_kv, k_rope, mla_q_norm_g[l], mla_w_uq[l],
                              mla_kv_norm_g[l], mla_w_ukv[l], pos)
        mix = jnp.concatenate([rms_norm(a_out, swa_out_norm_g[l]),
                               rms_norm(b_out, mla_out_norm_g[l])], axis=-1) @ w_o[l]
        h = layer_norm(ALPHA * h + mix, ln1_g[l], ln1_b[l])
        ffn = hierarchical_moe(h, moe_w_group[l], moe_b_group[l], moe_w_router[l], moe_b_router[l],
                               moe_w_gate[l], moe_w_up[l], moe_w_down[l])
        h = layer_norm(ALPHA * h + ffn, ln2_g[l], ln2_b[l])
    return h[:, BLOCK:]
```

```python
import numpy as np
from contextlib import ExitStack
import concourse.bass as bass
import concourse.mybir as mybir
from concourse.bass_utils import run_bass_kernel_spmd

F32 = mybir.dt.float32
BF16 = mybir.dt.bfloat16
AF = mybir.ActivationFunctionType
ALU = mybir.AluOpType
AX = mybir.AxisListType

NCH = 18
NBLK = NCH * 4
NSTEP = 8
LN_EPS = 1e-5
RMS_EPS = 1e-6
ALPHA = 2.0 ** 0.25
NEGB = -30000.0
NEXP = 32
KVW = 1152
SWW = 384
MOE_T = 2048
SLOT = 512
NSLOT = 12
ROWW = 1032
U32 = mybir.dt.uint32


class _Op:
    __slots__ = ("eng", "fn", "idx", "eidx", "deps", "need_inc", "dsem", "dcount", "is_dma", "inc_no", "bar")


class Sched:
    ENG = ("pe", "act", "dve", "pool", "sp")

    def __init__(self):
        self.ops = {e: [] for e in self.ENG}
        self.last_w = {}
        self.readers = {}
        self.seen = {e: {} for e in self.ENG}
        self.seen_dma = {e: set() for e in self.ENG}
        self.dcounts = {}
        self.dsems = {}
        self.n = 0
        self.pending = {e: None for e in self.ENG}
        self.capture = None

    def barrier(self):
        last = [self.ops[e][-1] for e in self.ENG if self.ops[e] and not self.ops[e][-1].is_dma]
        last = []
        for e in self.ENG:
            for op in reversed(self.ops[e]):
                if not op.is_dma:
                    last.append(op); break
        for op in last:
            op.need_inc = True
        dm = [(self.dsems[k], c) for k, c in self.dcounts.items()]
        for e in self.ENG:
            self.pending[e] = (last, dm)

    def add(self, eng, fn, reads=(), writes=(), dsem=None):
        if self.capture is not None:
            self.capture.append((eng, fn, list(reads), list(writes), dsem))
            return None
        op = _Op()
        op.eng = eng; op.fn = fn; op.idx = self.n; self.n += 1
        op.eidx = len(self.ops[eng]); op.need_inc = False; op.dsem = dsem
        op.is_dma = dsem is not None; op.inc_no = None
        if op.is_dma:
            self.dcounts[id(dsem)] = self.dcounts.get(id(dsem), 0) + 16
            self.dsems[id(dsem)] = dsem
            op.dcount = self.dcounts[id(dsem)]
        deps = []
        for k in reads:
            w = self.last_w.get(k)
            if w is not None:
                deps.append((w, "raw"))
            if isinstance(k, tuple) and k[0] in ("B", "T"):
                for r in self.readers.get(k, ()):
                    if r.eng != eng:
                        deps.append((r, "war"))
        for k in writes:
            w = self.last_w.get(k)
            if w is not None:
                deps.append((w, "waw"))
            for r in self.readers.get(k, ()):
                deps.append((r, "war"))
        final_eng = {}
        final_dma = []
        for d, kind in deps:
            if d is op:
                continue
            if d.is_dma:
                if d.idx not in self.seen_dma[eng]:
                    self.seen_dma[eng].add(d.idx)
                    final_dma.append(d)
                continue
            if d.eng == eng and not op.is_dma:
                if eng == "pe":
                    continue
                if kind != "raw":
                    continue
            if d.eidx <= self.seen[eng].get(d.eng, -1):
                continue
            if d.eng not in final_eng or final_eng[d.eng].eidx < d.eidx:
                final_eng[d.eng] = d
        for f, d in final_eng.items():
            self.seen[eng][f] = d.eidx
            d.need_inc = True
        op.deps = list(final_eng.values()) + final_dma
        op.bar = self.pending[eng]
        if op.bar is not None:
            self.pending[eng] = None
            for d in op.bar[0]:
                if d.eng != eng:
                    self.seen[eng][d.eng] = max(self.seen[eng].get(d.eng, -1), d.eidx)
        for k in writes:
            self.last_w[k] = op
            self.readers[k] = []
        for k in reads:
            lst = self.readers.setdefault(k, [])
            if not op.is_dma:
                lst[:] = [r for r in lst if r.is_dma or r.eng != eng]
            lst.append(op)
        self.ops[eng].append(op)
        return op

    def emit(self, block, sems):
        for e in self.ENG:
            c = 0
            for op in self.ops[e]:
                if op.need_inc and not op.is_dma:
                    c += 1
                    op.inc_no = c
        me = self

        def run(e, engobj):
            for op in me.ops[e]:
                if op.bar is not None:
                    for d in op.bar[0]:
                        engobj.wait_ge(sems[d.eng], d.inc_no)
                    for sm, cnt in op.bar[1]:
                        engobj.wait_ge(sm, cnt)
                for d in op.deps:
                    if d.is_dma:
                        engobj.wait_ge(d.dsem, d.dcount)
                    else:
                        engobj.wait_ge(sems[d.eng], d.inc_no)
                ins = op.fn(engobj)
                if op.is_dma:
                    ins.then_inc(op.dsem, 16)
                elif op.need_inc:
                    ins.then_inc(sems[e], 1)
            if e == "sp":
                for k, s in me.dsems.items():
                    engobj.wait_ge(s, me.dcounts[k])

        @block.tensor
        def _(eng):
            run("pe", eng)

        @block.scalar
        def _(eng):
            run("act", eng)

        @block.vector
        def _(eng):
            run("dve", eng)

        @block.gpsimd
        def _(eng):
            run("pool", eng)

        @block.sync
        def _(eng):
            run("sp", eng)


def build_nc(n_step=NSTEP, n_kchunk=NCH - 1, moe=True, n_exp=NEXP, dbg_h1=False, sparse=True):
    nc = bass.Bass("TRN2", target_bir_lowering=False)
    S = Sched()

    def din(name, shape, dt=F32):
        return nc.dram_tensor(name, list(shape), dt, kind="ExternalInput").ap()

    xs = din("xs", [NBLK * 128, 1024])
    cosT = din("cosT", [128, NBLK * 128])
    sinT = din("sinT", [128, NBLK * 128])
    kbm = din("kbm", [128, NBLK])
    kbs = din("kbs", [128, NBLK])
    cst = din("cst", [128, 128 * 4 + 512 * 2])
    WK_d = din("WK", [1024, 896])
    WQ_d = din("WQ", [1024, 1280])
    WUQ_d = din("WUQ", [256, 1024])
    WUK_d = din("WUK", [256, 512])
    WUV_d = din("WUV", [256, 512])
    WOA_d = din("WOA", [128, 4 * 1024])
    WOB_d = din("WOB", [128, 4 * 1024])
    WR_d = din("WR", [1024, 36])
    vec_d = din("vec", [128, 40])
    rows_d = din("rows", [7, 1024])
    if sparse:
        wg2_d = din("wg2", [NEXP * 128, 2048])
        wu2_d = din("wu2", [NEXP * 128, 2048])
        wd2_d = din("wd2", [NEXP * 128, 2048])
        cst2_d = din("cst2", [128, 128 + 8 + NSLOT])
        xs_d = nc.dram_tensor("xs_scr", [NSLOT * SLOT, ROWW], F32, kind="Internal").ap()
        ffn_d = nc.dram_tensor("ffn_scr", [NSLOT * SLOT, 1024], F32, kind="Internal").ap()
    else:
        wg_d = din("wg", [NEXP, 1024, 256])
        wu_d = din("wu", [NEXP, 1024, 256])
        wd_d = din("wd", [NEXP, 256, 1024])
    out_d = nc.dram_tensor("out", [NSTEP * 512, 1024], F32, kind="ExternalOutput").ap()
    kv_d = nc.dram_tensor("kv_scr", [NBLK, 128, KVW], BF16, kind="Internal").ap()
    sw_d = nc.dram_tensor("sw_scr", [NBLK, 128, SWW], BF16, kind="Internal").ap()
    h1_d = nc.dram_tensor("h1_scr", [NSTEP * 512, 1024], F32, kind="Internal").ap()

    with ExitStack() as es:
        cur = [es]

        def sb(name, shape, dt):
            return cur[0].enter_context(nc.sbuf_tensor(name, list(shape), dt))

        def ps(name, shape, dt):
            return es.enter_context(nc.psum_tensor(name, list(shape), dt))

        def sem(name):
            return es.enter_context(nc.semaphore(name))

        sems = {e: sem("s_" + e) for e in Sched.ENG}
        B = [ps("B%d" % i, [128, 512], F32) for i in range(6)]
        T = [ps("T%d" % i, [128, 1024], BF16) for i in range(2)]

        def kB(i):
            return ("B", i)

        def kT(i):
            return ("T", i)

        ident = sb("ident", [128, 128], BF16)
        ones = sb("ones", [128, 128], BF16)
        onesg = sb("onesg", [128, 2, 128], BF16)
        maskP = sb("maskP", [128, 512], BF16)
        maskC = sb("maskC", [128, 512], BF16)
        identf = sb("identf", [128, 128], F32)
        onesf = sb("onesf", [128, 128], F32)
        kbm_t = sb("kbm_t", [128, NBLK], F32)
        kbs_t = sb("kbs_t", [128, NBLK], F32)
        vec = sb("vec_s", [128, 40], F32)
        sinkexp = sb("sinkexp", [128, 4], F32)
        epsc = sb("epsc", [128, 2], F32)
        epsln = epsc[:, 0:1]
        epsrms = epsc[:, 1:2]
        S.add("dve", lambda e: e.memset(epsc[:, 0:1], LN_EPS), writes=["epsc0"])
        S.add("dve", lambda e: e.memset(epsc[:, 1:2], RMS_EPS), reads=["epsc0"], writes=["epsc"])
        st = [sb("st%d" % i, [128, 2, 6], F32) for i in range(4)]
        mv = [sb("mv%d" % i, [128, 4], F32) for i in range(4)]
        zt = sb("zt", [128, ROWW], F32)
        d_z = sem("d_z")
        S.add("pool", lambda e: e.memset(zt[:], 0.0), writes=["zt"])
        nzb = NSLOT * SLOT // 128
        zfill = [0]
        allz = [("xsz", i) for i in range(nzb)]

        def emit_zfill(n):
            while sparse and n > 0 and zfill[0] < nzb:
                i = zfill[0]; zfill[0] += 1
                S.add("sp", lambda e, i=i: e.dma_start(out=xs_d[i * 128:(i + 1) * 128, :], in_=zt[:]), reads=["zt"], writes=[("xsz", i)], dsem=d_z)
                n -= 1
        e_att = ExitStack()
        cur[0] = e_att
        d_c = [sem("d_c%d" % i) for i in range(28)]
        _ci = [0]

        import os as _os
        _lim = int(_os.environ.get("KDBG_NCLOAD", "999"))
        _skipms = _os.environ.get("KDBG_SKIPMS", "0") == "1"

        def cload(out, in_, key, q="pool"):
            if _ci[0] >= _lim:
                _ci[0] += 1
                return
            s = d_c[_ci[0]]; _ci[0] += 1
            S.add(q, lambda e: e.dma_start(out=out, in_=in_), writes=[key], dsem=s)

        cload(ident[:], cst[:, 0:128], "ident")
        cload(ones[:], cst[:, 128:256], "ones")
        cload(onesg[:, 0, :], cst[:, 256:384], "onesg0")
        cload(onesg[:, 1, :], cst[:, 384:512], "onesg1")
        cload(maskP[:], cst[:, 512:1024], "maskP")
        cload(maskC[:], cst[:, 1024:1536], "maskC")
        cload(identf[:], cst[:, 0:128], "identf", q="sp")
        cload(onesf[:], cst[:, 128:256], "onesf", q="sp")
        cload(kbm_t[:], kbm[:, :], "kbm", q="sp")
        cload(kbs_t[:], kbs[:, :], "kbs", q="sp")
        cload(vec[:], vec_d[:, :], "vec", q="sp")
        VG, VB, VQG, VKG, VGA, VGB, VSK = 0, 8, 16, 18, 20, 24, 28

        NX = 8
        xt = [sb("xt%d" % i, [128, 1024], F32) for i in range(NX)]
        d_x = [sem("d_x%d" % i) for i in range(NX)]
        xnb = sb("xnb", [128, 4, 1024], BF16)
        hT = sb("hT", [128, 8, 512], BF16)
        cs_t = sb("cs_t", [128, 512], F32)
        sn_t = sb("sn_t", [128, 512], F32)
        d_cs = sem("d_cs"); d_sn = sem("d_sn")
        t1 = [sb("t1_%d" % i, [128, 512], F32) for i in range(2)]
        t2 = [sb("t2_%d" % i, [128, 512], F32) for i in range(2)]
        sq = sb("sq", [128, 4, 512], BF16)
        rstd_t = sb("rstd_t", [128, 512], F32)
        e_k = ExitStack()
        cur[0] = e_k
        WK = sb("WK_s", [128, 8, 896], BF16)
        WUK = sb("WUK_s", [128, 2, 512], BF16)
        WUV = sb("WUV_s", [128, 2, 512], BF16)
        cload(WK[:], WK_d.rearrange("(k p) f -> p k f", p=128), "WK")
        cload(WUK[:], WUK_d.rearrange("(k p) f -> p k f", p=128), "WUK")
        cload(WUV[:], WUV_d.rearrange("(k p) f -> p k f", p=128), "WUV")
        ckvn = sb("ckvn", [128, 2, 512], BF16)
        kvrec = [sb("kvrec%d" % i, [128, 4, KVW], BF16) for i in range(2)]
        swrec = [sb("swrec%d" % i, [128, 4, SWW], BF16) for i in range(2)]
        d_kvw = [sem("d_kvw%d" % i) for i in range(2)]
        d_sww = [sem("d_sww%d" % i) for i in range(2)]

        for i in range(2):
            if not _skipms:
                S.add("pool", (lambda t: (lambda e: e.memset(t[:], 0.0)))(swrec[i]), writes=[("swrec", i)])

        xr = [0]

        def chunk_loads(ch):
            slots = []
            for t in range(4):
                s = xr[0]; xr[0] = (xr[0] + 1) % NX
                slots.append(s)
                blk = ch * 4 + t
                S.add("sp", (lambda s=s, blk=blk: (lambda e: e.dma_start(out=xt[s][:], in_=xs[blk * 128:(blk + 1) * 128, :])))(),
                      writes=[("x", s)], dsem=d_x[s])
            return slots

        def rope_loads(ch):
            S.add("sp", lambda e: e.dma_start(out=cs_t[:], in_=cosT[:, ch * 512:(ch + 1) * 512]), writes=["cs"], dsem=d_cs)
            S.add("sp", lambda e: e.dma_start(out=sn_t[:], in_=sinT[:, ch * 512:(ch + 1) * 512]), writes=["sn"], dsem=d_sn)

        def front(slots, want_res):
            _sub = int(_os.environ.get("KDBG_SUB", "99"))
            for t in range(4):
                s = slots[t]
                x_ = xt[s]
                st_, mv_ = st[t], mv[t]
                S.add("dve", lambda e, x_=x_, st_=st_: e.bn_stats(out=st_[:, 0, :], in_=x_[:, 0:512]), reads=[("x", s)], writes=[("st", t, 0)])
                S.add("dve", lambda e, x_=x_, st_=st_: e.bn_stats(out=st_[:, 1, :], in_=x_[:, 512:1024]), reads=[("x", s)], writes=[("st", t, 1)])
                S.add("dve", lambda e, st_=st_, mv_=mv_: e.bn_aggr(out=mv_[:, 0:2], in_=st_[:, :, :]), reads=[("st", t, 0), ("st", t, 1)], writes=[("mv", t, 0)])
                if _sub < 2:
                    continue
                S.add("act", lambda e, mv_=mv_: e.activation(out=mv_[:, 2:3], in_=mv_[:, 1:2], func=AF.Ln, bias=epsln[:, 0:1], scale=1.0),
                      reads=[("mv", t, 0), "epsc"], writes=[("mv", t, 1)])
                S.add("act", lambda e, mv_=mv_: e.activation(out=mv_[:, 2:3], in_=mv_[:, 2:3], func=AF.Exp, scale=-0.5),
                      reads=[("mv", t, 1)], writes=[("mv", t, 1)])
                S.add("dve", lambda e, mv_=mv_: e.scalar_tensor_tensor(out=mv_[:, 3:4], in0=mv_[:, 0:1], scalar=-1.0, in1=mv_[:, 2:3], op0=ALU.mult, op1=ALU.mult),
                      reads=[("mv", t, 0), ("mv", t, 1)], writes=[("mv", t, 2)])
                if _sub < 3:
                    continue
                S.add("act", lambda e, x_=x_, mv_=mv_, t=t: e.activation(out=xnb[:, t, :], in_=x_[:], func=AF.Identity, bias=mv_[:, 3:4], scale=mv_[:, 2:3]),
                      reads=[("x", s), ("mv", t, 1), ("mv", t, 2)], writes=[("xnb", t)])
                if want_res:
                    S.add("act", lambda e, x_=x_, mv_=mv_: e.activation(out=x_[:], in_=x_[:], func=AF.Identity, bias=mv_[:, 3:4], scale=mv_[:, 2:3]),
                          reads=[("x", s), ("mv", t, 1), ("mv", t, 2)], writes=[("x", s)])
                    S.add("pool", lambda e, x_=x_: e.tensor_tensor(out=x_[:], in0=x_[:], in1=gabc[:], op=ALU.mult), reads=[("x", s), "gabc"], writes=[("x", s)])
                    S.add("pool", lambda e, x_=x_: e.tensor_tensor(out=x_[:], in0=x_[:], in1=babc[:], op=ALU.add), reads=[("x", s), "babc"], writes=[("x", s)])
            for kp in range(4):
                if _sub < 4:
                    continue
                Tb = T[kp % 2]
                for kk in range(2):
                    k = 2 * kp + kk
                    for t in range(4):
                        S.add("pe", lambda e, Tb=Tb, kk=kk, t=t, k=k: e.transpose(out=Tb[:, (kk * 4 + t) * 128:(kk * 4 + t + 1) * 128],
                                                                                in_=xnb[:, t, k * 128:(k + 1) * 128], identity=ident[:]),
                              reads=[("xnb", t), "ident"], writes=[kT(kp % 2)])
                for kk in range(2):
                    if _sub < 5:
                        continue
                    k = 2 * kp + kk
                    _ev = _os.environ.get("KDBG_EV", "")
                    if (_ev == "act" and kk == 1) or (_ev == "dve" and kk == 0):
                        continue
                    if (kp % 2 == 0 and _ev != "alldve") or _ev == "allact":
                        S.add("act", lambda e, Tb=Tb, kk=kk, k=k: e.activation(out=hT[:, k, :], in_=Tb[:, kk * 512:(kk + 1) * 512], func=AF.Identity,
                                                                                bias=vec[:, VB + k:VB + k + 1], scale=vec[:, VG + k:VG + k + 1]),
                              reads=[kT(kp % 2), "vec"], writes=[("hT", k)])
                    else:
                        S.add("dve", lambda e, Tb=Tb, kk=kk, k=k: e.tensor_scalar(out=hT[:, k, :], in0=Tb[:, kk * 512:(kk + 1) * 512],
                                                                                   scalar1=vec[:, VG + k:VG + k + 1], scalar2=vec[:, VB + k:VB + k + 1],
                                                                                   op0=ALU.mult, op1=ALU.add),
                              reads=[kT(kp % 2), "vec"], writes=[("hT", k)])

        hT_all = [("hT", k) for k in range(8)]

        def proj(bi, W, c0, m, wkey, ncols=512):
            for k in range(8):
                S.add("pe", lambda e, k=k: e.matmul(B[bi][0:m, 0:ncols], lhsT=W[:, k, c0:c0 + m], rhs=hT[:, k, 0:ncols], start=(k == 0), stop=(k == 7)),
                      reads=[("hT", k), wkey], writes=[kB(bi)])

        def projP(pap, pkey, W, c0, wkey):
            for k in range(8):
                S.add("pe", lambda e, k=k: e.matmul(pap, lhsT=W[:, k, c0:c0 + 128], rhs=hT[:, k, :], start=(k == 0), stop=(k == 7)),
                      reads=[("hT", k), wkey], writes=[pkey])

        def rope_applyP(qap, qkey, rap, rkey, out_ap, okey, i):
            S.add("dve", lambda e: e.tensor_tensor(out=t1[i][:, :], in0=qap, in1=cs_t[:, :], op=ALU.mult), reads=[qkey, "cs"], writes=[("t1", i)])
            S.add("dve", lambda e: e.tensor_tensor(out=t2[i][:, :], in0=rap, in1=sn_t[:, :], op=ALU.mult), reads=[rkey, "sn"], writes=[("t2", i)])
            S.add("pool", lambda e: e.tensor_tensor(out=out_ap, in0=t1[i][:, :], in1=t2[i][:, :], op=ALU.add), reads=[("t1", i), ("t2", i)], writes=[okey])

        def rope_apply(bq, br, out_ap, okey, np_=128, i=0, o3=False):
            def v(t):
                a = t[0:np_, :]
                return a.rearrange("p (t c) -> p t c", t=4) if o3 else a
            S.add("dve", lambda e: e.tensor_tensor(out=t1[i][0:np_, :], in0=B[bq][0:np_, :], in1=cs_t[0:np_, :], op=ALU.mult), reads=[kB(bq), "cs"], writes=[("t1", i)])
            S.add("dve", lambda e: e.tensor_tensor(out=t2[i][0:np_, :], in0=B[br][0:np_, :], in1=sn_t[0:np_, :], op=ALU.mult), reads=[kB(br), "sn"], writes=[("t2", i)])
            S.add("pool", lambda e: e.tensor_tensor(out=out_ap, in0=v(t1[i]), in1=v(t2[i]), op=ALU.add), reads=[("t1", i), ("t2", i)], writes=[okey])

        def rms_feat(banks, gcol, nfeat, out_tile, okeys, bsum, in_keys=None, src_sb=None):
            n = len(banks)
            for c in range(n):
                rk = in_keys[c]
                S.add("act", lambda e, c=c: e.activation(out=sq[:, c, :], in_=banks[c], func=AF.Square), reads=[rk], writes=[("sq", c)])
            for c in range(n):
                S.add("pe", lambda e, c=c: e.matmul(B[bsum][:, :], lhsT=ones[:], rhs=sq[:, c, :], start=(c == 0), stop=(c == n - 1)),
                      reads=[("sq", c), "ones"], writes=[kB(bsum)])
            S.add("act", lambda e: e.activation(out=rstd_t[:], in_=B[bsum][:, :], func=AF.Ln, bias=epsrms[:, 0:1], scale=1.0 / nfeat),
                  reads=[kB(bsum), "epsc"], writes=["rstd"])
            S.add("act", lambda e: e.activation(out=rstd_t[:], in_=rstd_t[:], func=AF.Exp, scale=-0.5), reads=["rstd"], writes=["rstd"])
            for c in range(n):
                rk = in_keys[c]
                eng = "dve"
                S.add(eng, lambda e, c=c: e.scalar_tensor_tensor(out=out_tile[:, c, :], in0=banks[c], scalar=vec[:, gcol + c:gcol + c + 1], in1=rstd_t[:],
                                                                  op0=ALU.mult, op1=ALU.mult), reads=[rk, "rstd", "vec"], writes=[okeys[c]])

        def front_parts(slots_):
            cap = []
            S.capture = cap
            front(slots_, False)
            S.capture = None
            fi = next(i for i, o in enumerate(cap) if o[0] == "pe")
            return cap[:fi], cap[fi:]

        def emit_list(lst):
            for eng_, fn_, r_, w_, ds_ in lst:
                S.add(eng_, fn_, reads=r_, writes=w_, dsem=ds_)

        kslots = {}
        if n_kchunk > 0:
            kslots[0] = chunk_loads(0)
            if n_kchunk > 1:
                kslots[1] = chunk_loads(1)
            pa, pb = front_parts(kslots[0])
            emit_list(pa); emit_list(pb)
        for ch in range(n_kchunk):
            rope_loads(ch)
            if ch + 2 < n_kchunk:
                kslots[ch + 2] = chunk_loads(ch + 2)
            emit_zfill(3)
            kr = kvrec[ch % 2]; sr = swrec[ch % 2]
            kvk = ("kvrec", ch % 2); swk = ("swrec", ch % 2)
            proj(0, WK, 0, 128, "WK")
            proj(1, WK, 128, 128, "WK")
            proj(2, WK, 256, 128, "WK")
            proj(3, WK, 384, 128, "WK")
            proj(4, WK, 640, 128, "WK")
            proj(5, WK, 768, 128, "WK")
            if ch + 1 < n_kchunk:
                pa, pb = front_parts(kslots[ch + 1])
                emit_list(pa)
            else:
                pb = []
            vap = T[0][:, :].bitcast(F32)
            for t in range(4):
                for k in range(8):
                    S.add("pe", lambda e, t=t, k=k: e.matmul(vap[:, t * 128:(t + 1) * 128], lhsT=hT[:, k, t * 128:(t + 1) * 128], rhs=WK[:, k, 512:640],
                                                             start=(k == 0), stop=(k == 7)), reads=[("hT", k), "WK"], writes=[kT(0)])
            b4v = vap.rearrange("p (t c) -> p t c", t=4)
            S.add("act", lambda e, sr=sr, b4v=b4v: e.activation(out=sr[:, :, 128:192], in_=b4v[:, :, 0:64], func=AF.Copy), reads=[kT(0)], writes=[swk])
            S.add("act", lambda e, sr=sr, b4v=b4v: e.activation(out=sr[:, :, 320:384], in_=b4v[:, :, 64:128], func=AF.Copy), reads=[kT(0)], writes=[swk])
            emit_list(pb)
            rope_apply(0, 1, sr[:, :, 0:128], swk, i=0, o3=True)
            rope_apply(4, 5, kr[:, :, 512:640], kvk, i=1, o3=True)
            rms_feat([B[2][:, :], B[3][:, :]], VKG, 256.0, ckvn, [("ckvn", 0), ("ckvn", 1)], 0, in_keys=[kB(2), kB(3)])
            for h in range(4):
                bi = [1, 2, 3, 5][h]
                for k in range(2):
                    S.add("pe", lambda e, h=h, k=k, bi=bi: e.matmul(B[bi][:, :], lhsT=WUK[:, k, h * 128:(h + 1) * 128], rhs=ckvn[:, k, :], start=(k == 0), stop=(k == 1)),
                          reads=[("ckvn", k), "WUK"], writes=[kB(bi)])
                src = B[bi][:, :].rearrange("p (t c) -> p t c", t=4)
                if h % 2 == 0:
                    S.add("act", lambda e, h=h, src=src, kr=kr: e.activation(out=kr[:, :, h * 128:(h + 1) * 128], in_=src, func=AF.Copy), reads=[kB(bi)], writes=[kvk])
                else:
                    S.add("dve", lambda e, h=h, src=src, kr=kr: e.tensor_copy(out=kr[:, :, h * 128:(h + 1) * 128], in_=src), reads=[kB(bi)], writes=[kvk])
            for t in range(4):
                bi = [0, 4, 1, 2][t]
                for k in range(2):
                    S.add("pe", lambda e, t=t, k=k, bi=bi: e.matmul(B[bi][:, :], lhsT=ckvn[:, k, t * 128:(t + 1) * 128], rhs=WUV[:, k, :], start=(k == 0), stop=(k == 1)),
                          reads=[("ckvn", k), "WUV"], writes=[kB(bi)])
                if t % 2 == 0:
                    S.add("act", lambda e, t=t, bi=bi, kr=kr: e.activation(out=kr[:, t, 640:1152], in_=B[bi][:, :], func=AF.Copy), reads=[kB(bi)], writes=[kvk])
                else:
                    S.add("dve", lambda e, t=t, bi=bi, kr=kr: e.tensor_copy(out=kr[:, t, 640:1152], in_=B[bi][:, :]), reads=[kB(bi)], writes=[kvk])
            S.add("sp", lambda e, kr=kr, ch=ch: e.dma_start(out=kv_d[ch * 4:(ch + 1) * 4].rearrange("b p f -> p b f"), in_=kr[:]),
                  reads=[kvk], writes=[("kvd", ch)], dsem=d_kvw[ch % 2])
            S.add("sp", lambda e, sr=sr, ch=ch: e.dma_start(out=sw_d[ch * 4:(ch + 1) * 4].rearrange("b p f -> p b f"), in_=sr[:]),
                  reads=[swk], writes=[("swd", ch)], dsem=d_sww[ch % 2])

        emit_zfill(nzb)
        S.barrier()
        e_k.close()
        e_q = ExitStack()
        cur[0] = e_q
        sinkbc = sb("sinkbc", [128, 512], F32)
        g1bc = sb("g1bc", [128, 1024], F32)
        b1bc = sb("b1bc", [128, 1024], F32)
        gabc = sb("gabc", [128, 1024], F32)
        babc = sb("babc", [128, 1024], F32)
        WQ = sb("WQ_s", [128, 8, 1280], BF16)
        WUQ = sb("WUQ_s", [128, 2, 1024], BF16)
        WOA = sb("WOA_s", [128, 4, 1024], BF16)
        WOB = sb("WOB_s", [128, 4, 1024], BF16)
        if n_step > 0:
            cload(g1bc[:], rows_d[0, :].partition_broadcast(128), "g1bc", q="sp")
            cload(b1bc[:], rows_d[1, :].partition_broadcast(128), "b1bc", q="sp")
            cload(WQ[:], WQ_d.rearrange("(k p) f -> p k f", p=128), "WQ")
            cload(WUQ[:], WUQ_d.rearrange("(k p) f -> p k f", p=128), "WUQ")
            cload(WOA[:], WOA_d.rearrange("p (c f) -> p c f", c=4), "WOA")
            cload(WOB[:], WOB_d.rearrange("p (c f) -> p c f", c=4), "WOB")
            cload(gabc[:], rows_d[2, :].partition_broadcast(128), "gabc", q="sp")
            cload(babc[:], rows_d[3, :].partition_broadcast(128), "babc", q="sp")
            S.add("pool", lambda e: e.tensor_scalar(out=gabc[:], in0=gabc[:], scalar1=ALPHA, scalar2=None, op0=ALU.mult), reads=["gabc"], writes=["gabc"])
            S.add("pool", lambda e: e.tensor_scalar(out=babc[:], in0=babc[:], scalar1=ALPHA, scalar2=None, op0=ALU.mult), reads=["babc"], writes=["babc"])
            S.add("act", lambda e: e.activation(out=sinkexp[:], in_=vec[:, VSK:VSK + 4], func=AF.Exp), reads=["vec"], writes=["sinkexp"])
            for c in range(4):
                S.add("dve", lambda e, c=c: e.tensor_scalar(out=sinkbc[:, c * 128:(c + 1) * 128], in0=maskC[:, 0:128], scalar1=0.0, scalar2=sinkexp[:, c:c + 1], op0=ALU.mult, op1=ALU.add),
                      reads=["maskC", "sinkexp"], writes=["sinkbc"])
        qaT = sb("qaT", [128, 4, 512], BF16)
        cqn = sb("cqn", [128, 2, 512], BF16)
        qnT = sb("qnT", [128, 4, 512], BF16)
        qrT = sb("qrT", [128, 4, 512], BF16)
        swt = sb("swt", [128, 5, SWW], BF16)
        d_swt = sem("d_swt")
        NKR = 4
        kvt = [sb("kvt%d" % i, [128, KVW], BF16) for i in range(NKR)]
        d_kvt = [sem("d_kvt%d" % i) for i in range(NKR)]
        NPB = 8
        Pb = [sb("Pb%d" % i, [128, 512], BF16) for i in range(NPB)]
        accs = sb("accs", [128, 4, 512], F32)
        rec = sb("rec", [128, 512], F32)
        aT = sb("aT", [128, 4, 512], F32)
        anT = sb("anT", [128, 4, 512], BF16)
        bT = sb("bT", [128, 4, 512], F32)
        bnT = sb("bnT", [128, 4, 512], BF16)
        rbuf = [accs[:, 0:2, :].rearrange("p h q -> p (h q)"), accs[:, 2:4, :].rearrange("p h q -> p (h q)")]
        d_h1 = [sem("d_h1_%d" % i) for i in range(2)]
        kvi = [0]
        pbi = [0]
        scale_mla = 192.0 ** -0.5

        if n_step > 0:
            S.add("pool", lambda e: e.memset(qrT[:], 0.0), writes=[("qrT", h) for h in range(4)])
        next_slots = chunk_loads(2) if n_step > 0 else None
        def swt_load(ch):
            S.add("sp", lambda e: e.dma_start(out=swt[:], in_=sw_d[ch * 4 - 1:ch * 4 + 4].rearrange("b p f -> p b f")),
                  reads=[("swd", ch - 1), ("swd", ch)], writes=["swt"], dsem=d_swt)

        def qa_proj(use_t):
            for c in range(4):
                if use_t:
                    qp = (T[0][:, :].bitcast(F32), kT(0)); rp = (T[1][:, :].bitcast(F32), kT(1))
                else:
                    qp = (B[2 * (c % 2)][:, :], kB(2 * (c % 2))); rp = (B[2 * (c % 2) + 1][:, :], kB(2 * (c % 2) + 1))
                projP(qp[0], qp[1], WQ, c * 128, "WQ")
                projP(rp[0], rp[1], WQ, 512 + c * 128, "WQ")
                rope_applyP(qp[0], qp[1], rp[0], rp[1], qaT[:, c, :], ("qaT", c), c % 2)

        hoisted = False
        pending_tail = []
        HOIST = _os.environ.get("KDBG_NOHOIST", "0") != "1"
        for j in range(n_step):
            ch = 2 * j + 2
            slots = next_slots
            if not hoisted:
                rope_loads(ch)
                swt_load(ch)
            if not hoisted:
                front(slots, True)
                qa_proj(False)
            HS = []
            S.capture = []
            proj(4, WQ, 1024, 128, "WQ")
            proj(5, WQ, 1152, 128, "WQ")
            rms_feat([B[4][:, :], B[5][:, :]], VQG, 256.0, cqn, [("cqn", 0), ("cqn", 1)], 0, in_keys=[kB(4), kB(5)])
            HS.append(S.capture); S.capture = []
            for h in range(4):
                bi = 1 + h
                for k in range(2):
                    S.add("pe", lambda e, h=h, k=k, bi=bi: e.matmul(B[bi][:, :], lhsT=WUQ[:, k, h * 128:(h + 1) * 128], rhs=cqn[:, k, :], start=(k == 0), stop=(k == 1)),
                          reads=[("cqn", k), "WUQ"], writes=[kB(bi)])
                if h % 2 == 0:
                    S.add("act", lambda e, h=h, bi=bi: e.activation(out=qnT[:, h, :], in_=B[bi][:, :], func=AF.Copy), reads=[kB(bi)], writes=[("qnT", h)])
                else:
                    S.add("dve", lambda e, h=h, bi=bi: e.tensor_copy(out=qnT[:, h, :], in_=B[bi][:, :]), reads=[kB(bi)], writes=[("qnT", h)])
            HS.append(S.capture); S.capture = []
            for pr in range(2):
                bq, br = (0, 5) if pr == 0 else (1, 2)
                for k in range(2):
                    S.add("pe", lambda e, pr=pr, k=k, bq=bq: e.matmul(B[bq][:, :], lhsT=WUQ[:, k, 512 + pr * 128:512 + (pr + 1) * 128], rhs=cqn[:, k, :], start=(k == 0), stop=(k == 1)),
                          reads=[("cqn", k), "WUQ"], writes=[kB(bq)])
                for k in range(2):
                    S.add("pe", lambda e, pr=pr, k=k, br=br: e.matmul(B[br][:, :], lhsT=WUQ[:, k, 768 + pr * 128:768 + (pr + 1) * 128], rhs=cqn[:, k, :], start=(k == 0), stop=(k == 1)),
                          reads=[("cqn", k), "WUQ"], writes=[kB(br)])
                S.add("dve", lambda e, bq=bq, pr=pr: e.tensor_tensor(out=t1[pr][:, :], in0=B[bq][:, :], in1=cs_t[:, :], op=ALU.mult), reads=[kB(bq), "cs"], writes=[("t1", pr)])
                S.add("dve", lambda e, br=br, pr=pr: e.tensor_tensor(out=t2[pr][:, :], in0=B[br][:, :], in1=sn_t[:, :], op=ALU.mult), reads=[kB(br), "sn"], writes=[("t2", pr)])
                for hh in range(2):
                    S.add("pool", lambda e, pr=pr, hh=hh: e.tensor_tensor(out=qrT[hh * 64:(hh + 1) * 64, 2 * pr + hh, :], in0=t1[pr][hh * 64:(hh + 1) * 64, :], in1=t2[pr][hh * 64:(hh + 1) * 64, :], op=ALU.add),
                          reads=[("t1", pr), ("t2", pr)], writes=[("qrT", 2 * pr + hh)])
            HS.append(S.capture); S.capture = []
            swa_p = {}
            OS = [(B[4][:, :], kB(4), B[5][:, :], kB(5)), (T[0][:, :].bitcast(F32), kT(0), T[1][:, :].bitcast(F32), kT(1))]

            def swa_st(qb):
                for g in range(2):
                    for kk in range(2):
                        kbi = qb + kk
                        bi = g * 2 + kk
                        S.add("pe", lambda e, g=g, kbi=kbi, bi=bi: e.matmul(B[bi][:, :].rearrange("p (c i) -> p c i", c=4),
                                                                             lhsT=swt[g * 64:(g + 1) * 64, kbi, 0:128],
                                                                             rhs=qaT[g * 64:(g + 1) * 64, :, qb * 128:(qb + 1) * 128], start=True, stop=True),
                              reads=["swt"] + [("qaT", c) for c in range(4)], writes=[kB(bi)])

            def swa_exp(qb):
                swa_p[qb] = []
                for g in range(2):
                    for kk in range(2):
                        kbi = qb + kk
                        slotblk = ch * 4 - 1 + kbi
                        bi = g * 2 + kk
                        pi = pbi[0]; pbi[0] = (pbi[0] + 1) % NPB
                        swa_p[qb].append(pi)
                        S.add("act", lambda e, bi=bi, pi=pi, slotblk=slotblk: e.activation(out=Pb[pi][:], in_=B[bi][:, :], func=AF.Exp,
                                                                                            bias=kbs_t[:, slotblk:slotblk + 1], scale=0.125),
                              reads=[kB(bi), "kbs"], writes=[("Pb", pi)])
                        mk = maskP if kk == 0 else maskC
                        mkk = "maskP" if kk == 0 else "maskC"
                        S.add("dve" if g == 0 else "pool", lambda e, pi=pi, mk=mk: e.tensor_tensor(out=Pb[pi][:], in0=Pb[pi][:], in1=mk[:], op=ALU.mult), reads=[("Pb", pi), mkk], writes=[("Pb", pi)])

            def swa_pv(qb):
                oap, ok_, sap, sk_ = OS[qb % 2]
                u = 0
                for g in range(2):
                    for kk in range(2):
                        kbi = qb + kk
                        pi = swa_p[qb][u]; u += 1
                        first = (g == 0 and kk == 0); last = (g == 1 and kk == 1)
                        S.add("pe", lambda e, g=g, kbi=kbi, pi=pi, first=first, last=last: e.matmul(oap, lhsT=swt[:, kbi, 128 + g * 128:256 + g * 128], rhs=Pb[pi][:],
                                                                                                   start=first, stop=last), reads=["swt", ("Pb", pi)], writes=[ok_])
                        S.add("pe", lambda e, g=g, pi=pi, first=first, last=last: e.matmul(sap, lhsT=onesg[:, g, :], rhs=Pb[pi][:], start=first, stop=last),
                              reads=["onesg%d" % g, ("Pb", pi)], writes=[sk_])

            def swa_epi(qb):
                oap, ok_, sap, sk_ = OS[qb % 2]
                for c in range(4):
                    S.add("act", lambda e, c=c: e.activation(out=rec[:, c * 128:(c + 1) * 128], in_=sap[:, c * 128:(c + 1) * 128], func=AF.Ln, bias=sinkexp[:, c:c + 1], scale=1.0),
                          reads=[sk_, "sinkexp"], writes=["rec"])
                S.add("act", lambda e: e.activation(out=rec[:], in_=rec[:], func=AF.Exp, scale=-1.0), reads=["rec"], writes=["rec"])
                S.add("dve", lambda e: e.tensor_tensor(out=aT[:, :, qb * 128:(qb + 1) * 128], in0=oap.rearrange("p (c i) -> p c i", c=4),
                                                       in1=rec[:].rearrange("p (c i) -> p c i", c=4), op=ALU.mult), reads=[ok_, "rec"], writes=[("aT", qb)])

            swa_st(0); swa_exp(0)
            for qb in range(4):
                if qb + 1 < 4:
                    swa_st(qb + 1)
                swa_pv(qb)
                if qb + 1 < 4:
                    swa_exp(qb + 1)
                swa_epi(qb)
            HS.append(S.capture); S.capture = None
            TS = pending_tail
            pending_tail = []
            for si in range(max(len(HS), len(TS))):
                if si < len(HS):
                    emit_list(HS[si])
                if si < len(TS):
                    emit_list(TS[si])
            if j + 1 < n_step:
                next_slots = chunk_loads(ch + 2)
            aT_keys = [("aT", q) for q in range(4)]
            for c in range(4):
                S.add("act", lambda e, c=c: e.activation(out=sq[:, c, :], in_=aT[:, c, :], func=AF.Square), reads=aT_keys, writes=[("sq", c)])
            for c in range(4):
                S.add("pe", lambda e, c=c: e.matmul(B[0][:, :], lhsT=ones[:], rhs=sq[:, c, :], start=(c == 0), stop=(c == 3)), reads=[("sq", c), "ones"], writes=[kB(0)])
            S.add("act", lambda e: e.activation(out=rstd_t[:], in_=B[0][:, :], func=AF.Ln, bias=epsrms[:, 0:1], scale=1.0 / 512.0), reads=[kB(0), "epsc"], writes=["rstd"])
            S.add("act", lambda e: e.activation(out=rstd_t[:], in_=rstd_t[:], func=AF.Exp, scale=-0.5), reads=["rstd"], writes=["rstd"])
            for c in range(4):
                S.add("dve", lambda e, c=c: e.scalar_tensor_tensor(out=anT[:, c, :], in0=aT[:, c, :], scalar=vec[:, VGA + c:VGA + c + 1], in1=rstd_t[:], op0=ALU.mult, op1=ALU.mult),
                      reads=aT_keys + ["rstd", "vec"], writes=[("anT", c)])
            S.add("pool", lambda e: e.memset(accs[:], 0.0), writes=[("accs", h) for h in range(4)])
            kblocks = [(3, None)] + [(s, None) for s in range(4, ch * 4)] + [(ch * 4 + d, d) for d in range(4)]
            nkb = len(kblocks)
            units = [(idx, sblk, dg, h) for idx, (sblk, dg) in enumerate(kblocks) for h in range(4)]
            kslot = {}

            def mla_st(ui):
                idx, sblk, dg, h = units[ui]
                if h == 0:
                    ks = kvi[0]; kvi[0] = (kvi[0] + 1) % NKR
                    kslot[idx] = ks
                    S.add("sp", lambda e, ks=ks, sblk=sblk: e.dma_start(out=kvt[ks][:], in_=kv_d[sblk]), reads=[("kvd", sblk // 4)], writes=[("kvt", ks)], dsem=d_kvt[ks])
                ks = kslot[idx]
                q0 = 0 if dg is None else dg * 128
                sbk = 4 + (ui % 2)
                hp = (h % 2) * 64
                S.add("pe", lambda e: e.matmul(B[sbk][:, q0:512], lhsT=kvt[ks][:, h * 128:(h + 1) * 128], rhs=qnT[:, h, q0:512], start=True, stop=False),
                      reads=[("kvt", ks), ("qnT", h)], writes=[kB(sbk)])
                S.add("pe", lambda e: e.matmul(B[sbk][:, q0:512], lhsT=kvt[ks][:, 512:640], rhs=qrT[:, h, q0:512], start=False, stop=True),
                      reads=[("kvt", ks), ("qrT", h)], writes=[kB(sbk)])

            def mla_rest(ui):
                idx, sblk, dg, h = units[ui]
                ks = kslot[idx]
                q0 = 0 if dg is None else dg * 128
                sbk = 4 + (ui % 2)
                pi = pbi[0]; pbi[0] = (pbi[0] + 1) % NPB
                S.add("act", lambda e: e.activation(out=Pb[pi][:, q0:512], in_=B[sbk][:, q0:512], func=AF.Exp, bias=kbm_t[:, sblk:sblk + 1], scale=scale_mla),
                      reads=[kB(sbk), "kbm"], writes=[("Pb", pi)])
                if dg is not None:
                    S.add("pool", lambda e: e.tensor_tensor(out=Pb[pi][:, q0:q0 + 128], in0=Pb[pi][:, q0:q0 + 128], in1=maskC[:, 0:128], op=ALU.mult),
                          reads=[("Pb", pi), "maskC"], writes=[("Pb", pi)])
                S.add("pe", lambda e: e.matmul(B[h][:, q0:512], lhsT=kvt[ks][:, 640 + h * 128:640 + (h + 1) * 128], rhs=Pb[pi][:, q0:512],
                                               start=(idx == 0), stop=(idx == nkb - 1), skip_group_check=True),
                      reads=[("kvt", ks), ("Pb", pi)], writes=[kB(h)])
                S.add("pool" if h == 3 else "dve", lambda e: e.tensor_tensor(out=accs[:, h, q0:512], in0=accs[:, h, q0:512], in1=Pb[pi][:, q0:512], op=ALU.add),
                      reads=[("accs", h), ("Pb", pi)], writes=[("accs", h)])

            side = []
            hoisted = False
            if HOIST and j + 1 < n_step:
                S.capture = side
                rope_loads(ch + 2)
                swt_load(ch + 2)
                front(next_slots, True)
                qa_proj(True)
                S.capture = None
                hoisted = True
            nside = len(side)
            per_unit = max(1, -(-nside // max(1, int(len(units) * 0.8) - 4)))
            sp_ = [0]

            def emit_side(n):
                while n > 0 and sp_[0] < nside:
                    eng_, fn_, r_, w_, ds_ = side[sp_[0]]; sp_[0] += 1
                    S.add(eng_, fn_, reads=r_, writes=w_, dsem=ds_)
                    n -= 1

            mla_st(0)
            for ui in range(len(units)):
                if ui + 1 < len(units):
                    mla_st(ui + 1)
                mla_rest(ui)
                if ui >= 4:
                    emit_side(per_unit)
            emit_side(nside)
            for h in range(4):
                sbk = 4 + (h % 2)
                S.add("pe", lambda e, h=h, sbk=sbk: e.matmul(B[sbk][:, :], lhsT=onesf[:], rhs=accs[:, h, :], start=True, stop=True), reads=[("accs", h), "onesf"], writes=[kB(sbk)])
                S.add("act", lambda e, sbk=sbk: e.activation(out=rec[:], in_=B[sbk][:, :], func=AF.Ln), reads=[kB(sbk)], writes=["rec"])
                S.add("act", lambda e: e.activation(out=rec[:], in_=rec[:], func=AF.Exp, scale=-1.0), reads=["rec"], writes=["rec"])
                S.add("dve", lambda e, h=h: e.tensor_tensor(out=bT[:, h, :], in0=B[h][:, :], in1=rec[:], op=ALU.mult), reads=[kB(h), "rec"], writes=[("bT", h)])
            S.capture = []
            rms_feat([bT[:, h, :] for h in range(4)], VGB, 512.0, bnT, [("bnT", h) for h in range(4)], 0, in_keys=[("bT", h) for h in range(4)], src_sb=True)
            pending_tail.append(S.capture); S.capture = None
            for t in range(4):
                S.capture = []
                s = slots[t]
                rb = rbuf[t % 2]
                rk = [("accs", 2 * (t % 2)), ("accs", 2 * (t % 2) + 1)]
                for half in range(2):
                    bi = 1 + 2 * (t % 2) + half
                    for c in range(4):
                        S.add("pe", lambda e, c=c, t=t, bi=bi, half=half: e.matmul(B[bi][:, :], lhsT=anT[:, c, t * 128:(t + 1) * 128], rhs=WOA[:, c, half * 512:(half + 1) * 512],
                                                                                  start=(c == 0), stop=False), reads=[("anT", c), "WOA"], writes=[kB(bi)])
                    for c in range(4):
                        S.add("pe", lambda e, c=c, t=t, bi=bi, half=half: e.matmul(B[bi][:, :], lhsT=bnT[:, c, t * 128:(t + 1) * 128], rhs=WOB[:, c, half * 512:(half + 1) * 512],
                                                                                  start=False, stop=(c == 3)), reads=[("bnT", c), "WOB"], writes=[kB(bi)])
                    S.add("dve", lambda e, bi=bi, half=half, rb=rb, s=s: e.tensor_tensor(out=rb[:, half * 512:(half + 1) * 512], in0=B[bi][:, :], in1=xt[s][:, half * 512:(half + 1) * 512], op=ALU.add),
                          reads=[kB(bi), ("x", s)], writes=rk)
                ln_tok(S, rb, rk, st[t], mv[t], t, g1bc, b1bc, "g1bc", "b1bc", epsln)
                row0 = (j * 4 + t) * 128
                S.add("sp", lambda e, rb=rb, row0=row0: e.dma_start(out=(out_d if dbg_h1 else h1_d)[row0:row0 + 128, :], in_=rb[:]), reads=rk, writes=[("h1d", j * 4 + t)], dsem=d_h1[t % 2])
                pending_tail.append(S.capture); S.capture = None
        for seg_ in pending_tail:
            emit_list(seg_)
        pending_tail = []

        S.barrier()
        e_q.close()
        e_att.close()
        cur[0] = es
        if moe and n_step > 0 and sparse:
            moe_sparse_phase(locals())
        if moe and n_step > 0 and not sparse:
            ntok = n_step * 512
            T_ = min(MOE_T, ntok)
            npass = ntok // T_
            nb = T_ // 128
            WR = sb("WR_s", [128, 8, 36], F32)
            rbias = sb("rbias", [128, 36], F32)
            cload(WR[:], WR_d.rearrange("(k p) f -> p k f", p=128), "WR", q="sp")
            cload(rbias[:], rows_d[4, 0:36].partition_broadcast(128), "rbias", q="sp")
            g2bc = sb("g2bc", [128, 1024], F32)
            b2bc = sb("b2bc", [128, 1024], F32)
            cload(g2bc[:], rows_d[5, :].partition_broadcast(128), "g2bc", q="sp")
            cload(b2bc[:], rows_d[6, :].partition_broadcast(128), "b2bc", q="sp")
            h1T = sb("h1T", [128, 8, T_], BF16)
            h1T32 = sb("h1T32", [128, 8, 128], F32)
            accm = sb("accm", [128, nb, 1024], F32)
            comb = sb("comb", [128, nb, 32], F32)
            hb = [sb("hb%d" % i, [128, 1024], F32) for i in range(2)]
            d_hb = [sem("d_hb%d" % i) for i in range(2)]
            NW = 3
            wg_s = [sb("wg_s%d" % i, [128, 8, 256], BF16) for i in range(NW)]
            wu_s = [sb("wu_s%d" % i, [128, 8, 256], BF16) for i in range(NW)]
            wd_s = [sb("wd_s%d" % i, [128, 2, 1024], BF16) for i in range(NW)]
            d_wg = [sem("d_wg%d" % i) for i in range(NW)]
            d_wu = [sem("d_wu%d" % i) for i in range(NW)]
            d_wd = [sem("d_wd%d" % i) for i in range(NW)]
            sgs = [sb("sg%d" % i, [128, 2, 512], BF16) for i in range(2)]
            hid = [sb("hid%d" % i, [128, 2, 512], BF16) for i in range(2)]
            lg = sb("lg", [128, 36], F32)
            rt = sb("rt", [128, 64], F32)
            d_out = [sem("d_out%d" % i) for i in range(2)]

            def wload(e_, slot):
                S.add("pool", lambda e: e.dma_start(out=wg_s[slot][:], in_=wg_d[e_].rearrange("(k p) f -> p k f", p=128)), writes=[("wg", slot)], dsem=d_wg[slot])
                S.add("pool", lambda e: e.dma_start(out=wu_s[slot][:], in_=wu_d[e_].rearrange("(k p) f -> p k f", p=128)), writes=[("wu", slot)], dsem=d_wu[slot])
                S.add("pool", lambda e: e.dma_start(out=wd_s[slot][:], in_=wd_d[e_].rearrange("(k p) f -> p k f", p=128)), writes=[("wd", slot)], dsem=d_wd[slot])

            for p in range(npass):
                seq = list(range(n_exp))
                for i0 in range(min(NW, n_exp)):
                    wload(seq[i0], i0 % NW)
                S.add("pool", lambda e: e.memset(accm[:], 0.0), writes=[("accm", b) for b in range(nb)])
                for b in range(nb):
                    gb = p * nb + b
                    hb_ = hb[b % 2]; hk = ("hb", b % 2)
                    S.add("sp", lambda e, hb_=hb_, gb=gb: e.dma_start(out=hb_[:], in_=h1_d[gb * 128:(gb + 1) * 128, :]), reads=[("h1d", gb)], writes=[hk], dsem=d_hb[b % 2])
                    for k in range(8):
                        bi = k // 4
                        S.add("pe", lambda e, k=k, bi=bi, hb_=hb_: e.transpose(out=B[bi][:, (k % 4) * 128:(k % 4 + 1) * 128], in_=hb_[:, k * 128:(k + 1) * 128], identity=identf[:]),
                              reads=[hk, "identf"], writes=[kB(bi)])
                    S.add("act", lambda e: e.activation(out=h1T32[:, 0:4, :], in_=B[0][:, :].rearrange("p (k i) -> p k i", k=4), func=AF.Copy),
                          reads=[kB(0)], writes=[("h1T32", 0)])
                    S.add("dve", lambda e: e.tensor_copy(out=h1T32[:, 4:8, :], in_=B[1][:, :].rearrange("p (k i) -> p k i", k=4)),
                          reads=[kB(1)], writes=[("h1T32", 1)])
                    S.add("pool", lambda e, b=b: e.tensor_copy(out=h1T[:, :, b * 128:(b + 1) * 128], in_=h1T32[:, :, :]),
                          reads=[("h1T32", 0), ("h1T32", 1)], writes=[("h1T", b)])
                    for k in range(8):
                        S.add("pe", lambda e, k=k: e.matmul(B[2][:, 0:36], lhsT=h1T32[:, k, :], rhs=WR[:, k, :], start=(k == 0), stop=(k == 7)),
                              reads=[("h1T32", k // 4), "WR"], writes=[kB(2)])
                    route(S, B[2], kB(2), lg, rt, rbias, comb, b)
                ntg = T_ // 512
                pairs = [(ei, tg) for ei in range(n_exp) for tg in range(ntg)]
                DB = [(B[4][:, :], kB(4)), (B[5][:, :], kB(5)), (T[0][:, :].bitcast(F32), kT(0)), (T[1][:, :].bitcast(F32), kT(1))]
                dbi = [0]

                def moe_gu(pi_, fc):
                    ei, tg = pairs[pi_]
                    slot = ei % NW
                    hkeys = [("h1T", tg * 4 + q) for q in range(4)]
                    for k in range(8):
                        S.add("pe", lambda e, k=k: e.matmul(B[fc][:, :], lhsT=wg_s[slot][:, k, fc * 128:(fc + 1) * 128], rhs=h1T[:, k, tg * 512:(tg + 1) * 512],
                                                            start=(k == 0), stop=(k == 7)), reads=[("wg", slot)] + hkeys, writes=[kB(fc)])
                    for k in range(8):
                        S.add("pe", lambda e, k=k: e.matmul(B[2 + fc][:, :], lhsT=wu_s[slot][:, k, fc * 128:(fc + 1) * 128], rhs=h1T[:, k, tg * 512:(tg + 1) * 512],
                                                            start=(k == 0), stop=(k == 7)), reads=[("wu", slot)] + hkeys, writes=[kB(2 + fc)])
                    sg_ = sgs[pi_ % 2]; hd = hid[pi_ % 2]
                    S.add("act", lambda e: e.activation(out=sg_[:, fc, :], in_=B[fc][:, :], func=AF.Silu), reads=[kB(fc)], writes=[("sg", pi_ % 2, fc)])
                    S.add("dve", lambda e: e.tensor_tensor(out=hd[:, fc, :], in0=B[2 + fc][:, :], in1=sg_[:, fc, :], op=ALU.mult),
                          reads=[kB(2 + fc), ("sg", pi_ % 2, fc)], writes=[("hid", pi_ % 2, fc)])

                def moe_d(pi_):
                    ei, tg = pairs[pi_]
                    slot = ei % NW
                    hd = hid[pi_ % 2]
                    for tb in range(4):
                        b = tg * 4 + tb
                        for half in range(2):
                            dap, dk = DB[dbi[0]]; dbi[0] = (dbi[0] + 1) % len(DB)
                            for fc in range(2):
                                S.add("pe", lambda e, fc=fc, dap=dap, tb=tb, half=half: e.matmul(dap, lhsT=hd[:, fc, tb * 128:(tb + 1) * 128], rhs=wd_s[slot][:, fc, half * 512:(half + 1) * 512],
                                                                                                start=(fc == 0), stop=(fc == 1)),
                                      reads=[("hid", pi_ % 2, 0), ("hid", pi_ % 2, 1), ("wd", slot)], writes=[dk])
                            S.add("dve", lambda e, dap=dap, b=b, half=half: e.scalar_tensor_tensor(out=accm[:, b, half * 512:(half + 1) * 512], in0=dap, scalar=comb[:, b, ei:ei + 1],
                                                                                                  in1=accm[:, b, half * 512:(half + 1) * 512], op0=ALU.mult, op1=ALU.add),
                                  reads=[dk, ("comb", b), ("accm", b)], writes=[("accm", b)])
                    if tg == ntg - 1 and ei + NW < n_exp:
                        wload(ei + NW, slot)

                npairs = len(pairs)
                moe_gu(0, 0); moe_gu(0, 1)
                for pi_ in range(npairs):
                    if pi_ + 1 < npairs:
                        moe_gu(pi_ + 1, 0)
                    moe_d(pi_)
                    if pi_ + 1 < npairs:
                        moe_gu(pi_ + 1, 1)
                for b in range(nb):
                    gb = p * nb + b
                    hb_ = hb[b % 2]; hk = ("hb", b % 2)
                    S.add("sp", lambda e, hb_=hb_, gb=gb: e.dma_start(out=hb_[:], in_=h1_d[gb * 128:(gb + 1) * 128, :]), reads=[("h1d", gb)], writes=[hk], dsem=d_hb[b % 2])
                    S.add("dve", lambda e, hb_=hb_, b=b: e.scalar_tensor_tensor(out=hb_[:], in0=hb_[:], scalar=ALPHA, in1=accm[:, b, :], op0=ALU.mult, op1=ALU.add),
                          reads=[hk, ("accm", b)], writes=[hk])
                    ln_tok(S, hb_, hk, st[b % 4], mv[b % 4], b % 4, g2bc, b2bc, "g2bc", "b2bc", epsln)
                    S.add("sp", lambda e, hb_=hb_, gb=gb: e.dma_start(out=out_d[gb * 128:(gb + 1) * 128, :], in_=hb_[:]), reads=[hk], writes=[("outd", gb)], dsem=d_out[b % 2])

        block = es.enter_context(nc.Block())
        S.emit(block, sems)
    return nc


def ln_tok(S, buf, bkey, st_, mv_, t, gbc, bbc, gk, bk, epsln, geng="pool", beng="pool"):
    bks = list(bkey) if isinstance(bkey, list) else [bkey]
    S.add("dve", lambda e: e.bn_stats(out=st_[:, 0, :], in_=buf[:, 0:512]), reads=bks, writes=[("st", t, 0)])
    S.add("dve", lambda e: e.bn_stats(out=st_[:, 1, :], in_=buf[:, 512:1024]), reads=bks, writes=[("st", t, 1)])
    S.add("dve", lambda e: e.bn_aggr(out=mv_[:, 0:2], in_=st_[:, :, :]), reads=[("st", t, 0), ("st", t, 1)], writes=[("mv", t, 0)])
    S.add("act", lambda e: e.activation(out=mv_[:, 2:3], in_=mv_[:, 1:2], func=AF.Ln, bias=epsln[:, 0:1], scale=1.0), reads=[("mv", t, 0), "epsc"], writes=[("mv", t, 1)])
    S.add("act", lambda e: e.activation(out=mv_[:, 2:3], in_=mv_[:, 2:3], func=AF.Exp, scale=-0.5), reads=[("mv", t, 1)], writes=[("mv", t, 1)])
    S.add("dve", lambda e: e.scalar_tensor_tensor(out=mv_[:, 3:4], in0=mv_[:, 0:1], scalar=-1.0, in1=mv_[:, 2:3], op0=ALU.mult, op1=ALU.mult),
          reads=[("mv", t, 0), ("mv", t, 1)], writes=[("mv", t, 2)])
    S.add("act", lambda e: e.activation(out=buf[:], in_=buf[:], func=AF.Identity, bias=mv_[:, 3:4], scale=mv_[:, 2:3]), reads=bks + [("mv", t, 1), ("mv", t, 2)], writes=bks)
    S.add(geng, lambda e: e.tensor_tensor(out=buf[:], in0=buf[:], in1=gbc[:], op=ALU.mult), reads=bks + [gk], writes=bks)
    S.add(beng, lambda e: e.tensor_tensor(out=buf[:], in0=buf[:], in1=bbc[:], op=ALU.add), reads=bks + [bk], writes=bks)


def route(S, Bl, bkey, lg, rt, rbias, comb, b, ohg_out=None, c8_out=None, tag=0, defer=None):
    def add(eng, fn, r, w):
        if defer is None:
            S.add(eng, fn, reads=r, writes=w)
        else:
            defer.append((eng, fn, r, w))

    def D(fn, r, w):
        add("dve", fn, r, w)
    LG = ("lg", tag)
    GM, NGM, GS, GT, M1, M2, DD, ED, DEN, W1, W2 = range(11)
    OHG, ING, OH1, ING2, OH2, C8, GE = 16, 20, 28, 36, 44, 52, 60
    K = ("rt", tag)
    D(lambda e: e.tensor_tensor(out=lg[:], in0=Bl[:, 0:36], in1=rbias[:], op=ALU.add), [bkey, "rbias"], [LG])
    D(lambda e: e.tensor_reduce(out=rt[:, GM:GM + 1], in_=lg[:, 0:4], axis=AX.X, op=ALU.max), [LG], [K])
    D(lambda e: e.tensor_scalar(out=rt[:, NGM:NGM + 1], in0=rt[:, GM:GM + 1], scalar1=-1.0, scalar2=None, op0=ALU.mult), [K], [K])
    add("act", lambda e: e.activation(out=rt[:, GE:GE + 4], in_=lg[:, 0:4], func=AF.Exp, bias=rt[:, NGM:NGM + 1], scale=1.0, accum_out=rt[:, GS:GS + 1]), [LG, K], [K])
    D(lambda e: e.reciprocal(out=rt[:, GT:GT + 1], in_=rt[:, GS:GS + 1]), [K], [K])
    D(lambda e: e.tensor_scalar(out=rt[:, OHG:OHG + 4], in0=lg[:, 0:4], scalar1=rt[:, GM:GM + 1], scalar2=None, op0=ALU.is_equal), [LG, K], [K])
    D(lambda e: e.tensor_scalar(out=rt[:, ING:ING + 8], in0=lg[:, 4:12], scalar1=rt[:, OHG:OHG + 1], scalar2=None, op0=ALU.mult), [LG, K], [K])
    for g in range(1, 4):
        D(lambda e, g=g: e.scalar_tensor_tensor(out=rt[:, ING:ING + 8], in0=lg[:, 4 + 8 * g:12 + 8 * g], scalar=rt[:, OHG + g:OHG + g + 1], in1=rt[:, ING:ING + 8], op0=ALU.mult, op1=ALU.add), [LG, K], [K])
    D(lambda e: e.tensor_reduce(out=rt[:, M1:M1 + 1], in_=rt[:, ING:ING + 8], axis=AX.X, op=ALU.max), [K], [K])
    D(lambda e: e.tensor_scalar(out=rt[:, OH1:OH1 + 8], in0=rt[:, ING:ING + 8], scalar1=rt[:, M1:M1 + 1], scalar2=None, op0=ALU.is_equal), [K], [K])
    D(lambda e: e.scalar_tensor_tensor(out=rt[:, ING2:ING2 + 8], in0=rt[:, OH1:OH1 + 8], scalar=-1e30, in1=rt[:, ING:ING + 8], op0=ALU.mult, op1=ALU.add), [K], [K])
    D(lambda e: e.tensor_reduce(out=rt[:, M2:M2 + 1], in_=rt[:, ING2:ING2 + 8], axis=AX.X, op=ALU.max), [K], [K])
    D(lambda e: e.tensor_scalar(out=rt[:, OH2:OH2 + 8], in0=rt[:, ING2:ING2 + 8], scalar1=rt[:, M2:M2 + 1], scalar2=None, op0=ALU.is_equal), [K], [K])
    D(lambda e: e.tensor_tensor(out=rt[:, DD:DD + 1], in0=rt[:, M2:M2 + 1], in1=rt[:, M1:M1 + 1], op=ALU.subtract), [K], [K])
    add("act", lambda e: e.activation(out=rt[:, ED:ED + 1], in_=rt[:, DD:DD + 1], func=AF.Exp), [K], [K])
    D(lambda e: e.tensor_scalar(out=rt[:, DEN:DEN + 1], in0=rt[:, ED:ED + 1], scalar1=1.0, scalar2=None, op0=ALU.add), [K], [K])
    D(lambda e: e.reciprocal(out=rt[:, DEN:DEN + 1], in_=rt[:, DEN:DEN + 1]), [K], [K])
    D(lambda e: e.tensor_tensor(out=rt[:, W1:W1 + 1], in0=rt[:, GT:GT + 1], in1=rt[:, DEN:DEN + 1], op=ALU.mult), [K], [K])
    D(lambda e: e.tensor_tensor(out=rt[:, W2:W2 + 1], in0=rt[:, W1:W1 + 1], in1=rt[:, ED:ED + 1], op=ALU.mult), [K], [K])
    D(lambda e: e.tensor_scalar(out=rt[:, C8:C8 + 8], in0=rt[:, OH1:OH1 + 8], scalar1=rt[:, W1:W1 + 1], scalar2=None, op0=ALU.mult), [K], [K])
    D(lambda e: e.scalar_tensor_tensor(out=rt[:, C8:C8 + 8], in0=rt[:, OH2:OH2 + 8], scalar=rt[:, W2:W2 + 1], in1=rt[:, C8:C8 + 8], op0=ALU.mult, op1=ALU.add), [K], [K])
    if ohg_out is not None:
        D(lambda e: e.tensor_copy(out=ohg_out[:, b, :], in_=rt[:, OHG:OHG + 4]), [K], [("OHG", b)])
        D(lambda e: e.tensor_copy(out=c8_out[:, b, :], in_=rt[:, C8:C8 + 8]), [K], [("C8", b)])
        return
    for g in range(4):
        D(lambda e, g=g: e.tensor_scalar(out=comb[:, b, 8 * g:8 * g + 8], in0=rt[:, C8:C8 + 8], scalar1=rt[:, OHG + g:OHG + g + 1], scalar2=None, op0=ALU.mult), [K], [("comb", b)])


def _rot_perm64():
    return (np.arange(64) + 32) % 64


def host_layout(inputs):
    f = np.float32
    x = np.asarray(inputs["x"], f)
    meta = np.asarray(inputs["meta_tokens"], f)
    w_in = np.asarray(inputs["w_in"], f)[0]
    rp = _rot_perm64()
    q_a = w_in[:, 0:512]; k_a = w_in[:, 512:640]; v_a = w_in[:, 640:768]
    c_q = w_in[:, 768:1024]; c_kv = w_in[:, 1024:1280]; k_r = w_in[:, 1280:1344]
    k_a_rot = np.concatenate([k_a[:, h * 64:(h + 1) * 64][:, rp] for h in range(2)], axis=1)
    k_r_rot = k_r[:, rp]
    WK = np.concatenate([k_a, k_a_rot, c_kv, v_a, k_r, k_r, k_r_rot, k_r_rot], axis=1)
    qcols, qrcols = [], []
    for c in range(4):
        for h in (c, 4 + c):
            blk = q_a[:, h * 64:(h + 1) * 64]
            qcols.append(blk); qrcols.append(blk[:, rp])
    WQ = np.concatenate(qcols + qrcols + [c_q], axis=1)
    w_uq = np.asarray(inputs["mla_w_uq"], f)[0]
    nope = [w_uq[:, h * 192:h * 192 + 128] for h in range(4)]
    rope = [w_uq[:, h * 192 + 128:h * 192 + 192] for h in range(4)]
    WUQ = np.concatenate(nope + rope + [r[:, rp] for r in rope], axis=1)
    w_ukv = np.asarray(inputs["mla_w_ukv"], f)[0]
    WUK = np.concatenate([w_ukv[:, h * 256:h * 256 + 128] for h in range(4)], axis=1)
    WUV = np.concatenate([w_ukv[:, h * 256 + 128:h * 256 + 256] for h in range(4)], axis=1)
    w_o = np.asarray(inputs["w_o"], f)[0]
    WOA = np.zeros((128, 4, 1024), f)
    for g in range(2):
        for c in range(4):
            h = 4 * g + c
            WOA[g * 64:(g + 1) * 64, c, :] = w_o[h * 64:(h + 1) * 64, :]
    WOB = np.zeros((128, 4, 1024), f)
    for h in range(4):
        WOB[:, h, :] = w_o[512 + h * 128:512 + (h + 1) * 128, :]
    WR = np.concatenate([np.asarray(inputs["moe_w_group"], f)[0], np.asarray(inputs["moe_w_router"], f)[0]], axis=1)
    vec = np.zeros((128, 40), f)
    vec[:, 0:8] = np.asarray(inputs["ln_in_g"], f).reshape(8, 128).T
    vec[:, 8:16] = np.asarray(inputs["ln_in_b"], f).reshape(8, 128).T
    vec[:, 16:18] = np.asarray(inputs["mla_q_norm_g"], f)[0].reshape(2, 128).T
    vec[:, 18:20] = np.asarray(inputs["mla_kv_norm_g"], f)[0].reshape(2, 128).T
    ga = np.asarray(inputs["swa_out_norm_g"], f)[0]
    sk = np.asarray(inputs["swa_sinks"], f)[0]
    for g in range(2):
        for c in range(4):
            h = 4 * g + c
            vec[g * 64:(g + 1) * 64, 20 + c] = ga[h * 64:(h + 1) * 64]
            vec[g * 64:(g + 1) * 64, 28 + c] = sk[h]
    vec[:, 24:28] = np.asarray(inputs["mla_out_norm_g"], f)[0].reshape(4, 128).T
    rows = np.zeros((7, 1024), f)
    rows[0] = np.asarray(inputs["ln1_g"], f)[0]; rows[1] = np.asarray(inputs["ln1_b"], f)[0]
    rows[2] = np.asarray(inputs["ln_in_g"], f); rows[3] = np.asarray(inputs["ln_in_b"], f)
    rows[4, 0:4] = np.asarray(inputs["moe_b_group"], f)[0]; rows[4, 4:36] = np.asarray(inputs["moe_b_router"], f)[0]
    rows[5] = np.asarray(inputs["ln2_g"], f)[0]; rows[6] = np.asarray(inputs["ln2_b"], f)[0]
    cst = np.zeros((128, 1536), f)
    cst[:, 0:128] = np.eye(128, dtype=f)
    cst[:, 128:256] = 1.0
    cst[:, 256:320] = 1.0
    cst[:, 448:512] = 1.0
    p = np.arange(128)[:, None]; i = np.arange(128)[None, :]
    cst[:, 512:1024] = np.tile((p > i).astype(f), (1, 4))
    cst[:, 1024:1536] = np.tile((p <= i).astype(f), (1, 4))
    cst2 = np.zeros((128, 128 + 8 + NSLOT), f)
    cst2[:, 0:128] = (p < i).astype(f)
    cst2[:, 128:136] = np.arange(8)[None, :] * 128 + np.arange(128)[:, None]
    cst2[:, 136:136 + NSLOT] = (np.arange(NSLOT) * SLOT)[None, :]
    blk0 = np.zeros((128, 1024), f); blk0[112:] = meta
    zblk = np.zeros((128, 1024), f)
    pos_blk0 = np.maximum(np.arange(128) - 112, 0).astype(f)
    inv_freq = (10000.0 ** (-np.arange(0, 64, 2, dtype=f) / f(64))).astype(f)
    shared = dict(cst=cst, WK=WK, WQ=WQ, WUQ=WUQ, WUK=WUK, WUV=WUV, WOA=WOA.reshape(128, 4096), WOB=WOB.reshape(128, 4096), WR=WR, vec=vec,
                  rows=rows,
                  wg2=np.asarray(inputs["moe_w_gate"], f)[0].reshape(NEXP, 8, 128, 256).transpose(0, 2, 1, 3).reshape(NEXP * 128, 2048),
                  wu2=np.asarray(inputs["moe_w_up"], f)[0].reshape(NEXP, 8, 128, 256).transpose(0, 2, 1, 3).reshape(NEXP * 128, 2048),
                  wd2=np.asarray(inputs["moe_w_down"], f)[0].reshape(NEXP, 2, 128, 1024).transpose(0, 2, 1, 3).reshape(NEXP * 128, 2048),
                  cst2=cst2)
    shared = {k: np.ascontiguousarray(v) for k, v in shared.items()}
    in_maps = []
    for core in range(8):
        b, hf = core // 2, core % 2
        xb = x[b]
        blocks = [zblk, zblk, zblk, blk0]
        pos = [np.zeros(128, f)] * 3 + [pos_blk0]
        kbm = np.zeros((128, NBLK), f); kbs = np.zeros((128, NBLK), f)
        kbm[:, 0:3] = NEGB; kbm[:112, 3] = NEGB; kbs[:112, 3] = NEGB
        if hf == 0:
            blocks += [zblk, zblk, zblk, blk0]
            pos += [np.zeros(128, f)] * 3 + [pos_blk0]
            kbm[:, 4:8] = NEGB; kbs[:112, 7] = NEGB
        for t in range(64):
            blocks.append(xb[t * 128:(t + 1) * 128])
            pos.append((16 + t * 128 + np.arange(128)).astype(f))
        if hf == 1:
            blocks += [zblk] * 4
            pos += [np.zeros(128, f)] * 4
        xs_ = np.ascontiguousarray(np.concatenate(blocks, axis=0))
        posv = np.concatenate(pos)
        ang = posv[:, None] * inv_freq[None, :]
        cos64 = np.concatenate([np.cos(ang), np.cos(ang)], axis=1)
        sin64 = np.concatenate([-np.sin(ang), np.sin(ang)], axis=1)
        cosT_ = np.ascontiguousarray(np.concatenate([cos64, cos64], axis=1).T.astype(f))
        sinT_ = np.ascontiguousarray(np.concatenate([sin64, sin64], axis=1).T.astype(f))
        m = dict(shared)
        m.update(xs=xs_, cosT=cosT_, sinT=sinT_, kbm=kbm, kbs=kbs)
        in_maps.append(m)
    return in_maps


_NC_CACHE = {}


def kernel(**inputs):
    in_maps = host_layout(inputs)
    if "nc" not in _NC_CACHE:
        _NC_CACHE["nc"] = build_nc()
    nc = _NC_CACHE["nc"]
    res = run_bass_kernel_spmd(nc, in_maps, core_ids=list(range(8)))
    out = np.zeros((4, 8192, 1024), np.float32)
    for core in range(8):
        b, hf = core // 2, core % 2
        o = res.results[core]["out"]
        for j in range(NSTEP):
            xc = 2 * j + hf
            out[b, xc * 512:(xc + 1) * 512] = o[j * 512:(j + 1) * 512]
    return out


def moe_sparse_phase(L):
    S = L["S"]; sb = L["sb"]; sem = L["sem"]; cload = L["cload"]; B = L["B"]; T = L["T"]; kB = L["kB"]; kT = L["kT"]
    n_step = L["n_step"]; h1_d = L["h1_d"]; out_d = L["out_d"]; rows_d = L["rows_d"]; WR_d = L["WR_d"]
    identf = L["identf"]; onesf = L["onesf"]; ident = L["ident"]; st = L["st"]; mv = L["mv"]; epsln = L["epsln"]
    wg2_d = L["wg2_d"]; wu2_d = L["wu2_d"]; wd2_d = L["wd2_d"]; cst2_d = L["cst2_d"]; xs_d = L["xs_d"]; ffn_d = L["ffn_d"]
    nblk = n_step * 4
    nslot = nblk // 4 + 4
    assert nslot <= NSLOT

    WR = sb("WR_s", [128, 8, 36], F32)
    rbias = sb("rbias", [128, 36], F32)
    cst2 = sb("cst2_s", [128, 128 + 8 + NSLOT], F32)
    g2bc = sb("g2bc", [128, 1024], F32)
    b2bc = sb("b2bc", [128, 1024], F32)
    cload(WR[:], WR_d.rearrange("(k p) f -> p k f", p=128), "WR", q="sp")
    cload(rbias[:], rows_d[4, 0:36].partition_broadcast(128), "rbias", q="sp")
    cload(cst2[:], cst2_d[:, :], "cst2", q="sp")
    cload(g2bc[:], rows_d[5, :].partition_broadcast(128), "g2bc", q="sp")
    cload(b2bc[:], rows_d[6, :].partition_broadcast(128), "b2bc", q="sp")
    allz = L["allz"]
    UT = cst2[:, 0:128]
    JP = cst2[:, 128:136]
    SLST = cst2[:, 136:136 + NSLOT]
    NHB = 8
    hb = [sb("hb%d" % i, [128, 1024], F32) for i in range(NHB)]
    d_hb = [sem("d_hb%d" % i) for i in range(NHB)]
    d_out = [sem("d_out%d" % i) for i in range(6)]
    h1T32 = sb("h1T32", [128, 8, 128], F32)
    OHGt = sb("OHGt", [128, 32, 4], F32)
    C8t = sb("C8t", [128, 32, 8], F32)
    CS = sb("CS", [128, 32, 4], F32)
    PRE = sb("PRE", [128, 32, 4], F32)
    OFF = sb("OFF", [128, 32, 4], F32)
    sm = sb("sm", [128, 64], F32)
    POSF = sb("POSF", [128, 32], F32)
    POSI = sb("POSI", [128, 32], U32)
    WIDXF = sb("WIDXF", [128, NSLOT, 8], F32)
    WIDX = sb("WIDX", [128, NSLOT, 8], U32)
    if nblk < 32:
        S.add("dve", lambda e: e.memset(OHGt[:], 0.0), writes=[("OHG", b) for b in range(32)])

    GRP = 4
    lg4 = [sb("lg4_%d" % i, [128, 36], F32) for i in range(GRP)]
    rt4 = [sb("rt4_%d" % i, [128, 64], F32) for i in range(GRP)]
    for g0 in range(0, nblk, GRP):
        chains = []
        for i in range(GRP):
            b = g0 + i
            hb_ = hb[b % NHB]; hk = ("hb", b % NHB)
            rb_ = 2 + i
            S.add("sp", lambda e, hb_=hb_, b=b: e.dma_start(out=hb_[:], in_=h1_d[b * 128:(b + 1) * 128, :]), reads=[("h1d", b)], writes=[hk], dsem=d_hb[b % NHB])
            for k in range(8):
                bi = k // 4
                S.add("pe", lambda e, k=k, bi=bi, hb_=hb_: e.transpose(out=B[bi][:, (k % 4) * 128:(k % 4 + 1) * 128], in_=hb_[:, k * 128:(k + 1) * 128], identity=identf[:]),
                      reads=[hk, "identf"], writes=[kB(bi)])
            S.add("act", lambda e: e.activation(out=h1T32[:, 0:4, :], in_=B[0][:, :].rearrange("p (k i) -> p k i", k=4), func=AF.Copy), reads=[kB(0)], writes=[("h1T32", 0)])
            S.add("act", lambda e: e.activation(out=h1T32[:, 4:8, :], in_=B[1][:, :].rearrange("p (k i) -> p k i", k=4), func=AF.Copy), reads=[kB(1)], writes=[("h1T32", 1)])
            for k in range(8):
                S.add("pe", lambda e, k=k, rb_=rb_: e.matmul(B[rb_][:, 0:36], lhsT=h1T32[:, k, :], rhs=WR[:, k, :], start=(k == 0), stop=(k == 7)),
                      reads=[("h1T32", k // 4), "WR"], writes=[kB(rb_)])
            ch_ = []
            route(S, B[rb_], kB(rb_), lg4[i], rt4[i], rbias, None, b, ohg_out=OHGt, c8_out=C8t, tag=i, defer=ch_)
            chains.append(ch_)
        for opi in range(max(len(c) for c in chains)):
            for c in chains:
                if opi < len(c):
                    eng, fn, r, w = c[opi]
                    S.add(eng, fn, reads=r, writes=w)
    allohg = [("OHG", b) for b in range(32)]
    flat = lambda t: t[:, :, :].rearrange("p b g -> p (b g)")
    S.add("pe", lambda e: e.matmul(B[3][:, 0:128], lhsT=onesf[:], rhs=flat(OHGt), start=True, stop=True), reads=allohg + ["onesf"], writes=[kB(3)])
    S.add("pe", lambda e: e.matmul(B[4][:, 0:128], lhsT=UT, rhs=flat(OHGt), start=True, stop=True), reads=allohg + ["cst2"], writes=[kB(4)])
    S.add("dve", lambda e: e.tensor_copy(out=flat(CS), in_=B[3][:, 0:128]), reads=[kB(3)], writes=["CS"])
    S.add("act", lambda e: e.activation(out=flat(PRE), in_=B[4][:, 0:128], func=AF.Copy), reads=[kB(4)], writes=["PRE"])
    D = lambda fn, r, w: S.add("dve", fn, reads=r, writes=w)
    NG, PC, BASE, END, TMP8, GS, AA = 0, 4, 8, 12, 16, 24, 36
    D(lambda e: e.tensor_reduce(out=sm[:, NG:NG + 4], in_=CS[:, :, :].rearrange("p b g -> p g b"), axis=AX.X, op=ALU.add), ["CS"], ["sm"])
    for g in range(4):
        D(lambda e, g=g: e.tensor_scalar(out=sm[:, TMP8:TMP8 + 8], in0=SLST[:, 0:8], scalar1=sm[:, NG + g:NG + g + 1], scalar2=None, op0=ALU.is_lt), ["sm", "cst2"], ["sm"])
        D(lambda e, g=g: e.tensor_reduce(out=sm[:, PC + g:PC + g + 1], in_=sm[:, TMP8:TMP8 + 8], axis=AX.X, op=ALU.add), ["sm"], ["sm"])
    D(lambda e: e.tensor_scalar(out=sm[:, PC:PC + 4], in0=sm[:, PC:PC + 4], scalar1=float(SLOT), scalar2=None, op0=ALU.mult), ["sm"], ["sm"])
    D(lambda e: e.memset(sm[:, BASE:BASE + 1], 0.0), ["sm"], ["sm"])
    for g in range(1, 4):
        D(lambda e, g=g: e.tensor_tensor(out=sm[:, BASE + g:BASE + g + 1], in0=sm[:, BASE + g - 1:BASE + g], in1=sm[:, PC + g - 1:PC + g], op=ALU.add), ["sm"], ["sm"])
    D(lambda e: e.tensor_tensor(out=sm[:, END:END + 4], in0=sm[:, BASE:BASE + 4], in1=sm[:, PC:PC + 4], op=ALU.add), ["sm"], ["sm"])
    D(lambda e: e.tensor_copy(out=OFF[:, 0, :], in_=sm[:, BASE:BASE + 4]), ["sm"], ["OFF"])
    for b in range(1, 32):
        D(lambda e, b=b: e.tensor_tensor(out=OFF[:, b, :], in0=OFF[:, b - 1, :], in1=CS[:, b - 1, :], op=ALU.add), ["OFF", "CS"], ["OFF"])
    D(lambda e: e.tensor_tensor(out=flat(PRE), in0=flat(PRE), in1=flat(OFF), op=ALU.add), ["PRE", "OFF"], ["PRE"])
    D(lambda e: e.tensor_tensor(out=flat(PRE), in0=flat(PRE), in1=flat(OHGt), op=ALU.mult), ["PRE"] + allohg, ["PRE"])
    D(lambda e: e.tensor_reduce(out=POSF[:], in_=PRE[:, :, :], axis=AX.X, op=ALU.add), ["PRE"], ["POSF"])
    D(lambda e: e.tensor_copy(out=POSI[:], in_=POSF[:]), ["POSF"], ["POSI"])
    D(lambda e: e.tensor_scalar(out=sm[:, GS:GS + NSLOT], in0=SLST, scalar1=sm[:, END:END + 1], scalar2=None, op0=ALU.is_ge), ["sm", "cst2"], ["sm"])
    for g in range(1, 3):
        D(lambda e, g=g: e.scalar_tensor_tensor(out=sm[:, GS:GS + NSLOT], in0=SLST, scalar=sm[:, END + g:END + g + 1], in1=sm[:, GS:GS + NSLOT], op0=ALU.is_ge, op1=ALU.add), ["sm", "cst2"], ["sm"])
    D(lambda e: e.tensor_scalar(out=sm[:, AA:AA + NSLOT], in0=sm[:, GS:GS + NSLOT], scalar1=1024.0, scalar2=None, op0=ALU.mult), ["sm"], ["sm"])
    for s_ in range(NSLOT):
        D(lambda e, s_=s_: e.tensor_scalar(out=WIDXF[:, s_, :], in0=JP, scalar1=sm[:, AA + s_:AA + s_ + 1], scalar2=None, op0=ALU.add), ["sm", "cst2"], ["WIDXF"])
    D(lambda e: e.tensor_copy(out=WIDX[:, :, :].rearrange("p s j -> p (s j)"), in_=WIDXF[:, :, :].rearrange("p s j -> p (s j)")), ["WIDXF"], ["WIDX"])

    NRT = 4
    rowt = [sb("rowt%d" % i, [128, ROWW], F32) for i in range(NRT)]
    d_rl = [sem("d_rl%d" % i) for i in range(NRT)]
    d_rs = [sem("d_rs%d" % i) for i in range(NRT)]
    scat_cap = []
    S.capture = scat_cap
    for b in range(nblk):
        r_ = rowt[b % NRT]; rk = ("rowt", b % NRT)
        S.add("sp", lambda e, r_=r_, b=b: e.dma_start(out=r_[:, 0:1024], in_=h1_d[b * 128:(b + 1) * 128, :]), reads=[("h1d", b)], writes=[rk], dsem=d_rl[b % NRT])
        S.add("dve", lambda e, r_=r_, b=b: e.tensor_copy(out=r_[:, 1024:1032], in_=C8t[:, b, :]), reads=[("C8", b), rk], writes=[(rk, "c")])
        S.add("pool", lambda e, r_=r_, b=b: e.indirect_dma_start(out=xs_d[:, :], out_offset=bass.IndirectOffsetOnAxis(ap=POSI[:, b:b + 1], axis=0), in_=r_[:], in_offset=None),
              reads=[rk, (rk, "c"), "POSI"] + allz, writes=[("xs", b)], dsem=d_rs[b % NRT])
    S.capture = None
    allxs = [("xs", b) for b in range(nblk)]

    NW = 4
    wg_s = [sb("wg_s%d" % i, [128, 2048], BF16) for i in range(NW)]
    wu_s = [sb("wu_s%d" % i, [128, 2048], BF16) for i in range(NW)]
    wd_s = [sb("wd_s%d" % i, [128, 2048], BF16) for i in range(NW)]
    d_wg = [sem("d_wg%d" % i) for i in range(NW)]
    d_wu = [sem("d_wu%d" % i) for i in range(NW)]
    d_wd = [sem("d_wd%d" % i) for i in range(NW)]
    sgs = [sb("sg%d" % i, [128, 2, 512], BF16) for i in range(2)]
    hid = [sb("hid%d" % i, [128, 2, 512], BF16) for i in range(2)]
    ups = [sb("ups%d" % i, [128, 2, 512], F32) for i in range(2)]
    xsb = [sb("xsb%d" % i, [128, ROWW], F32) for i in range(2)]
    d_xl = [sem("d_xl%d" % i) for i in range(2)]
    x16 = [sb("x16_%d" % i, [128, 1024], BF16) for i in range(2)]
    xsT = [sb("xsT%d" % i, [128, 8, 512], BF16) for i in range(2)]
    c8s = [sb("c8s%d" % i, [128, 4, 8], F32) for i in range(2)]
    accs = [sb("maccs%d" % i, [128, 4, 1024], F32) for i in range(2)]
    d_fs = [sem("d_fs%d" % i) for i in range(2)]
    pairs = [(s_, j) for s_ in range(nslot) for j in range(8)]
    npairs = len(pairs)

    def wload(pi_):
        s_, j = pairs[pi_]
        slot = pi_ % NW
        off = bass.IndirectOffsetOnAxis(ap=WIDX[:, s_, j:j + 1], axis=0)
        S.add("pool", lambda e: e.indirect_dma_start(out=wg_s[slot][:], out_offset=None, in_=wg2_d[:, :], in_offset=off), reads=["WIDX"], writes=[("wg", slot)], dsem=d_wg[slot])
        S.add("pool", lambda e: e.indirect_dma_start(out=wu_s[slot][:], out_offset=None, in_=wu2_d[:, :], in_offset=off), reads=["WIDX"], writes=[("wu", slot)], dsem=d_wu[slot])
        S.add("pool", lambda e: e.indirect_dma_start(out=wd_s[slot][:], out_offset=None, in_=wd2_d[:, :], in_offset=off), reads=["WIDX"], writes=[("wd", slot)], dsem=d_wd[slot])

    def slot_prep(s_):
        par = s_ % 2
        for t in range(4):
            xb = xsb[t % 2]; xk = ("xsb", t % 2)
            r0 = s_ * SLOT + t * 128
            S.add("sp", lambda e, xb=xb, r0=r0: e.dma_start(out=xb[:], in_=xs_d[r0:r0 + 128, :]), reads=allxs, writes=[xk], dsem=d_xl[t % 2])
            S.add("act", lambda e, xb=xb, t=t: e.activation(out=x16[t % 2][:], in_=xb[:, 0:1024], func=AF.Copy), reads=[xk], writes=[("x16", t % 2)])
            S.add("pool", lambda e, xb=xb, t=t: e.tensor_copy(out=c8s[par][:, t, :], in_=xb[:, 1024:1032]), reads=[xk], writes=[("c8s", par, t)])
            S.add("act", lambda e, xb=xb, t=t: e.activation(out=accs[par][:, t, :], in_=xb[:, 0:1024], func=AF.Copy, scale=ALPHA), reads=[xk], writes=[("maccs", par, t)])
            for k in range(8):
                Tb = T[k // 4]
                S.add("pe", lambda e, Tb=Tb, k=k, t=t: e.transpose(out=Tb[:, (k % 4) * 128:(k % 4 + 1) * 128], in_=x16[t % 2][:, k * 128:(k + 1) * 128], identity=ident[:]),
                      reads=[("x16", t % 2), "ident"], writes=[kT(k // 4)])
            S.add("act", lambda e, t=t: e.activation(out=xsT[par][:, 0:4, t * 128:(t + 1) * 128], in_=T[0][:, 0:512].rearrange("p (k i) -> p k i", k=4), func=AF.Copy),
                  reads=[kT(0)], writes=[("xsT", par, t)])
            S.add("dve", lambda e, t=t: e.tensor_copy(out=xsT[par][:, 4:8, t * 128:(t + 1) * 128], in_=T[1][:, 0:512].rearrange("p (k i) -> p k i", k=4)),
                  reads=[kT(1)], writes=[("xsT", par, t, 1)])

    DB = [(B[4][:, :], kB(4)), (B[5][:, :], kB(5)), (T[0][:, :].bitcast(F32), kT(0)), (T[1][:, :].bitcast(F32), kT(1))]
    dbi = [0]

    def moe_gu(pi_, fc):
        s_, j = pairs[pi_]
        slot = pi_ % NW
        par = s_ % 2
        xkeys = [("xsT", par, t) for t in range(4)] + [("xsT", par, t, 1) for t in range(4)]
        for k in range(8):
            S.add("pe", lambda e, k=k: e.matmul(B[fc][:, :], lhsT=wg_s[slot][:, k * 256 + fc * 128:k * 256 + (fc + 1) * 128], rhs=xsT[par][:, k, :], start=(k == 0), stop=(k == 7)),
                  reads=[("wg", slot)] + xkeys, writes=[kB(fc)])
        for k in range(8):
            S.add("pe", lambda e, k=k: e.matmul(B[2 + fc][:, :], lhsT=wu_s[slot][:, k * 256 + fc * 128:k * 256 + (fc + 1) * 128], rhs=xsT[par][:, k, :], start=(k == 0), stop=(k == 7)),
                  reads=[("wu", slot)] + xkeys, writes=[kB(2 + fc)])
        sg_ = sgs[pi_ % 2]; hd = hid[pi_ % 2]; up_ = ups[pi_ % 2]
        S.add("act", lambda e: e.activation(out=sg_[:, fc, :], in_=B[fc][:, :], func=AF.Silu), reads=[kB(fc)], writes=[("sg", pi_ % 2, fc)])
        S.add("act", lambda e: e.activation(out=up_[:, fc, :], in_=B[2 + fc][:, :], func=AF.Copy), reads=[kB(2 + fc)], writes=[("up", pi_ % 2, fc)])
        S.add("pool", lambda e: e.tensor_tensor(out=hd[:, fc, :], in0=up_[:, fc, :], in1=sg_[:, fc, :], op=ALU.mult), reads=[("up", pi_ % 2, fc), ("sg", pi_ % 2, fc)], writes=[("hid", pi_ % 2, fc)])

    def moe_d(pi_):
        s_, j = pairs[pi_]
        slot = pi_ % NW
        par = s_ % 2
        hd = hid[pi_ % 2]
        ac = accs[par]
        for tb in range(4):
            for half in range(2):
                dap, dk = DB[dbi[0]]; dbi[0] = (dbi[0] + 1) % len(DB)
                for fc in range(2):
                    S.add("pe", lambda e, fc=fc, dap=dap, tb=tb, half=half: e.matmul(dap, lhsT=hd[:, fc, tb * 128:(tb + 1) * 128], rhs=wd_s[slot][:, fc * 1024 + half * 512:fc * 1024 + (half + 1) * 512],
                                                                                    start=(fc == 0), stop=(fc == 1)),
                          reads=[("hid", pi_ % 2, 0), ("hid", pi_ % 2, 1), ("wd", slot)], writes=[dk])
                ak = ("maccs", par, tb)
                if True:
                    S.add("dve", lambda e, dap=dap, tb=tb, half=half: e.scalar_tensor_tensor(out=ac[:, tb, half * 512:(half + 1) * 512], in0=dap, scalar=c8s[par][:, tb, j:j + 1],
                                                                                            in1=ac[:, tb, half * 512:(half + 1) * 512], op0=ALU.mult, op1=ALU.add),
                          reads=[dk, ("c8s", par, tb), ak], writes=[ak])
        if j == 7:
            S.add("sp", lambda e: e.dma_start(out=ffn_d[s_ * SLOT:(s_ + 1) * SLOT, :].rearrange("(t p) f -> p t f", p=128), in_=ac[:]),
                  reads=[("maccs", par, tb) for tb in range(4)], writes=[("ffn", s_)], dsem=d_fs[par])
        if pi_ + NW < npairs:
            wload(pi_ + NW)

    for i0 in range(min(NW, npairs)):
        wload(i0)
    for eng_, fn_, r_, w_, ds_ in scat_cap:
        S.add(eng_, fn_, reads=r_, writes=w_, dsem=ds_)
    slot_prep(0)
    moe_gu(0, 0); moe_gu(0, 1)
    for pi_ in range(npairs):
        s_, j = pairs[pi_]
        if j == 2 and s_ + 1 < nslot:
            slot_prep(s_ + 1)
        if pi_ + 1 < npairs:
            moe_gu(pi_ + 1, 0)
        moe_d(pi_)
        if pi_ + 1 < npairs:
            moe_gu(pi_ + 1, 1)
    allffn = [("ffn", s_) for s_ in range(nslot)]

    fb = [t_[:, 0:1024] for t_ in (rowt + xsb)]
    ND = len(fb)
    d_fb = [sem("d_fb%d" % i) for i in range(ND)]
    LOOK = ND - 1
    def ln_parts(b):
        fb_ = fb[b % ND]; fk = ("fb", b % ND)
        cap = []
        S.capture = cap
        ln_tok(S, fb_, fk, st[b % 4], mv[b % 4], b % 4, g2bc, b2bc, "g2bc", "b2bc", epsln, geng="dve", beng="dve")
        S.add("sp", lambda e: e.dma_start(out=out_d[b * 128:(b + 1) * 128, :], in_=fb_), reads=[fk], writes=[("outd", b)], dsem=d_out[b % ND])
        S.capture = None
        return cap[:7], cap[7:]

    def emit_(lst):
        for eng_, fn_, r_, w_, ds_ in lst:
            S.add(eng_, fn_, reads=r_, writes=w_, dsem=ds_)

    for b in range(min(ND, nblk)):
        fb_ = fb[b % ND]; fk = ("fb", b % ND)
        S.add("pool", lambda e, fb_=fb_, b=b: e.indirect_dma_start(out=fb_, out_offset=None, in_=ffn_d[:, :], in_offset=bass.IndirectOffsetOnAxis(ap=POSI[:, b:b + 1], axis=0)),
              reads=allffn + ["POSI"], writes=[fk], dsem=d_fb[b % ND])
    prev2 = None
    for b in range(nblk):
        s1, s2 = ln_parts(b)
        emit_(s1)
        if prev2 is not None:
            emit_(prev2)
        prev2 = s2
        nb_ = b + LOOK
        if nb_ < nblk and b >= 1:
            pass
        nb_ = b - 1 + ND
        if b >= 1 and nb_ < nblk:
            fbn = fb[nb_ % ND]; fkn = ("fb", nb_ % ND)
            S.add("pool", lambda e, fbn=fbn, nb_=nb_: e.indirect_dma_start(out=fbn, out_offset=None, in_=ffn_d[:, :], in_offset=bass.IndirectOffsetOnAxis(ap=POSI[:, nb_:nb_ + 1], axis=0)),
                  reads=allffn + ["POSI"], writes=[fkn], dsem=d_fb[nb_ % ND])
    emit_(prev2)
```

```python
import numpy as np
from contextlib import ExitStack
import concourse.bass as bass
import concourse.mybir as mybir
from concourse.bass_utils import run_bass_kernel_spmd

F32 = mybir.dt.float32
BF16 = mybir.dt.bfloat16
AF = mybir.ActivationFunctionType
ALU = mybir.AluOpType
AX = mybir.AxisListType

NCH = 18
NBLK = NCH * 4
NSTEP = 8
LN_EPS = 1e-5
RMS_EPS = 1e-6
ALPHA = 2.0 ** 0.25
NEGB = -30000.0
NEXP = 32
KVW = 1152
SWW = 384
MOE_T = 2048
SLOT = 512
NSLOT = 12
ROWW = 1032
U32 = mybir.dt.uint32


class _Op:
    __slots__ = ("eng", "fn", "idx", "eidx", "deps", "need_inc", "dsem", "dcount", "is_dma", "inc_no", "bar")


class Sched:
    ENG = ("pe", "act", "dve", "pool", "sp")

    def __init__(self):
        self.ops = {e: [] for e in self.ENG}
        self.last_w = {}
        self.readers = {}
        self.seen = {e: {} for e in self.ENG}
        self.seen_dma = {e: set() for e in self.ENG}
        self.dcounts = {}
        self.dsems = {}
        self.n = 0
        self.pending = {e: None for e in self.ENG}
        self.capture = None

    def barrier(self):
        last = [self.ops[e][-1] for e in self.ENG if self.ops[e] and not self.ops[e][-1].is_dma]
        last = []
        for e in self.ENG:
            for op in reversed(self.ops[e]):
                if not op.is_dma:
                    last.append(op); break
        for op in last:
            op.need_inc = True
        dm = [(self.dsems[k], c) for k, c in self.dcounts.items()]
        for e in self.ENG:
            self.pending[e] = (last, dm)

    def add(self, eng, fn, reads=(), writes=(), dsem=None):
        if self.capture is not None:
            self.capture.append((eng, fn, list(reads), list(writes), dsem))
            return None
        op = _Op()
        op.eng = eng; op.fn = fn; op.idx = self.n; self.n += 1
        op.eidx = len(self.ops[eng]); op.need_inc = False; op.dsem = dsem
        op.is_dma = dsem is not None; op.inc_no = None
        if op.is_dma:
            self.dcounts[id(dsem)] = self.dcounts.get(id(dsem), 0) + 16
            self.dsems[id(dsem)] = dsem
            op.dcount = self.dcounts[id(dsem)]
        deps = []
        for k in reads:
            w = self.last_w.get(k)
            if w is not None:
                deps.append((w, "raw"))
            if isinstance(k, tuple) and k[0] in ("B", "T"):
                for r in self.readers.get(k, ()):
                    if r.eng != eng:
                        deps.append((r, "war"))
        for k in writes:
            w = self.last_w.get(k)
            if w is not None:
                deps.append((w, "waw"))
            for r in self.readers.get(k, ()):
                deps.append((r, "war"))
        final_eng = {}
        final_dma = []
        for d, kind in deps:
            if d is op:
                continue
            if d.is_dma:
                if d.idx not in self.seen_dma[eng]:
                    self.seen_dma[eng].add(d.idx)
                    final_dma.append(d)
                continue
            if d.eng == eng and not op.is_dma:
                if eng == "pe":
                    continue
                if kind != "raw":
                    continue
            if d.eidx <= self.seen[eng].get(d.eng, -1):
                continue
            if d.eng not in final_eng or final_eng[d.eng].eidx < d.eidx:
                final_eng[d.eng] = d
        for f, d in final_eng.items():
            self.seen[eng][f] = d.eidx
            d.need_inc = True
        op.deps = list(final_eng.values()) + final_dma
        op.bar = self.pending[eng]
        if op.bar is not None:
            self.pending[eng] = None
            for d in op.bar[0]:
                if d.eng != eng:
                    self.seen[eng][d.eng] = max(self.seen[eng].get(d.eng, -1), d.eidx)
        for k in writes:
            self.last_w[k] = op
            self.readers[k] = []
        for k in reads:
            lst = self.readers.setdefault(k, [])
            if not op.is_dma:
                lst[:] = [r for r in lst if r.is_dma or r.eng != eng]
            lst.append(op)
        self.ops[eng].append(op)
        return op

    def emit(self, block, sems):
        for e in self.ENG:
            c = 0
            for op in self.ops[e]:
                if op.need_inc and not op.is_dma:
                    c += 1
                    op.inc_no = c
        me = self

        def run(e, engobj):
            for op in me.ops[e]:
                if op.bar is not None:
                    for d in op.bar[0]:
                        engobj.wait_ge(sems[d.eng], d.inc_no)
                    for sm, cnt in op.bar[1]:
                        engobj.wait_ge(sm, cnt)
                for d in op.deps:
                    if d.is_dma:
                        engobj.wait_ge(d.dsem, d.dcount)
                    else:
                        engobj.wait_ge(sems[d.eng], d.inc_no)
                ins = op.fn(engobj)
                if op.is_dma:
                    ins.then_inc(op.dsem, 16)
                elif op.need_inc:
                    ins.then_inc(sems[e], 1)
            if e == "sp":
                for k, s in me.dsems.items():
                    engobj.wait_ge(s, me.dcounts[k])

        @block.tensor
        def _(eng):
            run("pe", eng)

        @block.scalar
        def _(eng):
            run("act", eng)

        @block.vector
        def _(eng):
            run("dve", eng)

        @block.gpsimd
        def _(eng):
            run("pool", eng)

        @block.sync
        def _(eng):
            run("sp", eng)


def build_nc(n_step=NSTEP, n_kchunk=NCH - 1, moe=True, n_exp=NEXP, dbg_h1=False, sparse=True):
    nc = bass.Bass("TRN2", target_bir_lowering=False)
    S = Sched()

    def din(name, shape, dt=F32):
        return nc.dram_tensor(name, list(shape), dt, kind="ExternalInput").ap()

    xs = din("xs", [NBLK * 128, 1024])
    cosT = din("cosT", [128, NBLK * 128])
    sinT = din("sinT", [128, NBLK * 128])
    kbm = din("kbm", [128, NBLK])
    kbs = din("kbs", [128, NBLK])
    cst = din("cst", [128, 128 * 4 + 512 * 2])
    WK_d = din("WK", [1024, 896])
    WQ_d = din("WQ", [1024, 1280])
    WUQ_d = din("WUQ", [256, 1024])
    WUK_d = din("WUK", [256, 512])
    WUV_d = din("WUV", [256, 512])
    WOA_d = din("WOA", [128, 4 * 1024])
    WOB_d = din("WOB", [128, 4 * 1024])
    WR_d = din("WR", [1024, 36])
    vec_d = din("vec", [128, 40])
    rows_d = din("rows", [7, 1024])
    if sparse:
        wg2_d = din("wg2", [NEXP * 128, 2048])
        wu2_d = din("wu2", [NEXP * 128, 2048])
        wd2_d = din("wd2", [NEXP * 128, 2048])
        cst2_d = din("cst2", [128, 128 + 8 + NSLOT])
        xs_d = nc.dram_tensor("xs_scr", [NSLOT * SLOT, ROWW], F32, kind="Internal").ap()
        ffn_d = nc.dram_tensor("ffn_scr", [NSLOT * SLOT, 1024], F32, kind="Internal").ap()
    else:
        wg_d = din("wg", [NEXP, 1024, 256])
        wu_d = din("wu", [NEXP, 1024, 256])
        wd_d = din("wd", [NEXP, 256, 1024])
    out_d = nc.dram_tensor("out", [NSTEP * 512, 1024], F32, kind="ExternalOutput").ap()
    kv_d = nc.dram_tensor("kv_scr", [NBLK, 128, KVW], BF16, kind="Internal").ap()
    sw_d = nc.dram_tensor("sw_scr", [NBLK, 128, SWW], BF16, kind="Internal").ap()
    h1_d = nc.dram_tensor("h1_scr", [NSTEP * 512, 1024], F32, kind="Internal").ap()

    with ExitStack() as es:
        cur = [es]

        def sb(name, shape, dt):
            return cur[0].enter_context(nc.sbuf_tensor(name, list(shape), dt))

        def ps(name, shape, dt):
            return es.enter_context(nc.psum_tensor(name, list(shape), dt))

        def sem(name):
            return es.enter_context(nc.semaphore(name))

        sems = {e: sem("s_" + e) for e in Sched.ENG}
        B = [ps("B%d" % i, [128, 512], F32) for i in range(6)]
        T = [ps("T%d" % i, [128, 1024], BF16) for i in range(2)]

        def kB(i):
            return ("B", i)

        def kT(i):
            return ("T", i)

        ident = sb("ident", [128, 128], BF16)
        ones = sb("ones", [128, 128], BF16)
        onesg = sb("onesg", [128, 2, 128], BF16)
        maskP = sb("maskP", [128, 512], BF16)
        maskC = sb("maskC", [128, 512], BF16)
        identf = sb("identf", [128, 128], F32)
        onesf = sb("onesf", [128, 128], F32)
        kbm_t = sb("kbm_t", [128, NBLK], F32)
        kbs_t = sb("kbs_t", [128, NBLK], F32)
        vec = sb("vec_s", [128, 40], F32)
        sinkexp = sb("sinkexp", [128, 4], F32)
        epsc = sb("epsc", [128, 2], F32)
        epsln = epsc[:, 0:1]
        epsrms = epsc[:, 1:2]
        S.add("dve", lambda e: e.memset(epsc[:, 0:1], LN_EPS), writes=["epsc0"])
        S.add("dve", lambda e: e.memset(epsc[:, 1:2], RMS_EPS), reads=["epsc0"], writes=["epsc"])
        st = [sb("st%d" % i, [128, 2, 6], F32) for i in range(4)]
        mv = [sb("mv%d" % i, [128, 4], F32) for i in range(4)]
        zt = sb("zt", [128, ROWW], F32)
        d_z = sem("d_z")
        S.add("pool", lambda e: e.memset(zt[:], 0.0), writes=["zt"])
        nzb = NSLOT * SLOT // 128
        zfill = [0]
        allz = [("xsz", i) for i in range(nzb)]

        def emit_zfill(n):
            while sparse and n > 0 and zfill[0] < nzb:
                i = zfill[0]; zfill[0] += 1
                S.add("sp", lambda e, i=i: e.dma_start(out=xs_d[i * 128:(i + 1) * 128, :], in_=zt[:]), reads=["zt"], writes=[("xsz", i)], dsem=d_z)
                n -= 1
        e_att = ExitStack()
        cur[0] = e_att
        d_c = [sem("d_c%d" % i) for i in range(28)]
        _ci = [0]

        import os as _os
        _lim = int(_os.environ.get("KDBG_NCLOAD", "999"))
        _skipms = _os.environ.get("KDBG_SKIPMS", "0") == "1"

        def cload(out, in_, key, q="pool"):
            if _ci[0] >= _lim:
                _ci[0] += 1
                return
            s = d_c[_ci[0]]; _ci[0] += 1
            S.add(q, lambda e: e.dma_start(out=out, in_=in_), writes=[key], dsem=s)

        cload(ident[:], cst[:, 0:128], "ident")
        cload(ones[:], cst[:, 128:256], "ones")
        cload(onesg[:, 0, :], cst[:, 256:384], "onesg0")
        cload(onesg[:, 1, :], cst[:, 384:512], "onesg1")
        cload(maskP[:], cst[:, 512:1024], "maskP")
        cload(maskC[:], cst[:, 1024:1536], "maskC")
        cload(identf[:], cst[:, 0:128], "identf", q="sp")
        cload(onesf[:], cst[:, 128:256], "onesf", q="sp")
        cload(kbm_t[:], kbm[:, :], "kbm", q="sp")
        cload(kbs_t[:], kbs[:, :], "kbs", q="sp")
        cload(vec[:], vec_d[:, :], "vec", q="sp")
        VG, VB, VQG, VKG, VGA, VGB, VSK = 0, 8, 16, 18, 20, 24, 28

        NX = 8
        xt = [sb("xt%d" % i, [128, 1024], F32) for i in range(NX)]
        d_x = [sem("d_x%d" % i) for i in range(NX)]
        xnb = sb("xnb", [128, 4, 1024], BF16)
        hT = sb("hT", [128, 8, 512], BF16)
        cs_t = sb("cs_t", [128, 512], F32)
        sn_t = sb("sn_t", [128, 512], F32)
        d_cs = sem("d_cs"); d_sn = sem("d_sn")
        t1 = [sb("t1_%d" % i, [128, 512], F32) for i in range(2)]
        t2 = [sb("t2_%d" % i, [128, 512], F32) for i in range(2)]
        sq = sb("sq", [128, 4, 512], BF16)
        rstd_t = sb("rstd_t", [128, 512], F32)
        e_k = ExitStack()
        cur[0] = e_k
        WK = sb("WK_s", [128, 8, 896], BF16)
        WUK = sb("WUK_s", [128, 2, 512], BF16)
        WUV = sb("WUV_s", [128, 2, 512], BF16)
        cload(WK[:], WK_d.rearrange("(k p) f -> p k f", p=128), "WK")
        cload(WUK[:], WUK_d.rearrange("(k p) f -> p k f", p=128), "WUK")
        cload(WUV[:], WUV_d.rearrange("(k p) f -> p k f", p=128), "WUV")
        ckvn = sb("ckvn", [128, 2, 512], BF16)
        kvrec = [sb("kvrec%d" % i, [128, 4, KVW], BF16) for i in range(2)]
        swrec = [sb("swrec%d" % i, [128, 4, SWW], BF16) for i in range(2)]
        d_kvw = [sem("d_kvw%d" % i) for i in range(2)]
        d_sww = [sem("d_sww%d" % i) for i in range(2)]

        for i in range(2):
            if not _skipms:
                S.add("pool", (lambda t: (lambda e: e.memset(t[:], 0.0)))(swrec[i]), writes=[("swrec", i)])

        xr = [0]

        def chunk_loads(ch):
            slots = []
            for t in range(4):
                s = xr[0]; xr[0] = (xr[0] + 1) % NX
                slots.append(s)
                blk = ch * 4 + t
                S.add("sp", (lambda s=s, blk=blk: (lambda e: e.dma_start(out=xt[s][:], in_=xs[blk * 128:(blk + 1) * 128, :])))(),
                      writes=[("x", s)], dsem=d_x[s])
            return slots

        def rope_loads(ch):
            S.add("sp", lambda e: e.dma_start(out=cs_t[:], in_=cosT[:, ch * 512:(ch + 1) * 512]), writes=["cs"], dsem=d_cs)
            S.add("sp", lambda e: e.dma_start(out=sn_t[:], in_=sinT[:, ch * 512:(ch + 1) * 512]), writes=["sn"], dsem=d_sn)

        def front(slots, want_res):
            _sub = int(_os.environ.get("KDBG_SUB", "99"))
            for t in range(4):
                s = slots[t]
                x_ = xt[s]
                st_, mv_ = st[t], mv[t]
                S.add("dve", lambda e, x_=x_, st_=st_: e.bn_stats(out=st_[:, 0, :], in_=x_[:, 0:512]), reads=[("x", s)], writes=[("st", t, 0)])
                S.add("dve", lambda e, x_=x_, st_=st_: e.bn_stats(out=st_[:, 1, :], in_=x_[:, 512:1024]), reads=[("x", s)], writes=[("st", t, 1)])
                S.add("dve", lambda e, st_=st_, mv_=mv_: e.bn_aggr(out=mv_[:, 0:2], in_=st_[:, :, :]), reads=[("st", t, 0), ("st", t, 1)], writes=[("mv", t, 0)])
                if _sub < 2:
                    continue
                S.add("act", lambda e, mv_=mv_: e.activation(out=mv_[:, 2:3], in_=mv_[:, 1:2], func=AF.Ln, bias=epsln[:, 0:1], scale=1.0),
                      reads=[("mv", t, 0), "epsc"], writes=[("mv", t, 1)])
                S.add("act", lambda e, mv_=mv_: e.activation(out=mv_[:, 2:3], in_=mv_[:, 2:3], func=AF.Exp, scale=-0.5),
                      reads=[("mv", t, 1)], writes=[("mv", t, 1)])
                S.add("dve", lambda e, mv_=mv_: e.scalar_tensor_tensor(out=mv_[:, 3:4], in0=mv_[:, 0:1], scalar=-1.0, in1=mv_[:, 2:3], op0=ALU.mult, op1=ALU.mult),
                      reads=[("mv", t, 0), ("mv", t, 1)], writes=[("mv", t, 2)])
                if _sub < 3:
                    continue
                S.add("act", lambda e, x_=x_, mv_=mv_, t=t: e.activation(out=xnb[:, t, :], in_=x_[:], func=AF.Identity, bias=mv_[:, 3:4], scale=mv_[:, 2:3]),
                      reads=[("x", s), ("mv", t, 1), ("mv", t, 2)], writes=[("xnb", t)])
                if want_res:
                    S.add("pool", lambda e, x_=x_, mv_=mv_: e.tensor_scalar(out=x_[:], in0=x_[:], scalar1=mv_[:, 2:3], scalar2=mv_[:, 3:4], op0=ALU.mult, op1=ALU.add),
                          reads=[("x", s), ("mv", t, 1), ("mv", t, 2)], writes=[("x", s)])
                    S.add("pool", lambda e, x_=x_: e.tensor_tensor(out=x_[:], in0=x_[:], in1=gabc[:], op=ALU.mult), reads=[("x", s), "gabc"], writes=[("x", s)])
                    S.add("pool", lambda e, x_=x_: e.tensor_tensor(out=x_[:], in0=x_[:], in1=babc[:], op=ALU.add), reads=[("x", s), "babc"], writes=[("x", s)])
            for kp in range(4):
                if _sub < 4:
                    continue
                Tb = T[kp % 2]
                for kk in range(2):
                    k = 2 * kp + kk
                    for t in range(4):
                        S.add("pe", lambda e, Tb=Tb, kk=kk, t=t, k=k: e.transpose(out=Tb[:, (kk * 4 + t) * 128:(kk * 4 + t + 1) * 128],
                                                                                in_=xnb[:, t, k * 128:(k + 1) * 128], identity=ident[:]),
                              reads=[("xnb", t), "ident"], writes=[kT(kp % 2)])
                for kk in range(2):
                    if _sub < 5:
                        continue
                    k = 2 * kp + kk
                    _ev = _os.environ.get("KDBG_EV", "")
                    if (_ev == "act" and kk == 1) or (_ev == "dve" and kk == 0):
                        continue
                    if (kp % 2 == 0 and _ev != "alldve") or _ev == "allact":
                        S.add("act", lambda e, Tb=Tb, kk=kk, k=k: e.activation(out=hT[:, k, :], in_=Tb[:, kk * 512:(kk + 1) * 512], func=AF.Identity,
                                                                                bias=vec[:, VB + k:VB + k + 1], scale=vec[:, VG + k:VG + k + 1]),
                              reads=[kT(kp % 2), "vec"], writes=[("hT", k)])
                    else:
                        S.add("dve", lambda e, Tb=Tb, kk=kk, k=k: e.tensor_scalar(out=hT[:, k, :], in0=Tb[:, kk * 512:(kk + 1) * 512],
                                                                                   scalar1=vec[:, VG + k:VG + k + 1], scalar2=vec[:, VB + k:VB + k + 1],
                                                                                   op0=ALU.mult, op1=ALU.add),
                              reads=[kT(kp % 2), "vec"], writes=[("hT", k)])

        hT_all = [("hT", k) for k in range(8)]

        def proj(bi, W, c0, m, wkey, ncols=512):
            for k in range(8):
                S.add("pe", lambda e, k=k: e.matmul(B[bi][0:m, 0:ncols], lhsT=W[:, k, c0:c0 + m], rhs=hT[:, k, 0:ncols], start=(k == 0), stop=(k == 7)),
                      reads=[("hT", k), wkey], writes=[kB(bi)])

        def projP(pap, pkey, W, c0, wkey):
            for k in range(8):
                S.add("pe", lambda e, k=k: e.matmul(pap, lhsT=W[:, k, c0:c0 + 128], rhs=hT[:, k, :], start=(k == 0), stop=(k == 7)),
                      reads=[("hT", k), wkey], writes=[pkey])

        def rope_applyP(qap, qkey, rap, rkey, out_ap, okey, i):
            S.add("dve", lambda e: e.tensor_tensor(out=t1[i][:, :], in0=qap, in1=cs_t[:, :], op=ALU.mult), reads=[qkey, "cs"], writes=[("t1", i)])
            S.add("dve", lambda e: e.tensor_tensor(out=t2[i][:, :], in0=rap, in1=sn_t[:, :], op=ALU.mult), reads=[rkey, "sn"], writes=[("t2", i)])
            S.add("pool", lambda e: e.tensor_tensor(out=out_ap, in0=t1[i][:, :], in1=t2[i][:, :], op=ALU.add), reads=[("t1", i), ("t2", i)], writes=[okey])

        def rope_apply(bq, br, out_ap, okey, np_=128, i=0, o3=False):
            def v(t):
                a = t[0:np_, :]
                return a.rearrange("p (t c) -> p t c", t=4) if o3 else a
            S.add("dve", lambda e: e.tensor_tensor(out=t1[i][0:np_, :], in0=B[bq][0:np_, :], in1=cs_t[0:np_, :], op=ALU.mult), reads=[kB(bq), "cs"], writes=[("t1", i)])
            S.add("dve", lambda e: e.tensor_tensor(out=t2[i][0:np_, :], in0=B[br][0:np_, :], in1=sn_t[0:np_, :], op=ALU.mult), reads=[kB(br), "sn"], writes=[("t2", i)])
            S.add("pool", lambda e: e.tensor_tensor(out=out_ap, in0=v(t1[i]), in1=v(t2[i]), op=ALU.add), reads=[("t1", i), ("t2", i)], writes=[okey])

        def rms_feat(banks, gcol, nfeat, out_tile, okeys, bsum, in_keys=None, src_sb=None):
            n = len(banks)
            for c in range(n):
                rk = in_keys[c]
                S.add("act", lambda e, c=c: e.activation(out=sq[:, c, :], in_=banks[c], func=AF.Square), reads=[rk], writes=[("sq", c)])
            for c in range(n):
                S.add("pe", lambda e, c=c: e.matmul(B[bsum][:, :], lhsT=ones[:], rhs=sq[:, c, :], start=(c == 0), stop=(c == n - 1)),
                      reads=[("sq", c), "ones"], writes=[kB(bsum)])
            S.add("act", lambda e: e.activation(out=rstd_t[:], in_=B[bsum][:, :], func=AF.Ln, bias=epsrms[:, 0:1], scale=1.0 / nfeat),
                  reads=[kB(bsum), "epsc"], writes=["rstd"])
            S.add("act", lambda e: e.activation(out=rstd_t[:], in_=rstd_t[:], func=AF.Exp, scale=-0.5), reads=["rstd"], writes=["rstd"])
            for c in range(n):
                rk = in_keys[c]
                eng = "dve"
                S.add(eng, lambda e, c=c: e.scalar_tensor_tensor(out=out_tile[:, c, :], in0=banks[c], scalar=vec[:, gcol + c:gcol + c + 1], in1=rstd_t[:],
                                                                  op0=ALU.mult, op1=ALU.mult), reads=[rk, "rstd", "vec"], writes=[okeys[c]])

        def front_parts(slots_):
            cap = []
            S.capture = cap
            front(slots_, False)
            S.capture = None
            fi = next(i for i, o in enumerate(cap) if o[0] == "pe")
            return cap[:fi], cap[fi:]

        def emit_list(lst):
            for eng_, fn_, r_, w_, ds_ in lst:
                S.add(eng_, fn_, reads=r_, writes=w_, dsem=ds_)

        kslots = {}
        if n_kchunk > 0:
            kslots[0] = chunk_loads(0)
            if n_kchunk > 1:
                kslots[1] = chunk_loads(1)
            pa, pb = front_parts(kslots[0])
            emit_list(pa); emit_list(pb)
        for ch in range(n_kchunk):
            rope_loads(ch)
            if ch + 2 < n_kchunk:
                kslots[ch + 2] = chunk_loads(ch + 2)
            emit_zfill(3)
            kr = kvrec[ch % 2]; sr = swrec[ch % 2]
            kvk = ("kvrec", ch % 2); swk = ("swrec", ch % 2)
            proj(0, WK, 0, 128, "WK")
            proj(1, WK, 128, 128, "WK")
            proj(2, WK, 256, 128, "WK")
            proj(3, WK, 384, 128, "WK")
            proj(4, WK, 640, 128, "WK")
            proj(5, WK, 768, 128, "WK")
            if ch + 1 < n_kchunk:
                pa, pb = front_parts(kslots[ch + 1])
                emit_list(pa)
            else:
                pb = []
            vap = T[0][:, :].bitcast(F32)
            for t in range(4):
                for k in range(8):
                    S.add("pe", lambda e, t=t, k=k: e.matmul(vap[:, t * 128:(t + 1) * 128], lhsT=hT[:, k, t * 128:(t + 1) * 128], rhs=WK[:, k, 512:640],
                                                             start=(k == 0), stop=(k == 7)), reads=[("hT", k), "WK"], writes=[kT(0)])
            b4v = vap.rearrange("p (t c) -> p t c", t=4)
            S.add("act", lambda e, sr=sr, b4v=b4v: e.activation(out=sr[:, :, 128:192], in_=b4v[:, :, 0:64], func=AF.Copy), reads=[kT(0)], writes=[swk])
            S.add("act", lambda e, sr=sr, b4v=b4v: e.activation(out=sr[:, :, 320:384], in_=b4v[:, :, 64:128], func=AF.Copy), reads=[kT(0)], writes=[swk])
            emit_list(pb)
            rope_apply(0, 1, sr[:, :, 0:128], swk, i=0, o3=True)
            rope_apply(4, 5, kr[:, :, 512:640], kvk, i=1, o3=True)
            rms_feat([B[2][:, :], B[3][:, :]], VKG, 256.0, ckvn, [("ckvn", 0), ("ckvn", 1)], 0, in_keys=[kB(2), kB(3)])
            for h in range(4):
                bi = [1, 2, 3, 5][h]
                for k in range(2):
                    S.add("pe", lambda e, h=h, k=k, bi=bi: e.matmul(B[bi][:, :], lhsT=WUK[:, k, h * 128:(h + 1) * 128], rhs=ckvn[:, k, :], start=(k == 0), stop=(k == 1)),
                          reads=[("ckvn", k), "WUK"], writes=[kB(bi)])
                src = B[bi][:, :].rearrange("p (t c) -> p t c", t=4)
                if h % 2 == 0:
                    S.add("act", lambda e, h=h, src=src, kr=kr: e.activation(out=kr[:, :, h * 128:(h + 1) * 128], in_=src, func=AF.Copy), reads=[kB(bi)], writes=[kvk])
                else:
                    S.add("dve", lambda e, h=h, src=src, kr=kr: e.tensor_copy(out=kr[:, :, h * 128:(h + 1) * 128], in_=src), reads=[kB(bi)], writes=[kvk])
            for t in range(4):
                bi = [0, 4, 1, 2][t]
                for k in range(2):
                    S.add("pe", lambda e, t=t, k=k, bi=bi: e.matmul(B[bi][:, :], lhsT=ckvn[:, k, t * 128:(t + 1) * 128], rhs=WUV[:, k, :], start=(k == 0), stop=(k == 1)),
                          reads=[("ckvn", k), "WUV"], writes=[kB(bi)])
                if t % 2 == 0:
                    S.add("act", lambda e, t=t, bi=bi, kr=kr: e.activation(out=kr[:, t, 640:1152], in_=B[bi][:, :], func=AF.Copy), reads=[kB(bi)], writes=[kvk])
                else:
                    S.add("dve", lambda e, t=t, bi=bi, kr=kr: e.tensor_copy(out=kr[:, t, 640:1152], in_=B[bi][:, :]), reads=[kB(bi)], writes=[kvk])
            S.add("sp", lambda e, kr=kr, ch=ch: e.dma_start(out=kv_d[ch * 4:(ch + 1) * 4].rearrange("b p f -> p b f"), in_=kr[:]),
                  reads=[kvk], writes=[("kvd", ch)], dsem=d_kvw[ch % 2])
            S.add("sp", lambda e, sr=sr, ch=ch: e.dma_start(out=sw_d[ch * 4:(ch + 1) * 4].rearrange("b p f -> p b f"), in_=sr[:]),
                  reads=[swk], writes=[("swd", ch)], dsem=d_sww[ch % 2])

        emit_zfill(nzb)
        S.barrier()
        e_k.close()
        e_q = ExitStack()
        cur[0] = e_q
        sinkbc = sb("sinkbc", [128, 512], F32)
        g1bc = sb("g1bc", [128, 1024], F32)
        b1bc = sb("b1bc", [128, 1024], F32)
        gabc = sb("gabc", [128, 1024], F32)
        babc = sb("babc", [128, 1024], F32)
        WQ = sb("WQ_s", [128, 8, 1280], BF16)
        WUQ = sb("WUQ_s", [128, 2, 1024], BF16)
        WOA = sb("WOA_s", [128, 4, 1024], BF16)
        WOB = sb("WOB_s", [128, 4, 1024], BF16)
        if n_step > 0:
            cload(g1bc[:], rows_d[0, :].partition_broadcast(128), "g1bc", q="sp")
            cload(b1bc[:], rows_d[1, :].partition_broadcast(128), "b1bc", q="sp")
            cload(WQ[:], WQ_d.rearrange("(k p) f -> p k f", p=128), "WQ")
            cload(WUQ[:], WUQ_d.rearrange("(k p) f -> p k f", p=128), "WUQ")
            cload(WOA[:], WOA_d.rearrange("p (c f) -> p c f", c=4), "WOA")
            cload(WOB[:], WOB_d.rearrange("p (c f) -> p c f", c=4), "WOB")
            cload(gabc[:], rows_d[2, :].partition_broadcast(128), "gabc", q="sp")
            cload(babc[:], rows_d[3, :].partition_broadcast(128), "babc", q="sp")
            S.add("pool", lambda e: e.tensor_scalar(out=gabc[:], in0=gabc[:], scalar1=ALPHA, scalar2=None, op0=ALU.mult), reads=["gabc"], writes=["gabc"])
            S.add("pool", lambda e: e.tensor_scalar(out=babc[:], in0=babc[:], scalar1=ALPHA, scalar2=None, op0=ALU.mult), reads=["babc"], writes=["babc"])
            S.add("act", lambda e: e.activation(out=sinkexp[:], in_=vec[:, VSK:VSK + 4], func=AF.Exp), reads=["vec"], writes=["sinkexp"])
            for c in range(4):
                S.add("dve", lambda e, c=c: e.tensor_scalar(out=sinkbc[:, c * 128:(c + 1) * 128], in0=maskC[:, 0:128], scalar1=0.0, scalar2=sinkexp[:, c:c + 1], op0=ALU.mult, op1=ALU.add),
                      reads=["maskC", "sinkexp"], writes=["sinkbc"])
        qaT = sb("qaT", [128, 4, 512], BF16)
        cqn = sb("cqn", [128, 2, 512], BF16)
        qnT = sb("qnT", [128, 4, 512], BF16)
        qrT = sb("qrT", [128, 4, 512], BF16)
        swt = sb("swt", [128, 5, SWW], BF16)
        d_swt = sem("d_swt")
        NKR = 4
        kvt = [sb("kvt%d" % i, [128, KVW], BF16) for i in range(NKR)]
        d_kvt = [sem("d_kvt%d" % i) for i in range(NKR)]
        NPB = 8
        Pb = [sb("Pb%d" % i, [128, 512], BF16) for i in range(NPB)]
        accs = sb("accs", [128, 4, 512], F32)
        rec = sb("rec", [128, 512], F32)
        aT = sb("aT", [128, 4, 512], F32)
        anT = sb("anT", [128, 4, 512], BF16)
        bT = sb("bT", [128, 4, 512], F32)
        bnT = sb("bnT", [128, 4, 512], BF16)
        rbuf = [accs[:, 0:2, :].rearrange("p h q -> p (h q)"), accs[:, 2:4, :].rearrange("p h q -> p (h q)")]
        d_h1 = [sem("d_h1_%d" % i) for i in range(2)]
        kvi = [0]
        pbi = [0]
        scale_mla = 192.0 ** -0.5

        if n_step > 0:
            S.add("pool", lambda e: e.memset(qrT[:], 0.0), writes=[("qrT", h) for h in range(4)])
        next_slots = chunk_loads(2) if n_step > 0 else None
        def swt_load(ch):
            S.add("sp", lambda e: e.dma_start(out=swt[:], in_=sw_d[ch * 4 - 1:ch * 4 + 4].rearrange("b p f -> p b f")),
                  reads=[("swd", ch - 1), ("swd", ch)], writes=["swt"], dsem=d_swt)

        def qa_proj(use_t):
            for c in range(4):
                if use_t:
                    qp = (T[0][:, :].bitcast(F32), kT(0)); rp = (T[1][:, :].bitcast(F32), kT(1))
                else:
                    qp = (B[2 * (c % 2)][:, :], kB(2 * (c % 2))); rp = (B[2 * (c % 2) + 1][:, :], kB(2 * (c % 2) + 1))
                projP(qp[0], qp[1], WQ, c * 128, "WQ")
                projP(rp[0], rp[1], WQ, 512 + c * 128, "WQ")
                rope_applyP(qp[0], qp[1], rp[0], rp[1], qaT[:, c, :], ("qaT", c), c % 2)

        hoisted = False
        pending_tail = []
        HOIST = _os.environ.get("KDBG_NOHOIST", "0") != "1"
        for j in range(n_step):
            ch = 2 * j + 2
            slots = next_slots
            if not hoisted:
                rope_loads(ch)
                swt_load(ch)
            if not hoisted:
                front(slots, True)
                qa_proj(False)
            HS = []
            S.capture = []
            proj(4, WQ, 1024, 128, "WQ")
            proj(5, WQ, 1152, 128, "WQ")
            rms_feat([B[4][:, :], B[5][:, :]], VQG, 256.0, cqn, [("cqn", 0), ("cqn", 1)], 0, in_keys=[kB(4), kB(5)])
            HS.append(S.capture); S.capture = []
            for h in range(4):
                bi = 1 + h
                for k in range(2):
                    S.add("pe", lambda e, h=h, k=k, bi=bi: e.matmul(B[bi][:, :], lhsT=WUQ[:, k, h * 128:(h + 1) * 128], rhs=cqn[:, k, :], start=(k == 0), stop=(k == 1)),
                          reads=[("cqn", k), "WUQ"], writes=[kB(bi)])
                if h % 2 == 0:
                    S.add("act", lambda e, h=h, bi=bi: e.activation(out=qnT[:, h, :], in_=B[bi][:, :], func=AF.Copy), reads=[kB(bi)], writes=[("qnT", h)])
                else:
                    S.add("dve", lambda e, h=h, bi=bi: e.tensor_copy(out=qnT[:, h, :], in_=B[bi][:, :]), reads=[kB(bi)], writes=[("qnT", h)])
            HS.append(S.capture); S.capture = []
            for pr in range(2):
                bq, br = (0, 5) if pr == 0 else (1, 2)
                for k in range(2):
                    S.add("pe", lambda e, pr=pr, k=k, bq=bq: e.matmul(B[bq][:, :], lhsT=WUQ[:, k, 512 + pr * 128:512 + (pr + 1) * 128], rhs=cqn[:, k, :], start=(k == 0), stop=(k == 1)),
                          reads=[("cqn", k), "WUQ"], writes=[kB(bq)])
                for k in range(2):
                    S.add("pe", lambda e, pr=pr, k=k, br=br: e.matmul(B[br][:, :], lhsT=WUQ[:, k, 768 + pr * 128:768 + (pr + 1) * 128], rhs=cqn[:, k, :], start=(k == 0), stop=(k == 1)),
                          reads=[("cqn", k), "WUQ"], writes=[kB(br)])
                S.add("dve", lambda e, bq=bq, pr=pr: e.tensor_tensor(out=t1[pr][:, :], in0=B[bq][:, :], in1=cs_t[:, :], op=ALU.mult), reads=[kB(bq), "cs"], writes=[("t1", pr)])
                S.add("dve", lambda e, br=br, pr=pr: e.tensor_tensor(out=t2[pr][:, :], in0=B[br][:, :], in1=sn_t[:, :], op=ALU.mult), reads=[kB(br), "sn"], writes=[("t2", pr)])
                for hh in range(2):
                    S.add("pool", lambda e, pr=pr, hh=hh: e.tensor_tensor(out=qrT[hh * 64:(hh + 1) * 64, 2 * pr + hh, :], in0=t1[pr][hh * 64:(hh + 1) * 64, :], in1=t2[pr][hh * 64:(hh + 1) * 64, :], op=ALU.add),
                          reads=[("t1", pr), ("t2", pr)], writes=[("qrT", 2 * pr + hh)])
            HS.append(S.capture); S.capture = []
            swa_p = {}
            OS = [(B[4][:, :], kB(4), B[5][:, :], kB(5)), (T[0][:, :].bitcast(F32), kT(0), T[1][:, :].bitcast(F32), kT(1))]

            def swa_st(qb):
                for g in range(2):
                    for kk in range(2):
                        kbi = qb + kk
                        bi = g * 2 + kk
                        S.add("pe", lambda e, g=g, kbi=kbi, bi=bi: e.matmul(B[bi][:, :].rearrange("p (c i) -> p c i", c=4),
                                                                             lhsT=swt[g * 64:(g + 1) * 64, kbi, 0:128],
                                                                             rhs=qaT[g * 64:(g + 1) * 64, :, qb * 128:(qb + 1) * 128], start=True, stop=True),
                              reads=["swt"] + [("qaT", c) for c in range(4)], writes=[kB(bi)])

            def swa_exp(qb):
                swa_p[qb] = []
                for g in range(2):
                    for kk in range(2):
                        kbi = qb + kk
                        slotblk = ch * 4 - 1 + kbi
                        bi = g * 2 + kk
                        pi = pbi[0]; pbi[0] = (pbi[0] + 1) % NPB
                        swa_p[qb].append(pi)
                        S.add("act", lambda e, bi=bi, pi=pi, slotblk=slotblk: e.activation(out=Pb[pi][:], in_=B[bi][:, :], func=AF.Exp,
                                                                                            bias=kbs_t[:, slotblk:slotblk + 1], scale=0.125),
                              reads=[kB(bi), "kbs"], writes=[("Pb", pi)])
                        mk = maskP if kk == 0 else maskC
                        mkk = "maskP" if kk == 0 else "maskC"
                        S.add("dve" if g == 0 else "pool", lambda e, pi=pi, mk=mk: e.tensor_tensor(out=Pb[pi][:], in0=Pb[pi][:], in1=mk[:], op=ALU.mult), reads=[("Pb", pi), mkk], writes=[("Pb", pi)])

            def swa_pv(qb):
                oap, ok_, sap, sk_ = OS[qb % 2]
                u = 0
                for g in range(2):
                    for kk in range(2):
                        kbi = qb + kk
                        pi = swa_p[qb][u]; u += 1
                        first = (g == 0 and kk == 0); last = (g == 1 and kk == 1)
                        S.add("pe", lambda e, g=g, kbi=kbi, pi=pi, first=first, last=last: e.matmul(oap, lhsT=swt[:, kbi, 128 + g * 128:256 + g * 128], rhs=Pb[pi][:],
                                                                                                   start=first, stop=last), reads=["swt", ("Pb", pi)], writes=[ok_])
                        S.add("pe", lambda e, g=g, pi=pi, first=first, last=last: e.matmul(sap, lhsT=onesg[:, g, :], rhs=Pb[pi][:], start=first, stop=last),
                              reads=["onesg%d" % g, ("Pb", pi)], writes=[sk_])

            def swa_epi(qb):
                oap, ok_, sap, sk_ = OS[qb % 2]
                for c in range(4):
                    S.add("act", lambda e, c=c: e.activation(out=rec[:, c * 128:(c + 1) * 128], in_=sap[:, c * 128:(c + 1) * 128], func=AF.Ln, bias=sinkexp[:, c:c + 1], scale=1.0),
                          reads=[sk_, "sinkexp"], writes=["rec"])
                S.add("act", lambda e: e.activation(out=rec[:], in_=rec[:], func=AF.Exp, scale=-1.0), reads=["rec"], writes=["rec"])
                S.add("dve", lambda e: e.tensor_tensor(out=aT[:, :, qb * 128:(qb + 1) * 128], in0=oap.rearrange("p (c i) -> p c i", c=4),
                                                       in1=rec[:].rearrange("p (c i) -> p c i", c=4), op=ALU.mult), reads=[ok_, "rec"], writes=[("aT", qb)])

            swa_st(0); swa_exp(0)
            for qb in range(4):
                if qb + 1 < 4:
                    swa_st(qb + 1)
                swa_pv(qb)
                if qb + 1 < 4:
                    swa_exp(qb + 1)
                swa_epi(qb)
            HS.append(S.capture); S.capture = None
            TS = pending_tail
            pending_tail = []
            for si in range(max(len(HS), len(TS))):
                if si < len(HS):
                    emit_list(HS[si])
                if si < len(TS):
                    emit_list(TS[si])
            if j + 1 < n_step:
                next_slots = chunk_loads(ch + 2)
            aT_keys = [("aT", q) for q in range(4)]
            for c in range(4):
                S.add("act", lambda e, c=c: e.activation(out=sq[:, c, :], in_=aT[:, c, :], func=AF.Square), reads=aT_keys, writes=[("sq", c)])
            for c in range(4):
                S.add("pe", lambda e, c=c: e.matmul(B[0][:, :], lhsT=ones[:], rhs=sq[:, c, :], start=(c == 0), stop=(c == 3)), reads=[("sq", c), "ones"], writes=[kB(0)])
            S.add("act", lambda e: e.activation(out=rstd_t[:], in_=B[0][:, :], func=AF.Ln, bias=epsrms[:, 0:1], scale=1.0 / 512.0), reads=[kB(0), "epsc"], writes=["rstd"])
            S.add("act", lambda e: e.activation(out=rstd_t[:], in_=rstd_t[:], func=AF.Exp, scale=-0.5), reads=["rstd"], writes=["rstd"])
            for c in range(4):
                S.add("dve", lambda e, c=c: e.scalar_tensor_tensor(out=anT[:, c, :], in0=aT[:, c, :], scalar=vec[:, VGA + c:VGA + c + 1], in1=rstd_t[:], op0=ALU.mult, op1=ALU.mult),
                      reads=aT_keys + ["rstd", "vec"], writes=[("anT", c)])
            S.add("pool", lambda e: e.memset(accs[:], 0.0), writes=[("accs", h) for h in range(4)])
            kblocks = [(3, None)] + [(s, None) for s in range(4, ch * 4)] + [(ch * 4 + d, d) for d in range(4)]
            nkb = len(kblocks)
            units = [(idx, sblk, dg, h) for idx, (sblk, dg) in enumerate(kblocks) for h in range(4)]
            kslot = {}

            def mla_st(ui):
                idx, sblk, dg, h = units[ui]
                if h == 0:
                    ks = kvi[0]; kvi[0] = (kvi[0] + 1) % NKR
                    kslot[idx] = ks
                    S.add("sp", lambda e, ks=ks, sblk=sblk: e.dma_start(out=kvt[ks][:], in_=kv_d[sblk]), reads=[("kvd", sblk // 4)], writes=[("kvt", ks)], dsem=d_kvt[ks])
                ks = kslot[idx]
                q0 = 0 if dg is None else dg * 128
                sbk = 4 + (ui % 2)
                hp = (h % 2) * 64
                S.add("pe", lambda e: e.matmul(B[sbk][:, q0:512], lhsT=kvt[ks][:, h * 128:(h + 1) * 128], rhs=qnT[:, h, q0:512], start=True, stop=False),
                      reads=[("kvt", ks), ("qnT", h)], writes=[kB(sbk)])
                S.add("pe", lambda e: e.matmul(B[sbk][:, q0:512], lhsT=kvt[ks][:, 512:640], rhs=qrT[:, h, q0:512], start=False, stop=True),
                      reads=[("kvt", ks), ("qrT", h)], writes=[kB(sbk)])

            def mla_rest(ui):
                idx, sblk, dg, h = units[ui]
                ks = kslot[idx]
                q0 = 0 if dg is None else dg * 128
                sbk = 4 + (ui % 2)
                pi = pbi[0]; pbi[0] = (pbi[0] + 1) % NPB
                S.add("act", lambda e: e.activation(out=Pb[pi][:, q0:512], in_=B[sbk][:, q0:512], func=AF.Exp, bias=kbm_t[:, sblk:sblk + 1], scale=scale_mla),
                      reads=[kB(sbk), "kbm"], writes=[("Pb", pi)])
                if dg is not None:
                    S.add("pool", lambda e: e.tensor_tensor(out=Pb[pi][:, q0:q0 + 128], in0=Pb[pi][:, q0:q0 + 128], in1=maskC[:, 0:128], op=ALU.mult),
                          reads=[("Pb", pi), "maskC"], writes=[("Pb", pi)])
                S.add("pe", lambda e: e.matmul(B[h][:, q0:512], lhsT=kvt[ks][:, 640 + h * 128:640 + (h + 1) * 128], rhs=Pb[pi][:, q0:512],
                                               start=(idx == 0), stop=(idx == nkb - 1), skip_group_check=True),
                      reads=[("kvt", ks), ("Pb", pi)], writes=[kB(h)])
                S.add("dve" if h % 2 == 0 else "pool", lambda e: e.tensor_tensor(out=accs[:, h, q0:512], in0=accs[:, h, q0:512], in1=Pb[pi][:, q0:512], op=ALU.add),
                      reads=[("accs", h), ("Pb", pi)], writes=[("accs", h)])

            side = []
            hoisted = False
            if HOIST and j + 1 < n_step:
                S.capture = side
                rope_loads(ch + 2)
                swt_load(ch + 2)
                front(next_slots, True)
                qa_proj(True)
                S.capture = None
                hoisted = True
            nside = len(side)
            per_unit = max(1, -(-nside // max(1, int(len(units) * 0.8) - 4)))
            sp_ = [0]

            def emit_side(n):
                while n > 0 and sp_[0] < nside:
                    eng_, fn_, r_, w_, ds_ = side[sp_[0]]; sp_[0] += 1
                    S.add(eng_, fn_, reads=r_, writes=w_, dsem=ds_)
                    n -= 1

            mla_st(0)
            for ui in range(len(units)):
                if ui + 1 < len(units):
                    mla_st(ui + 1)
                mla_rest(ui)
                if ui >= 4:
                    emit_side(per_unit)
            emit_side(nside)
            for h in range(4):
                sbk = 4 + (h % 2)
                S.add("pe", lambda e, h=h, sbk=sbk: e.matmul(B[sbk][:, :], lhsT=onesf[:], rhs=accs[:, h, :], start=True, stop=True), reads=[("accs", h), "onesf"], writes=[kB(sbk)])
                S.add("act", lambda e, sbk=sbk: e.activation(out=rec[:], in_=B[sbk][:, :], func=AF.Ln), reads=[kB(sbk)], writes=["rec"])
                S.add("act", lambda e: e.activation(out=rec[:], in_=rec[:], func=AF.Exp, scale=-1.0), reads=["rec"], writes=["rec"])
                S.add("dve", lambda e, h=h: e.tensor_tensor(out=bT[:, h, :], in0=B[h][:, :], in1=rec[:], op=ALU.mult), reads=[kB(h), "rec"], writes=[("bT", h)])
            S.capture = []
            rms_feat([bT[:, h, :] for h in range(4)], VGB, 512.0, bnT, [("bnT", h) for h in range(4)], 0, in_keys=[("bT", h) for h in range(4)], src_sb=True)
            pending_tail.append(S.capture); S.capture = None
            for t in range(4):
                S.capture = []
                s = slots[t]
                rb = rbuf[t % 2]
                rk = [("accs", 2 * (t % 2)), ("accs", 2 * (t % 2) + 1)]
                for half in range(2):
                    bi = 1 + 2 * (t % 2) + half
                    for c in range(4):
                        S.add("pe", lambda e, c=c, t=t, bi=bi, half=half: e.matmul(B[bi][:, :], lhsT=anT[:, c, t * 128:(t + 1) * 128], rhs=WOA[:, c, half * 512:(half + 1) * 512],
                                                                                  start=(c == 0), stop=False), reads=[("anT", c), "WOA"], writes=[kB(bi)])
                    for c in range(4):
                        S.add("pe", lambda e, c=c, t=t, bi=bi, half=half: e.matmul(B[bi][:, :], lhsT=bnT[:, c, t * 128:(t + 1) * 128], rhs=WOB[:, c, half * 512:(half + 1) * 512],
                                                                                  start=False, stop=(c == 3)), reads=[("bnT", c), "WOB"], writes=[kB(bi)])
                    S.add("dve", lambda e, bi=bi, half=half, rb=rb, s=s: e.tensor_tensor(out=rb[:, half * 512:(half + 1) * 512], in0=B[bi][:, :], in1=xt[s][:, half * 512:(half + 1) * 512], op=ALU.add),
                          reads=[kB(bi), ("x", s)], writes=rk)
                ln_tok(S, rb, rk, st[t], mv[t], t, g1bc, b1bc, "g1bc", "b1bc", epsln)
                row0 = (j * 4 + t) * 128
                S.add("sp", lambda e, rb=rb, row0=row0: e.dma_start(out=(out_d if dbg_h1 else h1_d)[row0:row0 + 128, :], in_=rb[:]), reads=rk, writes=[("h1d", j * 4 + t)], dsem=d_h1[t % 2])
                pending_tail.append(S.capture); S.capture = None
        for seg_ in pending_tail:
            emit_list(seg_)
        pending_tail = []

        S.barrier()
        e_q.close()
        e_att.close()
        cur[0] = es
        if moe and n_step > 0 and sparse:
            moe_sparse_phase(locals())
        if moe and n_step > 0 and not sparse:
            ntok = n_step * 512
            T_ = min(MOE_T, ntok)
            npass = ntok // T_
            nb = T_ // 128
            WR = sb("WR_s", [128, 8, 36], F32)
            rbias = sb("rbias", [128, 36], F32)
            cload(WR[:], WR_d.rearrange("(k p) f -> p k f", p=128), "WR", q="sp")
            cload(rbias[:], rows_d[4, 0:36].partition_broadcast(128), "rbias", q="sp")
            g2bc = sb("g2bc", [128, 1024], F32)
            b2bc = sb("b2bc", [128, 1024], F32)
            cload(g2bc[:], rows_d[5, :].partition_broadcast(128), "g2bc", q="sp")
            cload(b2bc[:], rows_d[6, :].partition_broadcast(128), "b2bc", q="sp")
            h1T = sb("h1T", [128, 8, T_], BF16)
            h1T32 = sb("h1T32", [128, 8, 128], F32)
            accm = sb("accm", [128, nb, 1024], F32)
            comb = sb("comb", [128, nb, 32], F32)
            hb = [sb("hb%d" % i, [128, 1024], F32) for i in range(2)]
            d_hb = [sem("d_hb%d" % i) for i in range(2)]
            NW = 3
            wg_s = [sb("wg_s%d" % i, [128, 8, 256], BF16) for i in range(NW)]
            wu_s = [sb("wu_s%d" % i, [128, 8, 256], BF16) for i in range(NW)]
            wd_s = [sb("wd_s%d" % i, [128, 2, 1024], BF16) for i in range(NW)]
            d_wg = [sem("d_wg%d" % i) for i in range(NW)]
            d_wu = [sem("d_wu%d" % i) for i in range(NW)]
            d_wd = [sem("d_wd%d" % i) for i in range(NW)]
            sgs = [sb("sg%d" % i, [128, 2, 512], BF16) for i in range(2)]
            hid = [sb("hid%d" % i, [128, 2, 512], BF16) for i in range(2)]
            lg = sb("lg", [128, 36], F32)
            rt = sb("rt", [128, 64], F32)
            d_out = [sem("d_out%d" % i) for i in range(2)]

            def wload(e_, slot):
                S.add("pool", lambda e: e.dma_start(out=wg_s[slot][:], in_=wg_d[e_].rearrange("(k p) f -> p k f", p=128)), writes=[("wg", slot)], dsem=d_wg[slot])
                S.add("pool", lambda e: e.dma_start(out=wu_s[slot][:], in_=wu_d[e_].rearrange("(k p) f -> p k f", p=128)), writes=[("wu", slot)], dsem=d_wu[slot])
                S.add("pool", lambda e: e.dma_start(out=wd_s[slot][:], in_=wd_d[e_].rearrange("(k p) f -> p k f", p=128)), writes=[("wd", slot)], dsem=d_wd[slot])

            for p in range(npass):
                seq = list(range(n_exp))
                for i0 in range(min(NW, n_exp)):
                    wload(seq[i0], i0 % NW)
                S.add("pool", lambda e: e.memset(accm[:], 0.0), writes=[("accm", b) for b in range(nb)])
                for b in range(nb):
                    gb = p * nb + b
                    hb_ = hb[b % 2]; hk = ("hb", b % 2)
                    S.add("sp", lambda e, hb_=hb_, gb=gb: e.dma_start(out=hb_[:], in_=h1_d[gb * 128:(gb + 1) * 128, :]), reads=[("h1d", gb)], writes=[hk], dsem=d_hb[b % 2])
                    for k in range(8):
                        bi = k // 4
                        S.add("pe", lambda e, k=k, bi=bi, hb_=hb_: e.transpose(out=B[bi][:, (k % 4) * 128:(k % 4 + 1) * 128], in_=hb_[:, k * 128:(k + 1) * 128], identity=identf[:]),
                              reads=[hk, "identf"], writes=[kB(bi)])
                    S.add("act", lambda e: e.activation(out=h1T32[:, 0:4, :], in_=B[0][:, :].rearrange("p (k i) -> p k i", k=4), func=AF.Copy),
                          reads=[kB(0)], writes=[("h1T32", 0)])
                    S.add("dve", lambda e: e.tensor_copy(out=h1T32[:, 4:8, :], in_=B[1][:, :].rearrange("p (k i) -> p k i", k=4)),
                          reads=[kB(1)], writes=[("h1T32", 1)])
                    S.add("pool", lambda e, b=b: e.tensor_copy(out=h1T[:, :, b * 128:(b + 1) * 128], in_=h1T32[:, :, :]),
                          reads=[("h1T32", 0), ("h1T32", 1)], writes=[("h1T", b)])
                    for k in range(8):
                        S.add("pe", lambda e, k=k: e.matmul(B[2][:, 0:36], lhsT=h1T32[:, k, :], rhs=WR[:, k, :], start=(k == 0), stop=(k == 7)),
                              reads=[("h1T32", k // 4), "WR"], writes=[kB(2)])
                    route(S, B[2], kB(2), lg, rt, rbias, comb, b)
                ntg = T_ // 512
                pairs = [(ei, tg) for ei in range(n_exp) for tg in range(ntg)]
                DB = [(B[4][:, :], kB(4)), (B[5][:, :], kB(5)), (T[0][:, :].bitcast(F32), kT(0)), (T[1][:, :].bitcast(F32), kT(1))]
                dbi = [0]

                def moe_gu(pi_, fc):
                    ei, tg = pairs[pi_]
                    slot = ei % NW
                    hkeys = [("h1T", tg * 4 + q) for q in range(4)]
                    for k in range(8):
                        S.add("pe", lambda e, k=k: e.matmul(B[fc][:, :], lhsT=wg_s[slot][:, k, fc * 128:(fc + 1) * 128], rhs=h1T[:, k, tg * 512:(tg + 1) * 512],
                                                            start=(k == 0), stop=(k == 7)), reads=[("wg", slot)] + hkeys, writes=[kB(fc)])
                    for k in range(8):
                        S.add("pe", lambda e, k=k: e.matmul(B[2 + fc][:, :], lhsT=wu_s[slot][:, k, fc * 128:(fc + 1) * 128], rhs=h1T[:, k, tg * 512:(tg + 1) * 512],
                                                            start=(k == 0), stop=(k == 7)), reads=[("wu", slot)] + hkeys, writes=[kB(2 + fc)])
                    sg_ = sgs[pi_ % 2]; hd = hid[pi_ % 2]
                    S.add("act", lambda e: e.activation(out=sg_[:, fc, :], in_=B[fc][:, :], func=AF.Silu), reads=[kB(fc)], writes=[("sg", pi_ % 2, fc)])
                    S.add("dve", lambda e: e.tensor_tensor(out=hd[:, fc, :], in0=B[2 + fc][:, :], in1=sg_[:, fc, :], op=ALU.mult),
                          reads=[kB(2 + fc), ("sg", pi_ % 2, fc)], writes=[("hid", pi_ % 2, fc)])

                def moe_d(pi_):
                    ei, tg = pairs[pi_]
                    slot = ei % NW
                    hd = hid[pi_ % 2]
                    for tb in range(4):
                        b = tg * 4 + tb
                        for half in range(2):
                            dap, dk = DB[dbi[0]]; dbi[0] = (dbi[0] + 1) % len(DB)
                            for fc in range(2):
                                S.add("pe", lambda e, fc=fc, dap=dap, tb=tb, half=half: e.matmul(dap, lhsT=hd[:, fc, tb * 128:(tb + 1) * 128], rhs=wd_s[slot][:, fc, half * 512:(half + 1) * 512],
                                                                                                start=(fc == 0), stop=(fc == 1)),
                                      reads=[("hid", pi_ % 2, 0), ("hid", pi_ % 2, 1), ("wd", slot)], writes=[dk])
                            S.add("dve", lambda e, dap=dap, b=b, half=half: e.scalar_tensor_tensor(out=accm[:, b, half * 512:(half + 1) * 512], in0=dap, scalar=comb[:, b, ei:ei + 1],
                                                                                                  in1=accm[:, b, half * 512:(half + 1) * 512], op0=ALU.mult, op1=ALU.add),
                                  reads=[dk, ("comb", b), ("accm", b)], writes=[("accm", b)])
                    if tg == ntg - 1 and ei + NW < n_exp:
                        wload(ei + NW, slot)

                npairs = len(pairs)
                moe_gu(0, 0); moe_gu(0, 1)
                for pi_ in range(npairs):
                    if pi_ + 1 < npairs:
                        moe_gu(pi_ + 1, 0)
                    moe_d(pi_)
                    if pi_ + 1 < npairs:
                        moe_gu(pi_ + 1, 1)
                for b in range(nb):
                    gb = p * nb + b
                    hb_ = hb[b % 2]; hk = ("hb", b % 2)
                    S.add("sp", lambda e, hb_=hb_, gb=gb: e.dma_start(out=hb_[:], in_=h1_d[gb * 128:(gb + 1) * 128, :]), reads=[("h1d", gb)], writes=[hk], dsem=d_hb[b % 2])
                    S.add("dve", lambda e, hb_=hb_, b=b: e.scalar_tensor_tensor(out=hb_[:], in0=hb_[:], scalar=ALPHA, in1=accm[:, b, :], op0=ALU.mult, op1=ALU.add),
                          reads=[hk, ("accm", b)], writes=[hk])
                    ln_tok(S, hb_, hk, st[b % 4], mv[b % 4], b % 4, g2bc, b2bc, "g2bc", "b2bc", epsln)
                    S.add("sp", lambda e, hb_=hb_, gb=gb: e.dma_start(out=out_d[gb * 128:(gb + 1) * 128, :], in_=hb_[:]), reads=[hk], writes=[("outd", gb)], dsem=d_out[b % 2])

        block = es.enter_context(nc.Block())
        S.emit(block, sems)
    return nc


def ln_tok(S, buf, bkey, st_, mv_, t, gbc, bbc, gk, bk, epsln, geng="pool", beng="pool"):
    bks = list(bkey) if isinstance(bkey, list) else [bkey]
    S.add("dve", lambda e: e.bn_stats(out=st_[:, 0, :], in_=buf[:, 0:512]), reads=bks, writes=[("st", t, 0)])
    S.add("dve", lambda e: e.bn_stats(out=st_[:, 1, :], in_=buf[:, 512:1024]), reads=bks, writes=[("st", t, 1)])
    S.add("dve", lambda e: e.bn_aggr(out=mv_[:, 0:2], in_=st_[:, :, :]), reads=[("st", t, 0), ("st", t, 1)], writes=[("mv", t, 0)])
    S.add("act", lambda e: e.activation(out=mv_[:, 2:3], in_=mv_[:, 1:2], func=AF.Ln, bias=epsln[:, 0:1], scale=1.0), reads=[("mv", t, 0), "epsc"], writes=[("mv", t, 1)])
    S.add("act", lambda e: e.activation(out=mv_[:, 2:3], in_=mv_[:, 2:3], func=AF.Exp, scale=-0.5), reads=[("mv", t, 1)], writes=[("mv", t, 1)])
    S.add("dve", lambda e: e.scalar_tensor_tensor(out=mv_[:, 3:4], in0=mv_[:, 0:1], scalar=-1.0, in1=mv_[:, 2:3], op0=ALU.mult, op1=ALU.mult),
          reads=[("mv", t, 0), ("mv", t, 1)], writes=[("mv", t, 2)])
    S.add("act", lambda e: e.activation(out=buf[:], in_=buf[:], func=AF.Identity, bias=mv_[:, 3:4], scale=mv_[:, 2:3]), reads=bks + [("mv", t, 1), ("mv", t, 2)], writes=bks)
    S.add(geng, lambda e: e.tensor_tensor(out=buf[:], in0=buf[:], in1=gbc[:], op=ALU.mult), reads=bks + [gk], writes=bks)
    S.add(beng, lambda e: e.tensor_tensor(out=buf[:], in0=buf[:], in1=bbc[:], op=ALU.add), reads=bks + [bk], writes=bks)


def route(S, Bl, bkey, lg, rt, rbias, comb, b, ohg_out=None, c8_out=None, tag=0, defer=None):
    def add(eng, fn, r, w):
        if defer is None:
            S.add(eng, fn, reads=r, writes=w)
        else:
            defer.append((eng, fn, r, w))

    def D(fn, r, w):
        add("dve", fn, r, w)
    LG = ("lg", tag)
    GM, NGM, GS, GT, M1, M2, DD, ED, DEN, W1, W2 = range(11)
    OHG, ING, OH1, ING2, OH2, C8, GE = 16, 20, 28, 36, 44, 52, 60
    K = ("rt", tag)
    D(lambda e: e.tensor_tensor(out=lg[:], in0=Bl[:, 0:36], in1=rbias[:], op=ALU.add), [bkey, "rbias"], [LG])
    D(lambda e: e.tensor_reduce(out=rt[:, GM:GM + 1], in_=lg[:, 0:4], axis=AX.X, op=ALU.max), [LG], [K])
    D(lambda e: e.tensor_scalar(out=rt[:, NGM:NGM + 1], in0=rt[:, GM:GM + 1], scalar1=-1.0, scalar2=None, op0=ALU.mult), [K], [K])
    add("act", lambda e: e.activation(out=rt[:, GE:GE + 4], in_=lg[:, 0:4], func=AF.Exp, bias=rt[:, NGM:NGM + 1], scale=1.0, accum_out=rt[:, GS:GS + 1]), [LG, K], [K])
    D(lambda e: e.reciprocal(out=rt[:, GT:GT + 1], in_=rt[:, GS:GS + 1]), [K], [K])
    D(lambda e: e.tensor_scalar(out=rt[:, OHG:OHG + 4], in0=lg[:, 0:4], scalar1=rt[:, GM:GM + 1], scalar2=None, op0=ALU.is_equal), [LG, K], [K])
    D(lambda e: e.tensor_scalar(out=rt[:, ING:ING + 8], in0=lg[:, 4:12], scalar1=rt[:, OHG:OHG + 1], scalar2=None, op0=ALU.mult), [LG, K], [K])
    for g in range(1, 4):
        D(lambda e, g=g: e.scalar_tensor_tensor(out=rt[:, ING:ING + 8], in0=lg[:, 4 + 8 * g:12 + 8 * g], scalar=rt[:, OHG + g:OHG + g + 1], in1=rt[:, ING:ING + 8], op0=ALU.mult, op1=ALU.add), [LG, K], [K])
    D(lambda e: e.tensor_reduce(out=rt[:, M1:M1 + 1], in_=rt[:, ING:ING + 8], axis=AX.X, op=ALU.max), [K], [K])
    D(lambda e: e.tensor_scalar(out=rt[:, OH1:OH1 + 8], in0=rt[:, ING:ING + 8], scalar1=rt[:, M1:M1 + 1], scalar2=None, op0=ALU.is_equal), [K], [K])
    D(lambda e: e.scalar_tensor_tensor(out=rt[:, ING2:ING2 + 8], in0=rt[:, OH1:OH1 + 8], scalar=-1e30, in1=rt[:, ING:ING + 8], op0=ALU.mult, op1=ALU.add), [K], [K])
    D(lambda e: e.tensor_reduce(out=rt[:, M2:M2 + 1], in_=rt[:, ING2:ING2 + 8], axis=AX.X, op=ALU.max), [K], [K])
    D(lambda e: e.tensor_scalar(out=rt[:, OH2:OH2 + 8], in0=rt[:, ING2:ING2 + 8], scalar1=rt[:, M2:M2 + 1], scalar2=None, op0=ALU.is_equal), [K], [K])
    D(lambda e: e.tensor_tensor(out=rt[:, DD:DD + 1], in0=rt[:, M2:M2 + 1], in1=rt[:, M1:M1 + 1], op=ALU.subtract), [K], [K])
    add("act", lambda e: e.activation(out=rt[:, ED:ED + 1], in_=rt[:, DD:DD + 1], func=AF.Exp), [K], [K])
    D(lambda e: e.tensor_scalar(out=rt[:, DEN:DEN + 1], in0=rt[:, ED:ED + 1], scalar1=1.0, scalar2=None, op0=ALU.add), [K], [K])
    D(lambda e: e.reciprocal(out=rt[:, DEN:DEN + 1], in_=rt[:, DEN:DEN + 1]), [K], [K])
    D(lambda e: e.tensor_tensor(out=rt[:, W1:W1 + 1], in0=rt[:, GT:GT + 1], in1=rt[:, DEN:DEN + 1], op=ALU.mult), [K], [K])
    D(lambda e: e.tensor_tensor(out=rt[:, W2:W2 + 1], in0=rt[:, W1:W1 + 1], in1=rt[:, ED:ED + 1], op=ALU.mult), [K], [K])
    D(lambda e: e.tensor_scalar(out=rt[:, C8:C8 + 8], in0=rt[:, OH1:OH1 + 8], scalar1=rt[:, W1:W1 + 1], scalar2=None, op0=ALU.mult), [K], [K])
    D(lambda e: e.scalar_tensor_tensor(out=rt[:, C8:C8 + 8], in0=rt[:, OH2:OH2 + 8], scalar=rt[:, W2:W2 + 1], in1=rt[:, C8:C8 + 8], op0=ALU.mult, op1=ALU.add), [K], [K])
    if ohg_out is not None:
        D(lambda e: e.tensor_copy(out=ohg_out[:, b, :], in_=rt[:, OHG:OHG + 4]), [K], [("OHG", b)])
        D(lambda e: e.tensor_copy(out=c8_out[:, b, :], in_=rt[:, C8:C8 + 8]), [K], [("C8", b)])
        return
    for g in range(4):
        D(lambda e, g=g: e.tensor_scalar(out=comb[:, b, 8 * g:8 * g + 8], in0=rt[:, C8:C8 + 8], scalar1=rt[:, OHG + g:OHG + g + 1], scalar2=None, op0=ALU.mult), [K], [("comb", b)])


def _rot_perm64():
    return (np.arange(64) + 32) % 64


def host_layout(inputs):
    f = np.float32
    x = np.asarray(inputs["x"], f)
    meta = np.asarray(inputs["meta_tokens"], f)
    w_in = np.asarray(inputs["w_in"], f)[0]
    rp = _rot_perm64()
    q_a = w_in[:, 0:512]; k_a = w_in[:, 512:640]; v_a = w_in[:, 640:768]
    c_q = w_in[:, 768:1024]; c_kv = w_in[:, 1024:1280]; k_r = w_in[:, 1280:1344]
    k_a_rot = np.concatenate([k_a[:, h * 64:(h + 1) * 64][:, rp] for h in range(2)], axis=1)
    k_r_rot = k_r[:, rp]
    WK = np.concatenate([k_a, k_a_rot, c_kv, v_a, k_r, k_r, k_r_rot, k_r_rot], axis=1)
    qcols, qrcols = [], []
    for c in range(4):
        for h in (c, 4 + c):
            blk = q_a[:, h * 64:(h + 1) * 64]
            qcols.append(blk); qrcols.append(blk[:, rp])
    WQ = np.concatenate(qcols + qrcols + [c_q], axis=1)
    w_uq = np.asarray(inputs["mla_w_uq"], f)[0]
    nope = [w_uq[:, h * 192:h * 192 + 128] for h in range(4)]
    rope = [w_uq[:, h * 192 + 128:h * 192 + 192] for h in range(4)]
    WUQ = np.concatenate(nope + rope + [r[:, rp] for r in rope], axis=1)
    w_ukv = np.asarray(inputs["mla_w_ukv"], f)[0]
    WUK = np.concatenate([w_ukv[:, h * 256:h * 256 + 128] for h in range(4)], axis=1)
    WUV = np.concatenate([w_ukv[:, h * 256 + 128:h * 256 + 256] for h in range(4)], axis=1)
    w_o = np.asarray(inputs["w_o"], f)[0]
    WOA = np.zeros((128, 4, 1024), f)
    for g in range(2):
        for c in range(4):
            h = 4 * g + c
            WOA[g * 64:(g + 1) * 64, c, :] = w_o[h * 64:(h + 1) * 64, :]
    WOB = np.zeros((128, 4, 1024), f)
    for h in range(4):
        WOB[:, h, :] = w_o[512 + h * 128:512 + (h + 1) * 128, :]
    WR = np.concatenate([np.asarray(inputs["moe_w_group"], f)[0], np.asarray(inputs["moe_w_router"], f)[0]], axis=1)
    vec = np.zeros((128, 40), f)
    vec[:, 0:8] = np.asarray(inputs["ln_in_g"], f).reshape(8, 128).T
    vec[:, 8:16] = np.asarray(inputs["ln_in_b"], f).reshape(8, 128).T
    vec[:, 16:18] = np.asarray(inputs["mla_q_norm_g"], f)[0].reshape(2, 128).T
    vec[:, 18:20] = np.asarray(inputs["mla_kv_norm_g"], f)[0].reshape(2, 128).T
    ga = np.asarray(inputs["swa_out_norm_g"], f)[0]
    sk = np.asarray(inputs["swa_sinks"], f)[0]
    for g in range(2):
        for c in range(4):
            h = 4 * g + c
            vec[g * 64:(g + 1) * 64, 20 + c] = ga[h * 64:(h + 1) * 64]
            vec[g * 64:(g + 1) * 64, 28 + c] = sk[h]
    vec[:, 24:28] = np.asarray(inputs["mla_out_norm_g"], f)[0].reshape(4, 128).T
    rows = np.zeros((7, 1024), f)
    rows[0] = np.asarray(inputs["ln1_g"], f)[0]; rows[1] = np.asarray(inputs["ln1_b"], f)[0]
    rows[2] = np.asarray(inputs["ln_in_g"], f); rows[3] = np.asarray(inputs["ln_in_b"], f)
    rows[4, 0:4] = np.asarray(inputs["moe_b_group"], f)[0]; rows[4, 4:36] = np.asarray(inputs["moe_b_router"], f)[0]
    rows[5] = np.asarray(inputs["ln2_g"], f)[0]; rows[6] = np.asarray(inputs["ln2_b"], f)[0]
    cst = np.zeros((128, 1536), f)
    cst[:, 0:128] = np.eye(128, dtype=f)
    cst[:, 128:256] = 1.0
    cst[:, 256:320] = 1.0
    cst[:, 448:512] = 1.0
    p = np.arange(128)[:, None]; i = np.arange(128)[None, :]
    cst[:, 512:1024] = np.tile((p > i).astype(f), (1, 4))
    cst[:, 1024:1536] = np.tile((p <= i).astype(f), (1, 4))
    cst2 = np.zeros((128, 128 + 8 + NSLOT), f)
    cst2[:, 0:128] = (p < i).astype(f)
    cst2[:, 128:136] = np.arange(8)[None, :] * 128 + np.arange(128)[:, None]
    cst2[:, 136:136 + NSLOT] = (np.arange(NSLOT) * SLOT)[None, :]
    blk0 = np.zeros((128, 1024), f); blk0[112:] = meta
    zblk = np.zeros((128, 1024), f)
    pos_blk0 = np.maximum(np.arange(128) - 112, 0).astype(f)
    inv_freq = (10000.0 ** (-np.arange(0, 64, 2, dtype=f) / f(64))).astype(f)
    shared = dict(cst=cst, WK=WK, WQ=WQ, WUQ=WUQ, WUK=WUK, WUV=WUV, WOA=WOA.reshape(128, 4096), WOB=WOB.reshape(128, 4096), WR=WR, vec=vec,
                  rows=rows,
                  wg2=np.asarray(inputs["moe_w_gate"], f)[0].reshape(NEXP, 8, 128, 256).transpose(0, 2, 1, 3).reshape(NEXP * 128, 2048),
                  wu2=np.asarray(inputs["moe_w_up"], f)[0].reshape(NEXP, 8, 128, 256).transpose(0, 2, 1, 3).reshape(NEXP * 128, 2048),
                  wd2=np.asarray(inputs["moe_w_down"], f)[0].reshape(NEXP, 2, 128, 1024).transpose(0, 2, 1, 3).reshape(NEXP * 128, 2048),
                  cst2=cst2)
    shared = {k: np.ascontiguousarray(v) for k, v in shared.items()}
    in_maps = []
    for core in range(8):
        b, hf = core // 2, core % 2
        xb = x[b]
        blocks = [zblk, zblk, zblk, blk0]
        pos = [np.zeros(128, f)] * 3 + [pos_blk0]
        kbm = np.zeros((128, NBLK), f); kbs = np.zeros((128, NBLK), f)
        kbm[:, 0:3] = NEGB; kbm[:112, 3] = NEGB; kbs[:112, 3] = NEGB
        if hf == 0:
            blocks += [zblk, zblk, zblk, blk0]
            pos += [np.zeros(128, f)] * 3 + [pos_blk0]
            kbm[:, 4:8] = NEGB; kbs[:112, 7] = NEGB
        for t in range(64):
            blocks.append(xb[t * 128:(t + 1) * 128])
            pos.append((16 + t * 128 + np.arange(128)).astype(f))
        if hf == 1:
            blocks += [zblk] * 4
            pos += [np.zeros(128, f)] * 4
        xs_ = np.ascontiguousarray(np.concatenate(blocks, axis=0))
        posv = np.concatenate(pos)
        ang = posv[:, None] * inv_freq[None, :]
        cos64 = np.concatenate([np.cos(ang), np.cos(ang)], axis=1)
        sin64 = np.concatenate([-np.sin(ang), np.sin(ang)], axis=1)
        cosT_ = np.ascontiguousarray(np.concatenate([cos64, cos64], axis=1).T.astype(f))
        sinT_ = np.ascontiguousarray(np.concatenate([sin64, sin64], axis=1).T.astype(f))
        m = dict(shared)
        m.update(xs=xs_, cosT=cosT_, sinT=sinT_, kbm=kbm, kbs=kbs)
        in_maps.append(m)
    return in_maps


_NC_CACHE = {}


def kernel(**inputs):
    in_maps = host_layout(inputs)
    if "nc" not in _NC_CACHE:
        _NC_CACHE["nc"] = build_nc()
    nc = _NC_CACHE["nc"]
    res = run_bass_kernel_spmd(nc, in_maps, core_ids=list(range(8)))
    out = np.zeros((4, 8192, 1024), np.float32)
    for core in range(8):
        b, hf = core // 2, core % 2
        o = res.results[core]["out"]
        for j in range(NSTEP):
            xc = 2 * j + hf
            out[b, xc * 512:(xc + 1) * 512] = o[j * 512:(j + 1) * 512]
    return out


def moe_sparse_phase(L):
    S = L["S"]; sb = L["sb"]; sem = L["sem"]; cload = L["cload"]; B = L["B"]; T = L["T"]; kB = L["kB"]; kT = L["kT"]
    n_step = L["n_step"]; h1_d = L["h1_d"]; out_d = L["out_d"]; rows_d = L["rows_d"]; WR_d = L["WR_d"]
    identf = L["identf"]; onesf = L["onesf"]; ident = L["ident"]; st = L["st"]; mv = L["mv"]; epsln = L["epsln"]
    wg2_d = L["wg2_d"]; wu2_d = L["wu2_d"]; wd2_d = L["wd2_d"]; cst2_d = L["cst2_d"]; xs_d = L["xs_d"]; ffn_d = L["ffn_d"]
    nblk = n_step * 4
    nslot = nblk // 4 + 4
    assert nslot <= NSLOT

    WR = sb("WR_s", [128, 8, 36], F32)
    rbias = sb("rbias", [128, 36], F32)
    cst2 = sb("cst2_s", [128, 128 + 8 + NSLOT], F32)
    g2bc = sb("g2bc", [128, 1024], F32)
    b2bc = sb("b2bc", [128, 1024], F32)
    cload(WR[:], WR_d.rearrange("(k p) f -> p k f", p=128), "WR", q="sp")
    cload(rbias[:], rows_d[4, 0:36].partition_broadcast(128), "rbias", q="sp")
    cload(cst2[:], cst2_d[:, :], "cst2", q="sp")
    cload(g2bc[:], rows_d[5, :].partition_broadcast(128), "g2bc", q="sp")
    cload(b2bc[:], rows_d[6, :].partition_broadcast(128), "b2bc", q="sp")
    allz = L["allz"]
    UT = cst2[:, 0:128]
    JP = cst2[:, 128:136]
    SLST = cst2[:, 136:136 + NSLOT]
    NHB = 8
    hb = [sb("hb%d" % i, [128, 1024], F32) for i in range(NHB)]
    d_hb = [sem("d_hb%d" % i) for i in range(NHB)]
    d_out = [sem("d_out%d" % i) for i in range(6)]
    h1T32s = [sb("h1T32_%d" % i, [128, 8, 128], F32) for i in range(2)]
    OHGt = sb("OHGt", [128, 32, 4], F32)
    C8t = sb("C8t", [128, 32, 8], F32)
    CS = sb("CS", [128, 32, 4], F32)
    PRE = sb("PRE", [128, 32, 4], F32)
    OFF = sb("OFF", [128, 32, 4], F32)
    sm = sb("sm", [128, 64], F32)
    POSF = sb("POSF", [128, 32], F32)
    POSI = sb("POSI", [128, 32], U32)
    WIDXF = sb("WIDXF", [128, NSLOT, 8], F32)
    WIDX = sb("WIDX", [128, NSLOT, 8], U32)
    if nblk < 32:
        S.add("dve", lambda e: e.memset(OHGt[:], 0.0), writes=[("OHG", b) for b in range(32)])

    GRP = 4
    lg4 = [sb("lg4_%d" % i, [128, 36], F32) for i in range(GRP)]
    rt4 = [sb("rt4_%d" % i, [128, 64], F32) for i in range(GRP)]
    for g0 in range(0, nblk, GRP):
        chains = []
        for i in range(GRP):
            b = g0 + i
            hb_ = hb[b % NHB]; hk = ("hb", b % NHB)
            rb_ = 2 + i
            h1T32 = h1T32s[b % 2]; hpar = b % 2
            S.add("sp", lambda e, hb_=hb_, b=b: e.dma_start(out=hb_[:], in_=h1_d[b * 128:(b + 1) * 128, :]), reads=[("h1d", b)], writes=[hk], dsem=d_hb[b % NHB])
            for k in range(8):
                bi = k // 4
                S.add("pe", lambda e, k=k, bi=bi, hb_=hb_: e.transpose(out=B[bi][:, (k % 4) * 128:(k % 4 + 1) * 128], in_=hb_[:, k * 128:(k + 1) * 128], identity=identf[:]),
                      reads=[hk, "identf"], writes=[kB(bi)])
            S.add("act", lambda e, h1T32=h1T32: e.activation(out=h1T32[:, 0:4, :], in_=B[0][:, :].rearrange("p (k i) -> p k i", k=4), func=AF.Copy), reads=[kB(0)], writes=[("h1T32", hpar, 0)])
            S.add("act", lambda e, h1T32=h1T32: e.activation(out=h1T32[:, 4:8, :], in_=B[1][:, :].rearrange("p (k i) -> p k i", k=4), func=AF.Copy), reads=[kB(1)], writes=[("h1T32", hpar, 1)])
            for k in range(8):
                S.add("pe", lambda e, k=k, rb_=rb_, h1T32=h1T32: e.matmul(B[rb_][:, 0:36], lhsT=h1T32[:, k, :], rhs=WR[:, k, :], start=(k == 0), stop=(k == 7)),
                      reads=[("h1T32", hpar, k // 4), "WR"], writes=[kB(rb_)])
            ch_ = []
            route(S, B[rb_], kB(rb_), lg4[i], rt4[i], rbias, None, b, ohg_out=OHGt, c8_out=C8t, tag=i, defer=ch_)
            chains.append(ch_)
        for opi in range(max(len(c) for c in chains)):
            for c in chains:
                if opi < len(c):
                    eng, fn, r, w = c[opi]
                    S.add(eng, fn, reads=r, writes=w)
    allohg = [("OHG", b) for b in range(32)]
    flat = lambda t: t[:, :, :].rearrange("p b g -> p (b g)")
    S.add("pe", lambda e: e.matmul(B[3][:, 0:128], lhsT=onesf[:], rhs=flat(OHGt), start=True, stop=True), reads=allohg + ["onesf"], writes=[kB(3)])
    S.add("pe", lambda e: e.matmul(B[4][:, 0:128], lhsT=UT, rhs=flat(OHGt), start=True, stop=True), reads=allohg + ["cst2"], writes=[kB(4)])
    S.add("dve", lambda e: e.tensor_copy(out=flat(CS), in_=B[3][:, 0:128]), reads=[kB(3)], writes=["CS"])
    S.add("act", lambda e: e.activation(out=flat(PRE), in_=B[4][:, 0:128], func=AF.Copy), reads=[kB(4)], writes=["PRE"])
    D = lambda fn, r, w: S.add("dve", fn, reads=r, writes=w)
    NG, PC, BASE, END, TMP8, GS, AA = 0, 4, 8, 12, 16, 24, 36
    D(lambda e: e.tensor_reduce(out=sm[:, NG:NG + 4], in_=CS[:, :, :].rearrange("p b g -> p g b"), axis=AX.X, op=ALU.add), ["CS"], ["sm"])
    for g in range(4):
        D(lambda e, g=g: e.tensor_scalar(out=sm[:, TMP8:TMP8 + 8], in0=SLST[:, 0:8], scalar1=sm[:, NG + g:NG + g + 1], scalar2=None, op0=ALU.is_lt), ["sm", "cst2"], ["sm"])
        D(lambda e, g=g: e.tensor_reduce(out=sm[:, PC + g:PC + g + 1], in_=sm[:, TMP8:TMP8 + 8], axis=AX.X, op=ALU.add), ["sm"], ["sm"])
    D(lambda e: e.tensor_scalar(out=sm[:, PC:PC + 4], in0=sm[:, PC:PC + 4], scalar1=float(SLOT), scalar2=None, op0=ALU.mult), ["sm"], ["sm"])
    D(lambda e: e.memset(sm[:, BASE:BASE + 1], 0.0), ["sm"], ["sm"])
    for g in range(1, 4):
        D(lambda e, g=g: e.tensor_tensor(out=sm[:, BASE + g:BASE + g + 1], in0=sm[:, BASE + g - 1:BASE + g], in1=sm[:, PC + g - 1:PC + g], op=ALU.add), ["sm"], ["sm"])
    D(lambda e: e.tensor_tensor(out=sm[:, END:END + 4], in0=sm[:, BASE:BASE + 4], in1=sm[:, PC:PC + 4], op=ALU.add), ["sm"], ["sm"])
    D(lambda e: e.tensor_copy(out=OFF[:, 0, :], in_=sm[:, BASE:BASE + 4]), ["sm"], ["OFF"])
    for b in range(1, 32):
        D(lambda e, b=b: e.tensor_tensor(out=OFF[:, b, :], in0=OFF[:, b - 1, :], in1=CS[:, b - 1, :], op=ALU.add), ["OFF", "CS"], ["OFF"])
    D(lambda e: e.tensor_tensor(out=flat(PRE), in0=flat(PRE), in1=flat(OFF), op=ALU.add), ["PRE", "OFF"], ["PRE"])
    D(lambda e: e.tensor_tensor(out=flat(PRE), in0=flat(PRE), in1=flat(OHGt), op=ALU.mult), ["PRE"] + allohg, ["PRE"])
    D(lambda e: e.tensor_reduce(out=POSF[:], in_=PRE[:, :, :], axis=AX.X, op=ALU.add), ["PRE"], ["POSF"])
    D(lambda e: e.tensor_copy(out=POSI[:], in_=POSF[:]), ["POSF"], ["POSI"])
    D(lambda e: e.tensor_scalar(out=sm[:, GS:GS + NSLOT], in0=SLST, scalar1=sm[:, END:END + 1], scalar2=None, op0=ALU.is_ge), ["sm", "cst2"], ["sm"])
    for g in range(1, 3):
        D(lambda e, g=g: e.scalar_tensor_tensor(out=sm[:, GS:GS + NSLOT], in0=SLST, scalar=sm[:, END + g:END + g + 1], in1=sm[:, GS:GS + NSLOT], op0=ALU.is_ge, op1=ALU.add), ["sm", "cst2"], ["sm"])
    D(lambda e: e.tensor_scalar(out=sm[:, AA:AA + NSLOT], in0=sm[:, GS:GS + NSLOT], scalar1=1024.0, scalar2=None, op0=ALU.mult), ["sm"], ["sm"])
    for s_ in range(NSLOT):
        D(lambda e, s_=s_: e.tensor_scalar(out=WIDXF[:, s_, :], in0=JP, scalar1=sm[:, AA + s_:AA + s_ + 1], scalar2=None, op0=ALU.add), ["sm", "cst2"], ["WIDXF"])
    D(lambda e: e.tensor_copy(out=WIDX[:, :, :].rearrange("p s j -> p (s j)"), in_=WIDXF[:, :, :].rearrange("p s j -> p (s j)")), ["WIDXF"], ["WIDX"])

    NRT = 4
    rowt = [sb("rowt%d" % i, [128, ROWW], F32) for i in range(NRT)]
    d_rl = [sem("d_rl%d" % i) for i in range(NRT)]
    d_rs = [sem("d_rs%d" % i) for i in range(NRT)]
    scat_cap = []
    S.capture = scat_cap
    for b in range(nblk):
        r_ = rowt[b % NRT]; rk = ("rowt", b % NRT)
        S.add("sp", lambda e, r_=r_, b=b: e.dma_start(out=r_[:, 0:1024], in_=h1_d[b * 128:(b + 1) * 128, :]), reads=[("h1d", b)], writes=[rk], dsem=d_rl[b % NRT])
        S.add("dve", lambda e, r_=r_, b=b: e.tensor_copy(out=r_[:, 1024:1032], in_=C8t[:, b, :]), reads=[("C8", b), rk], writes=[(rk, "c")])
        S.add("pool", lambda e, r_=r_, b=b: e.indirect_dma_start(out=xs_d[:, :], out_offset=bass.IndirectOffsetOnAxis(ap=POSI[:, b:b + 1], axis=0), in_=r_[:], in_offset=None),
              reads=[rk, (rk, "c"), "POSI"] + allz, writes=[("xs", b)], dsem=d_rs[b % NRT])
    S.capture = None
    allxs = [("xs", b) for b in range(nblk)]

    NW = 4
    wg_s = [sb("wg_s%d" % i, [128, 2048], BF16) for i in range(NW)]
    wu_s = [sb("wu_s%d" % i, [128, 2048], BF16) for i in range(NW)]
    wd_s = [sb("wd_s%d" % i, [128, 2048], BF16) for i in range(NW)]
    d_wg = [sem("d_wg%d" % i) for i in range(NW)]
    d_wu = [sem("d_wu%d" % i) for i in range(NW)]
    d_wd = [sem("d_wd%d" % i) for i in range(NW)]
    sgs = [sb("sg%d" % i, [128, 2, 512], BF16) for i in range(2)]
    hid = [sb("hid%d" % i, [128, 2, 512], BF16) for i in range(2)]
    ups = [sb("ups%d" % i, [128, 2, 512], F32) for i in range(2)]
    xsb = [sb("xsb%d" % i, [128, ROWW], F32) for i in range(2)]
    d_xl = [sem("d_xl%d" % i) for i in range(2)]
    x16 = [sb("x16_%d" % i, [128, 1024], BF16) for i in range(2)]
    xsT = [sb("xsT%d" % i, [128, 8, 512], BF16) for i in range(2)]
    c8s = [sb("c8s%d" % i, [128, 4, 8], F32) for i in range(2)]
    accs = [sb("maccs%d" % i, [128, 4, 1024], F32) for i in range(2)]
    d_fs = [sem("d_fs%d" % i) for i in range(2)]
    pairs = [(s_, j) for s_ in range(nslot) for j in range(8)]
    npairs = len(pairs)

    def wload(pi_):
        s_, j = pairs[pi_]
        slot = pi_ % NW
        off = bass.IndirectOffsetOnAxis(ap=WIDX[:, s_, j:j + 1], axis=0)
        S.add("pool", lambda e: e.indirect_dma_start(out=wg_s[slot][:], out_offset=None, in_=wg2_d[:, :], in_offset=off), reads=["WIDX"], writes=[("wg", slot)], dsem=d_wg[slot])
        S.add("pool", lambda e: e.indirect_dma_start(out=wu_s[slot][:], out_offset=None, in_=wu2_d[:, :], in_offset=off), reads=["WIDX"], writes=[("wu", slot)], dsem=d_wu[slot])
        S.add("pool", lambda e: e.indirect_dma_start(out=wd_s[slot][:], out_offset=None, in_=wd2_d[:, :], in_offset=off), reads=["WIDX"], writes=[("wd", slot)], dsem=d_wd[slot])

    def slot_prep(s_):
        par = s_ % 2
        for t in range(4):
            xb = xsb[t % 2]; xk = ("xsb", t % 2)
            r0 = s_ * SLOT + t * 128
            S.add("sp", lambda e, xb=xb, r0=r0: e.dma_start(out=xb[:], in_=xs_d[r0:r0 + 128, :]), reads=allxs, writes=[xk], dsem=d_xl[t % 2])
            S.add("act", lambda e, xb=xb, t=t: e.activation(out=x16[t % 2][:], in_=xb[:, 0:1024], func=AF.Copy), reads=[xk], writes=[("x16", t % 2)])
            S.add("pool", lambda e, xb=xb, t=t: e.tensor_copy(out=c8s[par][:, t, :], in_=xb[:, 1024:1032]), reads=[xk], writes=[("c8s", par, t)])
            S.add("act", lambda e, xb=xb, t=t: e.activation(out=accs[par][:, t, :], in_=xb[:, 0:1024], func=AF.Copy, scale=ALPHA), reads=[xk], writes=[("maccs", par, t)])
            for k in range(8):
                Tb = T[k // 4]
                S.add("pe", lambda e, Tb=Tb, k=k, t=t: e.transpose(out=Tb[:, (k % 4) * 128:(k % 4 + 1) * 128], in_=x16[t % 2][:, k * 128:(k + 1) * 128], identity=ident[:]),
                      reads=[("x16", t % 2), "ident"], writes=[kT(k // 4)])
            S.add("act", lambda e, t=t: e.activation(out=xsT[par][:, 0:4, t * 128:(t + 1) * 128], in_=T[0][:, 0:512].rearrange("p (k i) -> p k i", k=4), func=AF.Copy),
                  reads=[kT(0)], writes=[("xsT", par, t)])
            S.add("dve", lambda e, t=t: e.tensor_copy(out=xsT[par][:, 4:8, t * 128:(t + 1) * 128], in_=T[1][:, 0:512].rearrange("p (k i) -> p k i", k=4)),
                  reads=[kT(1)], writes=[("xsT", par, t, 1)])

    DB = [(B[4][:, :], kB(4)), (B[5][:, :], kB(5)), (T[0][:, :].bitcast(F32), kT(0)), (T[1][:, :].bitcast(F32), kT(1))]
    dbi = [0]

    def moe_gu(pi_, fc):
        s_, j = pairs[pi_]
        slot = pi_ % NW
        par = s_ % 2
        xkeys = [("xsT", par, t) for t in range(4)] + [("xsT", par, t, 1) for t in range(4)]
        for k in range(8):
            S.add("pe", lambda e, k=k: e.matmul(B[fc][:, :], lhsT=wg_s[slot][:, k * 256 + fc * 128:k * 256 + (fc + 1) * 128], rhs=xsT[par][:, k, :], start=(k == 0), stop=(k == 7)),
                  reads=[("wg", slot)] + xkeys, writes=[kB(fc)])
        for k in range(8):
            S.add("pe", lambda e, k=k: e.matmul(B[2 + fc][:, :], lhsT=wu_s[slot][:, k * 256 + fc * 128:k * 256 + (fc + 1) * 128], rhs=xsT[par][:, k, :], start=(k == 0), stop=(k == 7)),
                  reads=[("wu", slot)] + xkeys, writes=[kB(2 + fc)])
        sg_ = sgs[pi_ % 2]; hd = hid[pi_ % 2]; up_ = ups[pi_ % 2]
        S.add("act", lambda e: e.activation(out=sg_[:, fc, :], in_=B[fc][:, :], func=AF.Silu), reads=[kB(fc)], writes=[("sg", pi_ % 2, fc)])
        S.add("act", lambda e: e.activation(out=up_[:, fc, :], in_=B[2 + fc][:, :], func=AF.Copy), reads=[kB(2 + fc)], writes=[("up", pi_ % 2, fc)])
        S.add("pool", lambda e: e.tensor_tensor(out=hd[:, fc, :], in0=up_[:, fc, :], in1=sg_[:, fc, :], op=ALU.mult), reads=[("up", pi_ % 2, fc), ("sg", pi_ % 2, fc)], writes=[("hid", pi_ % 2, fc)])

    def moe_d(pi_):
        s_, j = pairs[pi_]
        slot = pi_ % NW
        par = s_ % 2
        hd = hid[pi_ % 2]
        ac = accs[par]
        for tb in range(4):
            for half in range(2):
                dap, dk = DB[dbi[0]]; dbi[0] = (dbi[0] + 1) % len(DB)
                for fc in range(2):
                    S.add("pe", lambda e, fc=fc, dap=dap, tb=tb, half=half: e.matmul(dap, lhsT=hd[:, fc, tb * 128:(tb + 1) * 128], rhs=wd_s[slot][:, fc * 1024 + half * 512:fc * 1024 + (half + 1) * 512],
                                                                                    start=(fc == 0), stop=(fc == 1)),
                          reads=[("hid", pi_ % 2, 0), ("hid", pi_ % 2, 1), ("wd", slot)], writes=[dk])
                ak = ("maccs", par, tb)
                if True:
                    S.add("dve", lambda e, dap=dap, tb=tb, half=half: e.scalar_tensor_tensor(out=ac[:, tb, half * 512:(half + 1) * 512], in0=dap, scalar=c8s[par][:, tb, j:j + 1],
                                                                                            in1=ac[:, tb, half * 512:(half + 1) * 512], op0=ALU.mult, op1=ALU.add),
                          reads=[dk, ("c8s", par, tb), ak], writes=[ak])
        if j == 7:
            S.add("sp", lambda e: e.dma_start(out=ffn_d[s_ * SLOT:(s_ + 1) * SLOT, :].rearrange("(t p) f -> p t f", p=128), in_=ac[:]),
                  reads=[("maccs", par, tb) for tb in range(4)], writes=[("ffn", s_)], dsem=d_fs[par])
        if pi_ + NW < npairs:
            wload(pi_ + NW)

    for i0 in range(min(NW, npairs)):
        wload(i0)
    for eng_, fn_, r_, w_, ds_ in scat_cap:
        S.add(eng_, fn_, reads=r_, writes=w_, dsem=ds_)
    slot_prep(0)
    moe_gu(0, 0); moe_gu(0, 1)
    for pi_ in range(npairs):
        s_, j = pairs[pi_]
        if j == 2 and s_ + 1 < nslot:
            slot_prep(s_ + 1)
        if pi_ + 1 < npairs:
            moe_gu(pi_ + 1, 0)
        moe_d(pi_)
        if pi_ + 1 < npairs:
            moe_gu(pi_ + 1, 1)
    allffn = [("ffn", s_) for s_ in range(nslot)]

    fb = [t_[:, 0:1024] for t_ in (rowt + xsb)]
    ND = len(fb)
    d_fb = [sem("d_fb%d" % i) for i in range(ND)]
    LOOK = ND - 1
    def ln_parts(b):
        fb_ = fb[b % ND]; fk = ("fb", b % ND)
        cap = []
        S.capture = cap
        ln_tok(S, fb_, fk, st[b % 4], mv[b % 4], b % 4, g2bc, b2bc, "g2bc", "b2bc", epsln, geng="dve", beng="dve")
        S.add("sp", lambda e: e.dma_start(out=out_d[b * 128:(b + 1) * 128, :], in_=fb_), reads=[fk], writes=[("outd", b)], dsem=d_out[b % ND])
        S.capture = None
        return cap[:7], cap[7:]

    def emit_(lst):
        for eng_, fn_, r_, w_, ds_ in lst:
            S.add(eng_, fn_, reads=r_, writes=w_, dsem=ds_)

    for b in range(min(ND, nblk)):
        fb_ = fb[b % ND]; fk = ("fb", b % ND)
        S.add("pool", lambda e, fb_=fb_, b=b: e.indirect_dma_start(out=fb_, out_offset=None, in_=ffn_d[:, :], in_offset=bass.IndirectOffsetOnAxis(ap=POSI[:, b:b + 1], axis=0)),
              reads=allffn + ["POSI"], writes=[fk], dsem=d_fb[b % ND])
    prev2 = None
    for b in range(nblk):
        s1, s2 = ln_parts(b)
        emit_(s1)
        if prev2 is not None:
            emit_(prev2)
        prev2 = s2
        nb_ = b + LOOK
        if nb_ < nblk and b >= 1:
            pass
        nb_ = b - 1 + ND
        if b >= 1 and nb_ < nblk:
            fbn = fb[nb_ % ND]; fkn = ("fb", nb_ % ND)
            S.add("pool", lambda e, fbn=fbn, nb_=nb_: e.indirect_dma_start(out=fbn, out_offset=None, in_=ffn_d[:, :], in_offset=bass.IndirectOffsetOnAxis(ap=POSI[:, nb_:nb_ + 1], axis=0)),
                  reads=allffn + ["POSI"], writes=[fkn], dsem=d_fb[nb_ % ND])
    emit_(prev2)
```

```python
import numpy as np
from contextlib import ExitStack
import concourse.bass as bass
import concourse.mybir as mybir
from concourse.bass_utils import run_bass_kernel_spmd

F32 = mybir.dt.float32
BF16 = mybir.dt.bfloat16
AF = mybir.ActivationFunctionType
ALU = mybir.AluOpType
AX = mybir.AxisListType

NCH = 18
NBLK = NCH * 4
NSTEP = 8
LN_EPS = 1e-5
RMS_EPS = 1e-6
ALPHA = 2.0 ** 0.25
NEGB = -30000.0
NEXP = 32
KVW = 1152
SWW = 384
MOE_T = 2048
SLOT = 512
NSLOT = 12
ROWW = 1032
U32 = mybir.dt.uint32


class _Op:
    __slots__ = ("eng", "fn", "idx", "eidx", "deps", "need_inc", "dsem", "dcount", "is_dma", "inc_no", "bar")


class Sched:
    ENG = ("pe", "act", "dve", "pool", "sp")

    def __init__(self):
        self.ops = {e: [] for e in self.ENG}
        self.last_w = {}
        self.readers = {}
        self.seen = {e: {} for e in self.ENG}
        self.seen_dma = {e: set() for e in self.ENG}
        self.dcounts = {}
        self.dsems = {}
        self.n = 0
        self.pending = {e: None for e in self.ENG}
        self.capture = None

    def barrier(self):
        last = [self.ops[e][-1] for e in self.ENG if self.ops[e] and not self.ops[e][-1].is_dma]
        last = []
        for e in self.ENG:
            for op in reversed(self.ops[e]):
                if not op.is_dma:
                    last.append(op); break
        for op in last:
            op.need_inc = True
        dm = [(self.dsems[k], c) for k, c in self.dcounts.items()]
        for e in self.ENG:
            self.pending[e] = (last, dm)

    def add(self, eng, fn, reads=(), writes=(), dsem=None):
        if self.capture is not None:
            self.capture.append((eng, fn, list(reads), list(writes), dsem))
            return None
        op = _Op()
        op.eng = eng; op.fn = fn; op.idx = self.n; self.n += 1
        op.eidx = len(self.ops[eng]); op.need_inc = False; op.dsem = dsem
        op.is_dma = dsem is not None; op.inc_no = None
        if op.is_dma:
            self.dcounts[id(dsem)] = self.dcounts.get(id(dsem), 0) + 16
            self.dsems[id(dsem)] = dsem
            op.dcount = self.dcounts[id(dsem)]
        deps = []
        for k in reads:
            w = self.last_w.get(k)
            if w is not None:
                deps.append((w, "raw"))
            if isinstance(k, tuple) and k[0] in ("B", "T"):
                for r in self.readers.get(k, ()):
                    if r.eng != eng:
                        deps.append((r, "war"))
        for k in writes:
            w = self.last_w.get(k)
            if w is not None:
                deps.append((w, "waw"))
            for r in self.readers.get(k, ()):
                deps.append((r, "war"))
        final_eng = {}
        final_dma = []
        for d, kind in deps:
            if d is op:
                continue
            if d.is_dma:
                if d.idx not in self.seen_dma[eng]:
                    self.seen_dma[eng].add(d.idx)
                    final_dma.append(d)
                continue
            if d.eng == eng and not op.is_dma:
                if eng == "pe":
                    continue
                if kind != "raw":
                    continue
            if d.eidx <= self.seen[eng].get(d.eng, -1):
                continue
            if d.eng not in final_eng or final_eng[d.eng].eidx < d.eidx:
                final_eng[d.eng] = d
        for f, d in final_eng.items():
            self.seen[eng][f] = d.eidx
            d.need_inc = True
        op.deps = list(final_eng.values()) + final_dma
        op.bar = self.pending[eng]
        if op.bar is not None:
            self.pending[eng] = None
            for d in op.bar[0]:
                if d.eng != eng:
                    self.seen[eng][d.eng] = max(self.seen[eng].get(d.eng, -1), d.eidx)
        for k in writes:
            self.last_w[k] = op
            self.readers[k] = []
        for k in reads:
            lst = self.readers.setdefault(k, [])
            if not op.is_dma:
                lst[:] = [r for r in lst if r.is_dma or r.eng != eng]
            lst.append(op)
        self.ops[eng].append(op)
        return op

    def emit(self, block, sems):
        for e in self.ENG:
            c = 0
            for op in self.ops[e]:
                if op.need_inc and not op.is_dma:
                    c += 1
                    op.inc_no = c
        me = self

        def run(e, engobj):
            for op in me.ops[e]:
                if op.bar is not None:
                    for d in op.bar[0]:
                        engobj.wait_ge(sems[d.eng], d.inc_no)
                    for sm, cnt in op.bar[1]:
                        engobj.wait_ge(sm, cnt)
                for d in op.deps:
                    if d.is_dma:
                        engobj.wait_ge(d.dsem, d.dcount)
                    else:
                        engobj.wait_ge(sems[d.eng], d.inc_no)
                ins = op.fn(engobj)
                if op.is_dma:
                    ins.then_inc(op.dsem, 16)
                elif op.need_inc:
                    ins.then_inc(sems[e], 1)
            if e == "sp":
                for k, s in me.dsems.items():
                    engobj.wait_ge(s, me.dcounts[k])

        @block.tensor
        def _(eng):
            run("pe", eng)

        @block.scalar
        def _(eng):
            run("act", eng)

        @block.vector
        def _(eng):
            run("dve", eng)

        @block.gpsimd
        def _(eng):
            run("pool", eng)

        @block.sync
        def _(eng):
            run("sp", eng)


def build_nc(n_step=NSTEP, n_kchunk=NCH - 1, moe=True, n_exp=NEXP, dbg_h1=False, sparse=True):
    nc = bass.Bass("TRN2", target_bir_lowering=False)
    S = Sched()

    def din(name, shape, dt=F32):
        return nc.dram_tensor(name, list(shape), dt, kind="ExternalInput").ap()

    xs = din("xs", [NBLK * 128, 1024])
    cosT = din("cosT", [128, NBLK * 128])
    sinT = din("sinT", [128, NBLK * 128])
    kbm = din("kbm", [128, NBLK])
    kbs = din("kbs", [128, NBLK])
    cst = din("cst", [128, 128 * 4 + 512 * 2])
    WK_d = din("WK", [1024, 896])
    WQ_d = din("WQ", [1024, 1280])
    WUQ_d = din("WUQ", [256, 1024])
    WUK_d = din("WUK", [256, 512])
    WUV_d = din("WUV", [256, 512])
    WOA_d = din("WOA", [128, 4 * 1024])
    WOB_d = din("WOB", [128, 4 * 1024])
    WR_d = din("WR", [1024, 36])
    vec_d = din("vec", [128, 40])
    rows_d = din("rows", [7, 1024])
    if sparse:
        wg2_d = din("wg2", [NEXP * 128, 2048])
        wu2_d = din("wu2", [NEXP * 128, 2048])
        wd2_d = din("wd2", [NEXP * 128, 2048])
        cst2_d = din("cst2", [128, 128 + 8 + NSLOT])
        xs_d = nc.dram_tensor("xs_scr", [NSLOT * SLOT, ROWW], F32, kind="Internal").ap()
        ffn_d = nc.dram_tensor("ffn_scr", [NSLOT * SLOT, 1024], F32, kind="Internal").ap()
    else:
        wg_d = din("wg", [NEXP, 1024, 256])
        wu_d = din("wu", [NEXP, 1024, 256])
        wd_d = din("wd", [NEXP, 256, 1024])
    out_d = nc.dram_tensor("out", [NSTEP * 512, 1024], F32, kind="ExternalOutput").ap()
    kv_d = nc.dram_tensor("kv_scr", [NBLK, 128, KVW], BF16, kind="Internal").ap()
    sw_d = nc.dram_tensor("sw_scr", [NBLK, 128, SWW], BF16, kind="Internal").ap()
    h1_d = nc.dram_tensor("h1_scr", [NSTEP * 512, 1024], F32, kind="Internal").ap()

    with ExitStack() as es:
        cur = [es]

        def sb(name, shape, dt):
            return cur[0].enter_context(nc.sbuf_tensor(name, list(shape), dt))

        def ps(name, shape, dt):
            return es.enter_context(nc.psum_tensor(name, list(shape), dt))

        def sem(name):
            return es.enter_context(nc.semaphore(name))

        sems = {e: sem("s_" + e) for e in Sched.ENG}
        B = [ps("B%d" % i, [128, 512], F32) for i in range(6)]
        T = [ps("T%d" % i, [128, 1024], BF16) for i in range(2)]

        def kB(i):
            return ("B", i)

        def kT(i):
            return ("T", i)

        ident = sb("ident", [128, 128], BF16)
        ones = sb("ones", [128, 128], BF16)
        onesg = sb("onesg", [128, 2, 128], BF16)
        maskP = sb("maskP", [128, 512], BF16)
        maskC = sb("maskC", [128, 512], BF16)
        identf = sb("identf", [128, 128], F32)
        onesf = sb("onesf", [128, 128], F32)
        kbm_t = sb("kbm_t", [128, NBLK], F32)
        kbs_t = sb("kbs_t", [128, NBLK], F32)
        vec = sb("vec_s", [128, 40], F32)
        sinkexp = sb("sinkexp", [128, 4], F32)
        epsc = sb("epsc", [128, 2], F32)
        epsln = epsc[:, 0:1]
        epsrms = epsc[:, 1:2]
        S.add("dve", lambda e: e.memset(epsc[:, 0:1], LN_EPS), writes=["epsc0"])
        S.add("dve", lambda e: e.memset(epsc[:, 1:2], RMS_EPS), reads=["epsc0"], writes=["epsc"])
        st = [sb("st%d" % i, [128, 2, 6], F32) for i in range(4)]
        mv = [sb("mv%d" % i, [128, 4], F32) for i in range(4)]
        zt = sb("zt", [128, ROWW], F32)
        d_z = sem("d_z")
        S.add("pool", lambda e: e.memset(zt[:], 0.0), writes=["zt"])
        nzb = NSLOT * SLOT // 128
        zfill = [0]
        allz = [("xsz", i) for i in range(nzb)]

        def emit_zfill(n):
            while sparse and n > 0 and zfill[0] < nzb:
                i = zfill[0]; zfill[0] += 1
                S.add("sp", lambda e, i=i: e.dma_start(out=xs_d[i * 128:(i + 1) * 128, :], in_=zt[:]), reads=["zt"], writes=[("xsz", i)], dsem=d_z)
                n -= 1
        e_att = ExitStack()
        cur[0] = e_att
        d_c = [sem("d_c%d" % i) for i in range(28)]
        _ci = [0]

        import os as _os
        _lim = int(_os.environ.get("KDBG_NCLOAD", "999"))
        _skipms = _os.environ.get("KDBG_SKIPMS", "0") == "1"

        def cload(out, in_, key, q="pool"):
            if _ci[0] >= _lim:
                _ci[0] += 1
                return
            s = d_c[_ci[0]]; _ci[0] += 1
            S.add(q, lambda e: e.dma_start(out=out, in_=in_), writes=[key], dsem=s)

        cload(ident[:], cst[:, 0:128], "ident")
        cload(ones[:], cst[:, 128:256], "ones")
        cload(onesg[:, 0, :], cst[:, 256:384], "onesg0")
        cload(onesg[:, 1, :], cst[:, 384:512], "onesg1")
        cload(maskP[:], cst[:, 512:1024], "maskP")
        cload(maskC[:], cst[:, 1024:1536], "maskC")
        cload(identf[:], cst[:, 0:128], "identf", q="sp")
        cload(onesf[:], cst[:, 128:256], "onesf", q="sp")
        cload(kbm_t[:], kbm[:, :], "kbm", q="sp")
        cload(kbs_t[:], kbs[:, :], "kbs", q="sp")
        cload(vec[:], vec_d[:, :], "vec", q="sp")
        VG, VB, VQG, VKG, VGA, VGB, VSK = 0, 8, 16, 18, 20, 24, 28

        NX = 8
        xt = [sb("xt%d" % i, [128, 1024], F32) for i in range(NX)]
        d_x = [sem("d_x%d" % i) for i in range(NX)]
        xnb = sb("xnb", [128, 4, 1024], BF16)
        hT = sb("hT", [128, 8, 512], BF16)
        cs_t = sb("cs_t", [128, 512], F32)
        sn_t = sb("sn_t", [128, 512], F32)
        d_cs = sem("d_cs"); d_sn = sem("d_sn")
        t1 = [sb("t1_%d" % i, [128, 512], F32) for i in range(2)]
        t2 = [sb("t2_%d" % i, [128, 512], F32) for i in range(2)]
        sq = sb("sq", [128, 4, 512], BF16)
        rstd_t = sb("rstd_t", [128, 512], F32)
        e_k = ExitStack()
        cur[0] = e_k
        WK = sb("WK_s", [128, 8, 896], BF16)
        WUK = sb("WUK_s", [128, 2, 512], BF16)
        WUV = sb("WUV_s", [128, 2, 512], BF16)
        cload(WK[:], WK_d.rearrange("(k p) f -> p k f", p=128), "WK")
        cload(WUK[:], WUK_d.rearrange("(k p) f -> p k f", p=128), "WUK")
        cload(WUV[:], WUV_d.rearrange("(k p) f -> p k f", p=128), "WUV")
        ckvn = sb("ckvn", [128, 2, 512], BF16)
        kvrec = [sb("kvrec%d" % i, [128, 4, KVW], BF16) for i in range(2)]
        swrec = [sb("swrec%d" % i, [128, 4, SWW], BF16) for i in range(2)]
        d_kvw = [sem("d_kvw%d" % i) for i in range(2)]
        d_sww = [sem("d_sww%d" % i) for i in range(2)]

        for i in range(2):
            if not _skipms:
                S.add("pool", (lambda t: (lambda e: e.memset(t[:], 0.0)))(swrec[i]), writes=[("swrec", i)])

        xr = [0]

        def chunk_loads(ch):
            slots = []
            for t in range(4):
                s = xr[0]; xr[0] = (xr[0] + 1) % NX
                slots.append(s)
                blk = ch * 4 + t
                S.add("sp", (lambda s=s, blk=blk: (lambda e: e.dma_start(out=xt[s][:], in_=xs[blk * 128:(blk + 1) * 128, :])))(),
                      writes=[("x", s)], dsem=d_x[s])
            return slots

        def rope_loads(ch):
            S.add("sp", lambda e: e.dma_start(out=cs_t[:], in_=cosT[:, ch * 512:(ch + 1) * 512]), writes=["cs"], dsem=d_cs)
            S.add("sp", lambda e: e.dma_start(out=sn_t[:], in_=sinT[:, ch * 512:(ch + 1) * 512]), writes=["sn"], dsem=d_sn)

        def front(slots, want_res):
            _sub = int(_os.environ.get("KDBG_SUB", "99"))
            for t in range(4):
                s = slots[t]
                x_ = xt[s]
                st_, mv_ = st[t], mv[t]
                S.add("dve", lambda e, x_=x_, st_=st_: e.bn_stats(out=st_[:, 0, :], in_=x_[:, 0:512]), reads=[("x", s)], writes=[("st", t, 0)])
                S.add("dve", lambda e, x_=x_, st_=st_: e.bn_stats(out=st_[:, 1, :], in_=x_[:, 512:1024]), reads=[("x", s)], writes=[("st", t, 1)])
                S.add("dve", lambda e, st_=st_, mv_=mv_: e.bn_aggr(out=mv_[:, 0:2], in_=st_[:, :, :]), reads=[("st", t, 0), ("st", t, 1)], writes=[("mv", t, 0)])
                if _sub < 2:
                    continue
                S.add("act", lambda e, mv_=mv_: e.activation(out=mv_[:, 2:3], in_=mv_[:, 1:2], func=AF.Ln, bias=epsln[:, 0:1], scale=1.0),
                      reads=[("mv", t, 0), "epsc"], writes=[("mv", t, 1)])
                S.add("act", lambda e, mv_=mv_: e.activation(out=mv_[:, 2:3], in_=mv_[:, 2:3], func=AF.Exp, scale=-0.5),
                      reads=[("mv", t, 1)], writes=[("mv", t, 1)])
                S.add("dve", lambda e, mv_=mv_: e.scalar_tensor_tensor(out=mv_[:, 3:4], in0=mv_[:, 0:1], scalar=-1.0, in1=mv_[:, 2:3], op0=ALU.mult, op1=ALU.mult),
                      reads=[("mv", t, 0), ("mv", t, 1)], writes=[("mv", t, 2)])
                if _sub < 3:
                    continue
                S.add("act", lambda e, x_=x_, mv_=mv_, t=t: e.activation(out=xnb[:, t, :], in_=x_[:], func=AF.Identity, bias=mv_[:, 3:4], scale=mv_[:, 2:3]),
                      reads=[("x", s), ("mv", t, 1), ("mv", t, 2)], writes=[("xnb", t)])
                if want_res:
                    S.add("pool", lambda e, x_=x_, mv_=mv_: e.tensor_scalar(out=x_[:], in0=x_[:], scalar1=mv_[:, 2:3], scalar2=mv_[:, 3:4], op0=ALU.mult, op1=ALU.add),
                          reads=[("x", s), ("mv", t, 1), ("mv", t, 2)], writes=[("x", s)])
                    S.add("pool", lambda e, x_=x_: e.tensor_tensor(out=x_[:], in0=x_[:], in1=gabc[:], op=ALU.mult), reads=[("x", s), "gabc"], writes=[("x", s)])
                    S.add("pool", lambda e, x_=x_: e.tensor_tensor(out=x_[:], in0=x_[:], in1=babc[:], op=ALU.add), reads=[("x", s), "babc"], writes=[("x", s)])
            for kp in range(4):
                if _sub < 4:
                    continue
                Tb = T[kp % 2]
                for kk in range(2):
                    k = 2 * kp + kk
                    for t in range(4):
                        S.add("pe", lambda e, Tb=Tb, kk=kk, t=t, k=k: e.transpose(out=Tb[:, (kk * 4 + t) * 128:(kk * 4 + t + 1) * 128],
                                                                                in_=xnb[:, t, k * 128:(k + 1) * 128], identity=ident[:]),
                              reads=[("xnb", t), "ident"], writes=[kT(kp % 2)])
                for kk in range(2):
                    if _sub < 5:
                        continue
                    k = 2 * kp + kk
                    _ev = _os.environ.get("KDBG_EV", "")
                    if (_ev == "act" and kk == 1) or (_ev == "dve" and kk == 0):
                        continue
                    if (kp % 2 == 0 and _ev != "alldve") or _ev == "allact":
                        S.add("act", lambda e, Tb=Tb, kk=kk, k=k: e.activation(out=hT[:, k, :], in_=Tb[:, kk * 512:(kk + 1) * 512], func=AF.Identity,
                                                                                bias=vec[:, VB + k:VB + k + 1], scale=vec[:, VG + k:VG + k + 1]),
                              reads=[kT(kp % 2), "vec"], writes=[("hT", k)])
                    else:
                        S.add("dve", lambda e, Tb=Tb, kk=kk, k=k: e.tensor_scalar(out=hT[:, k, :], in0=Tb[:, kk * 512:(kk + 1) * 512],
                                                                                   scalar1=vec[:, VG + k:VG + k + 1], scalar2=vec[:, VB + k:VB + k + 1],
                                                                                   op0=ALU.mult, op1=ALU.add),
                              reads=[kT(kp % 2), "vec"], writes=[("hT", k)])

        hT_all = [("hT", k) for k in range(8)]

        def proj(bi, W, c0, m, wkey, ncols=512):
            for k in range(8):
                S.add("pe", lambda e, k=k: e.matmul(B[bi][0:m, 0:ncols], lhsT=W[:, k, c0:c0 + m], rhs=hT[:, k, 0:ncols], start=(k == 0), stop=(k == 7)),
                      reads=[("hT", k), wkey], writes=[kB(bi)])

        def projP(pap, pkey, W, c0, wkey):
            for k in range(8):
                S.add("pe", lambda e, k=k: e.matmul(pap, lhsT=W[:, k, c0:c0 + 128], rhs=hT[:, k, :], start=(k == 0), stop=(k == 7)),
                      reads=[("hT", k), wkey], writes=[pkey])

        def rope_applyP(qap, qkey, rap, rkey, out_ap, okey, i):
            S.add("dve", lambda e: e.tensor_tensor(out=t1[i][:, :], in0=qap, in1=cs_t[:, :], op=ALU.mult), reads=[qkey, "cs"], writes=[("t1", i)])
            S.add("dve", lambda e: e.tensor_tensor(out=t2[i][:, :], in0=rap, in1=sn_t[:, :], op=ALU.mult), reads=[rkey, "sn"], writes=[("t2", i)])
            S.add("pool", lambda e: e.tensor_tensor(out=out_ap, in0=t1[i][:, :], in1=t2[i][:, :], op=ALU.add), reads=[("t1", i), ("t2", i)], writes=[okey])

        def rope_apply(bq, br, out_ap, okey, np_=128, i=0, o3=False):
            def v(t):
                a = t[0:np_, :]
                return a.rearrange("p (t c) -> p t c", t=4) if o3 else a
            S.add("dve", lambda e: e.tensor_tensor(out=t1[i][0:np_, :], in0=B[bq][0:np_, :], in1=cs_t[0:np_, :], op=ALU.mult), reads=[kB(bq), "cs"], writes=[("t1", i)])
            S.add("dve", lambda e: e.tensor_tensor(out=t2[i][0:np_, :], in0=B[br][0:np_, :], in1=sn_t[0:np_, :], op=ALU.mult), reads=[kB(br), "sn"], writes=[("t2", i)])
            S.add("pool", lambda e: e.tensor_tensor(out=out_ap, in0=v(t1[i]), in1=v(t2[i]), op=ALU.add), reads=[("t1", i), ("t2", i)], writes=[okey])

        def rms_feat(banks, gcol, nfeat, out_tile, okeys, bsum, in_keys=None, src_sb=None):
            n = len(banks)
            for c in range(n):
                rk = in_keys[c]
                S.add("act", lambda e, c=c: e.activation(out=sq[:, c, :], in_=banks[c], func=AF.Square), reads=[rk], writes=[("sq", c)])
            for c in range(n):
                S.add("pe", lambda e, c=c: e.matmul(B[bsum][:, :], lhsT=ones[:], rhs=sq[:, c, :], start=(c == 0), stop=(c == n - 1)),
                      reads=[("sq", c), "ones"], writes=[kB(bsum)])
            S.add("act", lambda e: e.activation(out=rstd_t[:], in_=B[bsum][:, :], func=AF.Ln, bias=epsrms[:, 0:1], scale=1.0 / nfeat),
                  reads=[kB(bsum), "epsc"], writes=["rstd"])
            S.add("act", lambda e: e.activation(out=rstd_t[:], in_=rstd_t[:], func=AF.Exp, scale=-0.5), reads=["rstd"], writes=["rstd"])
            for c in range(n):
                rk = in_keys[c]
                eng = "dve"
                S.add(eng, lambda e, c=c: e.scalar_tensor_tensor(out=out_tile[:, c, :], in0=banks[c], scalar=vec[:, gcol + c:gcol + c + 1], in1=rstd_t[:],
                                                                  op0=ALU.mult, op1=ALU.mult), reads=[rk, "rstd", "vec"], writes=[okeys[c]])

        def front_parts(slots_):
            cap = []
            S.capture = cap
            front(slots_, False)
            S.capture = None
            fi = next(i for i, o in enumerate(cap) if o[0] == "pe")
            return cap[:fi], cap[fi:]

        def emit_list(lst):
            for eng_, fn_, r_, w_, ds_ in lst:
                S.add(eng_, fn_, reads=r_, writes=w_, dsem=ds_)

        kslots = {}
        if n_kchunk > 0:
            kslots[0] = chunk_loads(0)
            if n_kchunk > 1:
                kslots[1] = chunk_loads(1)
            pa, pb = front_parts(kslots[0])
            emit_list(pa); emit_list(pb)
        for ch in range(n_kchunk):
            rope_loads(ch)
            if ch + 2 < n_kchunk:
                kslots[ch + 2] = chunk_loads(ch + 2)
            emit_zfill(3)
            kr = kvrec[ch % 2]; sr = swrec[ch % 2]
            kvk = ("kvrec", ch % 2); swk = ("swrec", ch % 2)
            proj(0, WK, 0, 128, "WK")
            proj(1, WK, 128, 128, "WK")
            proj(2, WK, 256, 128, "WK")
            proj(3, WK, 384, 128, "WK")
            proj(4, WK, 640, 128, "WK")
            proj(5, WK, 768, 128, "WK")
            if ch + 1 < n_kchunk:
                pa, pb = front_parts(kslots[ch + 1])
                emit_list(pa)
            else:
                pb = []
            vap = T[0][:, :].bitcast(F32)
            for t in range(4):
                for k in range(8):
                    S.add("pe", lambda e, t=t, k=k: e.matmul(vap[:, t * 128:(t + 1) * 128], lhsT=hT[:, k, t * 128:(t + 1) * 128], rhs=WK[:, k, 512:640],
                                                             start=(k == 0), stop=(k == 7)), reads=[("hT", k), "WK"], writes=[kT(0)])
            b4v = vap.rearrange("p (t c) -> p t c", t=4)
            S.add("act", lambda e, sr=sr, b4v=b4v: e.activation(out=sr[:, :, 128:192], in_=b4v[:, :, 0:64], func=AF.Copy), reads=[kT(0)], writes=[swk])
            S.add("act", lambda e, sr=sr, b4v=b4v: e.activation(out=sr[:, :, 320:384], in_=b4v[:, :, 64:128], func=AF.Copy), reads=[kT(0)], writes=[swk])
            emit_list(pb)
            rope_apply(0, 1, sr[:, :, 0:128], swk, i=0, o3=True)
            rope_apply(4, 5, kr[:, :, 512:640], kvk, i=1, o3=True)
            rms_feat([B[2][:, :], B[3][:, :]], VKG, 256.0, ckvn, [("ckvn", 0), ("ckvn", 1)], 0, in_keys=[kB(2), kB(3)])
            for h in range(4):
                bi = [1, 2, 3, 5][h]
                for k in range(2):
                    S.add("pe", lambda e, h=h, k=k, bi=bi: e.matmul(B[bi][:, :], lhsT=WUK[:, k, h * 128:(h + 1) * 128], rhs=ckvn[:, k, :], start=(k == 0), stop=(k == 1)),
                          reads=[("ckvn", k), "WUK"], writes=[kB(bi)])
                src = B[bi][:, :].rearrange("p (t c) -> p t c", t=4)
                if h % 2 == 0:
                    S.add("act", lambda e, h=h, src=src, kr=kr: e.activation(out=kr[:, :, h * 128:(h + 1) * 128], in_=src, func=AF.Copy), reads=[kB(bi)], writes=[kvk])
                else:
                    S.add("dve", lambda e, h=h, src=src, kr=kr: e.tensor_copy(out=kr[:, :, h * 128:(h + 1) * 128], in_=src), reads=[kB(bi)], writes=[kvk])
            for t in range(4):
                bi = [0, 4, 1, 2][t]
                for k in range(2):
                    S.add("pe", lambda e, t=t, k=k, bi=bi: e.matmul(B[bi][:, :], lhsT=ckvn[:, k, t * 128:(t + 1) * 128], rhs=WUV[:, k, :], start=(k == 0), stop=(k == 1)),
                          reads=[("ckvn", k), "WUV"], writes=[kB(bi)])
                if t % 2 == 0:
                    S.add("act", lambda e, t=t, bi=bi, kr=kr: e.activation(out=kr[:, t, 640:1152], in_=B[bi][:, :], func=AF.Copy), reads=[kB(bi)], writes=[kvk])
                else:
                    S.add("dve", lambda e, t=t, bi=bi, kr=kr: e.tensor_copy(out=kr[:, t, 640:1152], in_=B[bi][:, :]), reads=[kB(bi)], writes=[kvk])
            S.add("sp", lambda e, kr=kr, ch=ch: e.dma_start(out=kv_d[ch * 4:(ch + 1) * 4].rearrange("b p f -> p b f"), in_=kr[:]),
                  reads=[kvk], writes=[("kvd", ch)], dsem=d_kvw[ch % 2])
            S.add("sp", lambda e, sr=sr, ch=ch: e.dma_start(out=sw_d[ch * 4:(ch + 1) * 4].rearrange("b p f -> p b f"), in_=sr[:]),
                  reads=[swk], writes=[("swd", ch)], dsem=d_sww[ch % 2])

        emit_zfill(nzb)
        S.barrier()
        e_k.close()
        e_q = ExitStack()
        cur[0] = e_q
        sinkbc = sb("sinkbc", [128, 512], F32)
        g1bc = sb("g1bc", [128, 1024], F32)
        b1bc = sb("b1bc", [128, 1024], F32)
        gabc = sb("gabc", [128, 1024], F32)
        babc = sb("babc", [128, 1024], F32)
        WQ = sb("WQ_s", [128, 8, 1280], BF16)
        WUQ = sb("WUQ_s", [128, 2, 1024], BF16)
        WOA = sb("WOA_s", [128, 4, 1024], BF16)
        WOB = sb("WOB_s", [128, 4, 1024], BF16)
        if n_step > 0:
            cload(g1bc[:], rows_d[0, :].partition_broadcast(128), "g1bc", q="sp")
            cload(b1bc[:], rows_d[1, :].partition_broadcast(128), "b1bc", q="sp")
            cload(WQ[:], WQ_d.rearrange("(k p) f -> p k f", p=128), "WQ")
            cload(WUQ[:], WUQ_d.rearrange("(k p) f -> p k f", p=128), "WUQ")
            cload(WOA[:], WOA_d.rearrange("p (c f) -> p c f", c=4), "WOA")
            cload(WOB[:], WOB_d.rearrange("p (c f) -> p c f", c=4), "WOB")
            cload(gabc[:], rows_d[2, :].partition_broadcast(128), "gabc", q="sp")
            cload(babc[:], rows_d[3, :].partition_broadcast(128), "babc", q="sp")
            S.add("pool", lambda e: e.tensor_scalar(out=gabc[:], in0=gabc[:], scalar1=ALPHA, scalar2=None, op0=ALU.mult), reads=["gabc"], writes=["gabc"])
            S.add("pool", lambda e: e.tensor_scalar(out=babc[:], in0=babc[:], scalar1=ALPHA, scalar2=None, op0=ALU.mult), reads=["babc"], writes=["babc"])
            S.add("act", lambda e: e.activation(out=sinkexp[:], in_=vec[:, VSK:VSK + 4], func=AF.Exp), reads=["vec"], writes=["sinkexp"])
            for c in range(4):
                S.add("dve", lambda e, c=c: e.tensor_scalar(out=sinkbc[:, c * 128:(c + 1) * 128], in0=maskC[:, 0:128], scalar1=0.0, scalar2=sinkexp[:, c:c + 1], op0=ALU.mult, op1=ALU.add),
                      reads=["maskC", "sinkexp"], writes=["sinkbc"])
        qaT = sb("qaT", [128, 4, 512], BF16)
        cqn = sb("cqn", [128, 2, 512], BF16)
        qnT = sb("qnT", [128, 4, 512], BF16)
        qrT = sb("qrT", [128, 4, 512], BF16)
        swt = sb("swt", [128, 5, SWW], BF16)
        d_swt = sem("d_swt")
        NKR = 4
        kvt = [sb("kvt%d" % i, [128, KVW], BF16) for i in range(NKR)]
        d_kvt = [sem("d_kvt%d" % i) for i in range(NKR)]
        NPB = 8
        Pb = [sb("Pb%d" % i, [128, 512], BF16) for i in range(NPB)]
        accs = sb("accs", [128, 4, 512], F32)
        rec = sb("rec", [128, 512], F32)
        aT = sb("aT", [128, 4, 512], F32)
        anT = sb("anT", [128, 4, 512], BF16)
        bT = sb("bT", [128, 4, 512], F32)
        bnT = sb("bnT", [128, 4, 512], BF16)
        rbuf = [accs[:, 0:2, :].rearrange("p h q -> p (h q)"), accs[:, 2:4, :].rearrange("p h q -> p (h q)")]
        d_h1 = [sem("d_h1_%d" % i) for i in range(2)]
        kvi = [0]
        pbi = [0]
        scale_mla = 192.0 ** -0.5

        if n_step > 0:
            S.add("pool", lambda e: e.memset(qrT[:], 0.0), writes=[("qrT", h) for h in range(4)])
        next_slots = chunk_loads(2) if n_step > 0 else None
        def swt_load(ch):
            S.add("sp", lambda e: e.dma_start(out=swt[:], in_=sw_d[ch * 4 - 1:ch * 4 + 4].rearrange("b p f -> p b f")),
                  reads=[("swd", ch - 1), ("swd", ch)], writes=["swt"], dsem=d_swt)

        def qa_proj(use_t):
            for c in range(4):
                if use_t:
                    qp = (T[0][:, :].bitcast(F32), kT(0)); rp = (T[1][:, :].bitcast(F32), kT(1))
                else:
                    qp = (B[2 * (c % 2)][:, :], kB(2 * (c % 2))); rp = (B[2 * (c % 2) + 1][:, :], kB(2 * (c % 2) + 1))
                projP(qp[0], qp[1], WQ, c * 128, "WQ")
                projP(rp[0], rp[1], WQ, 512 + c * 128, "WQ")
                rope_applyP(qp[0], qp[1], rp[0], rp[1], qaT[:, c, :], ("qaT", c), c % 2)

        hoisted = False
        pending_tail = []
        HOIST = _os.environ.get("KDBG_NOHOIST", "0") != "1"
        for j in range(n_step):
            ch = 2 * j + 2
            slots = next_slots
            if not hoisted:
                rope_loads(ch)
                swt_load(ch)
            if not hoisted:
                front(slots, True)
                qa_proj(False)
            HS = []
            S.capture = []
            proj(4, WQ, 1024, 128, "WQ")
            proj(5, WQ, 1152, 128, "WQ")
            rms_feat([B[4][:, :], B[5][:, :]], VQG, 256.0, cqn, [("cqn", 0), ("cqn", 1)], 0, in_keys=[kB(4), kB(5)])
            HS.append(S.capture); S.capture = []
            for h in range(4):
                bi = 1 + h
                for k in range(2):
                    S.add("pe", lambda e, h=h, k=k, bi=bi: e.matmul(B[bi][:, :], lhsT=WUQ[:, k, h * 128:(h + 1) * 128], rhs=cqn[:, k, :], start=(k == 0), stop=(k == 1)),
                          reads=[("cqn", k), "WUQ"], writes=[kB(bi)])
                if h % 2 == 0:
                    S.add("act", lambda e, h=h, bi=bi: e.activation(out=qnT[:, h, :], in_=B[bi][:, :], func=AF.Copy), reads=[kB(bi)], writes=[("qnT", h)])
                else:
                    S.add("dve", lambda e, h=h, bi=bi: e.tensor_copy(out=qnT[:, h, :], in_=B[bi][:, :]), reads=[kB(bi)], writes=[("qnT", h)])
            HS.append(S.capture); S.capture = []
            for pr in range(2):
                bq, br = (0, 5) if pr == 0 else (1, 2)
                for k in range(2):
                    S.add("pe", lambda e, pr=pr, k=k, bq=bq: e.matmul(B[bq][:, :], lhsT=WUQ[:, k, 512 + pr * 128:512 + (pr + 1) * 128], rhs=cqn[:, k, :], start=(k == 0), stop=(k == 1)),
                          reads=[("cqn", k), "WUQ"], writes=[kB(bq)])
                for k in range(2):
                    S.add("pe", lambda e, pr=pr, k=k, br=br: e.matmul(B[br][:, :], lhsT=WUQ[:, k, 768 + pr * 128:768 + (pr + 1) * 128], rhs=cqn[:, k, :], start=(k == 0), stop=(k == 1)),
                          reads=[("cqn", k), "WUQ"], writes=[kB(br)])
                S.add("dve", lambda e, bq=bq, pr=pr: e.tensor_tensor(out=t1[pr][:, :], in0=B[bq][:, :], in1=cs_t[:, :], op=ALU.mult), reads=[kB(bq), "cs"], writes=[("t1", pr)])
                S.add("dve", lambda e, br=br, pr=pr: e.tensor_tensor(out=t2[pr][:, :], in0=B[br][:, :], in1=sn_t[:, :], op=ALU.mult), reads=[kB(br), "sn"], writes=[("t2", pr)])
                for hh in range(2):
                    S.add("pool", lambda e, pr=pr, hh=hh: e.tensor_tensor(out=qrT[hh * 64:(hh + 1) * 64, 2 * pr + hh, :], in0=t1[pr][hh * 64:(hh + 1) * 64, :], in1=t2[pr][hh * 64:(hh + 1) * 64, :], op=ALU.add),
                          reads=[("t1", pr), ("t2", pr)], writes=[("qrT", 2 * pr + hh)])
            HS.append(S.capture); S.capture = []
            swa_p = {}
            OS = [(B[4][:, :], kB(4), B[5][:, :], kB(5)), (T[0][:, :].bitcast(F32), kT(0), T[1][:, :].bitcast(F32), kT(1))]

            def swa_st(qb):
                for g in range(2):
                    for kk in range(2):
                        kbi = qb + kk
                        bi = g * 2 + kk
                        S.add("pe", lambda e, g=g, kbi=kbi, bi=bi: e.matmul(B[bi][:, :].rearrange("p (c i) -> p c i", c=4),
                                                                             lhsT=swt[g * 64:(g + 1) * 64, kbi, 0:128],
                                                                             rhs=qaT[g * 64:(g + 1) * 64, :, qb * 128:(qb + 1) * 128], start=True, stop=True),
                              reads=["swt"] + [("qaT", c) for c in range(4)], writes=[kB(bi)])

            def swa_exp(qb):
                swa_p[qb] = []
                for g in range(2):
                    for kk in range(2):
                        kbi = qb + kk
                        slotblk = ch * 4 - 1 + kbi
                        bi = g * 2 + kk
                        pi = pbi[0]; pbi[0] = (pbi[0] + 1) % NPB
                        swa_p[qb].append(pi)
                        S.add("act", lambda e, bi=bi, pi=pi, slotblk=slotblk: e.activation(out=Pb[pi][:], in_=B[bi][:, :], func=AF.Exp,
                                                                                            bias=kbs_t[:, slotblk:slotblk + 1], scale=0.125),
                              reads=[kB(bi), "kbs"], writes=[("Pb", pi)])
                        mk = maskP if kk == 0 else maskC
                        mkk = "maskP" if kk == 0 else "maskC"
                        S.add("dve" if g == 0 else "pool", lambda e, pi=pi, mk=mk: e.tensor_tensor(out=Pb[pi][:], in0=Pb[pi][:], in1=mk[:], op=ALU.mult), reads=[("Pb", pi), mkk], writes=[("Pb", pi)])

            def swa_pv(qb):
                oap, ok_, sap, sk_ = OS[qb % 2]
                u = 0
                for g in range(2):
                    for kk in range(2):
                        kbi = qb + kk
                        pi = swa_p[qb][u]; u += 1
                        first = (g == 0 and kk == 0); last = (g == 1 and kk == 1)
                        S.add("pe", lambda e, g=g, kbi=kbi, pi=pi, first=first, last=last: e.matmul(oap, lhsT=swt[:, kbi, 128 + g * 128:256 + g * 128], rhs=Pb[pi][:],
                                                                                                   start=first, stop=last), reads=["swt", ("Pb", pi)], writes=[ok_])
                        S.add("pe", lambda e, g=g, pi=pi, first=first, last=last: e.matmul(sap, lhsT=onesg[:, g, :], rhs=Pb[pi][:], start=first, stop=last),
                              reads=["onesg%d" % g, ("Pb", pi)], writes=[sk_])

            def swa_epi(qb):
                oap, ok_, sap, sk_ = OS[qb % 2]
                for c in range(4):
                    S.add("act", lambda e, c=c: e.activation(out=rec[:, c * 128:(c + 1) * 128], in_=sap[:, c * 128:(c + 1) * 128], func=AF.Ln, bias=sinkexp[:, c:c + 1], scale=1.0),
                          reads=[sk_, "sinkexp"], writes=["rec"])
                S.add("act", lambda e: e.activation(out=rec[:], in_=rec[:], func=AF.Exp, scale=-1.0), reads=["rec"], writes=["rec"])
                S.add("dve", lambda e: e.tensor_tensor(out=aT[:, :, qb * 128:(qb + 1) * 128], in0=oap.rearrange("p (c i) -> p c i", c=4),
                                                       in1=rec[:].rearrange("p (c i) -> p c i", c=4), op=ALU.mult), reads=[ok_, "rec"], writes=[("aT", qb)])

            swa_st(0); swa_exp(0)
            for qb in range(4):
                if qb + 1 < 4:
                    swa_st(qb + 1)
                swa_pv(qb)
                if qb + 1 < 4:
                    swa_exp(qb + 1)
                swa_epi(qb)
            HS.append(S.capture); S.capture = None
            TS = pending_tail
            pending_tail = []
            for si in range(max(len(HS), len(TS))):
                if si < len(HS):
                    emit_list(HS[si])
                if si < len(TS):
                    emit_list(TS[si])
            if j + 1 < n_step:
                next_slots = chunk_loads(ch + 2)
            aT_keys = [("aT", q) for q in range(4)]
            for c in range(4):
                S.add("act", lambda e, c=c: e.activation(out=sq[:, c, :], in_=aT[:, c, :], func=AF.Square), reads=aT_keys, writes=[("sq", c)])
            for c in range(4):
                S.add("pe", lambda e, c=c: e.matmul(B[0][:, :], lhsT=ones[:], rhs=sq[:, c, :], start=(c == 0), stop=(c == 3)), reads=[("sq", c), "ones"], writes=[kB(0)])
            S.add("act", lambda e: e.activation(out=rstd_t[:], in_=B[0][:, :], func=AF.Ln, bias=epsrms[:, 0:1], scale=1.0 / 512.0), reads=[kB(0), "epsc"], writes=["rstd"])
            S.add("act", lambda e: e.activation(out=rstd_t[:], in_=rstd_t[:], func=AF.Exp, scale=-0.5), reads=["rstd"], writes=["rstd"])
            for c in range(4):
                S.add("dve", lambda e, c=c: e.scalar_tensor_tensor(out=anT[:, c, :], in0=aT[:, c, :], scalar=vec[:, VGA + c:VGA + c + 1], in1=rstd_t[:], op0=ALU.mult, op1=ALU.mult),
                      reads=aT_keys + ["rstd", "vec"], writes=[("anT", c)])
            S.add("pool", lambda e: e.memset(accs[:], 0.0), writes=[("accs", h) for h in range(4)])
            kblocks = [(3, None)] + [(s, None) for s in range(4, ch * 4)] + [(ch * 4 + d, d) for d in range(4)]
            nkb = len(kblocks)
            units = [(idx, sblk, dg, h) for idx, (sblk, dg) in enumerate(kblocks) for h in range(4)]
            kslot = {}

            def mla_st(ui):
                idx, sblk, dg, h = units[ui]
                if h == 0:
                    ks = kvi[0]; kvi[0] = (kvi[0] + 1) % NKR
                    kslot[idx] = ks
                    S.add("sp", lambda e, ks=ks, sblk=sblk: e.dma_start(out=kvt[ks][:], in_=kv_d[sblk]), reads=[("kvd", sblk // 4)], writes=[("kvt", ks)], dsem=d_kvt[ks])
                ks = kslot[idx]
                q0 = 0 if dg is None else dg * 128
                sbk = 4 + (ui % 2)
                hp = (h % 2) * 64
                S.add("pe", lambda e: e.matmul(B[sbk][:, q0:512], lhsT=kvt[ks][:, h * 128:(h + 1) * 128], rhs=qnT[:, h, q0:512], start=True, stop=False),
                      reads=[("kvt", ks), ("qnT", h)], writes=[kB(sbk)])
                S.add("pe", lambda e: e.matmul(B[sbk][:, q0:512], lhsT=kvt[ks][:, 512:640], rhs=qrT[:, h, q0:512], start=False, stop=True),
                      reads=[("kvt", ks), ("qrT", h)], writes=[kB(sbk)])

            def mla_rest(ui):
                idx, sblk, dg, h = units[ui]
                ks = kslot[idx]
                q0 = 0 if dg is None else dg * 128
                sbk = 4 + (ui % 2)
                pi = pbi[0]; pbi[0] = (pbi[0] + 1) % NPB
                S.add("act", lambda e: e.activation(out=Pb[pi][:, q0:512], in_=B[sbk][:, q0:512], func=AF.Exp, bias=kbm_t[:, sblk:sblk + 1], scale=scale_mla),
                      reads=[kB(sbk), "kbm"], writes=[("Pb", pi)])
                if dg is not None:
                    S.add("pool", lambda e: e.tensor_tensor(out=Pb[pi][:, q0:q0 + 128], in0=Pb[pi][:, q0:q0 + 128], in1=maskC[:, 0:128], op=ALU.mult),
                          reads=[("Pb", pi), "maskC"], writes=[("Pb", pi)])
                S.add("pe", lambda e: e.matmul(B[h][:, q0:512], lhsT=kvt[ks][:, 640 + h * 128:640 + (h + 1) * 128], rhs=Pb[pi][:, q0:512],
                                               start=(idx == 0), stop=(idx == nkb - 1), skip_group_check=True),
                      reads=[("kvt", ks), ("Pb", pi)], writes=[kB(h)])
                S.add("dve" if h % 2 == 0 else "pool", lambda e: e.tensor_tensor(out=accs[:, h, q0:512], in0=accs[:, h, q0:512], in1=Pb[pi][:, q0:512], op=ALU.add),
                      reads=[("accs", h), ("Pb", pi)], writes=[("accs", h)])

            side = []
            hoisted = False
            if HOIST and j + 1 < n_step:
                S.capture = side
                rope_loads(ch + 2)
                swt_load(ch + 2)
                front(next_slots, True)
                qa_proj(True)
                S.capture = None
                hoisted = True
            nside = len(side)
            per_unit = max(1, -(-nside // max(1, int(len(units) * 0.8) - 4)))
            sp_ = [0]

            def emit_side(n):
                while n > 0 and sp_[0] < nside:
                    eng_, fn_, r_, w_, ds_ = side[sp_[0]]; sp_[0] += 1
                    S.add(eng_, fn_, reads=r_, writes=w_, dsem=ds_)
                    n -= 1

            mla_st(0)
            for ui in range(len(units)):
                if ui + 1 < len(units):
                    mla_st(ui + 1)
                mla_rest(ui)
                if ui >= 4:
                    emit_side(per_unit)
            emit_side(nside)
            for h in range(4):
                sbk = 4 + (h % 2)
                S.add("pe", lambda e, h=h, sbk=sbk: e.matmul(B[sbk][:, :], lhsT=onesf[:], rhs=accs[:, h, :], start=True, stop=True), reads=[("accs", h), "onesf"], writes=[kB(sbk)])
                S.add("act", lambda e, sbk=sbk: e.activation(out=rec[:], in_=B[sbk][:, :], func=AF.Ln), reads=[kB(sbk)], writes=["rec"])
                S.add("act", lambda e: e.activation(out=rec[:], in_=rec[:], func=AF.Exp, scale=-1.0), reads=["rec"], writes=["rec"])
                S.add("dve", lambda e, h=h: e.tensor_tensor(out=bT[:, h, :], in0=B[h][:, :], in1=rec[:], op=ALU.mult), reads=[kB(h), "rec"], writes=[("bT", h)])
            S.capture = []
            rms_feat([bT[:, h, :] for h in range(4)], VGB, 512.0, bnT, [("bnT", h) for h in range(4)], 0, in_keys=[("bT", h) for h in range(4)], src_sb=True)
            pending_tail.append(S.capture); S.capture = None
            for t in range(4):
                S.capture = []
                s = slots[t]
                rb = rbuf[t % 2]
                rk = [("accs", 2 * (t % 2)), ("accs", 2 * (t % 2) + 1)]
                for half in range(2):
                    bi = 1 + 2 * (t % 2) + half
                    for c in range(4):
                        S.add("pe", lambda e, c=c, t=t, bi=bi, half=half: e.matmul(B[bi][:, :], lhsT=anT[:, c, t * 128:(t + 1) * 128], rhs=WOA[:, c, half * 512:(half + 1) * 512],
                                                                                  start=(c == 0), stop=False), reads=[("anT", c), "WOA"], writes=[kB(bi)])
                    for c in range(4):
                        S.add("pe", lambda e, c=c, t=t, bi=bi, half=half: e.matmul(B[bi][:, :], lhsT=bnT[:, c, t * 128:(t + 1) * 128], rhs=WOB[:, c, half * 512:(half + 1) * 512],
                                                                                  start=False, stop=(c == 3)), reads=[("bnT", c), "WOB"], writes=[kB(bi)])
                    S.add("dve", lambda e, bi=bi, half=half, rb=rb, s=s: e.tensor_tensor(out=rb[:, half * 512:(half + 1) * 512], in0=B[bi][:, :], in1=xt[s][:, half * 512:(half + 1) * 512], op=ALU.add),
                          reads=[kB(bi), ("x", s)], writes=rk)
                ln_tok(S, rb, rk, st[t], mv[t], t, g1bc, b1bc, "g1bc", "b1bc", epsln)
                row0 = (j * 4 + t) * 128
                S.add("sp", lambda e, rb=rb, row0=row0: e.dma_start(out=(out_d if dbg_h1 else h1_d)[row0:row0 + 128, :], in_=rb[:]), reads=rk, writes=[("h1d", j * 4 + t)], dsem=d_h1[t % 2])
                pending_tail.append(S.capture); S.capture = None
        for seg_ in pending_tail:
            emit_list(seg_)
        pending_tail = []

        S.barrier()
        e_q.close()
        e_att.close()
        cur[0] = es
        if moe and n_step > 0 and sparse:
            moe_sparse_phase(locals())
        if moe and n_step > 0 and not sparse:
            ntok = n_step * 512
            T_ = min(MOE_T, ntok)
            npass = ntok // T_
            nb = T_ // 128
            WR = sb("WR_s", [128, 8, 36], F32)
            rbias = sb("rbias", [128, 36], F32)
            cload(WR[:], WR_d.rearrange("(k p) f -> p k f", p=128), "WR", q="sp")
            cload(rbias[:], rows_d[4, 0:36].partition_broadcast(128), "rbias", q="sp")
            g2bc = sb("g2bc", [128, 1024], F32)
            b2bc = sb("b2bc", [128, 1024], F32)
            cload(g2bc[:], rows_d[5, :].partition_broadcast(128), "g2bc", q="sp")
            cload(b2bc[:], rows_d[6, :].partition_broadcast(128), "b2bc", q="sp")
            h1T = sb("h1T", [128, 8, T_], BF16)
            h1T32 = sb("h1T32", [128, 8, 128], F32)
            accm = sb("accm", [128, nb, 1024], F32)
            comb = sb("comb", [128, nb, 32], F32)
            hb = [sb("hb%d" % i, [128, 1024], F32) for i in range(2)]
            d_hb = [sem("d_hb%d" % i) for i in range(2)]
            NW = 3
            wg_s = [sb("wg_s%d" % i, [128, 8, 256], BF16) for i in range(NW)]
            wu_s = [sb("wu_s%d" % i, [128, 8, 256], BF16) for i in range(NW)]
            wd_s = [sb("wd_s%d" % i, [128, 2, 1024], BF16) for i in range(NW)]
            d_wg = [sem("d_wg%d" % i) for i in range(NW)]
            d_wu = [sem("d_wu%d" % i) for i in range(NW)]
            d_wd = [sem("d_wd%d" % i) for i in range(NW)]
            sgs = [sb("sg%d" % i, [128, 2, 512], BF16) for i in range(2)]
            hid = [sb("hid%d" % i, [128, 2, 512], BF16) for i in range(2)]
            lg = sb("lg", [128, 36], F32)
            rt = sb("rt", [128, 64], F32)
            d_out = [sem("d_out%d" % i) for i in range(2)]

            def wload(e_, slot):
                S.add("pool", lambda e: e.dma_start(out=wg_s[slot][:], in_=wg_d[e_].rearrange("(k p) f -> p k f", p=128)), writes=[("wg", slot)], dsem=d_wg[slot])
                S.add("pool", lambda e: e.dma_start(out=wu_s[slot][:], in_=wu_d[e_].rearrange("(k p) f -> p k f", p=128)), writes=[("wu", slot)], dsem=d_wu[slot])
                S.add("pool", lambda e: e.dma_start(out=wd_s[slot][:], in_=wd_d[e_].rearrange("(k p) f -> p k f", p=128)), writes=[("wd", slot)], dsem=d_wd[slot])

            for p in range(npass):
                seq = list(range(n_exp))
                for i0 in range(min(NW, n_exp)):
                    wload(seq[i0], i0 % NW)
                S.add("pool", lambda e: e.memset(accm[:], 0.0), writes=[("accm", b) for b in range(nb)])
                for b in range(nb):
                    gb = p * nb + b
                    hb_ = hb[b % 2]; hk = ("hb", b % 2)
                    S.add("sp", lambda e, hb_=hb_, gb=gb: e.dma_start(out=hb_[:], in_=h1_d[gb * 128:(gb + 1) * 128, :]), reads=[("h1d", gb)], writes=[hk], dsem=d_hb[b % 2])
                    for k in range(8):
                        bi = k // 4
                        S.add("pe", lambda e, k=k, bi=bi, hb_=hb_: e.transpose(out=B[bi][:, (k % 4) * 128:(k % 4 + 1) * 128], in_=hb_[:, k * 128:(k + 1) * 128], identity=identf[:]),
                              reads=[hk, "identf"], writes=[kB(bi)])
                    S.add("act", lambda e: e.activation(out=h1T32[:, 0:4, :], in_=B[0][:, :].rearrange("p (k i) -> p k i", k=4), func=AF.Copy),
                          reads=[kB(0)], writes=[("h1T32", 0)])
                    S.add("dve", lambda e: e.tensor_copy(out=h1T32[:, 4:8, :], in_=B[1][:, :].rearrange("p (k i) -> p k i", k=4)),
                          reads=[kB(1)], writes=[("h1T32", 1)])
                    S.add("pool", lambda e, b=b: e.tensor_copy(out=h1T[:, :, b * 128:(b + 1) * 128], in_=h1T32[:, :, :]),
                          reads=[("h1T32", 0), ("h1T32", 1)], writes=[("h1T", b)])
                    for k in range(8):
                        S.add("pe", lambda e, k=k: e.matmul(B[2][:, 0:36], lhsT=h1T32[:, k, :], rhs=WR[:, k, :], start=(k == 0), stop=(k == 7)),
                              reads=[("h1T32", k // 4), "WR"], writes=[kB(2)])
                    route(S, B[2], kB(2), lg, rt, rbias, comb, b)
                ntg = T_ // 512
                pairs = [(ei, tg) for ei in range(n_exp) for tg in range(ntg)]
                DB = [(B[4][:, :], kB(4)), (B[5][:, :], kB(5)), (T[0][:, :].bitcast(F32), kT(0)), (T[1][:, :].bitcast(F32), kT(1))]
                dbi = [0]

                def moe_gu(pi_, fc):
                    ei, tg = pairs[pi_]
                    slot = ei % NW
                    hkeys = [("h1T", tg * 4 + q) for q in range(4)]
                    for k in range(8):
                        S.add("pe", lambda e, k=k: e.matmul(B[fc][:, :], lhsT=wg_s[slot][:, k, fc * 128:(fc + 1) * 128], rhs=h1T[:, k, tg * 512:(tg + 1) * 512],
                                                            start=(k == 0), stop=(k == 7)), reads=[("wg", slot)] + hkeys, writes=[kB(fc)])
                    for k in range(8):
                        S.add("pe", lambda e, k=k: e.matmul(B[2 + fc][:, :], lhsT=wu_s[slot][:, k, fc * 128:(fc + 1) * 128], rhs=h1T[:, k, tg * 512:(tg + 1) * 512],
                                                            start=(k == 0), stop=(k == 7)), reads=[("wu", slot)] + hkeys, writes=[kB(2 + fc)])
                    sg_ = sgs[pi_ % 2]; hd = hid[pi_ % 2]
                    S.add("act", lambda e: e.activation(out=sg_[:, fc, :], in_=B[fc][:, :], func=AF.Silu), reads=[kB(fc)], writes=[("sg", pi_ % 2, fc)])
                    S.add("dve", lambda e: e.tensor_tensor(out=hd[:, fc, :], in0=B[2 + fc][:, :], in1=sg_[:, fc, :], op=ALU.mult),
                          reads=[kB(2 + fc), ("sg", pi_ % 2, fc)], writes=[("hid", pi_ % 2, fc)])

                def moe_d(pi_):
                    ei, tg = pairs[pi_]
                    slot = ei % NW
                    hd = hid[pi_ % 2]
                    for tb in range(4):
                        b = tg * 4 + tb
                        for half in range(2):
                            dap, dk = DB[dbi[0]]; dbi[0] = (dbi[0] + 1) % len(DB)
                            for fc in range(2):
                                S.add("pe", lambda e, fc=fc, dap=dap, tb=tb, half=half: e.matmul(dap, lhsT=hd[:, fc, tb * 128:(tb + 1) * 128], rhs=wd_s[slot][:, fc, half * 512:(half + 1) * 512],
                                                                                                start=(fc == 0), stop=(fc == 1)),
                                      reads=[("hid", pi_ % 2, 0), ("hid", pi_ % 2, 1), ("wd", slot)], writes=[dk])
                            S.add("dve", lambda e, dap=dap, b=b, half=half: e.scalar_tensor_tensor(out=accm[:, b, half * 512:(half + 1) * 512], in0=dap, scalar=comb[:, b, ei:ei + 1],
                                                                                                  in1=accm[:, b, half * 512:(half + 1) * 512], op0=ALU.mult, op1=ALU.add),
                                  reads=[dk, ("comb", b), ("accm", b)], writes=[("accm", b)])
                    if tg == ntg - 1 and ei + NW < n_exp:
                        wload(ei + NW, slot)

                npairs = len(pairs)
                moe_gu(0, 0); moe_gu(0, 1)
                for pi_ in range(npairs):
                    if pi_ + 1 < npairs:
                        moe_gu(pi_ + 1, 0)
                    moe_d(pi_)
                    if pi_ + 1 < npairs:
                        moe_gu(pi_ + 1, 1)
                for b in range(nb):
                    gb = p * nb + b
                    hb_ = hb[b % 2]; hk = ("hb", b % 2)
                    S.add("sp", lambda e, hb_=hb_, gb=gb: e.dma_start(out=hb_[:], in_=h1_d[gb * 128:(gb + 1) * 128, :]), reads=[("h1d", gb)], writes=[hk], dsem=d_hb[b % 2])
                    S.add("dve", lambda e, hb_=hb_, b=b: e.scalar_tensor_tensor(out=hb_[:], in0=hb_[:], scalar=ALPHA, in1=accm[:, b, :], op0=ALU.mult, op1=ALU.add),
                          reads=[hk, ("accm", b)], writes=[hk])
                    ln_tok(S, hb_, hk, st[b % 4], mv[b % 4], b % 4, g2bc, b2bc, "g2bc", "b2bc", epsln)
                    S.add("sp", lambda e, hb_=hb_, gb=gb: e.dma_start(out=out_d[gb * 128:(gb + 1) * 128, :], in_=hb_[:]), reads=[hk], writes=[("outd", gb)], dsem=d_out[b % 2])

        block = es.enter_context(nc.Block())
        S.emit(block, sems)
    return nc


def ln_tok(S, buf, bkey, st_, mv_, t, gbc, bbc, gk, bk, epsln, geng="pool", beng="pool"):
    bks = list(bkey) if isinstance(bkey, list) else [bkey]
    S.add("dve", lambda e: e.bn_stats(out=st_[:, 0, :], in_=buf[:, 0:512]), reads=bks, writes=[("st", t, 0)])
    S.add("dve", lambda e: e.bn_stats(out=st_[:, 1, :], in_=buf[:, 512:1024]), reads=bks, writes=[("st", t, 1)])
    S.add("dve", lambda e: e.bn_aggr(out=mv_[:, 0:2], in_=st_[:, :, :]), reads=[("st", t, 0), ("st", t, 1)], writes=[("mv", t, 0)])
    S.add("act", lambda e: e.activation(out=mv_[:, 2:3], in_=mv_[:, 1:2], func=AF.Ln, bias=epsln[:, 0:1], scale=1.0), reads=[("mv", t, 0), "epsc"], writes=[("mv", t, 1)])
    S.add("act", lambda e: e.activation(out=mv_[:, 2:3], in_=mv_[:, 2:3], func=AF.Exp, scale=-0.5), reads=[("mv", t, 1)], writes=[("mv", t, 1)])
    S.add("dve", lambda e: e.scalar_tensor_tensor(out=mv_[:, 3:4], in0=mv_[:, 0:1], scalar=-1.0, in1=mv_[:, 2:3], op0=ALU.mult, op1=ALU.mult),
          reads=[("mv", t, 0), ("mv", t, 1)], writes=[("mv", t, 2)])
    S.add("act", lambda e: e.activation(out=buf[:], in_=buf[:], func=AF.Identity, bias=mv_[:, 3:4], scale=mv_[:, 2:3]), reads=bks + [("mv", t, 1), ("mv", t, 2)], writes=bks)
    S.add(geng, lambda e: e.tensor_tensor(out=buf[:], in0=buf[:], in1=gbc[:], op=ALU.mult), reads=bks + [gk], writes=bks)
    S.add(beng, lambda e: e.tensor_tensor(out=buf[:], in0=buf[:], in1=bbc[:], op=ALU.add), reads=bks + [bk], writes=bks)


def route(S, Bl, bkey, lg, rt, rbias, comb, b, ohg_out=None, c8_out=None, tag=0, defer=None):
    def add(eng, fn, r, w):
        if defer is None:
            S.add(eng, fn, reads=r, writes=w)
        else:
            defer.append((eng, fn, r, w))

    def D(fn, r, w):
        add("dve", fn, r, w)
    LG = ("lg", tag)
    GM, NGM, GS, GT, M1, M2, DD, ED, DEN, W1, W2 = range(11)
    OHG, ING, OH1, ING2, OH2, C8, GE = 16, 20, 28, 36, 44, 52, 60
    K = ("rt", tag)
    D(lambda e: e.tensor_tensor(out=lg[:], in0=Bl[:, 0:36], in1=rbias[:], op=ALU.add), [bkey, "rbias"], [LG])
    D(lambda e: e.tensor_reduce(out=rt[:, GM:GM + 1], in_=lg[:, 0:4], axis=AX.X, op=ALU.max), [LG], [K])
    D(lambda e: e.tensor_scalar(out=rt[:, NGM:NGM + 1], in0=rt[:, GM:GM + 1], scalar1=-1.0, scalar2=None, op0=ALU.mult), [K], [K])
    add("act", lambda e: e.activation(out=rt[:, GE:GE + 4], in_=lg[:, 0:4], func=AF.Exp, bias=rt[:, NGM:NGM + 1], scale=1.0, accum_out=rt[:, GS:GS + 1]), [LG, K], [K])
    D(lambda e: e.reciprocal(out=rt[:, GT:GT + 1], in_=rt[:, GS:GS + 1]), [K], [K])
    D(lambda e: e.tensor_scalar(out=rt[:, OHG:OHG + 4], in0=lg[:, 0:4], scalar1=rt[:, GM:GM + 1], scalar2=None, op0=ALU.is_equal), [LG, K], [K])
    D(lambda e: e.tensor_scalar(out=rt[:, ING:ING + 8], in0=lg[:, 4:12], scalar1=rt[:, OHG:OHG + 1], scalar2=None, op0=ALU.mult), [LG, K], [K])
    for g in range(1, 4):
        D(lambda e, g=g: e.scalar_tensor_tensor(out=rt[:, ING:ING + 8], in0=lg[:, 4 + 8 * g:12 + 8 * g], scalar=rt[:, OHG + g:OHG + g + 1], in1=rt[:, ING:ING + 8], op0=ALU.mult, op1=ALU.add), [LG, K], [K])
    D(lambda e: e.tensor_reduce(out=rt[:, M1:M1 + 1], in_=rt[:, ING:ING + 8], axis=AX.X, op=ALU.max), [K], [K])
    D(lambda e: e.tensor_scalar(out=rt[:, OH1:OH1 + 8], in0=rt[:, ING:ING + 8], scalar1=rt[:, M1:M1 + 1], scalar2=None, op0=ALU.is_equal), [K], [K])
    D(lambda e: e.scalar_tensor_tensor(out=rt[:, ING2:ING2 + 8], in0=rt[:, OH1:OH1 + 8], scalar=-1e30, in1=rt[:, ING:ING + 8], op0=ALU.mult, op1=ALU.add), [K], [K])
    D(lambda e: e.tensor_reduce(out=rt[:, M2:M2 + 1], in_=rt[:, ING2:ING2 + 8], axis=AX.X, op=ALU.max), [K], [K])
    D(lambda e: e.tensor_scalar(out=rt[:, OH2:OH2 + 8], in0=rt[:, ING2:ING2 + 8], scalar1=rt[:, M2:M2 + 1], scalar2=None, op0=ALU.is_equal), [K], [K])
    D(lambda e: e.tensor_tensor(out=rt[:, DD:DD + 1], in0=rt[:, M2:M2 + 1], in1=rt[:, M1:M1 + 1], op=ALU.subtract), [K], [K])
    add("act", lambda e: e.activation(out=rt[:, ED:ED + 1], in_=rt[:, DD:DD + 1], func=AF.Exp), [K], [K])
    D(lambda e: e.tensor_scalar(out=rt[:, DEN:DEN + 1], in0=rt[:, ED:ED + 1], scalar1=1.0, scalar2=None, op0=ALU.add), [K], [K])
    D(lambda e: e.reciprocal(out=rt[:, DEN:DEN + 1], in_=rt[:, DEN:DEN + 1]), [K], [K])
    D(lambda e: e.tensor_tensor(out=rt[:, W1:W1 + 1], in0=rt[:, GT:GT + 1], in1=rt[:, DEN:DEN + 1], op=ALU.mult), [K], [K])
    D(lambda e: e.tensor_tensor(out=rt[:, W2:W2 + 1], in0=rt[:, W1:W1 + 1], in1=rt[:, ED:ED + 1], op=ALU.mult), [K], [K])
    D(lambda e: e.tensor_scalar(out=rt[:, C8:C8 + 8], in0=rt[:, OH1:OH1 + 8], scalar1=rt[:, W1:W1 + 1], scalar2=None, op0=ALU.mult), [K], [K])
    D(lambda e: e.scalar_tensor_tensor(out=rt[:, C8:C8 + 8], in0=rt[:, OH2:OH2 + 8], scalar=rt[:, W2:W2 + 1], in1=rt[:, C8:C8 + 8], op0=ALU.mult, op1=ALU.add), [K], [K])
    if ohg_out is not None:
        D(lambda e: e.tensor_copy(out=ohg_out[:, b, :], in_=rt[:, OHG:OHG + 4]), [K], [("OHG", b)])
        D(lambda e: e.tensor_copy(out=c8_out[:, b, :], in_=rt[:, C8:C8 + 8]), [K], [("C8", b)])
        return
    for g in range(4):
        D(lambda e, g=g: e.tensor_scalar(out=comb[:, b, 8 * g:8 * g + 8], in0=rt[:, C8:C8 + 8], scalar1=rt[:, OHG + g:OHG + g + 1], scalar2=None, op0=ALU.mult), [K], [("comb", b)])


def _rot_perm64():
    return (np.arange(64) + 32) % 64


def host_layout(inputs):
    f = np.float32
    x = np.asarray(inputs["x"], f)
    meta = np.asarray(inputs["meta_tokens"], f)
    w_in = np.asarray(inputs["w_in"], f)[0]
    rp = _rot_perm64()
    q_a = w_in[:, 0:512]; k_a = w_in[:, 512:640]; v_a = w_in[:, 640:768]
    c_q = w_in[:, 768:1024]; c_kv = w_in[:, 1024:1280]; k_r = w_in[:, 1280:1344]
    k_a_rot = np.concatenate([k_a[:, h * 64:(h + 1) * 64][:, rp] for h in range(2)], axis=1)
    k_r_rot = k_r[:, rp]
    WK = np.concatenate([k_a, k_a_rot, c_kv, v_a, k_r, k_r, k_r_rot, k_r_rot], axis=1)
    qcols, qrcols = [], []
    for c in range(4):
        for h in (c, 4 + c):
            blk = q_a[:, h * 64:(h + 1) * 64]
            qcols.append(blk); qrcols.append(blk[:, rp])
    WQ = np.concatenate(qcols + qrcols + [c_q], axis=1)
    w_uq = np.asarray(inputs["mla_w_uq"], f)[0]
    nope = [w_uq[:, h * 192:h * 192 + 128] for h in range(4)]
    rope = [w_uq[:, h * 192 + 128:h * 192 + 192] for h in range(4)]
    WUQ = np.concatenate(nope + rope + [r[:, rp] for r in rope], axis=1)
    w_ukv = np.asarray(inputs["mla_w_ukv"], f)[0]
    WUK = np.concatenate([w_ukv[:, h * 256:h * 256 + 128] for h in range(4)], axis=1)
    WUV = np.concatenate([w_ukv[:, h * 256 + 128:h * 256 + 256] for h in range(4)], axis=1)
    w_o = np.asarray(inputs["w_o"], f)[0]
    WOA = np.zeros((128, 4, 1024), f)
    for g in range(2):
        for c in range(4):
            h = 4 * g + c
            WOA[g * 64:(g + 1) * 64, c, :] = w_o[h * 64:(h + 1) * 64, :]
    WOB = np.zeros((128, 4, 1024), f)
    for h in range(4):
        WOB[:, h, :] = w_o[512 + h * 128:512 + (h + 1) * 128, :]
    WR = np.concatenate([np.asarray(inputs["moe_w_group"], f)[0], np.asarray(inputs["moe_w_router"], f)[0]], axis=1)
    vec = np.zeros((128, 40), f)
    vec[:, 0:8] = np.asarray(inputs["ln_in_g"], f).reshape(8, 128).T
    vec[:, 8:16] = np.asarray(inputs["ln_in_b"], f).reshape(8, 128).T
    vec[:, 16:18] = np.asarray(inputs["mla_q_norm_g"], f)[0].reshape(2, 128).T
    vec[:, 18:20] = np.asarray(inputs["mla_kv_norm_g"], f)[0].reshape(2, 128).T
    ga = np.asarray(inputs["swa_out_norm_g"], f)[0]
    sk = np.asarray(inputs["swa_sinks"], f)[0]
    for g in range(2):
        for c in range(4):
            h = 4 * g + c
            vec[g * 64:(g + 1) * 64, 20 + c] = ga[h * 64:(h + 1) * 64]
            vec[g * 64:(g + 1) * 64, 28 + c] = sk[h]
    vec[:, 24:28] = np.asarray(inputs["mla_out_norm_g"], f)[0].reshape(4, 128).T
    rows = np.zeros((7, 1024), f)
    rows[0] = np.asarray(inputs["ln1_g"], f)[0]; rows[1] = np.asarray(inputs["ln1_b"], f)[0]
    rows[2] = np.asarray(inputs["ln_in_g"], f); rows[3] = np.asarray(inputs["ln_in_b"], f)
    rows[4, 0:4] = np.asarray(inputs["moe_b_group"], f)[0]; rows[4, 4:36] = np.asarray(inputs["moe_b_router"], f)[0]
    rows[5] = np.asarray(inputs["ln2_g"], f)[0]; rows[6] = np.asarray(inputs["ln2_b"], f)[0]
    cst = np.zeros((128, 1536), f)
    cst[:, 0:128] = np.eye(128, dtype=f)
    cst[:, 128:256] = 1.0
    cst[:, 256:320] = 1.0
    cst[:, 448:512] = 1.0
    p = np.arange(128)[:, None]; i = np.arange(128)[None, :]
    cst[:, 512:1024] = np.tile((p > i).astype(f), (1, 4))
    cst[:, 1024:1536] = np.tile((p <= i).astype(f), (1, 4))
    cst2 = np.zeros((128, 128 + 8 + NSLOT), f)
    cst2[:, 0:128] = (p < i).astype(f)
    cst2[:, 128:136] = np.arange(8)[None, :] * 128 + np.arange(128)[:, None]
    cst2[:, 136:136 + NSLOT] = (np.arange(NSLOT) * SLOT)[None, :]
    blk0 = np.zeros((128, 1024), f); blk0[112:] = meta
    zblk = np.zeros((128, 1024), f)
    pos_blk0 = np.maximum(np.arange(128) - 112, 0).astype(f)
    inv_freq = (10000.0 ** (-np.arange(0, 64, 2, dtype=f) / f(64))).astype(f)
    shared = dict(cst=cst, WK=WK, WQ=WQ, WUQ=WUQ, WUK=WUK, WUV=WUV, WOA=WOA.reshape(128, 4096), WOB=WOB.reshape(128, 4096), WR=WR, vec=vec,
                  rows=rows,
                  wg2=np.asarray(inputs["moe_w_gate"], f)[0].reshape(NEXP, 8, 128, 256).transpose(0, 2, 1, 3).reshape(NEXP * 128, 2048),
                  wu2=np.asarray(inputs["moe_w_up"], f)[0].reshape(NEXP, 8, 128, 256).transpose(0, 2, 1, 3).reshape(NEXP * 128, 2048),
                  wd2=np.asarray(inputs["moe_w_down"], f)[0].reshape(NEXP, 2, 128, 1024).transpose(0, 2, 1, 3).reshape(NEXP * 128, 2048),
                  cst2=cst2)
    shared = {k: np.ascontiguousarray(v) for k, v in shared.items()}
    in_maps = []
    for core in range(8):
        b, hf = core // 2, core % 2
        xb = x[b]
        blocks = [zblk, zblk, zblk, blk0]
        pos = [np.zeros(128, f)] * 3 + [pos_blk0]
        kbm = np.zeros((128, NBLK), f); kbs = np.zeros((128, NBLK), f)
        kbm[:, 0:3] = NEGB; kbm[:112, 3] = NEGB; kbs[:112, 3] = NEGB
        if hf == 0:
            blocks += [zblk, zblk, zblk, blk0]
            pos += [np.zeros(128, f)] * 3 + [pos_blk0]
            kbm[:, 4:8] = NEGB; kbs[:112, 7] = NEGB
        for t in range(64):
            blocks.append(xb[t * 128:(t + 1) * 128])
            pos.append((16 + t * 128 + np.arange(128)).astype(f))
        if hf == 1:
            blocks += [zblk] * 4
            pos += [np.zeros(128, f)] * 4
        xs_ = np.ascontiguousarray(np.concatenate(blocks, axis=0))
        posv = np.concatenate(pos)
        ang = posv[:, None] * inv_freq[None, :]
        cos64 = np.concatenate([np.cos(ang), np.cos(ang)], axis=1)
        sin64 = np.concatenate([-np.sin(ang), np.sin(ang)], axis=1)
        cosT_ = np.ascontiguousarray(np.concatenate([cos64, cos64], axis=1).T.astype(f))
        sinT_ = np.ascontiguousarray(np.concatenate([sin64, sin64], axis=1).T.astype(f))
        m = dict(shared)
        m.update(xs=xs_, cosT=cosT_, sinT=sinT_, kbm=kbm, kbs=kbs)
        in_maps.append(m)
    return in_maps


_NC_CACHE = {}


def kernel(**inputs):
    in_maps = host_layout(inputs)
    if "nc" not in _NC_CACHE:
        _NC_CACHE["nc"] = build_nc()
    nc = _NC_CACHE["nc"]
    res = run_bass_kernel_spmd(nc, in_maps, core_ids=list(range(8)))
    out = np.zeros((4, 8192, 1024), np.float32)
    for core in range(8):
        b, hf = core // 2, core % 2
        o = res.results[core]["out"]
        for j in range(NSTEP):
            xc = 2 * j + hf
            out[b, xc * 512:(xc + 1) * 512] = o[j * 512:(j + 1) * 512]
    return out


def moe_sparse_phase(L):
    S = L["S"]; sb = L["sb"]; sem = L["sem"]; cload = L["cload"]; B = L["B"]; T = L["T"]; kB = L["kB"]; kT = L["kT"]
    n_step = L["n_step"]; h1_d = L["h1_d"]; out_d = L["out_d"]; rows_d = L["rows_d"]; WR_d = L["WR_d"]
    identf = L["identf"]; onesf = L["onesf"]; ident = L["ident"]; st = L["st"]; mv = L["mv"]; epsln = L["epsln"]
    wg2_d = L["wg2_d"]; wu2_d = L["wu2_d"]; wd2_d = L["wd2_d"]; cst2_d = L["cst2_d"]; xs_d = L["xs_d"]; ffn_d = L["ffn_d"]
    nblk = n_step * 4
    nslot = nblk // 4 + 3
    assert nslot <= NSLOT

    WR = sb("WR_s", [128, 8, 36], F32)
    rbias = sb("rbias", [128, 36], F32)
    cst2 = sb("cst2_s", [128, 128 + 8 + NSLOT], F32)
    g2bc = sb("g2bc", [128, 1024], F32)
    b2bc = sb("b2bc", [128, 1024], F32)
    cload(WR[:], WR_d.rearrange("(k p) f -> p k f", p=128), "WR", q="sp")
    cload(rbias[:], rows_d[4, 0:36].partition_broadcast(128), "rbias", q="sp")
    cload(cst2[:], cst2_d[:, :], "cst2", q="sp")
    cload(g2bc[:], rows_d[5, :].partition_broadcast(128), "g2bc", q="sp")
    cload(b2bc[:], rows_d[6, :].partition_broadcast(128), "b2bc", q="sp")
    allz = L["allz"]
    UT = cst2[:, 0:128]
    JP = cst2[:, 128:136]
    SLST = cst2[:, 136:136 + NSLOT]
    NHB = 8
    hb = [sb("hb%d" % i, [128, 1024], F32) for i in range(NHB)]
    d_hb = [sem("d_hb%d" % i) for i in range(NHB)]
    d_out = [sem("d_out%d" % i) for i in range(6)]
    h1T32 = sb("h1T32", [128, 8, 128], F32)
    OHGt = sb("OHGt", [128, 32, 4], F32)
    C8t = sb("C8t", [128, 32, 8], F32)
    CS = sb("CS", [128, 32, 4], F32)
    PRE = sb("PRE", [128, 32, 4], F32)
    OFF = sb("OFF", [128, 32, 4], F32)
    sm = sb("sm", [128, 64], F32)
    POSF = sb("POSF", [128, 32], F32)
    POSI = sb("POSI", [128, 32], U32)
    WIDXF = sb("WIDXF", [128, NSLOT, 8], F32)
    WIDX = sb("WIDX", [128, NSLOT, 8], U32)
    if nblk < 32:
        S.add("dve", lambda e: e.memset(OHGt[:], 0.0), writes=[("OHG", b) for b in range(32)])

    GRP = 4
    lg4 = [sb("lg4_%d" % i, [128, 36], F32) for i in range(GRP)]
    rt4 = [sb("rt4_%d" % i, [128, 64], F32) for i in range(GRP)]
    for g0 in range(0, nblk, GRP):
        chains = []
        for i in range(GRP):
            b = g0 + i
            hb_ = hb[b % NHB]; hk = ("hb", b % NHB)
            rb_ = 2 + i
            S.add("sp", lambda e, hb_=hb_, b=b: e.dma_start(out=hb_[:], in_=h1_d[b * 128:(b + 1) * 128, :]), reads=[("h1d", b)], writes=[hk], dsem=d_hb[b % NHB])
            for k in range(8):
                bi = k // 4
                S.add("pe", lambda e, k=k, bi=bi, hb_=hb_: e.transpose(out=B[bi][:, (k % 4) * 128:(k % 4 + 1) * 128], in_=hb_[:, k * 128:(k + 1) * 128], identity=identf[:]),
                      reads=[hk, "identf"], writes=[kB(bi)])
            S.add("act", lambda e: e.activation(out=h1T32[:, 0:4, :], in_=B[0][:, :].rearrange("p (k i) -> p k i", k=4), func=AF.Copy), reads=[kB(0)], writes=[("h1T32", 0)])
            S.add("act", lambda e: e.activation(out=h1T32[:, 4:8, :], in_=B[1][:, :].rearrange("p (k i) -> p k i", k=4), func=AF.Copy), reads=[kB(1)], writes=[("h1T32", 1)])
            for k in range(8):
                S.add("pe", lambda e, k=k, rb_=rb_: e.matmul(B[rb_][:, 0:36], lhsT=h1T32[:, k, :], rhs=WR[:, k, :], start=(k == 0), stop=(k == 7)),
                      reads=[("h1T32", k // 4), "WR"], writes=[kB(rb_)])
            ch_ = []
            route(S, B[rb_], kB(rb_), lg4[i], rt4[i], rbias, None, b, ohg_out=OHGt, c8_out=C8t, tag=i, defer=ch_)
            chains.append(ch_)
        for opi in range(max(len(c) for c in chains)):
            for c in chains:
                if opi < len(c):
                    eng, fn, r, w = c[opi]
                    S.add(eng, fn, reads=r, writes=w)
    allohg = [("OHG", b) for b in range(32)]
    flat = lambda t: t[:, :, :].rearrange("p b g -> p (b g)")
    S.add("pe", lambda e: e.matmul(B[3][:, 0:128], lhsT=onesf[:], rhs=flat(OHGt), start=True, stop=True), reads=allohg + ["onesf"], writes=[kB(3)])
    S.add("pe", lambda e: e.matmul(B[4][:, 0:128], lhsT=UT, rhs=flat(OHGt), start=True, stop=True), reads=allohg + ["cst2"], writes=[kB(4)])
    S.add("dve", lambda e: e.tensor_copy(out=flat(CS), in_=B[3][:, 0:128]), reads=[kB(3)], writes=["CS"])
    S.add("act", lambda e: e.activation(out=flat(PRE), in_=B[4][:, 0:128], func=AF.Copy), reads=[kB(4)], writes=["PRE"])
    D = lambda fn, r, w: S.add("dve", fn, reads=r, writes=w)
    NG, PC, BASE, END, TMP8, GS, AA = 0, 4, 8, 12, 16, 24, 36
    D(lambda e: e.tensor_reduce(out=sm[:, NG:NG + 4], in_=CS[:, :, :].rearrange("p b g -> p g b"), axis=AX.X, op=ALU.add), ["CS"], ["sm"])
    for g in range(4):
        D(lambda e, g=g: e.tensor_scalar(out=sm[:, TMP8:TMP8 + 8], in0=SLST[:, 0:8], scalar1=sm[:, NG + g:NG + g + 1], scalar2=None, op0=ALU.is_lt), ["sm", "cst2"], ["sm"])
        D(lambda e, g=g: e.tensor_reduce(out=sm[:, PC + g:PC + g + 1], in_=sm[:, TMP8:TMP8 + 8], axis=AX.X, op=ALU.add), ["sm"], ["sm"])
    D(lambda e: e.tensor_scalar(out=sm[:, PC:PC + 4], in0=sm[:, PC:PC + 4], scalar1=float(SLOT), scalar2=None, op0=ALU.mult), ["sm"], ["sm"])
    D(lambda e: e.memset(sm[:, BASE:BASE + 1], 0.0), ["sm"], ["sm"])
    for g in range(1, 4):
        D(lambda e, g=g: e.tensor_tensor(out=sm[:, BASE + g:BASE + g + 1], in0=sm[:, BASE + g - 1:BASE + g], in1=sm[:, PC + g - 1:PC + g], op=ALU.add), ["sm"], ["sm"])
    D(lambda e: e.tensor_tensor(out=sm[:, END:END + 4], in0=sm[:, BASE:BASE + 4], in1=sm[:, PC:PC + 4], op=ALU.add), ["sm"], ["sm"])
    D(lambda e: e.tensor_copy(out=OFF[:, 0, :], in_=sm[:, BASE:BASE + 4]), ["sm"], ["OFF"])
    for b in range(1, 32):
        D(lambda e, b=b: e.tensor_tensor(out=OFF[:, b, :], in0=OFF[:, b - 1, :], in1=CS[:, b - 1, :], op=ALU.add), ["OFF", "CS"], ["OFF"])
    D(lambda e: e.tensor_tensor(out=flat(PRE), in0=flat(PRE), in1=flat(OFF), op=ALU.add), ["PRE", "OFF"], ["PRE"])
    D(lambda e: e.tensor_tensor(out=flat(PRE), in0=flat(PRE), in1=flat(OHGt), op=ALU.mult), ["PRE"] + allohg, ["PRE"])
    D(lambda e: e.tensor_reduce(out=POSF[:], in_=PRE[:, :, :], axis=AX.X, op=ALU.add), ["PRE"], ["POSF"])
    D(lambda e: e.tensor_copy(out=POSI[:], in_=POSF[:]), ["POSF"], ["POSI"])
    D(lambda e: e.tensor_scalar(out=sm[:, GS:GS + NSLOT], in0=SLST, scalar1=sm[:, END:END + 1], scalar2=None, op0=ALU.is_ge), ["sm", "cst2"], ["sm"])
    for g in range(1, 3):
        D(lambda e, g=g: e.scalar_tensor_tensor(out=sm[:, GS:GS + NSLOT], in0=SLST, scalar=sm[:, END + g:END + g + 1], in1=sm[:, GS:GS + NSLOT], op0=ALU.is_ge, op1=ALU.add), ["sm", "cst2"], ["sm"])
    D(lambda e: e.tensor_scalar(out=sm[:, AA:AA + NSLOT], in0=sm[:, GS:GS + NSLOT], scalar1=1024.0, scalar2=None, op0=ALU.mult), ["sm"], ["sm"])
    for s_ in range(NSLOT):
        D(lambda e, s_=s_: e.tensor_scalar(out=WIDXF[:, s_, :], in0=JP, scalar1=sm[:, AA + s_:AA + s_ + 1], scalar2=None, op0=ALU.add), ["sm", "cst2"], ["WIDXF"])
    D(lambda e: e.tensor_copy(out=WIDX[:, :, :].rearrange("p s j -> p (s j)"), in_=WIDXF[:, :, :].rearrange("p s j -> p (s j)")), ["WIDXF"], ["WIDX"])

    NRT = 4
    rowt = [sb("rowt%d" % i, [128, ROWW], F32) for i in range(NRT)]
    d_rl = [sem("d_rl%d" % i) for i in range(NRT)]
    d_rs = [sem("d_rs%d" % i) for i in range(NRT)]
    scat_cap = []
    S.capture = scat_cap
    for b in range(nblk):
        r_ = rowt[b % NRT]; rk = ("rowt", b % NRT)
        S.add("sp", lambda e, r_=r_, b=b: e.dma_start(out=r_[:, 0:1024], in_=h1_d[b * 128:(b + 1) * 128, :]), reads=[("h1d", b)], writes=[rk], dsem=d_rl[b % NRT])
        S.add("dve", lambda e, r_=r_, b=b: e.tensor_copy(out=r_[:, 1024:1032], in_=C8t[:, b, :]), reads=[("C8", b), rk], writes=[(rk, "c")])
        S.add("pool", lambda e, r_=r_, b=b: e.indirect_dma_start(out=xs_d[:, :], out_offset=bass.IndirectOffsetOnAxis(ap=POSI[:, b:b + 1], axis=0), in_=r_[:], in_offset=None),
              reads=[rk, (rk, "c"), "POSI"] + allz, writes=[("xs", b)], dsem=d_rs[b % NRT])
    S.capture = None
    allxs = [("xs", b) for b in range(nblk)]

    NW = 4
    wg_s = [sb("wg_s%d" % i, [128, 2048], BF16) for i in range(NW)]
    wu_s = [sb("wu_s%d" % i, [128, 2048], BF16) for i in range(NW)]
    wd_s = [sb("wd_s%d" % i, [128, 2048], BF16) for i in range(NW)]
    d_wg = [sem("d_wg%d" % i) for i in range(NW)]
    d_wu = [sem("d_wu%d" % i) for i in range(NW)]
    d_wd = [sem("d_wd%d" % i) for i in range(NW)]
    sgs = [sb("sg%d" % i, [128, 2, 512], BF16) for i in range(2)]
    hid = [sb("hid%d" % i, [128, 2, 512], BF16) for i in range(2)]
    ups = [sb("ups%d" % i, [128, 2, 512], F32) for i in range(2)]
    xsb = [sb("xsb%d" % i, [128, ROWW], F32) for i in range(2)]
    d_xl = [sem("d_xl%d" % i) for i in range(2)]
    x16 = [sb("x16_%d" % i, [128, 1024], BF16) for i in range(2)]
    xsT = [sb("xsT%d" % i, [128, 8, 512], BF16) for i in range(2)]
    c8s = [sb("c8s%d" % i, [128, 4, 8], F32) for i in range(2)]
    accs = [sb("maccs%d" % i, [128, 4, 1024], F32) for i in range(2)]
    d_fs = [sem("d_fs%d" % i) for i in range(2)]
    pairs = [(s_, j) for s_ in range(nslot) for j in range(8)]
    npairs = len(pairs)

    def wload(pi_):
        s_, j = pairs[pi_]
        slot = pi_ % NW
        off = bass.IndirectOffsetOnAxis(ap=WIDX[:, s_, j:j + 1], axis=0)
        S.add("pool", lambda e: e.indirect_dma_start(out=wg_s[slot][:], out_offset=None, in_=wg2_d[:, :], in_offset=off), reads=["WIDX"], writes=[("wg", slot)], dsem=d_wg[slot])
        S.add("pool", lambda e: e.indirect_dma_start(out=wu_s[slot][:], out_offset=None, in_=wu2_d[:, :], in_offset=off), reads=["WIDX"], writes=[("wu", slot)], dsem=d_wu[slot])
        S.add("pool", lambda e: e.indirect_dma_start(out=wd_s[slot][:], out_offset=None, in_=wd2_d[:, :], in_offset=off), reads=["WIDX"], writes=[("wd", slot)], dsem=d_wd[slot])

    def slot_prep(s_):
        par = s_ % 2
        for t in range(4):
            xb = xsb[t % 2]; xk = ("xsb", t % 2)
            r0 = s_ * SLOT + t * 128
            S.add("sp", lambda e, xb=xb, r0=r0: e.dma_start(out=xb[:], in_=xs_d[r0:r0 + 128, :]), reads=allxs, writes=[xk], dsem=d_xl[t % 2])
            S.add("act", lambda e, xb=xb, t=t: e.activation(out=x16[t % 2][:], in_=xb[:, 0:1024], func=AF.Copy), reads=[xk], writes=[("x16", t % 2)])
            S.add("pool", lambda e, xb=xb, t=t: e.tensor_copy(out=c8s[par][:, t, :], in_=xb[:, 1024:1032]), reads=[xk], writes=[("c8s", par, t)])
            S.add("act", lambda e, xb=xb, t=t: e.activation(out=accs[par][:, t, :], in_=xb[:, 0:1024], func=AF.Copy, scale=ALPHA), reads=[xk], writes=[("maccs", par, t)])
            for k in range(8):
                Tb = T[k // 4]
                S.add("pe", lambda e, Tb=Tb, k=k, t=t: e.transpose(out=Tb[:, (k % 4) * 128:(k % 4 + 1) * 128], in_=x16[t % 2][:, k * 128:(k + 1) * 128], identity=ident[:]),
                      reads=[("x16", t % 2), "ident"], writes=[kT(k // 4)])
            S.add("act", lambda e, t=t: e.activation(out=xsT[par][:, 0:4, t * 128:(t + 1) * 128], in_=T[0][:, 0:512].rearrange("p (k i) -> p k i", k=4), func=AF.Copy),
                  reads=[kT(0)], writes=[("xsT", par, t)])
            S.add("dve", lambda e, t=t: e.tensor_copy(out=xsT[par][:, 4:8, t * 128:(t + 1) * 128], in_=T[1][:, 0:512].rearrange("p (k i) -> p k i", k=4)),
                  reads=[kT(1)], writes=[("xsT", par, t, 1)])

    DB = [(B[4][:, :], kB(4)), (B[5][:, :], kB(5)), (T[0][:, :].bitcast(F32), kT(0)), (T[1][:, :].bitcast(F32), kT(1))]
    dbi = [0]

    def moe_gu(pi_, fc):
        s_, j = pairs[pi_]
        slot = pi_ % NW
        par = s_ % 2
        xkeys = [("xsT", par, t) for t in range(4)] + [("xsT", par, t, 1) for t in range(4)]
        for k in range(8):
            S.add("pe", lambda e, k=k: e.matmul(B[fc][:, :], lhsT=wg_s[slot][:, k * 256 + fc * 128:k * 256 + (fc + 1) * 128], rhs=xsT[par][:, k, :], start=(k == 0), stop=(k == 7)),
                  reads=[("wg", slot)] + xkeys, writes=[kB(fc)])
        for k in range(8):
            S.add("pe", lambda e, k=k: e.matmul(B[2 + fc][:, :], lhsT=wu_s[slot][:, k * 256 + fc * 128:k * 256 + (fc + 1) * 128], rhs=xsT[par][:, k, :], start=(k == 0), stop=(k == 7)),
                  reads=[("wu", slot)] + xkeys, writes=[kB(2 + fc)])
        sg_ = sgs[pi_ % 2]; hd = hid[pi_ % 2]; up_ = ups[pi_ % 2]
        S.add("act", lambda e: e.activation(out=sg_[:, fc, :], in_=B[fc][:, :], func=AF.Silu), reads=[kB(fc)], writes=[("sg", pi_ % 2, fc)])
        S.add("act", lambda e: e.activation(out=up_[:, fc, :], in_=B[2 + fc][:, :], func=AF.Copy), reads=[kB(2 + fc)], writes=[("up", pi_ % 2, fc)])
        S.add("pool", lambda e: e.tensor_tensor(out=hd[:, fc, :], in0=up_[:, fc, :], in1=sg_[:, fc, :], op=ALU.mult), reads=[("up", pi_ % 2, fc), ("sg", pi_ % 2, fc)], writes=[("hid", pi_ % 2, fc)])

    def moe_d(pi_):
        s_, j = pairs[pi_]
        slot = pi_ % NW
        par = s_ % 2
        hd = hid[pi_ % 2]
        ac = accs[par]
        for tb in range(4):
            for half in range(2):
                dap, dk = DB[dbi[0]]; dbi[0] = (dbi[0] + 1) % len(DB)
                for fc in range(2):
                    S.add("pe", lambda e, fc=fc, dap=dap, tb=tb, half=half: e.matmul(dap, lhsT=hd[:, fc, tb * 128:(tb + 1) * 128], rhs=wd_s[slot][:, fc * 1024 + half * 512:fc * 1024 + (half + 1) * 512],
                                                                                    start=(fc == 0), stop=(fc == 1)),
                          reads=[("hid", pi_ % 2, 0), ("hid", pi_ % 2, 1), ("wd", slot)], writes=[dk])
                ak = ("maccs", par, tb)
                if True:
                    S.add("dve", lambda e, dap=dap, tb=tb, half=half: e.scalar_tensor_tensor(out=ac[:, tb, half * 512:(half + 1) * 512], in0=dap, scalar=c8s[par][:, tb, j:j + 1],
                                                                                            in1=ac[:, tb, half * 512:(half + 1) * 512], op0=ALU.mult, op1=ALU.add),
                          reads=[dk, ("c8s", par, tb), ak], writes=[ak])
        if j == 7:
            S.add("sp", lambda e: e.dma_start(out=ffn_d[s_ * SLOT:(s_ + 1) * SLOT, :].rearrange("(t p) f -> p t f", p=128), in_=ac[:]),
                  reads=[("maccs", par, tb) for tb in range(4)], writes=[("ffn", s_)], dsem=d_fs[par])
        if pi_ + NW < npairs:
            wload(pi_ + NW)

    for i0 in range(min(NW, npairs)):
        wload(i0)
    for eng_, fn_, r_, w_, ds_ in scat_cap:
        S.add(eng_, fn_, reads=r_, writes=w_, dsem=ds_)
    slot_prep(0)
    moe_gu(0, 0); moe_gu(0, 1)
    for pi_ in range(npairs):
        s_, j = pairs[pi_]
        if j == 2 and s_ + 1 < nslot:
            slot_prep(s_ + 1)
        if pi_ + 1 < npairs:
            moe_gu(pi_ + 1, 0)
        moe_d(pi_)
        if pi_ + 1 < npairs:
            moe_gu(pi_ + 1, 1)
    allffn = [("ffn", s_) for s_ in range(nslot)]

    fb = [t_[:, 0:1024] for t_ in (rowt + xsb)]
    ND = len(fb)
    d_fb = [sem("d_fb%d" % i) for i in range(ND)]
    LOOK = ND - 1
    def ln_parts(b):
        fb_ = fb[b % ND]; fk = ("fb", b % ND)
        cap = []
        S.capture = cap
        ln_tok(S, fb_, fk, st[b % 4], mv[b % 4], b % 4, g2bc, b2bc, "g2bc", "b2bc", epsln, geng="dve", beng="dve")
        S.add("sp", lambda e: e.dma_start(out=out_d[b * 128:(b + 1) * 128, :], in_=fb_), reads=[fk], writes=[("outd", b)], dsem=d_out[b % ND])
        S.capture = None
        return cap[:7], cap[7:]

    def emit_(lst):
        for eng_, fn_, r_, w_, ds_ in lst:
            S.add(eng_, fn_, reads=r_, writes=w_, dsem=ds_)

    for b in range(min(ND, nblk)):
        fb_ = fb[b % ND]; fk = ("fb", b % ND)
        S.add("pool", lambda e, fb_=fb_, b=b: e.indirect_dma_start(out=fb_, out_offset=None, in_=ffn_d[:, :], in_offset=bass.IndirectOffsetOnAxis(ap=POSI[:, b:b + 1], axis=0)),
              reads=allffn + ["POSI"], writes=[fk], dsem=d_fb[b % ND])
    prev2 = None
    for b in range(nblk):
        s1, s2 = ln_parts(b)
        emit_(s1)
        if prev2 is not None:
            emit_(prev2)
        prev2 = s2
        nb_ = b + LOOK
        if nb_ < nblk and b >= 1:
            pass
        nb_ = b - 1 + ND
        if b >= 1 and nb_ < nblk:
            fbn = fb[nb_ % ND]; fkn = ("fb", nb_ % ND)
            S.add("pool", lambda e, fbn=fbn, nb_=nb_: e.indirect_dma_start(out=fbn, out_offset=None, in_=ffn_d[:, :], in_offset=bass.IndirectOffsetOnAxis(ap=POSI[:, nb_:nb_ + 1], axis=0)),
                  reads=allffn + ["POSI"], writes=[fkn], dsem=d_fb[nb_ % ND])
    emit_(prev2)
```

```python
import numpy as np
from contextlib import ExitStack
import concourse.bass as bass
import concourse.mybir as mybir
from concourse.bass_utils import run_bass_kernel_spmd

F32 = mybir.dt.float32
BF16 = mybir.dt.bfloat16
AF = mybir.ActivationFunctionType
ALU = mybir.AluOpType
AX = mybir.AxisListType

NCH = 18
NBLK = NCH * 4
NSTEP = 8
LN_EPS = 1e-5
RMS_EPS = 1e-6
ALPHA = 2.0 ** 0.25
NEGB = -30000.0
NEXP = 32
KVW = 1152
SWW = 384
MOE_T = 2048
SLOT = 512
NSLOT = 12
ROWW = 1032
U32 = mybir.dt.uint32


class _Op:
    __slots__ = ("eng", "fn", "idx", "eidx", "deps", "need_inc", "dsem", "dcount", "is_dma", "inc_no", "bar")


class Sched:
    ENG = ("pe", "act", "dve", "pool", "sp")

    def __init__(self):
        self.ops = {e: [] for e in self.ENG}
        self.last_w = {}
        self.readers = {}
        self.seen = {e: {} for e in self.ENG}
        self.seen_dma = {e: set() for e in self.ENG}
        self.dcounts = {}
        self.dsems = {}
        self.n = 0
        self.pending = {e: None for e in self.ENG}
        self.capture = None

    def barrier(self):
        last = [self.ops[e][-1] for e in self.ENG if self.ops[e] and not self.ops[e][-1].is_dma]
        last = []
        for e in self.ENG:
            for op in reversed(self.ops[e]):
                if not op.is_dma:
                    last.append(op); break
        for op in last:
            op.need_inc = True
        dm = [(self.dsems[k], c) for k, c in self.dcounts.items()]
        for e in self.ENG:
            self.pending[e] = (last, dm)

    def add(self, eng, fn, reads=(), writes=(), dsem=None):
        if self.capture is not None:
            self.capture.append((eng, fn, list(reads), list(writes), dsem))
            return None
        op = _Op()
        op.eng = eng; op.fn = fn; op.idx = self.n; self.n += 1
        op.eidx = len(self.ops[eng]); op.need_inc = False; op.dsem = dsem
        op.is_dma = dsem is not None; op.inc_no = None
        if op.is_dma:
            self.dcounts[id(dsem)] = self.dcounts.get(id(dsem), 0) + 16
            self.dsems[id(dsem)] = dsem
            op.dcount = self.dcounts[id(dsem)]
        deps = []
        for k in reads:
            w = self.last_w.get(k)
            if w is not None:
                deps.append((w, "raw"))
            if isinstance(k, tuple) and k[0] in ("B", "T"):
                for r in self.readers.get(k, ()):
                    if r.eng != eng:
                        deps.append((r, "war"))
        for k in writes:
            w = self.last_w.get(k)
            if w is not None:
                deps.append((w, "waw"))
            for r in self.readers.get(k, ()):
                deps.append((r, "war"))
        final_eng = {}
        final_dma = []
        for d, kind in deps:
            if d is op:
                continue
            if d.is_dma:
                if d.idx not in self.seen_dma[eng]:
                    self.seen_dma[eng].add(d.idx)
                    final_dma.append(d)
                continue
            if d.eng == eng and not op.is_dma:
                if eng == "pe":
                    continue
                if kind != "raw":
                    continue
            if d.eidx <= self.seen[eng].get(d.eng, -1):
                continue
            if d.eng not in final_eng or final_eng[d.eng].eidx < d.eidx:
                final_eng[d.eng] = d
        for f, d in final_eng.items():
            self.seen[eng][f] = d.eidx
            d.need_inc = True
        op.deps = list(final_eng.values()) + final_dma
        op.bar = self.pending[eng]
        if op.bar is not None:
            self.pending[eng] = None
            for d in op.bar[0]:
                if d.eng != eng:
                    self.seen[eng][d.eng] = max(self.seen[eng].get(d.eng, -1), d.eidx)
        for k in writes:
            self.last_w[k] = op
            self.readers[k] = []
        for k in reads:
            lst = self.readers.setdefault(k, [])
            if not op.is_dma:
                lst[:] = [r for r in lst if r.is_dma or r.eng != eng]
            lst.append(op)
        self.ops[eng].append(op)
        return op

    def emit(self, block, sems):
        for e in self.ENG:
            c = 0
            for op in self.ops[e]:
                if op.need_inc and not op.is_dma:
                    c += 1
                    op.inc_no = c
        me = self

        def run(e, engobj):
            for op in me.ops[e]:
                if op.bar is not None:
                    for d in op.bar[0]:
                        engobj.wait_ge(sems[d.eng], d.inc_no)
                    for sm, cnt in op.bar[1]:
                        engobj.wait_ge(sm, cnt)
                for d in op.deps:
                    if d.is_dma:
                        engobj.wait_ge(d.dsem, d.dcount)
                    else:
                        engobj.wait_ge(sems[d.eng], d.inc_no)
                ins = op.fn(engobj)
                if op.is_dma:
                    ins.then_inc(op.dsem, 16)
                elif op.need_inc:
                    ins.then_inc(sems[e], 1)
            if e == "sp":
                for k, s in me.dsems.items():
                    engobj.wait_ge(s, me.dcounts[k])

        @block.tensor
        def _(eng):
            run("pe", eng)

        @block.scalar
        def _(eng):
            run("act", eng)

        @block.vector
        def _(eng):
            run("dve", eng)

        @block.gpsimd
        def _(eng):
            run("pool", eng)

        @block.sync
        def _(eng):
            run("sp", eng)


def build_nc(n_step=NSTEP, n_kchunk=NCH - 1, moe=True, n_exp=NEXP, dbg_h1=False, sparse=True):
    nc = bass.Bass("TRN2", target_bir_lowering=False)
    S = Sched()

    def din(name, shape, dt=F32):
        return nc.dram_tensor(name, list(shape), dt, kind="ExternalInput").ap()

    xs = din("xs", [NBLK * 128, 1024])
    cosT = din("cosT", [128, NBLK * 128])
    sinT = din("sinT", [128, NBLK * 128])
    kbm = din("kbm", [128, NBLK])
    kbs = din("kbs", [128, NBLK])
    cst = din("cst", [128, 128 * 4 + 512 * 2])
    WK_d = din("WK", [1024, 896])
    WQ_d = din("WQ", [1024, 1280])
    WUQ_d = din("WUQ", [256, 1024])
    WUK_d = din("WUK", [256, 512])
    WUV_d = din("WUV", [256, 512])
    WOA_d = din("WOA", [128, 4 * 1024])
    WOB_d = din("WOB", [128, 4 * 1024])
    WR_d = din("WR", [1024, 36])
    vec_d = din("vec", [128, 40])
    rows_d = din("rows", [7, 1024])
    if sparse:
        wg2_d = din("wg2", [NEXP * 128, 2048])
        wu2_d = din("wu2", [NEXP * 128, 2048])
        wd2_d = din("wd2", [NEXP * 128, 2048])
        cst2_d = din("cst2", [128, 128 + 8 + NSLOT])
        xs_d = nc.dram_tensor("xs_scr", [NSLOT * SLOT, ROWW], F32, kind="Internal").ap()
        ffn_d = nc.dram_tensor("ffn_scr", [NSLOT * SLOT, 1024], F32, kind="Internal").ap()
    else:
        wg_d = din("wg", [NEXP, 1024, 256])
        wu_d = din("wu", [NEXP, 1024, 256])
        wd_d = din("wd", [NEXP, 256, 1024])
    out_d = nc.dram_tensor("out", [NSTEP * 512, 1024], F32, kind="ExternalOutput").ap()
    kv_d = nc.dram_tensor("kv_scr", [NBLK, 128, KVW], BF16, kind="Internal").ap()
    sw_d = nc.dram_tensor("sw_scr", [NBLK, 128, SWW], BF16, kind="Internal").ap()
    h1_d = nc.dram_tensor("h1_scr", [NSTEP * 512, 1024], F32, kind="Internal").ap()

    with ExitStack() as es:
        cur = [es]

        def sb(name, shape, dt):
            return cur[0].enter_context(nc.sbuf_tensor(name, list(shape), dt))

        def ps(name, shape, dt):
            return es.enter_context(nc.psum_tensor(name, list(shape), dt))

        def sem(name):
            return es.enter_context(nc.semaphore(name))

        sems = {e: sem("s_" + e) for e in Sched.ENG}
        B = [ps("B%d" % i, [128, 512], F32) for i in range(6)]
        T = [ps("T%d" % i, [128, 1024], BF16) for i in range(2)]

        def kB(i):
            return ("B", i)

        def kT(i):
            return ("T", i)

        ident = sb("ident", [128, 128], BF16)
        ones = sb("ones", [128, 128], BF16)
        onesg = sb("onesg", [128, 2, 128], BF16)
        maskP = sb("maskP", [128, 512], BF16)
        maskC = sb("maskC", [128, 512], BF16)
        identf = sb("identf", [128, 128], F32)
        onesf = sb("onesf", [128, 128], F32)
        kbm_t = sb("kbm_t", [128, NBLK], F32)
        kbs_t = sb("kbs_t", [128, NBLK], F32)
        vec = sb("vec_s", [128, 40], F32)
        sinkexp = sb("sinkexp", [128, 4], F32)
        epsc = sb("epsc", [128, 2], F32)
        epsln = epsc[:, 0:1]
        epsrms = epsc[:, 1:2]
        S.add("dve", lambda e: e.memset(epsc[:, 0:1], LN_EPS), writes=["epsc0"])
        S.add("dve", lambda e: e.memset(epsc[:, 1:2], RMS_EPS), reads=["epsc0"], writes=["epsc"])
        st = [sb("st%d" % i, [128, 2, 6], F32) for i in range(4)]
        mv = [sb("mv%d" % i, [128, 4], F32) for i in range(4)]
        zt = sb("zt", [128, ROWW], F32)
        d_z = sem("d_z")
        S.add("pool", lambda e: e.memset(zt[:], 0.0), writes=["zt"])
        nzb = NSLOT * SLOT // 128
        zfill = [0]
        allz = [("xsz", nzb - 1)]

        def emit_zfill(n):
            while sparse and n > 0 and zfill[0] < nzb:
                i = zfill[0]; zfill[0] += 1
                S.add("sp", lambda e, i=i: e.dma_start(out=xs_d[i * 128:(i + 1) * 128, :], in_=zt[:]), reads=["zt"], writes=[("xsz", i)], dsem=d_z)
                n -= 1
        e_att = ExitStack()
        cur[0] = e_att
        d_c = [sem("d_c%d" % i) for i in range(28)]
        _ci = [0]

        import os as _os
        _lim = int(_os.environ.get("KDBG_NCLOAD", "999"))
        _skipms = _os.environ.get("KDBG_SKIPMS", "0") == "1"

        def cload(out, in_, key, q="pool"):
            if _ci[0] >= _lim:
                _ci[0] += 1
                return
            s = d_c[_ci[0]]; _ci[0] += 1
            S.add(q, lambda e: e.dma_start(out=out, in_=in_), writes=[key], dsem=s)

        cload(ident[:], cst[:, 0:128], "ident")
        cload(ones[:], cst[:, 128:256], "ones")
        cload(onesg[:, 0, :], cst[:, 256:384], "onesg0")
        cload(onesg[:, 1, :], cst[:, 384:512], "onesg1")
        cload(maskP[:], cst[:, 512:1024], "maskP")
        cload(maskC[:], cst[:, 1024:1536], "maskC")
        cload(identf[:], cst[:, 0:128], "identf", q="sp")
        cload(onesf[:], cst[:, 128:256], "onesf", q="sp")
        cload(kbm_t[:], kbm[:, :], "kbm", q="sp")
        cload(kbs_t[:], kbs[:, :], "kbs", q="sp")
        cload(vec[:], vec_d[:, :], "vec", q="sp")
        VG, VB, VQG, VKG, VGA, VGB, VSK = 0, 8, 16, 18, 20, 24, 28

        NX = 8
        xt = [sb("xt%d" % i, [128, 1024], F32) for i in range(NX)]
        d_x = [sem("d_x%d" % i) for i in range(NX)]
        xnb = sb("xnb", [128, 4, 1024], BF16)
        hT = sb("hT", [128, 8, 512], BF16)
        cs_t = sb("cs_t", [128, 512], F32)
        sn_t = sb("sn_t", [128, 512], F32)
        d_cs = sem("d_cs"); d_sn = sem("d_sn")
        t1 = [sb("t1_%d" % i, [128, 512], F32) for i in range(2)]
        t2 = [sb("t2_%d" % i, [128, 512], F32) for i in range(2)]
        sq = sb("sq", [128, 4, 512], BF16)
        rstd_t = sb("rstd_t", [128, 512], F32)
        e_k = ExitStack()
        cur[0] = e_k
        WK = sb("WK_s", [128, 8, 896], BF16)
        WUK = sb("WUK_s", [128, 2, 512], BF16)
        WUV = sb("WUV_s", [128, 2, 512], BF16)
        cload(WK[:], WK_d.rearrange("(k p) f -> p k f", p=128), "WK")
        cload(WUK[:], WUK_d.rearrange("(k p) f -> p k f", p=128), "WUK")
        cload(WUV[:], WUV_d.rearrange("(k p) f -> p k f", p=128), "WUV")
        ckvn = sb("ckvn", [128, 2, 512], BF16)
        kvrec = [sb("kvrec%d" % i, [128, 4, KVW], BF16) for i in range(2)]
        swrec = [sb("swrec%d" % i, [128, 4, SWW], BF16) for i in range(2)]
        d_kvw = [sem("d_kvw%d" % i) for i in range(2)]
        d_sww = [sem("d_sww%d" % i) for i in range(2)]

        for i in range(2):
            if not _skipms:
                S.add("pool", (lambda t: (lambda e: e.memset(t[:], 0.0)))(swrec[i]), writes=[("swrec", i)])

        xr = [0]

        def chunk_loads(ch):
            slots = []
            for t in range(4):
                s = xr[0]; xr[0] = (xr[0] + 1) % NX
                slots.append(s)
                blk = ch * 4 + t
                S.add("sp", (lambda s=s, blk=blk: (lambda e: e.dma_start(out=xt[s][:], in_=xs[blk * 128:(blk + 1) * 128, :])))(),
                      writes=[("x", s)], dsem=d_x[s])
            return slots

        def rope_loads(ch):
            S.add("sp", lambda e: e.dma_start(out=cs_t[:], in_=cosT[:, ch * 512:(ch + 1) * 512]), writes=["cs"], dsem=d_cs)
            S.add("sp", lambda e: e.dma_start(out=sn_t[:], in_=sinT[:, ch * 512:(ch + 1) * 512]), writes=["sn"], dsem=d_sn)

        def front(slots, want_res):
            _sub = int(_os.environ.get("KDBG_SUB", "99"))
            for t in range(4):
                s = slots[t]
                x_ = xt[s]
                st_, mv_ = st[t], mv[t]
                S.add("dve", lambda e, x_=x_, st_=st_: e.bn_stats(out=st_[:, 0, :], in_=x_[:, 0:512]), reads=[("x", s)], writes=[("st", t, 0)])
                S.add("dve", lambda e, x_=x_, st_=st_: e.bn_stats(out=st_[:, 1, :], in_=x_[:, 512:1024]), reads=[("x", s)], writes=[("st", t, 1)])
                S.add("dve", lambda e, st_=st_, mv_=mv_: e.bn_aggr(out=mv_[:, 0:2], in_=st_[:, :, :]), reads=[("st", t, 0), ("st", t, 1)], writes=[("mv", t, 0)])
                if _sub < 2:
                    continue
                S.add("act", lambda e, mv_=mv_: e.activation(out=mv_[:, 2:3], in_=mv_[:, 1:2], func=AF.Ln, bias=epsln[:, 0:1], scale=1.0),
                      reads=[("mv", t, 0), "epsc"], writes=[("mv", t, 1)])
                S.add("act", lambda e, mv_=mv_: e.activation(out=mv_[:, 2:3], in_=mv_[:, 2:3], func=AF.Exp, scale=-0.5),
                      reads=[("mv", t, 1)], writes=[("mv", t, 1)])
                S.add("dve", lambda e, mv_=mv_: e.scalar_tensor_tensor(out=mv_[:, 3:4], in0=mv_[:, 0:1], scalar=-1.0, in1=mv_[:, 2:3], op0=ALU.mult, op1=ALU.mult),
                      reads=[("mv", t, 0), ("mv", t, 1)], writes=[("mv", t, 2)])
                if _sub < 3:
                    continue
                S.add("act", lambda e, x_=x_, mv_=mv_, t=t: e.activation(out=xnb[:, t, :], in_=x_[:], func=AF.Identity, bias=mv_[:, 3:4], scale=mv_[:, 2:3]),
                      reads=[("x", s), ("mv", t, 1), ("mv", t, 2)], writes=[("xnb", t)])
                if want_res:
                    S.add("pool", lambda e, x_=x_, mv_=mv_: e.tensor_scalar(out=x_[:], in0=x_[:], scalar1=mv_[:, 2:3], scalar2=mv_[:, 3:4], op0=ALU.mult, op1=ALU.add),
                          reads=[("x", s), ("mv", t, 1), ("mv", t, 2)], writes=[("x", s)])
                    S.add("pool", lambda e, x_=x_: e.tensor_tensor(out=x_[:], in0=x_[:], in1=gabc[:], op=ALU.mult), reads=[("x", s), "gabc"], writes=[("x", s)])
                    S.add("pool", lambda e, x_=x_: e.tensor_tensor(out=x_[:], in0=x_[:], in1=babc[:], op=ALU.add), reads=[("x", s), "babc"], writes=[("x", s)])
            for kp in range(4):
                if _sub < 4:
                    continue
                Tb = T[kp % 2]
                for kk in range(2):
                    k = 2 * kp + kk
                    for t in range(4):
                        S.add("pe", lambda e, Tb=Tb, kk=kk, t=t, k=k: e.transpose(out=Tb[:, (kk * 4 + t) * 128:(kk * 4 + t + 1) * 128],
                                                                                in_=xnb[:, t, k * 128:(k + 1) * 128], identity=ident[:]),
                              reads=[("xnb", t), "ident"], writes=[kT(kp % 2)])
                for kk in range(2):
                    if _sub < 5:
                        continue
                    k = 2 * kp + kk
                    _ev = _os.environ.get("KDBG_EV", "")
                    if (_ev == "act" and kk == 1) or (_ev == "dve" and kk == 0):
                        continue
                    if (kp % 2 == 0 and _ev != "alldve") or _ev == "allact":
                        S.add("act", lambda e, Tb=Tb, kk=kk, k=k: e.activation(out=hT[:, k, :], in_=Tb[:, kk * 512:(kk + 1) * 512], func=AF.Identity,
                                                                                bias=vec[:, VB + k:VB + k + 1], scale=vec[:, VG + k:VG + k + 1]),
                              reads=[kT(kp % 2), "vec"], writes=[("hT", k)])
                    else:
                        S.add("dve", lambda e, Tb=Tb, kk=kk, k=k: e.tensor_scalar(out=hT[:, k, :], in0=Tb[:, kk * 512:(kk + 1) * 512],
                                                                                   scalar1=vec[:, VG + k:VG + k + 1], scalar2=vec[:, VB + k:VB + k + 1],
                                                                                   op0=ALU.mult, op1=ALU.add),
                              reads=[kT(kp % 2), "vec"], writes=[("hT", k)])

        hT_all = [("hT", k) for k in range(8)]

        def proj(bi, W, c0, m, wkey, ncols=512):
            for k in range(8):
                S.add("pe", lambda e, k=k: e.matmul(B[bi][0:m, 0:ncols], lhsT=W[:, k, c0:c0 + m], rhs=hT[:, k, 0:ncols], start=(k == 0), stop=(k == 7)),
                      reads=[("hT", k), wkey], writes=[kB(bi)])

        def projP(pap, pkey, W, c0, wkey):
            for k in range(8):
                S.add("pe", lambda e, k=k: e.matmul(pap, lhsT=W[:, k, c0:c0 + 128], rhs=hT[:, k, :], start=(k == 0), stop=(k == 7)),
                      reads=[("hT", k), wkey], writes=[pkey])

        def rope_applyP(qap, qkey, rap, rkey, out_ap, okey, i):
            S.add("dve", lambda e: e.tensor_tensor(out=t1[i][:, :], in0=qap, in1=cs_t[:, :], op=ALU.mult), reads=[qkey, "cs"], writes=[("t1", i)])
            S.add("dve", lambda e: e.tensor_tensor(out=t2[i][:, :], in0=rap, in1=sn_t[:, :], op=ALU.mult), reads=[rkey, "sn"], writes=[("t2", i)])
            S.add("pool", lambda e: e.tensor_tensor(out=out_ap, in0=t1[i][:, :], in1=t2[i][:, :], op=ALU.add), reads=[("t1", i), ("t2", i)], writes=[okey])

        def rope_apply(bq, br, out_ap, okey, np_=128, i=0, o3=False):
            def v(t):
                a = t[0:np_, :]
                return a.rearrange("p (t c) -> p t c", t=4) if o3 else a
            S.add("dve", lambda e: e.tensor_tensor(out=t1[i][0:np_, :], in0=B[bq][0:np_, :], in1=cs_t[0:np_, :], op=ALU.mult), reads=[kB(bq), "cs"], writes=[("t1", i)])
            S.add("dve", lambda e: e.tensor_tensor(out=t2[i][0:np_, :], in0=B[br][0:np_, :], in1=sn_t[0:np_, :], op=ALU.mult), reads=[kB(br), "sn"], writes=[("t2", i)])
            S.add("pool", lambda e: e.tensor_tensor(out=out_ap, in0=v(t1[i]), in1=v(t2[i]), op=ALU.add), reads=[("t1", i), ("t2", i)], writes=[okey])

        def rms_feat(banks, gcol, nfeat, out_tile, okeys, bsum, in_keys=None, src_sb=None):
            n = len(banks)
            for c in range(n):
                rk = in_keys[c]
                S.add("act", lambda e, c=c: e.activation(out=sq[:, c, :], in_=banks[c], func=AF.Square), reads=[rk], writes=[("sq", c)])
            for c in range(n):
                S.add("pe", lambda e, c=c: e.matmul(B[bsum][:, :], lhsT=ones[:], rhs=sq[:, c, :], start=(c == 0), stop=(c == n - 1)),
                      reads=[("sq", c), "ones"], writes=[kB(bsum)])
            S.add("act", lambda e: e.activation(out=rstd_t[:], in_=B[bsum][:, :], func=AF.Ln, bias=epsrms[:, 0:1], scale=1.0 / nfeat),
                  reads=[kB(bsum), "epsc"], writes=["rstd"])
            S.add("act", lambda e: e.activation(out=rstd_t[:], in_=rstd_t[:], func=AF.Exp, scale=-0.5), reads=["rstd"], writes=["rstd"])
            for c in range(n):
                rk = in_keys[c]
                eng = "dve"
                S.add(eng, lambda e, c=c: e.scalar_tensor_tensor(out=out_tile[:, c, :], in0=banks[c], scalar=vec[:, gcol + c:gcol + c + 1], in1=rstd_t[:],
                                                                  op0=ALU.mult, op1=ALU.mult), reads=[rk, "rstd", "vec"], writes=[okeys[c]])

        def front_parts(slots_):
            cap = []
            S.capture = cap
            front(slots_, False)
            S.capture = None
            fi = next(i for i, o in enumerate(cap) if o[0] == "pe")
            return cap[:fi], cap[fi:]

        def emit_list(lst):
            for eng_, fn_, r_, w_, ds_ in lst:
                S.add(eng_, fn_, reads=r_, writes=w_, dsem=ds_)

        kslots = {}
        if n_kchunk > 0:
            kslots[0] = chunk_loads(0)
            if n_kchunk > 1:
                kslots[1] = chunk_loads(1)
            pa, pb = front_parts(kslots[0])
            emit_list(pa); emit_list(pb)
        for ch in range(n_kchunk):
            rope_loads(ch)
            if ch + 2 < n_kchunk:
                kslots[ch + 2] = chunk_loads(ch + 2)
            emit_zfill(3)
            kr = kvrec[ch % 2]; sr = swrec[ch % 2]
            kvk = ("kvrec", ch % 2); swk = ("swrec", ch % 2)
            proj(0, WK, 0, 128, "WK")
            proj(1, WK, 128, 128, "WK")
            proj(2, WK, 256, 128, "WK")
            proj(3, WK, 384, 128, "WK")
            proj(4, WK, 640, 128, "WK")
            proj(5, WK, 768, 128, "WK")
            if ch + 1 < n_kchunk:
                pa, pb = front_parts(kslots[ch + 1])
                emit_list(pa)
            else:
                pb = []
            vap = T[0][:, :].bitcast(F32)
            for t in range(4):
                for k in range(8):
                    S.add("pe", lambda e, t=t, k=k: e.matmul(vap[:, t * 128:(t + 1) * 128], lhsT=hT[:, k, t * 128:(t + 1) * 128], rhs=WK[:, k, 512:640],
                                                             start=(k == 0), stop=(k == 7)), reads=[("hT", k), "WK"], writes=[kT(0)])
            b4v = vap.rearrange("p (t c) -> p t c", t=4)
            S.add("act", lambda e, sr=sr, b4v=b4v: e.activation(out=sr[:, :, 128:192], in_=b4v[:, :, 0:64], func=AF.Copy), reads=[kT(0)], writes=[swk])
            S.add("act", lambda e, sr=sr, b4v=b4v: e.activation(out=sr[:, :, 320:384], in_=b4v[:, :, 64:128], func=AF.Copy), reads=[kT(0)], writes=[swk])
            emit_list(pb)
            rope_apply(0, 1, sr[:, :, 0:128], swk, i=0, o3=True)
            rope_apply(4, 5, kr[:, :, 512:640], kvk, i=1, o3=True)
            rms_feat([B[2][:, :], B[3][:, :]], VKG, 256.0, ckvn, [("ckvn", 0), ("ckvn", 1)], 0, in_keys=[kB(2), kB(3)])
            for h in range(4):
                bi = [1, 2, 3, 5][h]
                for k in range(2):
                    S.add("pe", lambda e, h=h, k=k, bi=bi: e.matmul(B[bi][:, :], lhsT=WUK[:, k, h * 128:(h + 1) * 128], rhs=ckvn[:, k, :], start=(k == 0), stop=(k == 1)),
                          reads=[("ckvn", k), "WUK"], writes=[kB(bi)])
                src = B[bi][:, :].rearrange("p (t c) -> p t c", t=4)
                if h % 2 == 0:
                    S.add("act", lambda e, h=h, src=src, kr=kr: e.activation(out=kr[:, :, h * 128:(h + 1) * 128], in_=src, func=AF.Copy), reads=[kB(bi)], writes=[kvk])
                else:
                    S.add("dve", lambda e, h=h, src=src, kr=kr: e.tensor_copy(out=kr[:, :, h * 128:(h + 1) * 128], in_=src), reads=[kB(bi)], writes=[kvk])
            for t in range(4):
                bi = [0, 4, 1, 2][t]
                for k in range(2):
                    S.add("pe", lambda e, t=t, k=k, bi=bi: e.matmul(B[bi][:, :], lhsT=ckvn[:, k, t * 128:(t + 1) * 128], rhs=WUV[:, k, :], start=(k == 0), stop=(k == 1)),
                          reads=[("ckvn", k), "WUV"], writes=[kB(bi)])
                if t % 2 == 0:
                    S.add("act", lambda e, t=t, bi=bi, kr=kr: e.activation(out=kr[:, t, 640:1152], in_=B[bi][:, :], func=AF.Copy), reads=[kB(bi)], writes=[kvk])
                else:
                    S.add("dve", lambda e, t=t, bi=bi, kr=kr: e.tensor_copy(out=kr[:, t, 640:1152], in_=B[bi][:, :]), reads=[kB(bi)], writes=[kvk])
            S.add("sp", lambda e, kr=kr, ch=ch: e.dma_start(out=kv_d[ch * 4:(ch + 1) * 4].rearrange("b p f -> p b f"), in_=kr[:]),
                  reads=[kvk], writes=[("kvd", ch)], dsem=d_kvw[ch % 2])
            S.add("sp", lambda e, sr=sr, ch=ch: e.dma_start(out=sw_d[ch * 4:(ch + 1) * 4].rearrange("b p f -> p b f"), in_=sr[:]),
                  reads=[swk], writes=[("swd", ch)], dsem=d_sww[ch % 2])

        emit_zfill(nzb)
        S.barrier()
        e_k.close()
        e_q = ExitStack()
        cur[0] = e_q
        sinkbc = sb("sinkbc", [128, 512], F32)
        g1bc = sb("g1bc", [128, 1024], F32)
        b1bc = sb("b1bc", [128, 1024], F32)
        gabc = sb("gabc", [128, 1024], F32)
        babc = sb("babc", [128, 1024], F32)
        WQ = sb("WQ_s", [128, 8, 1280], BF16)
        WUQ = sb("WUQ_s", [128, 2, 1024], BF16)
        WOA = sb("WOA_s", [128, 4, 1024], BF16)
        WOB = sb("WOB_s", [128, 4, 1024], BF16)
        if n_step > 0:
            cload(g1bc[:], rows_d[0, :].partition_broadcast(128), "g1bc", q="sp")
            cload(b1bc[:], rows_d[1, :].partition_broadcast(128), "b1bc", q="sp")
            cload(WQ[:], WQ_d.rearrange("(k p) f -> p k f", p=128), "WQ")
            cload(WUQ[:], WUQ_d.rearrange("(k p) f -> p k f", p=128), "WUQ")
            cload(WOA[:], WOA_d.rearrange("p (c f) -> p c f", c=4), "WOA")
            cload(WOB[:], WOB_d.rearrange("p (c f) -> p c f", c=4), "WOB")
            cload(gabc[:], rows_d[2, :].partition_broadcast(128), "gabc", q="sp")
            cload(babc[:], rows_d[3, :].partition_broadcast(128), "babc", q="sp")
            S.add("pool", lambda e: e.tensor_scalar(out=gabc[:], in0=gabc[:], scalar1=ALPHA, scalar2=None, op0=ALU.mult), reads=["gabc"], writes=["gabc"])
            S.add("pool", lambda e: e.tensor_scalar(out=babc[:], in0=babc[:], scalar1=ALPHA, scalar2=None, op0=ALU.mult), reads=["babc"], writes=["babc"])
            S.add("act", lambda e: e.activation(out=sinkexp[:], in_=vec[:, VSK:VSK + 4], func=AF.Exp), reads=["vec"], writes=["sinkexp"])
            for c in range(4):
                S.add("dve", lambda e, c=c: e.tensor_scalar(out=sinkbc[:, c * 128:(c + 1) * 128], in0=maskC[:, 0:128], scalar1=0.0, scalar2=sinkexp[:, c:c + 1], op0=ALU.mult, op1=ALU.add),
                      reads=["maskC", "sinkexp"], writes=["sinkbc"])
        qaT = sb("qaT", [128, 4, 512], BF16)
        cqn = sb("cqn", [128, 2, 512], BF16)
        qnT = sb("qnT", [128, 4, 512], BF16)
        qrT = sb("qrT", [128, 4, 512], BF16)
        swt = sb("swt", [128, 5, SWW], BF16)
        d_swt = sem("d_swt")
        NKR = 4
        kvt = [sb("kvt%d" % i, [128, KVW], BF16) for i in range(NKR)]
        d_kvt = [sem("d_kvt%d" % i) for i in range(NKR)]
        NPB = 8
        Pb = [sb("Pb%d" % i, [128, 512], BF16) for i in range(NPB)]
        accs = sb("accs", [128, 4, 512], F32)
        rec = sb("rec", [128, 512], F32)
        aT = sb("aT", [128, 4, 512], F32)
        anT = sb("anT", [128, 4, 512], BF16)
        bT = sb("bT", [128, 4, 512], F32)
        bnT = sb("bnT", [128, 4, 512], BF16)
        rbuf = [accs[:, 0:2, :].rearrange("p h q -> p (h q)"), accs[:, 2:4, :].rearrange("p h q -> p (h q)")]
        d_h1 = [sem("d_h1_%d" % i) for i in range(2)]
        kvi = [0]
        pbi = [0]
        scale_mla = 192.0 ** -0.5

        if n_step > 0:
            S.add("pool", lambda e: e.memset(qrT[:], 0.0), writes=[("qrT", h) for h in range(4)])
        next_slots = chunk_loads(2) if n_step > 0 else None
        def swt_load(ch):
            S.add("sp", lambda e: e.dma_start(out=swt[:], in_=sw_d[ch * 4 - 1:ch * 4 + 4].rearrange("b p f -> p b f")),
                  reads=[("swd", ch - 1), ("swd", ch)], writes=["swt"], dsem=d_swt)

        def qa_proj(use_t):
            for c in range(4):
                if use_t:
                    qp = (T[0][:, :].bitcast(F32), kT(0)); rp = (T[1][:, :].bitcast(F32), kT(1))
                else:
                    qp = (B[2 * (c % 2)][:, :], kB(2 * (c % 2))); rp = (B[2 * (c % 2) + 1][:, :], kB(2 * (c % 2) + 1))
                projP(qp[0], qp[1], WQ, c * 128, "WQ")
                projP(rp[0], rp[1], WQ, 512 + c * 128, "WQ")
                rope_applyP(qp[0], qp[1], rp[0], rp[1], qaT[:, c, :], ("qaT", c), c % 2)

        hoisted = False
        pending_tail = []
        HOIST = _os.environ.get("KDBG_NOHOIST", "0") != "1"
        for j in range(n_step):
            ch = 2 * j + 2
            slots = next_slots
            if not hoisted:
                rope_loads(ch)
                swt_load(ch)
            if not hoisted:
                front(slots, True)
                qa_proj(False)
            HS = []
            S.capture = []
            proj(4, WQ, 1024, 128, "WQ")
            proj(5, WQ, 1152, 128, "WQ")
            rms_feat([B[4][:, :], B[5][:, :]], VQG, 256.0, cqn, [("cqn", 0), ("cqn", 1)], 0, in_keys=[kB(4), kB(5)])
            HS.append(S.capture); S.capture = []
            for h in range(4):
                bi = 1 + h
                for k in range(2):
                    S.add("pe", lambda e, h=h, k=k, bi=bi: e.matmul(B[bi][:, :], lhsT=WUQ[:, k, h * 128:(h + 1) * 128], rhs=cqn[:, k, :], start=(k == 0), stop=(k == 1)),
                          reads=[("cqn", k), "WUQ"], writes=[kB(bi)])
                if h % 2 == 0:
                    S.add("act", lambda e, h=h, bi=bi: e.activation(out=qnT[:, h, :], in_=B[bi][:, :], func=AF.Copy), reads=[kB(bi)], writes=[("qnT", h)])
                else:
                    S.add("dve", lambda e, h=h, bi=bi: e.tensor_copy(out=qnT[:, h, :], in_=B[bi][:, :]), reads=[kB(bi)], writes=[("qnT", h)])
            HS.append(S.capture); S.capture = []
            for pr in range(2):
                bq, br = (0, 5) if pr == 0 else (1, 2)
                for k in range(2):
                    S.add("pe", lambda e, pr=pr, k=k, bq=bq: e.matmul(B[bq][:, :], lhsT=WUQ[:, k, 512 + pr * 128:512 + (pr + 1) * 128], rhs=cqn[:, k, :], start=(k == 0), stop=(k == 1)),
                          reads=[("cqn", k), "WUQ"], writes=[kB(bq)])
                for k in range(2):
                    S.add("pe", lambda e, pr=pr, k=k, br=br: e.matmul(B[br][:, :], lhsT=WUQ[:, k, 768 + pr * 128:768 + (pr + 1) * 128], rhs=cqn[:, k, :], start=(k == 0), stop=(k == 1)),
                          reads=[("cqn", k), "WUQ"], writes=[kB(br)])
                S.add("dve", lambda e, bq=bq, pr=pr: e.tensor_tensor(out=t1[pr][:, :], in0=B[bq][:, :], in1=cs_t[:, :], op=ALU.mult), reads=[kB(bq), "cs"], writes=[("t1", pr)])
                S.add("dve", lambda e, br=br, pr=pr: e.tensor_tensor(out=t2[pr][:, :], in0=B[br][:, :], in1=sn_t[:, :], op=ALU.mult), reads=[kB(br), "sn"], writes=[("t2", pr)])
                for hh in range(2):
                    S.add("pool", lambda e, pr=pr, hh=hh: e.tensor_tensor(out=qrT[hh * 64:(hh + 1) * 64, 2 * pr + hh, :], in0=t1[pr][hh * 64:(hh + 1) * 64, :], in1=t2[pr][hh * 64:(hh + 1) * 64, :], op=ALU.add),
                          reads=[("t1", pr), ("t2", pr)], writes=[("qrT", 2 * pr + hh)])
            HS.append(S.capture); S.capture = []
            swa_p = {}
            OS = [(B[4][:, :], kB(4), B[5][:, :], kB(5)), (T[0][:, :].bitcast(F32), kT(0), T[1][:, :].bitcast(F32), kT(1))]

            def swa_st(qb):
                for g in range(2):
                    for kk in range(2):
                        kbi = qb + kk
                        bi = g * 2 + kk
                        S.add("pe", lambda e, g=g, kbi=kbi, bi=bi: e.matmul(B[bi][:, :].rearrange("p (c i) -> p c i", c=4),
                                                                             lhsT=swt[g * 64:(g + 1) * 64, kbi, 0:128],
                                                                             rhs=qaT[g * 64:(g + 1) * 64, :, qb * 128:(qb + 1) * 128], start=True, stop=True),
                              reads=["swt"] + [("qaT", c) for c in range(4)], writes=[kB(bi)])

            def swa_exp(qb):
                swa_p[qb] = []
                for g in range(2):
                    for kk in range(2):
                        kbi = qb + kk
                        slotblk = ch * 4 - 1 + kbi
                        bi = g * 2 + kk
                        pi = pbi[0]; pbi[0] = (pbi[0] + 1) % NPB
                        swa_p[qb].append(pi)
                        S.add("act", lambda e, bi=bi, pi=pi, slotblk=slotblk: e.activation(out=Pb[pi][:], in_=B[bi][:, :], func=AF.Exp,
                                                                                            bias=kbs_t[:, slotblk:slotblk + 1], scale=0.125),
                              reads=[kB(bi), "kbs"], writes=[("Pb", pi)])
                        mk = maskP if kk == 0 else maskC
                        mkk = "maskP" if kk == 0 else "maskC"
                        S.add("dve" if g == 0 else "pool", lambda e, pi=pi, mk=mk: e.tensor_tensor(out=Pb[pi][:], in0=Pb[pi][:], in1=mk[:], op=ALU.mult), reads=[("Pb", pi), mkk], writes=[("Pb", pi)])

            def swa_pv(qb):
                oap, ok_, sap, sk_ = OS[qb % 2]
                u = 0
                for g in range(2):
                    for kk in range(2):
                        kbi = qb + kk
                        pi = swa_p[qb][u]; u += 1
                        first = (g == 0 and kk == 0); last = (g == 1 and kk == 1)
                        S.add("pe", lambda e, g=g, kbi=kbi, pi=pi, first=first, last=last: e.matmul(oap, lhsT=swt[:, kbi, 128 + g * 128:256 + g * 128], rhs=Pb[pi][:],
                                                                                                   start=first, stop=last), reads=["swt", ("Pb", pi)], writes=[ok_])
                        S.add("pe", lambda e, g=g, pi=pi, first=first, last=last: e.matmul(sap, lhsT=onesg[:, g, :], rhs=Pb[pi][:], start=first, stop=last),
                              reads=["onesg%d" % g, ("Pb", pi)], writes=[sk_])

            def swa_epi(qb):
                oap, ok_, sap, sk_ = OS[qb % 2]
                for c in range(4):
                    S.add("act", lambda e, c=c: e.activation(out=rec[:, c * 128:(c + 1) * 128], in_=sap[:, c * 128:(c + 1) * 128], func=AF.Ln, bias=sinkexp[:, c:c + 1], scale=1.0),
                          reads=[sk_, "sinkexp"], writes=["rec"])
                S.add("act", lambda e: e.activation(out=rec[:], in_=rec[:], func=AF.Exp, scale=-1.0), reads=["rec"], writes=["rec"])
                S.add("dve", lambda e: e.tensor_tensor(out=aT[:, :, qb * 128:(qb + 1) * 128], in0=oap.rearrange("p (c i) -> p c i", c=4),
                                                       in1=rec[:].rearrange("p (c i) -> p c i", c=4), op=ALU.mult), reads=[ok_, "rec"], writes=[("aT", qb)])

            swa_st(0); swa_exp(0)
            for qb in range(4):
                if qb + 1 < 4:
                    swa_st(qb + 1)
                swa_pv(qb)
                if qb + 1 < 4:
                    swa_exp(qb + 1)
                swa_epi(qb)
            HS.append(S.capture); S.capture = None
            TS = pending_tail
            pending_tail = []
            for si in range(max(len(HS), len(TS))):
                if si < len(HS):
                    emit_list(HS[si])
                if si < len(TS):
                    emit_list(TS[si])
            if j + 1 < n_step:
                next_slots = chunk_loads(ch + 2)
            aT_keys = [("aT", q) for q in range(4)]
            for c in range(4):
                S.add("act", lambda e, c=c: e.activation(out=sq[:, c, :], in_=aT[:, c, :], func=AF.Square), reads=aT_keys, writes=[("sq", c)])
            for c in range(4):
                S.add("pe", lambda e, c=c: e.matmul(B[0][:, :], lhsT=ones[:], rhs=sq[:, c, :], start=(c == 0), stop=(c == 3)), reads=[("sq", c), "ones"], writes=[kB(0)])
            S.add("act", lambda e: e.activation(out=rstd_t[:], in_=B[0][:, :], func=AF.Ln, bias=epsrms[:, 0:1], scale=1.0 / 512.0), reads=[kB(0), "epsc"], writes=["rstd"])
            S.add("act", lambda e: e.activation(out=rstd_t[:], in_=rstd_t[:], func=AF.Exp, scale=-0.5), reads=["rstd"], writes=["rstd"])
            for c in range(4):
                S.add("dve", lambda e, c=c: e.scalar_tensor_tensor(out=anT[:, c, :], in0=aT[:, c, :], scalar=vec[:, VGA + c:VGA + c + 1], in1=rstd_t[:], op0=ALU.mult, op1=ALU.mult),
                      reads=aT_keys + ["rstd", "vec"], writes=[("anT", c)])
            S.add("pool", lambda e: e.memset(accs[:], 0.0), writes=[("accs", h) for h in range(4)])
            kblocks = [(3, None)] + [(s, None) for s in range(4, ch * 4)] + [(ch * 4 + d, d) for d in range(4)]
            nkb = len(kblocks)
            units = [(idx, sblk, dg, h) for idx, (sblk, dg) in enumerate(kblocks) for h in range(4)]
            kslot = {}

            def mla_st(ui):
                idx, sblk, dg, h = units[ui]
                if h == 0:
                    ks = kvi[0]; kvi[0] = (kvi[0] + 1) % NKR
                    kslot[idx] = ks
                    S.add("sp", lambda e, ks=ks, sblk=sblk: e.dma_start(out=kvt[ks][:], in_=kv_d[sblk]), reads=[("kvd", sblk // 4)], writes=[("kvt", ks)], dsem=d_kvt[ks])
                ks = kslot[idx]
                q0 = 0 if dg is None else dg * 128
                sbk = 4 + (ui % 2)
                hp = (h % 2) * 64
                S.add("pe", lambda e: e.matmul(B[sbk][:, q0:512], lhsT=kvt[ks][:, h * 128:(h + 1) * 128], rhs=qnT[:, h, q0:512], start=True, stop=False),
                      reads=[("kvt", ks), ("qnT", h)], writes=[kB(sbk)])
                S.add("pe", lambda e: e.matmul(B[sbk][:, q0:512], lhsT=kvt[ks][:, 512:640], rhs=qrT[:, h, q0:512], start=False, stop=True),
                      reads=[("kvt", ks), ("qrT", h)], writes=[kB(sbk)])

            def mla_rest(ui):
                idx, sblk, dg, h = units[ui]
                ks = kslot[idx]
                q0 = 0 if dg is None else dg * 128
                sbk = 4 + (ui % 2)
                pi = pbi[0]; pbi[0] = (pbi[0] + 1) % NPB
                S.add("act", lambda e: e.activation(out=Pb[pi][:, q0:512], in_=B[sbk][:, q0:512], func=AF.Exp, bias=kbm_t[:, sblk:sblk + 1], scale=scale_mla),
                      reads=[kB(sbk), "kbm"], writes=[("Pb", pi)])
                if dg is not None:
                    S.add("pool", lambda e: e.tensor_tensor(out=Pb[pi][:, q0:q0 + 128], in0=Pb[pi][:, q0:q0 + 128], in1=maskC[:, 0:128], op=ALU.mult),
                          reads=[("Pb", pi), "maskC"], writes=[("Pb", pi)])
                S.add("pe", lambda e: e.matmul(B[h][:, q0:512], lhsT=kvt[ks][:, 640 + h * 128:640 + (h + 1) * 128], rhs=Pb[pi][:, q0:512],
                                               start=(idx == 0), stop=(idx == nkb - 1), skip_group_check=True),
                      reads=[("kvt", ks), ("Pb", pi)], writes=[kB(h)])
                S.add("dve" if h % 2 == 0 else "pool", lambda e: e.tensor_tensor(out=accs[:, h, q0:512], in0=accs[:, h, q0:512], in1=Pb[pi][:, q0:512], op=ALU.add),
                      reads=[("accs", h), ("Pb", pi)], writes=[("accs", h)])

            side = []
            hoisted = False
            if HOIST and j + 1 < n_step:
                S.capture = side
                rope_loads(ch + 2)
                swt_load(ch + 2)
                front(next_slots, True)
                qa_proj(True)
                S.capture = None
                hoisted = True
            nside = len(side)
            per_unit = max(1, -(-nside // max(1, int(len(units) * 0.8) - 4)))
            sp_ = [0]

            def emit_side(n):
                while n > 0 and sp_[0] < nside:
                    eng_, fn_, r_, w_, ds_ = side[sp_[0]]; sp_[0] += 1
                    S.add(eng_, fn_, reads=r_, writes=w_, dsem=ds_)
                    n -= 1

            mla_st(0)
            for ui in range(len(units)):
                if ui + 1 < len(units):
                    mla_st(ui + 1)
                mla_rest(ui)
                if ui >= 4:
                    emit_side(per_unit)
            emit_side(nside)
            for h in range(4):
                sbk = 4 + (h % 2)
                S.add("pe", lambda e, h=h, sbk=sbk: e.matmul(B[sbk][:, :], lhsT=onesf[:], rhs=accs[:, h, :], start=True, stop=True), reads=[("accs", h), "onesf"], writes=[kB(sbk)])
                S.add("act", lambda e, sbk=sbk: e.activation(out=rec[:], in_=B[sbk][:, :], func=AF.Ln), reads=[kB(sbk)], writes=["rec"])
                S.add("act", lambda e: e.activation(out=rec[:], in_=rec[:], func=AF.Exp, scale=-1.0), reads=["rec"], writes=["rec"])
                S.add("dve", lambda e, h=h: e.tensor_tensor(out=bT[:, h, :], in0=B[h][:, :], in1=rec[:], op=ALU.mult), reads=[kB(h), "rec"], writes=[("bT", h)])
            S.capture = []
            rms_feat([bT[:, h, :] for h in range(4)], VGB, 512.0, bnT, [("bnT", h) for h in range(4)], 0, in_keys=[("bT", h) for h in range(4)], src_sb=True)
            pending_tail.append(S.capture); S.capture = None
            for t in range(4):
                S.capture = []
                s = slots[t]
                rb = rbuf[t % 2]
                rk = [("accs", 2 * (t % 2)), ("accs", 2 * (t % 2) + 1)]
                for half in range(2):
                    bi = 1 + 2 * (t % 2) + half
                    for c in range(4):
                        S.add("pe", lambda e, c=c, t=t, bi=bi, half=half: e.matmul(B[bi][:, :], lhsT=anT[:, c, t * 128:(t + 1) * 128], rhs=WOA[:, c, half * 512:(half + 1) * 512],
                                                                                  start=(c == 0), stop=False), reads=[("anT", c), "WOA"], writes=[kB(bi)])
                    for c in range(4):
                        S.add("pe", lambda e, c=c, t=t, bi=bi, half=half: e.matmul(B[bi][:, :], lhsT=bnT[:, c, t * 128:(t + 1) * 128], rhs=WOB[:, c, half * 512:(half + 1) * 512],
                                                                                  start=False, stop=(c == 3)), reads=[("bnT", c), "WOB"], writes=[kB(bi)])
                    S.add("dve", lambda e, bi=bi, half=half, rb=rb, s=s: e.tensor_tensor(out=rb[:, half * 512:(half + 1) * 512], in0=B[bi][:, :], in1=xt[s][:, half * 512:(half + 1) * 512], op=ALU.add),
                          reads=[kB(bi), ("x", s)], writes=rk)
                ln_tok(S, rb, rk, st[t], mv[t], t, g1bc, b1bc, "g1bc", "b1bc", epsln)
                row0 = (j * 4 + t) * 128
                S.add("sp", lambda e, rb=rb, row0=row0: e.dma_start(out=(out_d if dbg_h1 else h1_d)[row0:row0 + 128, :], in_=rb[:]), reads=rk, writes=[("h1d", j * 4 + t)], dsem=d_h1[t % 2])
                pending_tail.append(S.capture); S.capture = None
        for seg_ in pending_tail:
            emit_list(seg_)
        pending_tail = []

        S.barrier()
        e_q.close()
        e_att.close()
        cur[0] = es
        if moe and n_step > 0 and sparse:
            moe_sparse_phase(locals())
        if moe and n_step > 0 and not sparse:
            ntok = n_step * 512
            T_ = min(MOE_T, ntok)
            npass = ntok // T_
            nb = T_ // 128
            WR = sb("WR_s", [128, 8, 36], F32)
            rbias = sb("rbias", [128, 36], F32)
            cload(WR[:], WR_d.rearrange("(k p) f -> p k f", p=128), "WR", q="sp")
            cload(rbias[:], rows_d[4, 0:36].partition_broadcast(128), "rbias", q="sp")
            g2bc = sb("g2bc", [128, 1024], F32)
            b2bc = sb("b2bc", [128, 1024], F32)
            cload(g2bc[:], rows_d[5, :].partition_broadcast(128), "g2bc", q="sp")
            cload(b2bc[:], rows_d[6, :].partition_broadcast(128), "b2bc", q="sp")
            h1T = sb("h1T", [128, 8, T_], BF16)
            h1T32 = sb("h1T32", [128, 8, 128], F32)
            accm = sb("accm", [128, nb, 1024], F32)
            comb = sb("comb", [128, nb, 32], F32)
            hb = [sb("hb%d" % i, [128, 1024], F32) for i in range(2)]
            d_hb = [sem("d_hb%d" % i) for i in range(2)]
            NW = 3
            wg_s = [sb("wg_s%d" % i, [128, 8, 256], BF16) for i in range(NW)]
            wu_s = [sb("wu_s%d" % i, [128, 8, 256], BF16) for i in range(NW)]
            wd_s = [sb("wd_s%d" % i, [128, 2, 1024], BF16) for i in range(NW)]
            d_wg = [sem("d_wg%d" % i) for i in range(NW)]
            d_wu = [sem("d_wu%d" % i) for i in range(NW)]
            d_wd = [sem("d_wd%d" % i) for i in range(NW)]
            sgs = [sb("sg%d" % i, [128, 2, 512], BF16) for i in range(2)]
            hid = [sb("hid%d" % i, [128, 2, 512], BF16) for i in range(2)]
            lg = sb("lg", [128, 36], F32)
            rt = sb("rt", [128, 64], F32)
            d_out = [sem("d_out%d" % i) for i in range(2)]

            def wload(e_, slot):
                S.add("pool", lambda e: e.dma_start(out=wg_s[slot][:], in_=wg_d[e_].rearrange("(k p) f -> p k f", p=128)), writes=[("wg", slot)], dsem=d_wg[slot])
                S.add("pool", lambda e: e.dma_start(out=wu_s[slot][:], in_=wu_d[e_].rearrange("(k p) f -> p k f", p=128)), writes=[("wu", slot)], dsem=d_wu[slot])
                S.add("pool", lambda e: e.dma_start(out=wd_s[slot][:], in_=wd_d[e_].rearrange("(k p) f -> p k f", p=128)), writes=[("wd", slot)], dsem=d_wd[slot])

            for p in range(npass):
                seq = list(range(n_exp))
                for i0 in range(min(NW, n_exp)):
                    wload(seq[i0], i0 % NW)
                S.add("pool", lambda e: e.memset(accm[:], 0.0), writes=[("accm", b) for b in range(nb)])
                for b in range(nb):
                    gb = p * nb + b
                    hb_ = hb[b % 2]; hk = ("hb", b % 2)
                    S.add("sp", lambda e, hb_=hb_, gb=gb: e.dma_start(out=hb_[:], in_=h1_d[gb * 128:(gb + 1) * 128, :]), reads=[("h1d", gb)], writes=[hk], dsem=d_hb[b % 2])
                    for k in range(8):
                        bi = k // 4
                        S.add("pe", lambda e, k=k, bi=bi, hb_=hb_: e.transpose(out=B[bi][:, (k % 4) * 128:(k % 4 + 1) * 128], in_=hb_[:, k * 128:(k + 1) * 128], identity=identf[:]),
                              reads=[hk, "identf"], writes=[kB(bi)])
                    S.add("act", lambda e: e.activation(out=h1T32[:, 0:4, :], in_=B[0][:, :].rearrange("p (k i) -> p k i", k=4), func=AF.Copy),
                          reads=[kB(0)], writes=[("h1T32", 0)])
                    S.add("dve", lambda e: e.tensor_copy(out=h1T32[:, 4:8, :], in_=B[1][:, :].rearrange("p (k i) -> p k i", k=4)),
                          reads=[kB(1)], writes=[("h1T32", 1)])
                    S.add("pool", lambda e, b=b: e.tensor_copy(out=h1T[:, :, b * 128:(b + 1) * 128], in_=h1T32[:, :, :]),
                          reads=[("h1T32", 0), ("h1T32", 1)], writes=[("h1T", b)])
                    for k in range(8):
                        S.add("pe", lambda e, k=k: e.matmul(B[2][:, 0:36], lhsT=h1T32[:, k, :], rhs=WR[:, k, :], start=(k == 0), stop=(k == 7)),
                              reads=[("h1T32", k // 4), "WR"], writes=[kB(2)])
                    route(S, B[2], kB(2), lg, rt, rbias, comb, b)
                ntg = T_ // 512
                pairs = [(ei, tg) for ei in range(n_exp) for tg in range(ntg)]
                DB = [(B[4][:, :], kB(4)), (B[5][:, :], kB(5)), (T[0][:, :].bitcast(F32), kT(0)), (T[1][:, :].bitcast(F32), kT(1))]
                dbi = [0]

                def moe_gu(pi_, fc):
                    ei, tg = pairs[pi_]
                    slot = ei % NW
                    hkeys = [("h1T", tg * 4 + q) for q in range(4)]
                    for k in range(8):
                        S.add("pe", lambda e, k=k: e.matmul(B[fc][:, :], lhsT=wg_s[slot][:, k, fc * 128:(fc + 1) * 128], rhs=h1T[:, k, tg * 512:(tg + 1) * 512],
                                                            start=(k == 0), stop=(k == 7)), reads=[("wg", slot)] + hkeys, writes=[kB(fc)])
                    for k in range(8):
                        S.add("pe", lambda e, k=k: e.matmul(B[2 + fc][:, :], lhsT=wu_s[slot][:, k, fc * 128:(fc + 1) * 128], rhs=h1T[:, k, tg * 512:(tg + 1) * 512],
                                                            start=(k == 0), stop=(k == 7)), reads=[("wu", slot)] + hkeys, writes=[kB(2 + fc)])
                    sg_ = sgs[pi_ % 2]; hd = hid[pi_ % 2]
                    S.add("act", lambda e: e.activation(out=sg_[:, fc, :], in_=B[fc][:, :], func=AF.Silu), reads=[kB(fc)], writes=[("sg", pi_ % 2, fc)])
                    S.add("dve", lambda e: e.tensor_tensor(out=hd[:, fc, :], in0=B[2 + fc][:, :], in1=sg_[:, fc, :], op=ALU.mult),
                          reads=[kB(2 + fc), ("sg", pi_ % 2, fc)], writes=[("hid", pi_ % 2, fc)])

                def moe_d(pi_):
                    ei, tg = pairs[pi_]
                    slot = ei % NW
                    hd = hid[pi_ % 2]
                    for tb in range(4):
                        b = tg * 4 + tb
                        for half in range(2):
                            dap, dk = DB[dbi[0]]; dbi[0] = (dbi[0] + 1) % len(DB)
                            for fc in range(2):
                                S.add("pe", lambda e, fc=fc, dap=dap, tb=tb, half=half: e.matmul(dap, lhsT=hd[:, fc, tb * 128:(tb + 1) * 128], rhs=wd_s[slot][:, fc, half * 512:(half + 1) * 512],
                                                                                                start=(fc == 0), stop=(fc == 1)),
                                      reads=[("hid", pi_ % 2, 0), ("hid", pi_ % 2, 1), ("wd", slot)], writes=[dk])
                            S.add("dve", lambda e, dap=dap, b=b, half=half: e.scalar_tensor_tensor(out=accm[:, b, half * 512:(half + 1) * 512], in0=dap, scalar=comb[:, b, ei:ei + 1],
                                                                                                  in1=accm[:, b, half * 512:(half + 1) * 512], op0=ALU.mult, op1=ALU.add),
                                  reads=[dk, ("comb", b), ("accm", b)], writes=[("accm", b)])
                    if tg == ntg - 1 and ei + NW < n_exp:
                        wload(ei + NW, slot)

                npairs = len(pairs)
                moe_gu(0, 0); moe_gu(0, 1)
                for pi_ in range(npairs):
                    if pi_ + 1 < npairs:
                        moe_gu(pi_ + 1, 0)
                    moe_d(pi_)
                    if pi_ + 1 < npairs:
                        moe_gu(pi_ + 1, 1)
                for b in range(nb):
                    gb = p * nb + b
                    hb_ = hb[b % 2]; hk = ("hb", b % 2)
                    S.add("sp", lambda e, hb_=hb_, gb=gb: e.dma_start(out=hb_[:], in_=h1_d[gb * 128:(gb + 1) * 128, :]), reads=[("h1d", gb)], writes=[hk], dsem=d_hb[b % 2])
                    S.add("dve", lambda e, hb_=hb_, b=b: e.scalar_tensor_tensor(out=hb_[:], in0=hb_[:], scalar=ALPHA, in1=accm[:, b, :], op0=ALU.mult, op1=ALU.add),
                          reads=[hk, ("accm", b)], writes=[hk])
                    ln_tok(S, hb_, hk, st[b % 4], mv[b % 4], b % 4, g2bc, b2bc, "g2bc", "b2bc", epsln)
                    S.add("sp", lambda e, hb_=hb_, gb=gb: e.dma_start(out=out_d[gb * 128:(gb + 1) * 128, :], in_=hb_[:]), reads=[hk], writes=[("outd", gb)], dsem=d_out[b % 2])

        block = es.enter_context(nc.Block())
        S.emit(block, sems)
    return nc


def ln_tok(S, buf, bkey, st_, mv_, t, gbc, bbc, gk, bk, epsln, geng="pool", beng="pool"):
    bks = list(bkey) if isinstance(bkey, list) else [bkey]
    S.add("dve", lambda e: e.bn_stats(out=st_[:, 0, :], in_=buf[:, 0:512]), reads=bks, writes=[("st", t, 0)])
    S.add("dve", lambda e: e.bn_stats(out=st_[:, 1, :], in_=buf[:, 512:1024]), reads=bks, writes=[("st", t, 1)])
    S.add("dve", lambda e: e.bn_aggr(out=mv_[:, 0:2], in_=st_[:, :, :]), reads=[("st", t, 0), ("st", t, 1)], writes=[("mv", t, 0)])
    S.add("act", lambda e: e.activation(out=mv_[:, 2:3], in_=mv_[:, 1:2], func=AF.Ln, bias=epsln[:, 0:1], scale=1.0), reads=[("mv", t, 0), "epsc"], writes=[("mv", t, 1)])
    S.add("act", lambda e: e.activation(out=mv_[:, 2:3], in_=mv_[:, 2:3], func=AF.Exp, scale=-0.5), reads=[("mv", t, 1)], writes=[("mv", t, 1)])
    S.add("dve", lambda e: e.scalar_tensor_tensor(out=mv_[:, 3:4], in0=mv_[:, 0:1], scalar=-1.0, in1=mv_[:, 2:3], op0=ALU.mult, op1=ALU.mult),
          reads=[("mv", t, 0), ("mv", t, 1)], writes=[("mv", t, 2)])
    S.add("act", lambda e: e.activation(out=buf[:], in_=buf[:], func=AF.Identity, bias=mv_[:, 3:4], scale=mv_[:, 2:3]), reads=bks + [("mv", t, 1), ("mv", t, 2)], writes=bks)
    S.add(geng, lambda e: e.tensor_tensor(out=buf[:], in0=buf[:], in1=gbc[:], op=ALU.mult), reads=bks + [gk], writes=bks)
    S.add(beng, lambda e: e.tensor_tensor(out=buf[:], in0=buf[:], in1=bbc[:], op=ALU.add), reads=bks + [bk], writes=bks)


def route(S, Bl, bkey, lg, rt, rbias, comb, b, ohg_out=None, c8_out=None, tag=0, defer=None):
    def add(eng, fn, r, w):
        if defer is None:
            S.add(eng, fn, reads=r, writes=w)
        else:
            defer.append((eng, fn, r, w))

    def D(fn, r, w):
        add("dve", fn, r, w)
    LG = ("lg", tag)
    GM, NGM, GS, GT, M1, M2, DD, ED, DEN, W1, W2 = range(11)
    OHG, ING, OH1, ING2, OH2, C8, GE = 16, 20, 28, 36, 44, 52, 60
    K = ("rt", tag)
    D(lambda e: e.tensor_tensor(out=lg[:], in0=Bl[:, 0:36], in1=rbias[:], op=ALU.add), [bkey, "rbias"], [LG])
    D(lambda e: e.tensor_reduce(out=rt[:, GM:GM + 1], in_=lg[:, 0:4], axis=AX.X, op=ALU.max), [LG], [K])
    D(lambda e: e.tensor_scalar(out=rt[:, NGM:NGM + 1], in0=rt[:, GM:GM + 1], scalar1=-1.0, scalar2=None, op0=ALU.mult), [K], [K])
    add("act", lambda e: e.activation(out=rt[:, GE:GE + 4], in_=lg[:, 0:4], func=AF.Exp, bias=rt[:, NGM:NGM + 1], scale=1.0, accum_out=rt[:, GS:GS + 1]), [LG, K], [K])
    D(lambda e: e.reciprocal(out=rt[:, GT:GT + 1], in_=rt[:, GS:GS + 1]), [K], [K])
    D(lambda e: e.tensor_scalar(out=rt[:, OHG:OHG + 4], in0=lg[:, 0:4], scalar1=rt[:, GM:GM + 1], scalar2=None, op0=ALU.is_equal), [LG, K], [K])
    D(lambda e: e.tensor_scalar(out=rt[:, ING:ING + 8], in0=lg[:, 4:12], scalar1=rt[:, OHG:OHG + 1], scalar2=None, op0=ALU.mult), [LG, K], [K])
    for g in range(1, 4):
        D(lambda e, g=g: e.scalar_tensor_tensor(out=rt[:, ING:ING + 8], in0=lg[:, 4 + 8 * g:12 + 8 * g], scalar=rt[:, OHG + g:OHG + g + 1], in1=rt[:, ING:ING + 8], op0=ALU.mult, op1=ALU.add), [LG, K], [K])
    D(lambda e: e.tensor_reduce(out=rt[:, M1:M1 + 1], in_=rt[:, ING:ING + 8], axis=AX.X, op=ALU.max), [K], [K])
    D(lambda e: e.tensor_scalar(out=rt[:, OH1:OH1 + 8], in0=rt[:, ING:ING + 8], scalar1=rt[:, M1:M1 + 1], scalar2=None, op0=ALU.is_equal), [K], [K])
    D(lambda e: e.scalar_tensor_tensor(out=rt[:, ING2:ING2 + 8], in0=rt[:, OH1:OH1 + 8], scalar=-1e30, in1=rt[:, ING:ING + 8], op0=ALU.mult, op1=ALU.add), [K], [K])
    D(lambda e: e.tensor_reduce(out=rt[:, M2:M2 + 1], in_=rt[:, ING2:ING2 + 8], axis=AX.X, op=ALU.max), [K], [K])
    D(lambda e: e.tensor_scalar(out=rt[:, OH2:OH2 + 8], in0=rt[:, ING2:ING2 + 8], scalar1=rt[:, M2:M2 + 1], scalar2=None, op0=ALU.is_equal), [K], [K])
    D(lambda e: e.tensor_tensor(out=rt[:, DD:DD + 1], in0=rt[:, M2:M2 + 1], in1=rt[:, M1:M1 + 1], op=ALU.subtract), [K], [K])
    add("act", lambda e: e.activation(out=rt[:, ED:ED + 1], in_=rt[:, DD:DD + 1], func=AF.Exp), [K], [K])
    D(lambda e: e.tensor_scalar(out=rt[:, DEN:DEN + 1], in0=rt[:, ED:ED + 1], scalar1=1.0, scalar2=None, op0=ALU.add), [K], [K])
    D(lambda e: e.reciprocal(out=rt[:, DEN:DEN + 1], in_=rt[:, DEN:DEN + 1]), [K], [K])
    D(lambda e: e.tensor_tensor(out=rt[:, W1:W1 + 1], in0=rt[:, GT:GT + 1], in1=rt[:, DEN:DEN + 1], op=ALU.mult), [K], [K])
    D(lambda e: e.tensor_tensor(out=rt[:, W2:W2 + 1], in0=rt[:, W1:W1 + 1], in1=rt[:, ED:ED + 1], op=ALU.mult), [K], [K])
    D(lambda e: e.tensor_scalar(out=rt[:, C8:C8 + 8], in0=rt[:, OH1:OH1 + 8], scalar1=rt[:, W1:W1 + 1], scalar2=None, op0=ALU.mult), [K], [K])
    D(lambda e: e.scalar_tensor_tensor(out=rt[:, C8:C8 + 8], in0=rt[:, OH2:OH2 + 8], scalar=rt[:, W2:W2 + 1], in1=rt[:, C8:C8 + 8], op0=ALU.mult, op1=ALU.add), [K], [K])
    if ohg_out is not None:
        D(lambda e: e.tensor_copy(out=ohg_out[:, b, :], in_=rt[:, OHG:OHG + 4]), [K], [("OHG", b)])
        D(lambda e: e.tensor_copy(out=c8_out[:, b, :], in_=rt[:, C8:C8 + 8]), [K], [("C8", b)])
        return
    for g in range(4):
        D(lambda e, g=g: e.tensor_scalar(out=comb[:, b, 8 * g:8 * g + 8], in0=rt[:, C8:C8 + 8], scalar1=rt[:, OHG + g:OHG + g + 1], scalar2=None, op0=ALU.mult), [K], [("comb", b)])


def _rot_perm64():
    return (np.arange(64) + 32) % 64


def host_layout(inputs):
    f = np.float32
    x = np.asarray(inputs["x"], f)
    meta = np.asarray(inputs["meta_tokens"], f)
    w_in = np.asarray(inputs["w_in"], f)[0]
    rp = _rot_perm64()
    q_a = w_in[:, 0:512]; k_a = w_in[:, 512:640]; v_a = w_in[:, 640:768]
    c_q = w_in[:, 768:1024]; c_kv = w_in[:, 1024:1280]; k_r = w_in[:, 1280:1344]
    k_a_rot = np.concatenate([k_a[:, h * 64:(h + 1) * 64][:, rp] for h in range(2)], axis=1)
    k_r_rot = k_r[:, rp]
    WK = np.concatenate([k_a, k_a_rot, c_kv, v_a, k_r, k_r, k_r_rot, k_r_rot], axis=1)
    qcols, qrcols = [], []
    for c in range(4):
        for h in (c, 4 + c):
            blk = q_a[:, h * 64:(h + 1) * 64]
            qcols.append(blk); qrcols.append(blk[:, rp])
    WQ = np.concatenate(qcols + qrcols + [c_q], axis=1)
    w_uq = np.asarray(inputs["mla_w_uq"], f)[0]
    nope = [w_uq[:, h * 192:h * 192 + 128] for h in range(4)]
    rope = [w_uq[:, h * 192 + 128:h * 192 + 192] for h in range(4)]
    WUQ = np.concatenate(nope + rope + [r[:, rp] for r in rope], axis=1)
    w_ukv = np.asarray(inputs["mla_w_ukv"], f)[0]
    WUK = np.concatenate([w_ukv[:, h * 256:h * 256 + 128] for h in range(4)], axis=1)
    WUV = np.concatenate([w_ukv[:, h * 256 + 128:h * 256 + 256] for h in range(4)], axis=1)
    w_o = np.asarray(inputs["w_o"], f)[0]
    WOA = np.zeros((128, 4, 1024), f)
    for g in range(2):
        for c in range(4):
            h = 4 * g + c
            WOA[g * 64:(g + 1) * 64, c, :] = w_o[h * 64:(h + 1) * 64, :]
    WOB = np.zeros((128, 4, 1024), f)
    for h in range(4):
        WOB[:, h, :] = w_o[512 + h * 128:512 + (h + 1) * 128, :]
    WR = np.concatenate([np.asarray(inputs["moe_w_group"], f)[0], np.asarray(inputs["moe_w_router"], f)[0]], axis=1)
    vec = np.zeros((128, 40), f)
    vec[:, 0:8] = np.asarray(inputs["ln_in_g"], f).reshape(8, 128).T
    vec[:, 8:16] = np.asarray(inputs["ln_in_b"], f).reshape(8, 128).T
    vec[:, 16:18] = np.asarray(inputs["mla_q_norm_g"], f)[0].reshape(2, 128).T
    vec[:, 18:20] = np.asarray(inputs["mla_kv_norm_g"], f)[0].reshape(2, 128).T
    ga = np.asarray(inputs["swa_out_norm_g"], f)[0]
    sk = np.asarray(inputs["swa_sinks"], f)[0]
    for g in range(2):
        for c in range(4):
            h = 4 * g + c
            vec[g * 64:(g + 1) * 64, 20 + c] = ga[h * 64:(h + 1) * 64]
            vec[g * 64:(g + 1) * 64, 28 + c] = sk[h]
    vec[:, 24:28] = np.asarray(inputs["mla_out_norm_g"], f)[0].reshape(4, 128).T
    rows = np.zeros((7, 1024), f)
    rows[0] = np.asarray(inputs["ln1_g"], f)[0]; rows[1] = np.asarray(inputs["ln1_b"], f)[0]
    rows[2] = np.asarray(inputs["ln_in_g"], f); rows[3] = np.asarray(inputs["ln_in_b"], f)
    rows[4, 0:4] = np.asarray(inputs["moe_b_group"], f)[0]; rows[4, 4:36] = np.asarray(inputs["moe_b_router"], f)[0]
    rows[5] = np.asarray(inputs["ln2_g"], f)[0]; rows[6] = np.asarray(inputs["ln2_b"], f)[0]
    cst = np.zeros((128, 1536), f)
    cst[:, 0:128] = np.eye(128, dtype=f)
    cst[:, 128:256] = 1.0
    cst[:, 256:320] = 1.0
    cst[:, 448:512] = 1.0
    p = np.arange(128)[:, None]; i = np.arange(128)[None, :]
    cst[:, 512:1024] = np.tile((p > i).astype(f), (1, 4))
    cst[:, 1024:1536] = np.tile((p <= i).astype(f), (1, 4))
    cst2 = np.zeros((128, 128 + 8 + NSLOT), f)
    cst2[:, 0:128] = (p < i).astype(f)
    cst2[:, 128:136] = np.arange(8)[None, :] * 128 + np.arange(128)[:, None]
    cst2[:, 136:136 + NSLOT] = (np.arange(NSLOT) * SLOT)[None, :]
    blk0 = np.zeros((128, 1024), f); blk0[112:] = meta
    zblk = np.zeros((128, 1024), f)
    pos_blk0 = np.maximum(np.arange(128) - 112, 0).astype(f)
    inv_freq = (10000.0 ** (-np.arange(0, 64, 2, dtype=f) / f(64))).astype(f)
    shared = dict(cst=cst, WK=WK, WQ=WQ, WUQ=WUQ, WUK=WUK, WUV=WUV, WOA=WOA.reshape(128, 4096), WOB=WOB.reshape(128, 4096), WR=WR, vec=vec,
                  rows=rows,
                  wg2=np.asarray(inputs["moe_w_gate"], f)[0].reshape(NEXP, 8, 128, 256).transpose(0, 2, 1, 3).reshape(NEXP * 128, 2048),
                  wu2=np.asarray(inputs["moe_w_up"], f)[0].reshape(NEXP, 8, 128, 256).transpose(0, 2, 1, 3).reshape(NEXP * 128, 2048),
                  wd2=np.asarray(inputs["moe_w_down"], f)[0].reshape(NEXP, 2, 128, 1024).transpose(0, 2, 1, 3).reshape(NEXP * 128, 2048),
                  cst2=cst2)
    shared = {k: np.ascontiguousarray(v) for k, v in shared.items()}
    in_maps = []
    for core in range(8):
        b, hf = core // 2, core % 2
        xb = x[b]
        blocks = [zblk, zblk, zblk, blk0]
        pos = [np.zeros(128, f)] * 3 + [pos_blk0]
        kbm = np.zeros((128, NBLK), f); kbs = np.zeros((128, NBLK), f)
        kbm[:, 0:3] = NEGB; kbm[:112, 3] = NEGB; kbs[:112, 3] = NEGB
        if hf == 0:
            blocks += [zblk, zblk, zblk, blk0]
            pos += [np.zeros(128, f)] * 3 + [pos_blk0]
            kbm[:, 4:8] = NEGB; kbs[:112, 7] = NEGB
        for t in range(64):
            blocks.append(xb[t * 128:(t + 1) * 128])
            pos.append((16 + t * 128 + np.arange(128)).astype(f))
        if hf == 1:
            blocks += [zblk] * 4
            pos += [np.zeros(128, f)] * 4
        xs_ = np.ascontiguousarray(np.concatenate(blocks, axis=0))
        posv = np.concatenate(pos)
        ang = posv[:, None] * inv_freq[None, :]
        cos64 = np.concatenate([np.cos(ang), np.cos(ang)], axis=1)
        sin64 = np.concatenate([-np.sin(ang), np.sin(ang)], axis=1)
        cosT_ = np.ascontiguousarray(np.concatenate([cos64, cos64], axis=1).T.astype(f))
        sinT_ = np.ascontiguousarray(np.concatenate([sin64, sin64], axis=1).T.astype(f))
        m = dict(shared)
        m.update(xs=xs_, cosT=cosT_, sinT=sinT_, kbm=kbm, kbs=kbs)
        in_maps.append(m)
    return in_maps


_NC_CACHE = {}


def kernel(**inputs):
    in_maps = host_layout(inputs)
    if "nc" not in _NC_CACHE:
        _NC_CACHE["nc"] = build_nc()
    nc = _NC_CACHE["nc"]
    res = run_bass_kernel_spmd(nc, in_maps, core_ids=list(range(8)))
    out = np.zeros((4, 8192, 1024), np.float32)
    for core in range(8):
        b, hf = core // 2, core % 2
        o = res.results[core]["out"]
        for j in range(NSTEP):
            xc = 2 * j + hf
            out[b, xc * 512:(xc + 1) * 512] = o[j * 512:(j + 1) * 512]
    return out


def moe_sparse_phase(L):
    S = L["S"]; sb = L["sb"]; sem = L["sem"]; cload = L["cload"]; B = L["B"]; T = L["T"]; kB = L["kB"]; kT = L["kT"]
    n_step = L["n_step"]; h1_d = L["h1_d"]; out_d = L["out_d"]; rows_d = L["rows_d"]; WR_d = L["WR_d"]
    identf = L["identf"]; onesf = L["onesf"]; ident = L["ident"]; st = L["st"]; mv = L["mv"]; epsln = L["epsln"]
    wg2_d = L["wg2_d"]; wu2_d = L["wu2_d"]; wd2_d = L["wd2_d"]; cst2_d = L["cst2_d"]; xs_d = L["xs_d"]; ffn_d = L["ffn_d"]
    nblk = n_step * 4
    nslot = nblk // 4 + 3
    assert nslot <= NSLOT

    WR = sb("WR_s", [128, 8, 36], F32)
    rbias = sb("rbias", [128, 36], F32)
    cst2 = sb("cst2_s", [128, 128 + 8 + NSLOT], F32)
    g2bc = sb("g2bc", [128, 1024], F32)
    b2bc = sb("b2bc", [128, 1024], F32)
    cload(WR[:], WR_d.rearrange("(k p) f -> p k f", p=128), "WR", q="sp")
    cload(rbias[:], rows_d[4, 0:36].partition_broadcast(128), "rbias", q="sp")
    cload(cst2[:], cst2_d[:, :], "cst2", q="sp")
    cload(g2bc[:], rows_d[5, :].partition_broadcast(128), "g2bc", q="sp")
    cload(b2bc[:], rows_d[6, :].partition_broadcast(128), "b2bc", q="sp")
    allz = L["allz"]
    UT = cst2[:, 0:128]
    JP = cst2[:, 128:136]
    SLST = cst2[:, 136:136 + NSLOT]
    NHB = 8
    hb = [sb("hb%d" % i, [128, 1024], F32) for i in range(NHB)]
    d_hb = [sem("d_hb%d" % i) for i in range(NHB)]
    d_out = [sem("d_out%d" % i) for i in range(6)]
    h1T32 = sb("h1T32", [128, 8, 128], F32)
    OHGt = sb("OHGt", [128, 32, 4], F32)
    C8t = sb("C8t", [128, 32, 8], F32)
    CS = sb("CS", [128, 32, 4], F32)
    PRE = sb("PRE", [128, 32, 4], F32)
    OFF = sb("OFF", [128, 32, 4], F32)
    sm = sb("sm", [128, 64], F32)
    POSF = sb("POSF", [128, 32], F32)
    POSI = sb("POSI", [128, 32], U32)
    WIDXF = sb("WIDXF", [128, NSLOT, 8], F32)
    WIDX = sb("WIDX", [128, NSLOT, 8], U32)
    if nblk < 32:
        S.add("dve", lambda e: e.memset(OHGt[:], 0.0), writes=[("OHG", b) for b in range(32)])

    GRP = 4
    lg4 = [sb("lg4_%d" % i, [128, 36], F32) for i in range(GRP)]
    rt4 = [sb("rt4_%d" % i, [128, 64], F32) for i in range(GRP)]
    for g0 in range(0, nblk, GRP):
        chains = []
        for i in range(GRP):
            b = g0 + i
            hb_ = hb[b % NHB]; hk = ("hb", b % NHB)
            rb_ = 2 + i
            S.add("sp", lambda e, hb_=hb_, b=b: e.dma_start(out=hb_[:], in_=h1_d[b * 128:(b + 1) * 128, :]), reads=[("h1d", b)], writes=[hk], dsem=d_hb[b % NHB])
            for k in range(8):
                bi = k // 4
                S.add("pe", lambda e, k=k, bi=bi, hb_=hb_: e.transpose(out=B[bi][:, (k % 4) * 128:(k % 4 + 1) * 128], in_=hb_[:, k * 128:(k + 1) * 128], identity=identf[:]),
                      reads=[hk, "identf"], writes=[kB(bi)])
            S.add("act", lambda e: e.activation(out=h1T32[:, 0:4, :], in_=B[0][:, :].rearrange("p (k i) -> p k i", k=4), func=AF.Copy), reads=[kB(0)], writes=[("h1T32", 0)])
            S.add("act", lambda e: e.activation(out=h1T32[:, 4:8, :], in_=B[1][:, :].rearrange("p (k i) -> p k i", k=4), func=AF.Copy), reads=[kB(1)], writes=[("h1T32", 1)])
            for k in range(8):
                S.add("pe", lambda e, k=k, rb_=rb_: e.matmul(B[rb_][:, 0:36], lhsT=h1T32[:, k, :], rhs=WR[:, k, :], start=(k == 0), stop=(k == 7)),
                      reads=[("h1T32", k // 4), "WR"], writes=[kB(rb_)])
            ch_ = []
            route(S, B[rb_], kB(rb_), lg4[i], rt4[i], rbias, None, b, ohg_out=OHGt, c8_out=C8t, tag=i, defer=ch_)
            chains.append(ch_)
        for opi in range(max(len(c) for c in chains)):
            for c in chains:
                if opi < len(c):
                    eng, fn, r, w = c[opi]
                    S.add(eng, fn, reads=r, writes=w)
    allohg = [("OHG", b) for b in range(32)]
    flat = lambda t: t[:, :, :].rearrange("p b g -> p (b g)")
    S.add("pe", lambda e: e.matmul(B[3][:, 0:128], lhsT=onesf[:], rhs=flat(OHGt), start=True, stop=True), reads=allohg + ["onesf"], writes=[kB(3)])
    S.add("pe", lambda e: e.matmul(B[4][:, 0:128], lhsT=UT, rhs=flat(OHGt), start=True, stop=True), reads=allohg + ["cst2"], writes=[kB(4)])
    S.add("dve", lambda e: e.tensor_copy(out=flat(CS), in_=B[3][:, 0:128]), reads=[kB(3)], writes=["CS"])
    S.add("act", lambda e: e.activation(out=flat(PRE), in_=B[4][:, 0:128], func=AF.Copy), reads=[kB(4)], writes=["PRE"])
    D = lambda fn, r, w: S.add("dve", fn, reads=r, writes=w)
    NG, PC, BASE, END, TMP8, GS, AA = 0, 4, 8, 12, 16, 24, 36
    D(lambda e: e.tensor_reduce(out=sm[:, NG:NG + 4], in_=CS[:, :, :].rearrange("p b g -> p g b"), axis=AX.X, op=ALU.add), ["CS"], ["sm"])
    for g in range(4):
        D(lambda e, g=g: e.tensor_scalar(out=sm[:, TMP8:TMP8 + 8], in0=SLST[:, 0:8], scalar1=sm[:, NG + g:NG + g + 1], scalar2=None, op0=ALU.is_lt), ["sm", "cst2"], ["sm"])
        D(lambda e, g=g: e.tensor_reduce(out=sm[:, PC + g:PC + g + 1], in_=sm[:, TMP8:TMP8 + 8], axis=AX.X, op=ALU.add), ["sm"], ["sm"])
    D(lambda e: e.tensor_scalar(out=sm[:, PC:PC + 4], in0=sm[:, PC:PC + 4], scalar1=float(SLOT), scalar2=None, op0=ALU.mult), ["sm"], ["sm"])
    D(lambda e: e.memset(sm[:, BASE:BASE + 1], 0.0), ["sm"], ["sm"])
    for g in range(1, 4):
        D(lambda e, g=g: e.tensor_tensor(out=sm[:, BASE + g:BASE + g + 1], in0=sm[:, BASE + g - 1:BASE + g], in1=sm[:, PC + g - 1:PC + g], op=ALU.add), ["sm"], ["sm"])
    D(lambda e: e.tensor_tensor(out=sm[:, END:END + 4], in0=sm[:, BASE:BASE + 4], in1=sm[:, PC:PC + 4], op=ALU.add), ["sm"], ["sm"])
    D(lambda e: e.tensor_copy(out=OFF[:, 0, :], in_=sm[:, BASE:BASE + 4]), ["sm"], ["OFF"])
    for b in range(1, 32):
        D(lambda e, b=b: e.tensor_tensor(out=OFF[:, b, :], in0=OFF[:, b - 1, :], in1=CS[:, b - 1, :], op=ALU.add), ["OFF", "CS"], ["OFF"])
    D(lambda e: e.tensor_tensor(out=flat(PRE), in0=flat(PRE), in1=flat(OFF), op=ALU.add), ["PRE", "OFF"], ["PRE"])
    D(lambda e: e.tensor_tensor(out=flat(PRE), in0=flat(PRE), in1=flat(OHGt), op=ALU.mult), ["PRE"] + allohg, ["PRE"])
    D(lambda e: e.tensor_reduce(out=POSF[:], in_=PRE[:, :, :], axis=AX.X, op=ALU.add), ["PRE"], ["POSF"])
    D(lambda e: e.tensor_copy(out=POSI[:], in_=POSF[:]), ["POSF"], ["POSI"])
    D(lambda e: e.tensor_scalar(out=sm[:, GS:GS + NSLOT], in0=SLST, scalar1=sm[:, END:END + 1], scalar2=None, op0=ALU.is_ge), ["sm", "cst2"], ["sm"])
    for g in range(1, 3):
        D(lambda e, g=g: e.scalar_tensor_tensor(out=sm[:, GS:GS + NSLOT], in0=SLST, scalar=sm[:, END + g:END + g + 1], in1=sm[:, GS:GS + NSLOT], op0=ALU.is_ge, op1=ALU.add), ["sm", "cst2"], ["sm"])
    D(lambda e: e.tensor_scalar(out=sm[:, AA:AA + NSLOT], in0=sm[:, GS:GS + NSLOT], scalar1=1024.0, scalar2=None, op0=ALU.mult), ["sm"], ["sm"])
    for s_ in range(NSLOT):
        D(lambda e, s_=s_: e.tensor_scalar(out=WIDXF[:, s_, :], in0=JP, scalar1=sm[:, AA + s_:AA + s_ + 1], scalar2=None, op0=ALU.add), ["sm", "cst2"], ["WIDXF"])
    D(lambda e: e.tensor_copy(out=WIDX[:, :, :].rearrange("p s j -> p (s j)"), in_=WIDXF[:, :, :].rearrange("p s j -> p (s j)")), ["WIDXF"], ["WIDX"])

    NRT = 4
    rowt = [sb("rowt%d" % i, [128, ROWW], F32) for i in range(NRT)]
    d_rl = [sem("d_rl%d" % i) for i in range(NRT)]
    d_rs = [sem("d_rs%d" % i) for i in range(NRT)]
    scat_cap = []
    S.capture = scat_cap
    for b in range(nblk):
        r_ = rowt[b % NRT]; rk = ("rowt", b % NRT)
        S.add("sp", lambda e, r_=r_, b=b: e.dma_start(out=r_[:, 0:1024], in_=h1_d[b * 128:(b + 1) * 128, :]), reads=[("h1d", b)], writes=[rk], dsem=d_rl[b % NRT])
        S.add("dve", lambda e, r_=r_, b=b: e.tensor_copy(out=r_[:, 1024:1032], in_=C8t[:, b, :]), reads=[("C8", b), rk], writes=[(rk, "c")])
        S.add("pool", lambda e, r_=r_, b=b: e.indirect_dma_start(out=xs_d[:, :], out_offset=bass.IndirectOffsetOnAxis(ap=POSI[:, b:b + 1], axis=0), in_=r_[:], in_offset=None),
              reads=[rk, (rk, "c"), "POSI"] + allz, writes=[("xs", b)], dsem=d_rs[b % NRT])
    S.capture = None
    allxs = [("xs", b) for b in range(nblk)]

    NW = 4
    wg_s = [sb("wg_s%d" % i, [128, 2048], BF16) for i in range(NW)]
    wu_s = [sb("wu_s%d" % i, [128, 2048], BF16) for i in range(NW)]
    wd_s = [sb("wd_s%d" % i, [128, 2048], BF16) for i in range(NW)]
    d_wg = [sem("d_wg%d" % i) for i in range(NW)]
    d_wu = [sem("d_wu%d" % i) for i in range(NW)]
    d_wd = [sem("d_wd%d" % i) for i in range(NW)]
    sgs = [sb("sg%d" % i, [128, 2, 512], BF16) for i in range(2)]
    hid = [sb("hid%d" % i, [128, 2, 512], BF16) for i in range(2)]
    ups = [sb("ups%d" % i, [128, 2, 512], F32) for i in range(2)]
    xsb = [sb("xsb%d" % i, [128, ROWW], F32) for i in range(2)]
    d_xl = [sem("d_xl%d" % i) for i in range(2)]
    x16 = [sb("x16_%d" % i, [128, 1024], BF16) for i in range(2)]
    xsT = [sb("xsT%d" % i, [128, 8, 512], BF16) for i in range(2)]
    c8s = [sb("c8s%d" % i, [128, 4, 8], F32) for i in range(2)]
    accs = [sb("maccs%d" % i, [128, 4, 1024], F32) for i in range(2)]
    d_fs = [sem("d_fs%d" % i) for i in range(2)]
    pairs = [(s_, j) for s_ in range(nslot) for j in range(8)]
    npairs = len(pairs)

    def wload(pi_):
        s_, j = pairs[pi_]
        slot = pi_ % NW
        off = bass.IndirectOffsetOnAxis(ap=WIDX[:, s_, j:j + 1], axis=0)
        S.add("pool", lambda e: e.indirect_dma_start(out=wg_s[slot][:], out_offset=None, in_=wg2_d[:, :], in_offset=off), reads=["WIDX"], writes=[("wg", slot)], dsem=d_wg[slot])
        S.add("pool", lambda e: e.indirect_dma_start(out=wu_s[slot][:], out_offset=None, in_=wu2_d[:, :], in_offset=off), reads=["WIDX"], writes=[("wu", slot)], dsem=d_wu[slot])
        S.add("pool", lambda e: e.indirect_dma_start(out=wd_s[slot][:], out_offset=None, in_=wd2_d[:, :], in_offset=off), reads=["WIDX"], writes=[("wd", slot)], dsem=d_wd[slot])

    def slot_prep(s_):
        par = s_ % 2
        for t in range(4):
            xb = xsb[t % 2]; xk = ("xsb", t % 2)
            r0 = s_ * SLOT + t * 128
            S.add("sp", lambda e, xb=xb, r0=r0: e.dma_start(out=xb[:], in_=xs_d[r0:r0 + 128, :]), reads=allxs, writes=[xk], dsem=d_xl[t % 2])
            S.add("act", lambda e, xb=xb, t=t: e.activation(out=x16[t % 2][:], in_=xb[:, 0:1024], func=AF.Copy), reads=[xk], writes=[("x16", t % 2)])
            S.add("pool", lambda e, xb=xb, t=t: e.tensor_copy(out=c8s[par][:, t, :], in_=xb[:, 1024:1032]), reads=[xk], writes=[("c8s", par, t)])
            S.add("act", lambda e, xb=xb, t=t: e.activation(out=accs[par][:, t, :], in_=xb[:, 0:1024], func=AF.Copy, scale=ALPHA), reads=[xk], writes=[("maccs", par, t)])
            for k in range(8):
                Tb = T[k // 4]
                S.add("pe", lambda e, Tb=Tb, k=k, t=t: e.transpose(out=Tb[:, (k % 4) * 128:(k % 4 + 1) * 128], in_=x16[t % 2][:, k * 128:(k + 1) * 128], identity=ident[:]),
                      reads=[("x16", t % 2), "ident"], writes=[kT(k // 4)])
            S.add("act", lambda e, t=t: e.activation(out=xsT[par][:, 0:4, t * 128:(t + 1) * 128], in_=T[0][:, 0:512].rearrange("p (k i) -> p k i", k=4), func=AF.Copy),
                  reads=[kT(0)], writes=[("xsT", par, t)])
            S.add("dve", lambda e, t=t: e.tensor_copy(out=xsT[par][:, 4:8, t * 128:(t + 1) * 128], in_=T[1][:, 0:512].rearrange("p (k i) -> p k i", k=4)),
                  reads=[kT(1)], writes=[("xsT", par, t, 1)])

    DB = [(B[4][:, :], kB(4)), (B[5][:, :], kB(5)), (T[0][:, :].bitcast(F32), kT(0)), (T[1][:, :].bitcast(F32), kT(1))]
    dbi = [0]

    def moe_gu(pi_, fc):
        s_, j = pairs[pi_]
        slot = pi_ % NW
        par = s_ % 2
        xkeys = [("xsT", par, t) for t in range(4)] + [("xsT", par, t, 1) for t in range(4)]
        for k in range(8):
            S.add("pe", lambda e, k=k: e.matmul(B[fc][:, :], lhsT=wg_s[slot][:, k * 256 + fc * 128:k * 256 + (fc + 1) * 128], rhs=xsT[par][:, k, :], start=(k == 0), stop=(k == 7)),
                  reads=[("wg", slot)] + xkeys, writes=[kB(fc)])
        for k in range(8):
            S.add("pe", lambda e, k=k: e.matmul(B[2 + fc][:, :], lhsT=wu_s[slot][:, k * 256 + fc * 128:k * 256 + (fc + 1) * 128], rhs=xsT[par][:, k, :], start=(k == 0), stop=(k == 7)),
                  reads=[("wu", slot)] + xkeys, writes=[kB(2 + fc)])
        sg_ = sgs[pi_ % 2]; hd = hid[pi_ % 2]; up_ = ups[pi_ % 2]
        S.add("act", lambda e: e.activation(out=sg_[:, fc, :], in_=B[fc][:, :], func=AF.Silu), reads=[kB(fc)], writes=[("sg", pi_ % 2, fc)])
        S.add("act", lambda e: e.activation(out=up_[:, fc, :], in_=B[2 + fc][:, :], func=AF.Copy), reads=[kB(2 + fc)], writes=[("up", pi_ % 2, fc)])
        S.add("pool", lambda e: e.tensor_tensor(out=hd[:, fc, :], in0=up_[:, fc, :], in1=sg_[:, fc, :], op=ALU.mult), reads=[("up", pi_ % 2, fc), ("sg", pi_ % 2, fc)], writes=[("hid", pi_ % 2, fc)])

    def moe_d(pi_):
        s_, j = pairs[pi_]
        slot = pi_ % NW
        par = s_ % 2
        hd = hid[pi_ % 2]
        ac = accs[par]
        for tb in range(4):
            for half in range(2):
                dap, dk = DB[dbi[0]]; dbi[0] = (dbi[0] + 1) % len(DB)
                for fc in range(2):
                    S.add("pe", lambda e, fc=fc, dap=dap, tb=tb, half=half: e.matmul(dap, lhsT=hd[:, fc, tb * 128:(tb + 1) * 128], rhs=wd_s[slot][:, fc * 1024 + half * 512:fc * 1024 + (half + 1) * 512],
                                                                                    start=(fc == 0), stop=(fc == 1)),
                          reads=[("hid", pi_ % 2, 0), ("hid", pi_ % 2, 1), ("wd", slot)], writes=[dk])
                ak = ("maccs", par, tb)
                if True:
                    S.add("dve", lambda e, dap=dap, tb=tb, half=half: e.scalar_tensor_tensor(out=ac[:, tb, half * 512:(half + 1) * 512], in0=dap, scalar=c8s[par][:, tb, j:j + 1],
                                                                                            in1=ac[:, tb, half * 512:(half + 1) * 512], op0=ALU.mult, op1=ALU.add),
                          reads=[dk, ("c8s", par, tb), ak], writes=[ak])
        if j == 7:
            S.add("sp", lambda e: e.dma_start(out=ffn_d[s_ * SLOT:(s_ + 1) * SLOT, :].rearrange("(t p) f -> p t f", p=128), in_=ac[:]),
                  reads=[("maccs", par, tb) for tb in range(4)], writes=[("ffn", s_)], dsem=d_fs[par])
        if pi_ + NW < npairs:
            wload(pi_ + NW)

    for i0 in range(min(NW, npairs)):
        wload(i0)
    for eng_, fn_, r_, w_, ds_ in scat_cap:
        S.add(eng_, fn_, reads=r_, writes=w_, dsem=ds_)
    slot_prep(0)
    moe_gu(0, 0); moe_gu(0, 1)
    for pi_ in range(npairs):
        s_, j = pairs[pi_]
        if j == 2 and s_ + 1 < nslot:
            slot_prep(s_ + 1)
        if pi_ + 1 < npairs:
            moe_gu(pi_ + 1, 0)
        moe_d(pi_)
        if pi_ + 1 < npairs:
            moe_gu(pi_ + 1, 1)
    allffn = [("ffn", s_) for s_ in range(nslot)]

    fb = [t_[:, 0:1024] for t_ in (rowt + xsb)]
    ND = len(fb)
    d_fb = [sem("d_fb%d" % i) for i in range(ND)]
    LOOK = ND - 1
    def ln_parts(b):
        fb_ = fb[b % ND]; fk = ("fb", b % ND)
        cap = []
        S.capture = cap
        ln_tok(S, fb_, fk, st[b % 4], mv[b % 4], b % 4, g2bc, b2bc, "g2bc", "b2bc", epsln, geng="dve", beng="dve")
        S.add("sp", lambda e: e.dma_start(out=out_d[b * 128:(b + 1) * 128, :], in_=fb_), reads=[fk], writes=[("outd", b)], dsem=d_out[b % ND])
        S.capture = None
        return cap[:7], cap[7:]

    def emit_(lst):
        for eng_, fn_, r_, w_, ds_ in lst:
            S.add(eng_, fn_, reads=r_, writes=w_, dsem=ds_)

    for b in range(min(ND, nblk)):
        fb_ = fb[b % ND]; fk = ("fb", b % ND)
        S.add("pool", lambda e, fb_=fb_, b=b: e.indirect_dma_start(out=fb_, out_offset=None, in_=ffn_d[:, :], in_offset=bass.IndirectOffsetOnAxis(ap=POSI[:, b:b + 1], axis=0)),
              reads=allffn + ["POSI"], writes=[fk], dsem=d_fb[b % ND])
    prev2 = None
    for b in range(nblk):
        s1, s2 = ln_parts(b)
        emit_(s1)
        if prev2 is not None:
            emit_(prev2)
        prev2 = s2
        nb_ = b + LOOK
        if nb_ < nblk and b >= 1:
            pass
        nb_ = b - 1 + ND
        if b >= 1 and nb_ < nblk:
            fbn = fb[nb_ % ND]; fkn = ("fb", nb_ % ND)
            S.add("pool", lambda e, fbn=fbn, nb_=nb_: e.indirect_dma_start(out=fbn, out_offset=None, in_=ffn_d[:, :], in_offset=bass.IndirectOffsetOnAxis(ap=POSI[:, nb_:nb_ + 1], axis=0)),
                  reads=allffn + ["POSI"], writes=[fkn], dsem=d_fb[nb_ % ND])
    emit_(prev2)
```
